# Optimizing a Trainium2 kernel written in Bass

```python
import math
import jax, jax.numpy as jnp
from jax import lax
import numpy as np

D_MODEL = 1024
BATCH = 8
SEQ = 4096
DEPTH = 1

GRID_W = 64
CTX_LEN = 256
EPS = 1e-6

SSM_WIDTH = 512
SSM_GROUP = 16
SSM_GROUPS = SSM_WIDTH // SSM_GROUP
SSM_STATE = 64

NA_HEADS = 8
NA_HEAD_DIM = 64
NA_WIDTH = NA_HEADS * NA_HEAD_DIM
NA_ROWS_MAX = 8
NA_COLS = 16

COL_K = SSM_WIDTH
COL_V = COL_K + NA_WIDTH
COL_Q = COL_V + NA_WIDTH
COL_GA = COL_Q + NA_WIDTH
COL_GB = COL_GA + D_MODEL
IN_COLS = COL_GB + D_MODEL
CTX_COLS = COL_Q

PEER_HEADS = 8
PEER_KEYS = 128
PEER_EXPERTS = PEER_KEYS * PEER_KEYS
PEER_QDIM = 256
PEER_HALF = PEER_QDIM // 2
PEER_TOPK = 16
PEER_BLOCK = 128

kernel_name = 'hybrid_s5_natten_peer_dit_layer'


def rms_norm(t, g):
    tf = t.astype(jnp.float32)
    y = tf * lax.rsqrt(jnp.mean(tf * tf, axis=-1, keepdims=True) + EPS)
    return (y * g.astype(jnp.float32)).astype(t.dtype)


def modulate(t, g, shift, scale):
    return rms_norm(t, g) * (1 + scale) + shift


def adaln(cond, w_mod, b_mod, n_chunks):
    m = jax.nn.silu(cond) @ w_mod[:, :n_chunks * D_MODEL] + b_mod[:n_chunks * D_MODEL]
    return jnp.split(m, n_chunks, axis=-1)


def s5_discretise(p, direction):
    f32 = jnp.float32
    lam = lax.complex(p['ssm_a_re'][direction].astype(f32), p['ssm_a_im'][direction].astype(f32))
    dt = jnp.exp(p['ssm_log_dt'][direction].astype(f32))[:, None]
    lam_bar = jnp.exp(lam * dt)
    b = lax.complex(p['ssm_b_re'][direction].astype(f32), p['ssm_b_im'][direction].astype(f32))
    b_bar = ((lam_bar - 1) / lam)[..., None] * b
    cmat = lax.complex(p['ssm_c_re'][direction].astype(f32), p['ssm_c_im'][direction].astype(f32))
    return lam_bar, b_bar, cmat


def _linear_recurrence(e1, e2):
    a1, b1 = e1
    a2, b2 = e2
    return a2 * a1, a2 * b1 + b2


def s5_scan(lam_bar, bu, s0, reverse):
    if s0 is not None:
        bu = bu.at[-1 if reverse else 0].add(lam_bar * s0)
    a = jnp.broadcast_to(lam_bar, (bu.shape[0], 1) + lam_bar.shape)
    _, s = lax.associative_scan(_linear_recurrence, (a, bu), reverse=reverse, axis=0)
    return s


def s5_mixer(u_lat, u_ctx, p, with_ctx_out):
    def groups(u):
        n, b = u.shape[1], u.shape[0]
        return jnp.swapaxes(u.astype(jnp.float32), 0, 1).reshape(n, b, SSM_GROUPS, SSM_GROUP).astype(jnp.complex64)

    def readout(cmat, s):
        y = jnp.einsum('ghp,nbgp->nbgh', cmat, s).real
        return jnp.swapaxes(y.reshape(y.shape[0], y.shape[1], SSM_WIDTH), 0, 1)

    ul, uc = groups(u_lat), groups(u_ctx)
    d_skip = p['ssm_d'].astype(jnp.float32)
    y_lat = d_skip * u_lat.astype(jnp.float32)
    y_ctx = d_skip * u_ctx.astype(jnp.float32) if with_ctx_out else None
    for direction, reverse in ((0, False), (1, True)):
        lam_bar, b_bar, cmat = s5_discretise(p, direction)
        s_ctx = s5_scan(lam_bar, jnp.einsum('gph,nbgh->nbgp', b_bar, uc), None, reverse)
        s0 = s_ctx[0] if reverse else s_ctx[-1]
        s_lat = s5_scan(lam_bar, jnp.einsum('gph,nbgh->nbgp', b_bar, ul), s0, reverse)
        y_lat = y_lat + readout(cmat, s_lat)
        if with_ctx_out:
            y_ctx = y_ctx + readout(cmat, s_ctx)
    return y_lat, y_ctx


def s5_glu(y, p):
    z = jax.nn.gelu(y)
    return z * jax.nn.sigmoid(z @ p['w_glu'] + p['b_glu'])


def ctx_heads(t):
    b, n, _ = t.shape
    return t.reshape(b, n, NA_HEADS, NA_HEAD_DIM).transpose(0, 2, 1, 3)


def na_mixer(q, k, v, k_ctx, v_ctx, rpb):
    b, l, _ = q.shape
    rows = l // GRID_W
    kh = min(NA_ROWS_MAX, rows)
    n_band = kh * GRID_W

    def grid_heads(t):
        return t.reshape(b, rows, GRID_W, NA_HEADS, NA_HEAD_DIM).transpose(0, 3, 1, 2, 4)

    qg, kg, vg = grid_heads(q), grid_heads(k), grid_heads(v)
    cols = jnp.arange(GRID_W)
    c0 = jnp.clip(cols - NA_COLS // 2, 0, GRID_W - NA_COLS)
    col_in = (cols[None, :] >= c0[:, None]) & (cols[None, :] < c0[:, None] + NA_COLS)
    dc = jnp.clip(cols[None, :] - cols[:, None] + NA_COLS - 1, 0, 2 * NA_COLS - 2)
    rpb = rpb.astype(jnp.float32)
    scale = NA_HEAD_DIM ** -0.5

    def row_block(args):
        r, q_row = args
        r0 = jnp.clip(r - kh // 2, 0, rows - kh)
        k_band = lax.dynamic_slice_in_dim(kg, r0, kh, axis=2).reshape(b, NA_HEADS, n_band, NA_HEAD_DIM)
        v_band = lax.dynamic_slice_in_dim(vg, r0, kh, axis=2).reshape(b, NA_HEADS, n_band, NA_HEAD_DIM)
        dr = r0 + jnp.arange(kh) - r + NA_ROWS_MAX - 1
        bias = rpb[:, dr[:, None, None], dc[None, :, :]]
        bias = jnp.where(col_in[None, None], bias, -jnp.inf)
        bias = bias.transpose(0, 2, 1, 3).reshape(NA_HEADS, GRID_W, n_band)
        s_lat = jnp.einsum('bhqd,bhkd->bhqk', q_row, k_band).astype(jnp.float32) * scale + bias
        s_ctx = jnp.einsum('bhqd,bhkd->bhqk', q_row, k_ctx).astype(jnp.float32) * scale
        prob = jax.nn.softmax(jnp.concatenate([s_lat, s_ctx], axis=-1), axis=-1).astype(v.dtype)
        return (jnp.einsum('bhqk,bhkd->bhqd', prob[..., :n_band], v_band)
                + jnp.einsum('bhqk,bhkd->bhqd', prob[..., n_band:], v_ctx))

    o = lax.map(row_block, (jnp.arange(rows), jnp.moveaxis(qg, 2, 0)))
    return o.transpose(1, 0, 3, 2, 4).reshape(b, l, NA_WIDTH)


def ctx_attention(q_c, k_c, v_c):
    s = jnp.einsum('bhqd,bhkd->bhqk', q_c, k_c).astype(jnp.float32) * NA_HEAD_DIM ** -0.5
    prob = jax.nn.softmax(s, axis=-1).astype(v_c.dtype)
    o = jnp.einsum('bhqk,bhkd->bhqd', prob, v_c)
    b, _, n, _ = o.shape
    return o.transpose(0, 2, 1, 3).reshape(b, n, NA_WIDTH)


def peer_ffn(h, w_q, subkeys, u_tab, v_tab):
    b, l, d = h.shape
    q = (h @ w_q).reshape(b, l, PEER_HEADS, 2, PEER_HALF)
    s = jnp.einsum('blhnd,nkd->blhnk', q, subkeys).astype(jnp.float32)
    s_top, i_top = lax.top_k(s, PEER_TOPK)
    n_cand = PEER_TOPK * PEER_TOPK
    cand = (s_top[..., 0, :, None] + s_top[..., 1, None, :]).reshape(b, l, PEER_HEADS, n_cand)
    cand_idx = (i_top[..., 0, :, None] * PEER_KEYS + i_top[..., 1, None, :]).reshape(b, l, PEER_HEADS, n_cand)
    best, pos = lax.top_k(cand, PEER_TOPK)
    expert = jnp.take_along_axis(cand_idx, pos, axis=-1)
    gate = jax.nn.softmax(best, axis=-1)
    n_blk = (b * l) // PEER_BLOCK

    def block(args):
        h_blk, e_blk, g_blk = args
        a = jnp.einsum('td,thkd->thk', h_blk, u_tab[e_blk])
        w = (g_blk * jax.nn.gelu(a.astype(jnp.float32))).astype(v_tab.dtype)
        return jnp.einsum('thk,thkd->td', w, v_tab[e_blk])

    out = lax.map(block, (h.reshape(n_blk, PEER_BLOCK, d),
                          expert.reshape(n_blk, PEER_BLOCK, PEER_HEADS, PEER_TOPK),
                          gate.reshape(n_blk, PEER_BLOCK, PEER_HEADS, PEER_TOPK)))
    return out.reshape(b, l, d)


def hybrid_layer(x, ctx, c, c_ctx, p, with_ctx_out):
    shift1, scale1, gate1, shift2, scale2, gate2 = adaln(c[:, None, :], p['w_mod'], p['b_mod'], 6)
    mod_c = adaln(c_ctx[None, None, :], p['w_mod'], p['b_mod'], 6 if with_ctx_out else 2)

    hx = modulate(x, p['norm1_g'], shift1, scale1)
    hc = modulate(ctx, p['norm1_g'], mod_c[0], mod_c[1])
    px = hx @ p['w_in']
    u_x, k_x, v_x, q_x, ga_x, gb_x = jnp.split(px, [COL_K, COL_V, COL_Q, COL_GA, COL_GB], axis=-1)
    pc = hc @ p['w_in'][:, :(IN_COLS if with_ctx_out else CTX_COLS)]
    u_c, k_c, v_c = pc[..., :COL_K], pc[..., COL_K:COL_V], pc[..., COL_V:COL_Q]
    kc, vc = ctx_heads(k_c), ctx_heads(v_c)

    y_ssm_x, y_ssm_c = s5_mixer(u_x, u_c, p, with_ctx_out)
    branch_a = (s5_glu(y_ssm_x, p) @ p['w_branch_a']).astype(x.dtype)
    branch_b = na_mixer(q_x, k_x, v_x, kc, vc, p['na_rpb']) @ p['w_branch_b']
    merged = jax.nn.sigmoid(ga_x) * branch_a + jax.nn.sigmoid(gb_x) * branch_b
    x = x + gate1 * (merged @ p['w_out'])
    x = x + gate2 * peer_ffn(modulate(x, p['norm2_g'], shift2, scale2),
                             p['peer_w_q'], p['peer_subkeys'], p['peer_u'], p['peer_v'])

    if with_ctx_out:
        _, _, c_gate1, c_shift2, c_scale2, c_gate2 = mod_c
        q_c, ga_c, gb_c = pc[..., COL_Q:COL_GA], pc[..., COL_GA:COL_GB], pc[..., COL_GB:]
        branch_a_c = (s5_glu(y_ssm_c, p) @ p['w_branch_a']).astype(ctx.dtype)
        branch_b_c = ctx_attention(ctx_heads(q_c), kc, vc) @ p['w_branch_b']
        merged_c = jax.nn.sigmoid(ga_c) * branch_a_c + jax.nn.sigmoid(gb_c) * branch_b_c
        ctx = ctx + c_gate1 * (merged_c @ p['w_out'])
        ctx = ctx + c_gate2 * peer_ffn(modulate(ctx, p['norm2_g'], c_shift2, c_scale2),
                                       p['peer_w_q'], p['peer_subkeys'], p['peer_u'], p['peer_v'])
    return x, ctx


def setup_inputs(seed: int = 0) -> dict:
    key = jax.random.key(seed)
    ks = jax.random.split(key, 28)
    f32 = jnp.float32

    def nrm(k, shape, scale):
        return jax.random.normal(k, shape, f32) * scale

    L = DEPTH
    G, P, H = SSM_GROUPS, SSM_STATE, SSM_GROUP
    n_idx = jnp.arange(SSM_STATE, dtype=f32)
    return {
        'x': nrm(ks[0], (BATCH, SEQ, D_MODEL), 1.0),
        'c': nrm(ks[1], (BATCH, D_MODEL), 1.0),
        'ctx': nrm(ks[2], (BATCH, CTX_LEN, D_MODEL), 1.0),
        'c_ctx': nrm(ks[3], (D_MODEL,), 1.0),
        'w_mod': nrm(ks[4], (L, D_MODEL, 6 * D_MODEL), 0.5 * D_MODEL ** -0.5),
        'b_mod': nrm(ks[5], (L, 6 * D_MODEL), 0.01),
        'norm1_g': 1.0 + nrm(ks[6], (L, D_MODEL), 0.01),
        'norm2_g': 1.0 + nrm(ks[7], (L, D_MODEL), 0.01),
        'w_in': nrm(ks[8], (L, D_MODEL, IN_COLS), D_MODEL ** -0.5),
        'ssm_a_re': -0.5 + nrm(ks[9], (L, 2, G, P), 0.01),
        'ssm_a_im': math.pi * n_idx + nrm(ks[10], (L, 2, G, P), 0.01),
        'ssm_log_dt': jax.random.uniform(ks[11], (L, 2, G), f32, math.log(1e-3), math.log(1e-1)),
        'ssm_b_re': nrm(ks[12], (L, 2, G, P, H), (2 * H) ** -0.5),
        'ssm_b_im': nrm(ks[13], (L, 2, G, P, H), (2 * H) ** -0.5),
        'ssm_c_re': nrm(ks[14], (L, 2, G, H, P), P ** -0.5),
        'ssm_c_im': nrm(ks[15], (L, 2, G, H, P), P ** -0.5),
        'ssm_d': nrm(ks[16], (L, SSM_WIDTH), 1.0),
        'w_glu': nrm(ks[17], (L, SSM_WIDTH, SSM_WIDTH), SSM_WIDTH ** -0.5),
        'b_glu': nrm(ks[18], (L, SSM_WIDTH), 0.01),
        'w_branch_a': nrm(ks[19], (L, SSM_WIDTH, D_MODEL), SSM_WIDTH ** -0.5),
        'w_branch_b': nrm(ks[20], (L, NA_WIDTH, D_MODEL), NA_WIDTH ** -0.5),
        'na_rpb': nrm(ks[21], (L, NA_HEADS, 2 * NA_ROWS_MAX - 1, 2 * NA_COLS - 1), 0.02),
        'w_out': nrm(ks[22], (L, D_MODEL, D_MODEL), D_MODEL ** -0.5),
        'peer_w_q': nrm(ks[23], (L, D_MODEL, PEER_HEADS * PEER_QDIM), D_MODEL ** -0.5),
        'peer_subkeys': nrm(ks[24], (L, 2, PEER_KEYS, PEER_HALF), PEER_HALF ** -0.5),
        'peer_u': nrm(ks[25], (L, PEER_EXPERTS, D_MODEL), D_MODEL ** -0.5),
        'peer_v': nrm(ks[26], (L, PEER_EXPERTS, D_MODEL), PEER_HEADS ** -0.5),
        'final_g': 1.0 + nrm(ks[27], (D_MODEL,), 0.01),
    }


def reference(x, c, ctx, c_ctx, w_mod, b_mod, norm1_g, norm2_g, w_in,
              ssm_a_re, ssm_a_im, ssm_log_dt, ssm_b_re, ssm_b_im, ssm_c_re, ssm_c_im, ssm_d,
              w_glu, b_glu, w_branch_a, w_branch_b, na_rpb, w_out,
              peer_w_q, peer_subkeys, peer_u, peer_v, final_g):
    for i in range(DEPTH):
        p = {
            'w_mod': w_mod[i], 'b_mod': b_mod[i], 'norm1_g': norm1_g[i], 'norm2_g': norm2_g[i],
            'w_in': w_in[i],
            'ssm_a_re': ssm_a_re[i], 'ssm_a_im': ssm_a_im[i], 'ssm_log_dt': ssm_log_dt[i],
            'ssm_b_re': ssm_b_re[i], 'ssm_b_im': ssm_b_im[i], 'ssm_c_re': ssm_c_re[i], 'ssm_c_im': ssm_c_im[i],
            'ssm_d': ssm_d[i], 'w_glu': w_glu[i], 'b_glu': b_glu[i],
            'w_branch_a': w_branch_a[i], 'w_branch_b': w_branch_b[i], 'na_rpb': na_rpb[i], 'w_out': w_out[i],
            'peer_w_q': peer_w_q[i], 'peer_subkeys': peer_subkeys[i], 'peer_u': peer_u[i], 'peer_v': peer_v[i],
        }
        x, ctx = hybrid_layer(x, ctx, c, c_ctx, p, with_ctx_out=(i < DEPTH - 1))
    return rms_norm(x, final_g)
```

```python
import math
import numpy as np
import concourse.bass as bass
import concourse.mybir as mybir
from concourse.bass_utils import run_bass_kernel_spmd

F32 = mybir.dt.float32
BF16 = mybir.dt.bfloat16
I32 = mybir.dt.int32
U32 = mybir.dt.uint32
AF = mybir.ActivationFunctionType
ALU = mybir.AluOpType
AX = mybir.AxisListType


class _Op:
    __slots__ = ("eng", "fn", "deps", "seq", "is_dma", "semkey", "signal", "count", "waits")

    def __init__(self, eng, fn, seq, is_dma=False, semkey=None):
        self.eng = eng
        self.fn = fn
        self.deps = []
        self.seq = seq
        self.is_dma = is_dma
        self.semkey = semkey
        self.signal = is_dma
        self.count = 0
        self.waits = []


class Prog:
    ENGS = ("pe", "dve", "act", "pool", "sp")

    def __init__(self, nc):
        self.nc = nc
        self.ops = []
        self.writer = {}
        self.readers = {}
        import os
        self.same_sync = os.environ.get("KSAME", "1") == "1"

    def _add(self, op, reads, writes):
        deps = []
        for r in reads:
            w = self.writer.get(r)
            if w is not None:
                deps.append(w)
        for w_ in writes:
            w = self.writer.get(w_)
            if w is not None:
                deps.append(w)
            deps.extend(self.readers.get(w_, ()))
        op.deps = [d for d in set(deps) if d is not op]
        for r in reads:
            self.readers.setdefault(r, []).append(op)
        for w_ in writes:
            self.writer[w_] = op
            self.readers[w_] = []
        self.ops.append(op)
        return op

    def op(self, eng, fn, reads=(), writes=()):
        return self._add(_Op(eng, fn, len(self.ops)), reads, writes)

    def dma(self, eng, semkey, fn, reads=(), writes=()):
        return self._add(_Op(eng, fn, len(self.ops), True, semkey), reads, writes)

    def emit(self):
        nc = self.nc
        ops = self.ops
        for o in ops:
            for d in o.deps:
                if d.is_dma:
                    continue
                if d.eng == o.eng and not o.is_dma and (d.eng == "pe" or not self.same_sync):
                    continue
                d.signal = True
        keys = []
        for o in ops:
            k = ("dma", o.semkey) if o.is_dma else ("eng", o.eng)
            if o.signal and k not in keys:
                keys.append(k)
        cnt = {k: 0 for k in keys}
        dma_hist = {}
        for o in ops:
            if not o.signal:
                continue
            k = ("dma", o.semkey) if o.is_dma else ("eng", o.eng)
            cnt[k] += 1
            o.count = cnt[k]
            if o.is_dma:
                dma_hist.setdefault(o.semkey, []).append(o.seq)
        import bisect
        seen = {e: {} for e in self.ENGS}
        for o in ops:
            need = {}
            for d in o.deps:
                if d.is_dma:
                    k = ("dma", d.semkey)
                    c = bisect.bisect_left(dma_hist[d.semkey], o.seq)
                    v = 16 * c
                else:
                    if d.eng == o.eng and not o.is_dma and (d.eng == "pe" or not self.same_sync):
                        continue
                    k = ("eng", d.eng)
                    v = d.count
                if need.get(k, 0) < v:
                    need[k] = v
            sn = seen[o.eng]
            o.waits = []
            for k, v in need.items():
                if sn.get(k, 0) < v:
                    sn[k] = v
                    o.waits.append((k, v))
        self.n_sems = len(keys)
        from contextlib import ExitStack
        with ExitStack() as st:
            sem = {}
            for i, k in enumerate(keys):
                sem[k] = st.enter_context(nc.semaphore("s%d_%s" % (i, str(k[1]).replace(" ", ""))))
            block = st.enter_context(nc.Block())
            per = {e: [o for o in ops if o.eng == e] for e in self.ENGS}

            def run(engobj, lst):
                for o in lst:
                    for k, v in o.waits:
                        engobj.wait_ge(sem[k], v)
                    ins = o.fn(engobj)
                    if o.signal:
                        k = ("dma", o.semkey) if o.is_dma else ("eng", o.eng)
                        ins.then_inc(sem[k], 16 if o.is_dma else 1)

            @block.tensor
            def _(e):
                run(e, per["pe"])

            @block.vector
            def _(e):
                run(e, per["dve"])

            @block.scalar
            def _(e):
                run(e, per["act"])

            @block.gpsimd
            def _(e):
                run(e, per["pool"])

            @block.sync
            def _(e):
                run(e, per["sp"])

    def barrier(self):
        last = {}
        dmas = {}
        for o in self.ops:
            if o.is_dma:
                dmas[o.semkey] = o
            else:
                last[o.eng] = o
        deps = list(last.values()) + list(dmas.values())
        for e in self.ENGS:
            o = _Op(e, lambda eng: eng.nop(), len(self.ops))
            o.deps = [d for d in deps]
            self.ops.append(o)
        self.writer = {}
        self.readers = {}


class Rot:
    def __init__(self, name, n):
        self.name, self.n, self.i = name, n, -1

    def next(self):
        self.i = (self.i + 1) % self.n
        return self.i, "%s%d" % (self.name, self.i)


D = 1024
SEQ = 4096
CTX = 256
NTOK = SEQ + CTX
EPS = 1e-6


def build(stage=99, debug=False):
    import os
    KD = os.environ.get("KDBG", "")
    from contextlib import ExitStack
    nc = bass.Bass("TRN2", target_bir_lowering=False)
    P = Prog(nc)

    def din(name, shape, dt=F32):
        return nc.dram_tensor(name, shape, dt, kind="ExternalInput").ap()

    def dscr(name, shape, dt):
        return nc.dram_tensor(name, shape, dt, kind=("ExternalOutput" if debug else "Internal")).ap()

    x = din("x", [SEQ, D]); c = din("c", [1, D]); ctx = din("ctx", [CTX, D]); c_ctx = din("c_ctx", [1, D])
    w_mod = din("w_mod", [D, 6 * D]); b_mod = din("b_mod", [1, 6 * D])
    norm1_g = din("norm1_g", [1, D]); norm2_g = din("norm2_g", [1, D]); final_g = din("final_g", [1, D])
    w_in = din("w_in", [D, 4096])
    a_re = din("ssm_a_re", [2, 32, 64]); a_im = din("ssm_a_im", [2, 32, 64]); log_dt = din("ssm_log_dt", [2, 32])
    b_re = din("ssm_b_re", [2, 32, 64, 16]); b_im = din("ssm_b_im", [2, 32, 64, 16])
    c_re = din("ssm_c_re", [2, 32, 16, 64]); c_im = din("ssm_c_im", [2, 32, 16, 64])
    ssm_d = din("ssm_d", [512, 1]); w_glu = din("w_glu", [512, 512]); b_glu = din("b_glu", [512, 1])
    w_ba = din("w_branch_a", [512, D]); w_bb = din("w_branch_b", [512, D]); rpb = din("na_rpb", [8, 15, 31])
    w_out = din("w_out", [D, D]); w_q = din("peer_w_q", [D, 2048]); subkeys = din("peer_subkeys", [2, 128, 128])
    peer_uv = din("peer_uv", [16384, 2 * D])
    out = nc.dram_tensor("out", [SEQ, D], F32, kind="ExternalOutput").ap()

    uT_d = dscr("uT_d", [512, NTOK], F32)
    kT_d = dscr("kT_d", [512, NTOK], BF16)
    qT_d = dscr("qT_d", [512, SEQ], BF16)
    v_d = dscr("v_d", [NTOK, 512], BF16)
    gT_d = dscr("gT_d", [2048, SEQ], BF16)
    baT_d = dscr("baT_d", [D, SEQ], BF16)
    mgT_d = dscr("mgT_d", [D, SEQ], BF16)
    uvb_d = nc.dram_tensor("uvb_d", [16384, 2 * D], BF16, kind=("ExternalOutput" if (debug and stage == 3.5) else "Internal")).ap()

    top = ExitStack()
    with top:
        def sbuf(st, n, s, d=F32):
            return st.enter_context(nc.sbuf_tensor(n, s, d))

        def psum(st, n, s, d=F32):
            return st.enter_context(nc.psum_tensor(n, s, d))

        identf = sbuf(top, "identf", [128, 128])
        identb = sbuf(top, "identb", [128, 128], BF16)
        modB = sbuf(top, "modB", [128, 6 * D])
        P.op("pool", lambda e: e.iota(identf[:], pattern=[[1, 128]], base=0, channel_multiplier=-1,
                                      allow_small_or_imprecise_dtypes=True), writes=["identf"])
        P.op("dve", lambda e: e.tensor_single_scalar(out=identf[:], in_=identf[:], scalar=0.0, op=ALU.is_equal),
             reads=["identf"], writes=["identf"])
        P.op("dve", lambda e: e.tensor_copy(out=identb[:], in_=identf[:]), reads=["identf"], writes=["identb"])

        with ExitStack() as st:
            modcB = sbuf(st, "modcB", [128, 2 * D])
            wm = [sbuf(st, "wm%d" % i, [128, 8, 512]) for i in range(2)]
            w_in_sb = sbuf(st, "w_in_sb", [128, 8, 4096], BF16)
            st0 = ExitStack()
            cc = sbuf(st0, "cc", [128, 2, 8]); sc = sbuf(st0, "sc", [128, 2, 8]); scB = sbuf(st0, "scB", [128, 2, 8, 128])
            bmB = sbuf(st0, "bmB", [128, 6 * D]); gB = sbuf(st0, "gB", [128, 2, D])
            pmod = [psum(st0, "pmod%d" % i, [128, 512]) for i in range(2)]

            P.dma("sp", "c0", lambda e: e.dma_start(out=cc[:, 0, :], in_=c.rearrange("o (k p) -> p (o k)", p=128),
                                                    allow_slow_non_contiguous=True), writes=["cc"])
            P.dma("sp", "c0", lambda e: e.dma_start(out=cc[:, 1, :], in_=c_ctx.rearrange("o (k p) -> p (o k)", p=128),
                                                    allow_slow_non_contiguous=True), writes=["cc"])
            P.dma("act", "c1", lambda e: e.dma_start(out=bmB[:], in_=b_mod.to_broadcast([128, 6 * D])), writes=["bmB"])
            P.dma("act", "c1", lambda e: e.dma_start(out=gB[:, 0, :], in_=norm1_g.to_broadcast([128, D])), writes=["gB"])
            P.dma("act", "c1", lambda e: e.dma_start(out=gB[:, 1, :], in_=norm2_g.to_broadcast([128, D])), writes=["gB"])
            P.op("act", lambda e: e.activation(out=sc[:], in_=cc[:], func=AF.Silu), reads=["cc"], writes=["sc"])
            P.op("dve", lambda e: e.tensor_copy(out=scB[:], in_=sc[:].unsqueeze(3).to_broadcast([128, 2, 8, 128])),
                 reads=["sc"], writes=["scB"])
            w_mod_v = w_mod.rearrange("(k p) n -> p k n", p=128)
            for cch in range(12):
                bi = cch % 2
                P.dma("sp", "wm%d" % bi, (lambda bi, cch: lambda e: e.dma_start(out=wm[bi][:], in_=w_mod_v[:, :, cch * 512:(cch + 1) * 512]))(bi, cch),
                      writes=["wm%d" % bi])
                for which in range(2 if cch < 4 else 1):
                    for k in range(8):
                        P.op("pe", (lambda bi, which, k: lambda e: e.matmul(pmod[which][:], lhsT=scB[:, which, k, :], rhs=wm[bi][:, k, :],
                                                                            start=(k == 0), stop=(k == 7)))(bi, which, k),
                             reads=["scB", "wm%d" % bi], writes=["pmod%d" % which])
                    dst = modB if which == 0 else modcB
                    P.op("dve", (lambda dst, which, cch: lambda e: e.tensor_tensor(out=dst[:, cch * 512:(cch + 1) * 512], in0=pmod[which][:],
                                                                                   in1=bmB[:, cch * 512:(cch + 1) * 512], op=ALU.add))(dst, which, cch),
                         reads=["pmod%d" % which, "bmB"], writes=["modB" if which == 0 else "modcB"])
            for dst, key, off, gi in ((modB, "modB", D, 0), (modcB, "modcB", D, 0), (modB, "modB", 4 * D, 1)):
                P.op("dve", (lambda dst, off, gi: lambda e: e.scalar_tensor_tensor(out=dst[:, off:off + D], in0=dst[:, off:off + D], scalar=1.0,
                                                                                  in1=gB[:, gi, :], op0=ALU.add, op1=ALU.mult))(dst, off, gi),
                     reads=[key, "gB"], writes=[key])

            st0.close()
            P.barrier()
            w_in_v = w_in.rearrange("(k p) n -> p k n", p=128)
            for cch in range(8):
                bi = cch % 2
                P.dma("sp", "wm%d" % bi, (lambda bi, cch: lambda e: e.dma_start(out=wm[bi][:], in_=w_in_v[:, :, cch * 512:(cch + 1) * 512]))(bi, cch),
                      reads=[], writes=["wm%d" % bi])
                eng = ("pool", "dve")[cch % 2]
                P.op(eng, (lambda bi, cch: lambda e: e.tensor_copy(out=w_in_sb[:, :, cch * 512:(cch + 1) * 512], in_=wm[bi][:]))(bi, cch),
                     reads=["wm%d" % bi], writes=["w_in_sb"])

            xt = [sbuf(st, "xt%d" % i, [128, D]) for i in range(3)]; xr = Rot("xt", 3)
            junk = sbuf(st, "junkA", [128, D]); tmpA = sbuf(st, "tmpA", [128, D])
            ss = [sbuf(st, "ss%d" % i, [128, 4]) for i in range(2)]; ssr = Rot("ss", 2)
            hxb = [sbuf(st, "hxb%d" % i, [128, D], BF16) for i in range(2)]; hr = Rot("hxb", 2)
            hxT = [sbuf(st, "hxT%d" % i, [128, 8, 512], BF16) for i in range(2)]; hTr = Rot("hxT", 2)
            st_u = sbuf(st, "st_u", [128, 4, 512]); st_k = sbuf(st, "st_k", [128, 4, 512], BF16)
            st_q = sbuf(st, "st_q", [128, 4, 512], BF16); st_g = sbuf(st, "st_g", [128, 16, 512], BF16)
            st_v = sbuf(st, "st_v", [128, 4, 512], BF16)
            tp = [psum(st, "tpA%d" % i, [128, 8, 128], BF16) for i in range(2)]; tpr = Rot("tpA", 2)
            pj = [psum(st, "pj%d" % i, [128, 512]) for i in range(4)]; pjr = Rot("pj", 4)
            evac_i = [0]

            def evac(dst_ap, src_ap, reads, writes, func=None):
                if func is not None:
                    P.op("act", lambda e: e.activation(out=dst_ap, in_=src_ap, func=func), reads, writes)
                    return
                evac_i[0] += 1
                if evac_i[0] % 2:
                    P.op("act", lambda e: e.copy(out=dst_ap, in_=src_ap), reads, writes)
                else:
                    P.op("dve", lambda e: e.tensor_copy(out=dst_ap, in_=src_ap), reads, writes)

            chunks = [("ctx", 0, 256)] + [("lat", i * 512, 512) for i in range(8)]
            for kind, t0, n in chunks:
                src = ctx if kind == "ctx" else x
                mB, mkey = (modcB, "modcB") if kind == "ctx" else (modB, "modB")
                col0 = t0 if kind == "ctx" else CTX + t0
                hi, hkey = hTr.next()
                for t in range(n // 128):
                    xi, xkey = xr.next()
                    si, skey = ssr.next()
                    bi, bkey = hr.next()
                    pi, pkey = tpr.next()
                    r0 = t0 + t * 128
                    P.dma("sp", xkey, (lambda xi, r0, src: lambda e: e.dma_start(out=xt[xi][:], in_=src[r0:r0 + 128, :]))(xi, r0, src), writes=[xkey])
                    P.op("act", (lambda xi, si: lambda e: e.activation(out=junk[:], in_=xt[xi][:], func=AF.Square, accum_out=ss[si][:, 0:1]))(xi, si),
                         reads=[xkey], writes=["junkA", skey])
                    P.op("dve", (lambda si: lambda e: e.tensor_scalar(out=ss[si][:, 1:2], in0=ss[si][:, 0:1], scalar1=1.0 / D, scalar2=EPS,
                                                                      op0=ALU.mult, op1=ALU.add))(si), reads=[skey], writes=[skey])
                    P.op("act", (lambda si: lambda e: e.sqrt(out=ss[si][:, 2:3], in_=ss[si][:, 1:2]))(si), reads=[skey], writes=[skey])
                    P.op("dve", (lambda si: lambda e: e.reciprocal(out=ss[si][:, 3:4], in_=ss[si][:, 2:3]))(si), reads=[skey], writes=[skey])
                    P.op("dve", (lambda xi, si, mB: lambda e: e.scalar_tensor_tensor(out=tmpA[:], in0=xt[xi][:], scalar=ss[si][:, 3:4], in1=mB[:, D:2 * D],
                                                                                     op0=ALU.mult, op1=ALU.mult))(xi, si, mB),
                         reads=[xkey, skey, mkey], writes=["tmpA"])
                    P.op("dve", (lambda bi, mB: lambda e: e.tensor_tensor(out=hxb[bi][:], in0=tmpA[:], in1=mB[:, 0:D], op=ALU.add))(bi, mB),
                         reads=["tmpA", mkey], writes=[bkey])
                    for k in range(8):
                        P.op("pe", (lambda pi, bi, k: lambda e: e.transpose(tp[pi][:, k, :], hxb[bi][:, k * 128:(k + 1) * 128], identb[:]))(pi, bi, k),
                             reads=[bkey, "identb"], writes=[pkey])
                    P.op("act", (lambda hi, pi, t: lambda e: e.copy(out=hxT[hi][:, :, t * 128:(t + 1) * 128], in_=tp[pi][:]))(hi, pi, t),
                         reads=[pkey], writes=[hkey])
                cts = list(range(0, 8)) + (list(range(12, 32)) if kind == "lat" else [])
                for ct in cts:
                    qi, qkey = pjr.next()
                    for k in range(8):
                        P.op("pe", (lambda qi, hi, k, ct: lambda e: e.matmul(pj[qi][:, 0:n], lhsT=w_in_sb[:, k, ct * 128:(ct + 1) * 128], rhs=hxT[hi][:, k, 0:n],
                                                                             start=(k == 0), stop=(k == 7)))(qi, hi, k, ct),
                             reads=["w_in_sb", hkey], writes=[qkey])
                    if ct < 4:
                        evac(st_u[:, ct, 0:n], pj[qi][:, 0:n], [qkey], ["st_u"])
                    elif ct < 8:
                        evac(st_k[:, ct - 4, 0:n], pj[qi][:, 0:n], [qkey], ["st_k"])
                    elif ct < 16:
                        evac(st_q[:, ct - 12, 0:n], pj[qi][:, 0:n], [qkey], ["st_q"])
                    else:
                        evac(st_g[:, ct - 16, 0:n], pj[qi][:, 0:n], [qkey], ["st_g"], func=AF.Sigmoid)
                P.dma("sp", "stu", (lambda col0, n: lambda e: e.dma_start(out=uT_d.rearrange("(t p) n -> p t n", p=128)[:, :, col0:col0 + n], in_=st_u[:, :, 0:n]))(col0, n),
                      reads=["st_u"], writes=["uT_d"])
                P.dma("sp", "stk", (lambda col0, n: lambda e: e.dma_start(out=kT_d.rearrange("(t p) n -> p t n", p=128)[:, :, col0:col0 + n], in_=st_k[:, :, 0:n]))(col0, n),
                      reads=["st_k"], writes=["kT_d"])
                if kind == "lat":
                    P.dma("sp", "stq", (lambda t0: lambda e: e.dma_start(out=qT_d.rearrange("(t p) n -> p t n", p=128)[:, :, t0:t0 + 512], in_=st_q[:]))(t0),
                          reads=["st_q"], writes=["qT_d"])
                    P.dma("sp", "stg", (lambda t0: lambda e: e.dma_start(out=gT_d.rearrange("(t p) n -> p t n", p=128)[:, :, t0:t0 + 512], in_=st_g[:]))(t0),
                          reads=["st_g"], writes=["gT_d"])
                for t in range(n // 128):
                    qi, qkey = pjr.next()
                    for k in range(8):
                        P.op("pe", (lambda qi, hi, k, t: lambda e: e.matmul(pj[qi][:], lhsT=hxT[hi][:, k, t * 128:(t + 1) * 128], rhs=w_in_sb[:, k, 1024:1536],
                                                                            start=(k == 0), stop=(k == 7)))(qi, hi, k, t),
                             reads=["w_in_sb", hkey], writes=[qkey])
                    evac(st_v[:, t, :], pj[qi][:], [qkey], ["st_v"])
                nt = n // 128
                P.dma("sp", "stv", (lambda col0, nt: lambda e: e.dma_start(out=v_d[col0:col0 + nt * 128, :].rearrange("(t p) n -> p t n", p=128), in_=st_v[:, 0:nt, :]))(col0, nt),
                      reads=["st_v"], writes=["v_d"])
        P.barrier()
        if stage <= 1:
            return finish(nc, P, out)

        yT_d = dscr("yT_d", [512, SEQ], F32) if debug else None
        TWO_PI = 2.0 * math.pi
        with ExitStack() as stB:
            zT = sbuf(stB, "zT", [128, 4, SEQ], BF16)
            with ExitStack() as st:
                def t32(n):
                    return sbuf(st, n, [128, 32])
                are, aim, ldt = t32("are"), t32("aim"), t32("ldt")
                Bn = [sbuf(st, "Bn%d" % i, [128, 32, 16]) for i in range(2)]
                bb = [sbuf(st, "bb%d" % i, [128, 32, 16]) for i in range(2)]
                tmpb = sbuf(st, "tmpb", [128, 32, 16])
                Cn2 = [sbuf(st, "Cn2%d" % i, [128, 8, 2, 64]) for i in range(2)]
                dsk = sbuf(st, "dsk", [128, 4])
                maskf = sbuf(st, "maskf", [128, 4, 2]); mask2 = sbuf(st, "mask2", [128, 4, 2])
                pwr = sbuf(st, "pwr", [128, 13, 32]); pwi = sbuf(st, "pwi", [128, 13, 32]); npwi = sbuf(st, "npwi", [128, 13, 32])
                kint = sbuf(st, "kint", [128, 32], I32)
                names = ["dt", "er", "th", "mag", "kf", "rr", "half", "sn", "ah", "cq", "sinr", "cosr", "nre", "den", "rden",
                         "fre", "fim", "t1", "t2"]
                T = {n: t32("p_" + n) for n in names}
                uT_sb = [sbuf(st, "uT_sb%d" % i, [128, NTOK]) for i in range(1)]
                PL = [sbuf(st, "PL%d" % i, [128, 2, NTOK]) for i in range(2)]
                yT = sbuf(st, "yT", [128, SEQ])
                Z = [sbuf(st, "Z%d" % i, [128, 2, 128]) for i in range(2)]
                Zc = [sbuf(st, "Zc%d" % i, [128, 2, 128]) for i in range(2)]
                LB = [sbuf(st, "LB%d" % i, [128, 2, 128]) for i in range(2)]
                LC = [sbuf(st, "LC%d" % i, [128, 2, 128]) for i in range(2)]
                pz = [psum(st, "pz%d" % i, [128, 2, 128]) for i in range(2)]; pzr = Rot("pz", 2)
                pb = [psum(st, "pb%d" % i, [128, 512]) for i in range(3)]; pbr = Rot("pb", 3)
                py = [psum(st, "py%d" % i, [128, 512]) for i in range(2)]; pyr = Rot("py", 2)

                for gl in range(2):
                    sl = slice(gl * 64, (gl + 1) * 64)
                    for dst, srcp, key in ((are, a_re, "are"), (aim, a_im, "aim")):
                        P.dma("act", "pb0", (lambda dst, srcp, sl, gl: lambda e: e.dma_start(
                            out=dst[sl, :].rearrange("p (d g) -> p d g", d=2),
                            in_=srcp.rearrange("d (gp gl) p -> gl p d gp", gl=2)[gl], allow_slow_non_contiguous=True))(dst, srcp, sl, gl), writes=[key])
                    P.dma("act", "pb0", (lambda sl, gl: lambda e: e.dma_start(
                        out=ldt[sl, :].rearrange("p (d g) -> p d g", d=2),
                        in_=log_dt.rearrange("d (gp gl) -> gl d gp", gl=2)[gl:gl + 1].to_broadcast([64, 2, 16]), allow_slow_non_contiguous=True))(sl, gl), writes=["ldt"])
                    for i, srcp in enumerate((b_re, b_im)):
                        P.dma("act", "pb0", (lambda i, srcp, sl, gl: lambda e: e.dma_start(
                            out=Bn[i][sl].rearrange("p (d g) h -> p d g h", d=2),
                            in_=srcp.rearrange("d (gp gl) p h -> gl p d gp h", gl=2)[gl]))(i, srcp, sl, gl), writes=["Bn%d" % i])
                for i, srcp in enumerate((c_re, c_im)):
                    for j in range(2):
                        P.dma("act", "pb0", (lambda i, srcp, j: lambda e: e.dma_start(
                            out=Cn2[i][:, :, j, :].rearrange("p (d u) q -> p d u q", d=2),
                            in_=srcp.rearrange("d (ut g8) h p -> (g8 h) d ut p", g8=8)))(i, srcp, j), writes=["Cn2%d" % i])
                P.dma("act", "pb0", lambda e: e.dma_start(out=dsk[:], in_=ssm_d.rearrange("(ut p) o -> p (ut o)", p=128), allow_slow_non_contiguous=True), writes=["dsk"])
                P.op("pool", lambda e: e.iota(maskf[:], pattern=[[-32, 4], [-16, 2]], base=0, channel_multiplier=1, allow_small_or_imprecise_dtypes=True), writes=["maskf"])
                P.op("dve", lambda e: e.tensor_single_scalar(out=mask2[:], in_=maskf[:], scalar=0.0, op=ALU.is_ge), reads=["maskf"], writes=["mask2"])
                P.op("dve", lambda e: e.tensor_single_scalar(out=maskf[:], in_=maskf[:], scalar=16.0, op=ALU.is_lt), reads=["maskf", "mask2"], writes=["maskf"])
                P.op("dve", lambda e: e.tensor_tensor(out=maskf[:], in0=maskf[:], in1=mask2[:], op=ALU.mult), reads=["maskf", "mask2"], writes=["maskf"])

                PK = ["are", "aim", "ldt", "Bn0", "Bn1", "prm"]

                def dve(fn):
                    P.op("dve", fn, reads=PK, writes=["prm"])

                def act(fn):
                    P.op("act", fn, reads=PK, writes=["prm"])
                act(lambda e: e.activation(out=T["dt"][:], in_=ldt[:], func=AF.Exp))
                dve(lambda e: e.tensor_tensor(out=T["er"][:], in0=are[:], in1=T["dt"][:], op=ALU.mult))
                dve(lambda e: e.tensor_tensor(out=T["th"][:], in0=aim[:], in1=T["dt"][:], op=ALU.mult))
                act(lambda e: e.activation(out=T["mag"][:], in_=T["er"][:], func=AF.Exp))
                dve(lambda e: e.tensor_single_scalar(out=T["kf"][:], in_=T["th"][:], scalar=1.0 / TWO_PI, op=ALU.mult))
                dve(lambda e: e.tensor_copy(out=kint[:], in_=T["kf"][:]))
                dve(lambda e: e.tensor_copy(out=T["kf"][:], in_=kint[:]))
                dve(lambda e: e.scalar_tensor_tensor(out=T["rr"][:], in0=T["kf"][:], scalar=-TWO_PI, in1=T["th"][:], op0=ALU.mult, op1=ALU.add))
                dve(lambda e: e.tensor_single_scalar(out=T["half"][:], in_=T["rr"][:], scalar=0.5, op=ALU.mult))
                act(lambda e: e.activation(out=T["ah"][:], in_=T["half"][:], func=AF.Abs))
                dve(lambda e: e.tensor_scalar(out=T["t1"][:], in0=T["ah"][:], scalar1=-1.0, scalar2=math.pi / 2, op0=ALU.mult, op1=ALU.add))
                act(lambda e: e.activation(out=T["sn"][:], in_=T["half"][:], func=AF.Sin))
                act(lambda e: e.activation(out=T["cq"][:], in_=T["t1"][:], func=AF.Sin))
                dve(lambda e: e.scalar_tensor_tensor(out=T["sinr"][:], in0=T["sn"][:], scalar=2.0, in1=T["cq"][:], op0=ALU.mult, op1=ALU.mult))
                dve(lambda e: e.scalar_tensor_tensor(out=T["t2"][:], in0=T["sn"][:], scalar=-2.0, in1=T["sn"][:], op0=ALU.mult, op1=ALU.mult))
                dve(lambda e: e.tensor_single_scalar(out=T["cosr"][:], in_=T["t2"][:], scalar=1.0, op=ALU.add))
                dve(lambda e: e.tensor_tensor(out=pwr[:, 0, :], in0=T["mag"][:], in1=T["cosr"][:], op=ALU.mult))
                dve(lambda e: e.tensor_tensor(out=pwi[:, 0, :], in0=T["mag"][:], in1=T["sinr"][:], op=ALU.mult))
                dve(lambda e: e.tensor_single_scalar(out=T["nre"][:], in_=pwr[:, 0, :], scalar=-1.0, op=ALU.add))
                dve(lambda e: e.tensor_tensor(out=T["den"][:], in0=are[:], in1=are[:], op=ALU.mult))
                dve(lambda e: e.tensor_tensor(out=T["t1"][:], in0=aim[:], in1=aim[:], op=ALU.mult))
                dve(lambda e: e.tensor_tensor(out=T["den"][:], in0=T["den"][:], in1=T["t1"][:], op=ALU.add))
                dve(lambda e: e.reciprocal(out=T["rden"][:], in_=T["den"][:]))
                dve(lambda e: e.tensor_tensor(out=T["t1"][:], in0=T["nre"][:], in1=are[:], op=ALU.mult))
                dve(lambda e: e.tensor_tensor(out=T["t2"][:], in0=pwi[:, 0, :], in1=aim[:], op=ALU.mult))
                dve(lambda e: e.tensor_tensor(out=T["t1"][:], in0=T["t1"][:], in1=T["t2"][:], op=ALU.add))
                dve(lambda e: e.tensor_tensor(out=T["fre"][:], in0=T["t1"][:], in1=T["rden"][:], op=ALU.mult))
                dve(lambda e: e.tensor_tensor(out=T["t1"][:], in0=pwi[:, 0, :], in1=are[:], op=ALU.mult))
                dve(lambda e: e.tensor_tensor(out=T["t2"][:], in0=T["nre"][:], in1=aim[:], op=ALU.mult))
                dve(lambda e: e.tensor_tensor(out=T["t1"][:], in0=T["t1"][:], in1=T["t2"][:], op=ALU.subtract))
                dve(lambda e: e.tensor_tensor(out=T["fim"][:], in0=T["t1"][:], in1=T["rden"][:], op=ALU.mult))
                fr = T["fre"][:].unsqueeze(2).to_broadcast([128, 32, 16]); fi = T["fim"][:].unsqueeze(2).to_broadcast([128, 32, 16])
                dve(lambda e: e.tensor_tensor(out=bb[0][:], in0=Bn[0][:], in1=fr, op=ALU.mult))
                dve(lambda e: e.tensor_tensor(out=tmpb[:], in0=Bn[1][:], in1=fi, op=ALU.mult))
                dve(lambda e: e.tensor_tensor(out=bb[0][:], in0=bb[0][:], in1=tmpb[:], op=ALU.subtract))
                dve(lambda e: e.tensor_tensor(out=bb[1][:], in0=Bn[1][:], in1=fr, op=ALU.mult))
                dve(lambda e: e.tensor_tensor(out=tmpb[:], in0=Bn[0][:], in1=fi, op=ALU.mult))
                dve(lambda e: e.tensor_tensor(out=bb[1][:], in0=bb[1][:], in1=tmpb[:], op=ALU.add))
                for k in range(12):
                    dve((lambda k: lambda e: e.tensor_tensor(out=T["t1"][:], in0=pwr[:, k, :], in1=pwr[:, k, :], op=ALU.mult))(k))
                    dve((lambda k: lambda e: e.tensor_tensor(out=T["t2"][:], in0=pwi[:, k, :], in1=pwi[:, k, :], op=ALU.mult))(k))
                    dve((lambda k: lambda e: e.tensor_tensor(out=pwr[:, k + 1, :], in0=T["t1"][:], in1=T["t2"][:], op=ALU.subtract))(k))
                    dve((lambda k: lambda e: e.scalar_tensor_tensor(out=pwi[:, k + 1, :], in0=pwr[:, k, :], scalar=2.0, in1=pwi[:, k, :], op0=ALU.mult, op1=ALU.mult))(k))
                dve(lambda e: e.tensor_single_scalar(out=npwi[:], in_=pwi[:], scalar=-1.0, op=ALU.mult))

                def cmul_acc(hi_re, hi_im, lo_re, lo_im, k, u, key):
                    sr = pwr[:, k, u:u + 1]; si = pwi[:, k, u:u + 1]; nsi = npwi[:, k, u:u + 1]
                    for o_, a_, s_ in ((hi_re, lo_re, sr), (hi_re, lo_im, nsi), (hi_im, lo_re, si), (hi_im, lo_im, sr)):
                        P.op("dve", (lambda o_, a_, s_: lambda e: e.scalar_tensor_tensor(out=o_, in0=a_, scalar=s_, in1=o_, op0=ALU.mult, op1=ALU.add))(o_, a_, s_),
                             reads=[key, "prm"], writes=[key])

                def bk_scan(pl, c0, n, rev, u, key):
                    L = n.bit_length() - 1
                    re = pl[:, 0, c0:c0 + n]; im = pl[:, 1, c0:c0 + n]
                    for k in range(L):
                        s_ = 2 << k; h_ = 1 << k
                        vr = re.rearrange("p (m s) -> p m s", s=s_); vi = im.rearrange("p (m s) -> p m s", s=s_)
                        if not rev:
                            cmul_acc(vr[:, :, s_ - 1], vi[:, :, s_ - 1], vr[:, :, h_ - 1], vi[:, :, h_ - 1], k, u, key)
                        else:
                            cmul_acc(vr[:, :, 0], vi[:, :, 0], vr[:, :, h_], vi[:, :, h_], k, u, key)
                    for k in range(L - 2, -1, -1):
                        s_ = 2 << k; h_ = 1 << k
                        vr = re.rearrange("p (m s) -> p m s", s=s_); vi = im.rearrange("p (m s) -> p m s", s=s_)
                        if not rev:
                            cmul_acc(vr[:, 1:, h_ - 1], vi[:, 1:, h_ - 1], vr[:, :-1, s_ - 1], vi[:, :-1, s_ - 1], k, u, key)
                        else:
                            cmul_acc(vr[:, :-1, h_], vi[:, :-1, h_], vr[:, 1:, 0], vi[:, 1:, 0], k, u, key)

                segs = [(0, 256)] + [(CTX + i * 512, 512) for i in range(8)]
                units = [(ut, d_, gpl) for ut in range(4) for d_ in range(2) for gpl in range(4)]

                def stA(ix):
                    ut, d_, gpl = units[ix]
                    u = d_ * 16 + ut * 4 + gpl
                    bi = ix % 2; ub = 0; ukey = "uT_sb0"
                    zk, zck, lbk, lck, plk = "Z%d" % bi, "Zc%d" % bi, "LB%d" % bi, "LC%d" % bi, "PL%d" % bi
                    if ix % 8 == 0:
                        P.dma("sp", ukey, lambda e: e.dma_start(out=uT_sb[ub][:], in_=uT_d[ut * 128:(ut + 1) * 128, :]), reads=["uT_d"], writes=[ukey])
                    P.op("pool", lambda e: e.memset(Z[bi][:], 0.0), writes=[zk])
                    for j in range(2):
                        for gl in range(2):
                            cs = (2 * gpl + gl) * 16
                            P.op("pool", (lambda j, gl, cs: lambda e: e.tensor_copy(out=Z[bi][gl * 64:(gl + 1) * 64, j, cs:cs + 16], in_=bb[j][gl * 64:(gl + 1) * 64, u, :]))(j, gl, cs),
                                 reads=["prm"], writes=[zk])
                    zi, zkey = pzr.next()
                    for j in range(2):
                        P.op("pe", (lambda zi, j: lambda e: e.matmul(pz[zi][:, j, :], lhsT=Z[bi][:, j, :], rhs=identf[:], start=True, stop=True))(zi, j), reads=[zk, "identf"], writes=[zkey])
                    P.op("act", (lambda zi: lambda e: e.copy(out=LB[bi][:], in_=pz[zi][:]))(zi), reads=[zkey], writes=[lbk])
                    for j in range(2):
                        P.op("pool", (lambda j: lambda e: e.tensor_tensor(out=Zc[bi][:, j, :].rearrange("p (g q) -> p g q", g=2), in0=Cn2[j][:, d_ * 4 + ut, :, :],
                                                                         in1=maskf[:, gpl, :].unsqueeze(2).to_broadcast([128, 2, 64]), op=ALU.mult))(j),
                             reads=["Cn2%d" % j, "maskf"], writes=[zck])
                    zi2, zkey2 = pzr.next()
                    for j in range(2):
                        P.op("pe", (lambda zi2, j: lambda e: e.matmul(pz[zi2][:, j, :], lhsT=Zc[bi][:, j, :], rhs=identf[:], start=True, stop=True))(zi2, j), reads=[zck, "identf"], writes=[zkey2])
                    P.op("act", lambda e: e.copy(out=LC[bi][:, 0, :], in_=pz[zi2][:, 0, :]), reads=[zkey2], writes=[lck])
                    P.op("act", lambda e: e.mul(out=LC[bi][:, 1, :], in_=pz[zi2][:, 1, :], mul=-1.0), reads=[zkey2], writes=[lck])
                    for (c0, n) in segs:
                        for j in range(2):
                            qi, qkey = pbr.next()
                            P.op("pe", (lambda qi, j, c0, n: lambda e: e.matmul(pb[qi][:, 0:n], lhsT=LB[bi][:, j, :], rhs=uT_sb[ub][:, c0:c0 + n], start=True, stop=True))(qi, j, c0, n),
                                 reads=[lbk, ukey], writes=[qkey])
                            P.op("act", (lambda qi, j, c0, n: lambda e: e.copy(out=PL[bi][:, j, c0:c0 + n], in_=pb[qi][:, 0:n]))(qi, j, c0, n), reads=[qkey], writes=[plk])

                def stB(ix):
                    ut, d_, gpl = units[ix]
                    u = d_ * 16 + ut * 4 + gpl
                    bi = ix % 2; plk = "PL%d" % bi
                    rev = (d_ == 1)
                    bk_scan(PL[bi], 0, CTX, rev, u, plk)
                    if not rev:
                        cmul_acc(PL[bi][:, 0, CTX:CTX + 1], PL[bi][:, 1, CTX:CTX + 1], PL[bi][:, 0, CTX - 1:CTX], PL[bi][:, 1, CTX - 1:CTX], 0, u, plk)
                    else:
                        cmul_acc(PL[bi][:, 0, NTOK - 1:NTOK], PL[bi][:, 1, NTOK - 1:NTOK], PL[bi][:, 0, 0:1], PL[bi][:, 1, 0:1], 0, u, plk)
                    bk_scan(PL[bi], CTX, SEQ, rev, u, plk)

                def stC(ix):
                    ut, d_, gpl = units[ix]
                    bi = ix % 2; ub = 0; ukey = "uT_sb0"; lck, plk = "LC%d" % bi, "PL%d" % bi
                    first = (ix % 8 == 0)
                    for sgi in range(8):
                        c0 = CTX + sgi * 512
                        yi, ykey = pyr.next()
                        for j in range(2):
                            P.op("pe", (lambda yi, j, c0: lambda e: e.matmul(py[yi][:], lhsT=LC[bi][:, j, :], rhs=PL[bi][:, j, c0:c0 + 512], start=(j == 0), stop=(j == 1)))(yi, j, c0),
                                 reads=[lck, plk], writes=[ykey])
                        ysl = slice(sgi * 512, (sgi + 1) * 512)
                        if first:
                            P.op("dve", (lambda yi, c0, ysl: lambda e: e.scalar_tensor_tensor(out=yT[:, ysl], in0=uT_sb[ub][:, c0:c0 + 512], scalar=dsk[:, ut:ut + 1],
                                                                                              in1=py[yi][:], op0=ALU.mult, op1=ALU.add))(yi, c0, ysl),
                                 reads=[ykey, ukey, "dsk"], writes=["yT"])
                        else:
                            P.op("dve", (lambda yi, ysl: lambda e: e.tensor_tensor(out=yT[:, ysl], in0=yT[:, ysl], in1=py[yi][:], op=ALU.add))(yi, ysl), reads=[ykey], writes=["yT"])
                    if ix % 8 == 7:
                        if debug:
                            P.dma("sp", "dbgy", lambda e: e.dma_start(out=yT_d[ut * 128:(ut + 1) * 128, :], in_=yT[:]), reads=["yT"], writes=["yT_d"])
                        P.op("act", lambda e: e.activation(out=zT[:, ut, :], in_=yT[:], func=AF.Gelu_apprx_tanh), reads=["yT"], writes=["zT"])

                stA(0)
                for ix in range(32):
                    if ix + 1 < 32:
                        stA(ix + 1)
                    stB(ix)
                    stC(ix)
            P.barrier()
            with ExitStack() as st:
                wstgB_t = sbuf(st, "wstgB", [128, 4, 1024])
                w_glu_sb = sbuf(st, "w_glu_sb", [128, 4, 512], BF16); w_ba_sb = sbuf(st, "w_ba_sb", [128, 4, D], BF16)
                bglu = sbuf(st, "bglu", [128, 4])
                sg = [sbuf(st, "sg%d" % i, [128, 512], BF16) for i in range(2)]; sgr = Rot("sg", 2)
                glu = [sbuf(st, "glu%d" % i, [128, 4, 512], BF16) for i in range(2)]
                st_ba = [sbuf(st, "st_ba%d" % i, [128, 8, 512], BF16) for i in range(2)]
                pg = [psum(st, "pg%d" % i, [128, 512]) for i in range(3)]; pgr = Rot("pg", 3)
                pa = [psum(st, "pa%d" % i, [128, 512]) for i in range(3)]; par = Rot("pa", 3)
                P.dma("sp", "wl0", lambda e: e.dma_start(out=wstgB_t[:, :, 0:512], in_=w_glu.rearrange("(k p) n -> p k n", p=128)), writes=["wstgB"])
                P.op("dve", lambda e: e.tensor_copy(out=w_glu_sb[:], in_=wstgB_t[:, :, 0:512]), reads=["wstgB"], writes=["w_glu_sb"])
                P.dma("sp", "wl0", lambda e: e.dma_start(out=wstgB_t[:], in_=w_ba.rearrange("(k p) n -> p k n", p=128)), reads=["wstgB"], writes=["wstgB"])
                P.op("dve", lambda e: e.tensor_copy(out=w_ba_sb[:], in_=wstgB_t[:]), reads=["wstgB"], writes=["w_ba_sb"])
                P.dma("act", "wl1", lambda e: e.dma_start(out=bglu[:], in_=b_glu.rearrange("(k p) o -> p (k o)", p=128), allow_slow_non_contiguous=True), writes=["bglu"])
                for sgi in range(8):
                    gb_ = sgi % 2; gkey = "glu%d" % gb_; bakey = "st_ba%d" % gb_
                    ssl = slice(sgi * 512, (sgi + 1) * 512)
                    for ct in range(4):
                        gi, gk = pgr.next()
                        for k in range(4):
                            P.op("pe", (lambda gi, k, ct, ssl: lambda e: e.matmul(pg[gi][:], lhsT=w_glu_sb[:, k, ct * 128:(ct + 1) * 128], rhs=zT[:, k, ssl],
                                                                                  start=(k == 0), stop=(k == 3)))(gi, k, ct, ssl),
                                 reads=["w_glu_sb", "zT"], writes=[gk])
                        si_, sk_ = sgr.next()
                        P.op("act", (lambda si_, gi, ct: lambda e: e.activation(out=sg[si_][:], in_=pg[gi][:], func=AF.Sigmoid, bias=bglu[:, ct:ct + 1]))(si_, gi, ct),
                             reads=[gk, "bglu"], writes=[sk_])
                        P.op("dve", (lambda gb_, ct, si_, ssl: lambda e: e.tensor_tensor(out=glu[gb_][:, ct, :], in0=sg[si_][:], in1=zT[:, ct, ssl], op=ALU.mult))(gb_, ct, si_, ssl),
                             reads=[sk_, "zT"], writes=[gkey])
                    for ct2 in range(8):
                        ai, ak = par.next()
                        for k in range(4):
                            P.op("pe", (lambda ai, k, ct2, gb_: lambda e: e.matmul(pa[ai][:], lhsT=w_ba_sb[:, k, ct2 * 128:(ct2 + 1) * 128], rhs=glu[gb_][:, k, :],
                                                                                   start=(k == 0), stop=(k == 3)))(ai, k, ct2, gb_),
                                 reads=["w_ba_sb", gkey], writes=[ak])
                        if ct2 % 2:
                            P.op("act", (lambda gb_, ct2, ai: lambda e: e.copy(out=st_ba[gb_][:, ct2, :], in_=pa[ai][:]))(gb_, ct2, ai), reads=[ak], writes=[bakey])
                        else:
                            P.op("dve", (lambda gb_, ct2, ai: lambda e: e.tensor_copy(out=st_ba[gb_][:, ct2, :], in_=pa[ai][:]))(gb_, ct2, ai), reads=[ak], writes=[bakey])
                    P.dma("sp", bakey, (lambda gb_, ssl: lambda e: e.dma_start(out=baT_d.rearrange("(t p) n -> p t n", p=128)[:, :, ssl], in_=st_ba[gb_][:]))(gb_, ssl),
                          reads=[bakey], writes=["baT_d"])
        P.barrier()
        if stage <= 2:
            return finish(nc, P, out)

        attT_d = dscr("attT_d", [512, SEQ], BF16) if debug else None
        NEG = -30000.0
        with ExitStack() as stC:
            attT_sb = sbuf(stC, "attT_sb", [128, 4, SEQ], BF16)
            stC2 = ExitStack()
            kT_sb = sbuf(stC2, "kT_sb", [128, 4, NTOK], BF16); qT_sb = sbuf(stC2, "qT_sb", [128, 4, SEQ], BF16)
            BiasTT = sbuf(stC2, "BiasTT", [128, 8 * 14, 64])
            Vctx = sbuf(stC2, "Vctx", [128, 2, 512], BF16)
            ones_b = sbuf(stC2, "ones_b", [128, 128], BF16)
            P.dma("sp", "lc0", lambda e: e.dma_start(out=kT_sb[:], in_=kT_d.rearrange("(t p) n -> p t n", p=128)), reads=["kT_d"], writes=["kT_sb"])
            P.dma("act", "lc1", lambda e: e.dma_start(out=qT_sb[:], in_=qT_d.rearrange("(t p) n -> p t n", p=128)), reads=["qT_d"], writes=["qT_sb"])
            P.dma("act", "lc1", lambda e: e.dma_start(out=Vctx[:], in_=v_d[0:CTX, :].rearrange("(t p) n -> p t n", p=128)), reads=["v_d"], writes=["Vctx"])
            P.op("pool", lambda e: e.memset(ones_b[:], 1.0), writes=["ones_b"])
            with ExitStack() as st:
                rpbB = sbuf(st, "rpbB", [128, 8 * 14, 31]); tmpC = sbuf(st, "tmpC", [128, 8 * 14, 64])
                Dm = sbuf(st, "Dm", [128, 64]); eqm = [sbuf(st, "eqm%d" % i, [128, 64]) for i in range(2)]
                c0t = sbuf(st, "c0t", [128, 64]); kcv = sbuf(st, "kcv", [128, 64]); m2 = sbuf(st, "m2c", [128, 64])
                for half in range(2):
                    sl = slice(half * 64, (half + 1) * 64)
                    P.dma("sp", "lc2", (lambda sl, half: lambda e: e.dma_start(out=rpbB[sl].rearrange("p (h j) m -> p h (j m)", h=8),
                                                                              in_=rpb[:, half:half + 14, :].rearrange("h j m -> h (j m)").unsqueeze(0).to_broadcast([64, 8, 14 * 31])))(sl, half),
                          writes=["rpbB"])
                    P.op("pool", (lambda sl: lambda e: e.iota(Dm[sl], pattern=[[-1, 64]], base=15, channel_multiplier=1, allow_small_or_imprecise_dtypes=True))(sl), writes=["Dm"])
                    P.op("pool", (lambda sl: lambda e: e.iota(kcv[sl], pattern=[[0, 64]], base=0, channel_multiplier=1, allow_small_or_imprecise_dtypes=True))(sl), writes=["kcv"])
                P.op("pool", lambda e: e.iota(c0t[:], pattern=[[1, 64]], base=-8, channel_multiplier=0, allow_small_or_imprecise_dtypes=True), writes=["c0t"])
                P.op("dve", lambda e: e.tensor_scalar(out=c0t[:], in0=c0t[:], scalar1=0.0, scalar2=48.0, op0=ALU.max, op1=ALU.min), reads=["c0t"], writes=["c0t"])
                P.op("dve", lambda e: e.tensor_tensor(out=kcv[:], in0=kcv[:], in1=c0t[:], op=ALU.subtract), reads=["kcv", "c0t"], writes=["kcv"])
                P.op("dve", lambda e: e.tensor_single_scalar(out=m2[:], in_=kcv[:], scalar=0.0, op=ALU.is_ge), reads=["kcv"], writes=["m2c"])
                P.op("dve", lambda e: e.tensor_single_scalar(out=kcv[:], in_=kcv[:], scalar=15.0, op=ALU.is_le), reads=["kcv", "m2c"], writes=["kcv"])
                P.op("dve", lambda e: e.tensor_tensor(out=m2[:], in0=m2[:], in1=kcv[:], op=ALU.mult), reads=["kcv", "m2c"], writes=["m2c"])
                P.op("dve", lambda e: e.tensor_scalar(out=m2[:], in0=m2[:], scalar1=-1.0, scalar2=-NEG, op0=ALU.add, op1=ALU.mult), reads=["m2c"], writes=["m2c"])
                P.op("dve", lambda e: e.tensor_copy(out=BiasTT[:], in_=m2[:].unsqueeze(1).to_broadcast([128, 112, 64])), reads=["m2c"], writes=["BiasTT"])
                for m in range(31):
                    ei = m % 2; ek = "eqm%d" % ei
                    P.op("dve", (lambda ei, m: lambda e: e.tensor_single_scalar(out=eqm[ei][:], in_=Dm[:], scalar=float(m), op=ALU.is_equal))(ei, m), reads=["Dm"], writes=[ek])
                    P.op("pool", (lambda ei, m: lambda e: e.tensor_tensor(out=tmpC[:], in0=eqm[ei][:].unsqueeze(1).to_broadcast([128, 112, 64]),
                                                                          in1=rpbB[:, :, m:m + 1].to_broadcast([128, 112, 64]), op=ALU.mult))(ei, m),
                         reads=[ek, "rpbB"], writes=["tmpC"])
                    P.op("dve", lambda e: e.tensor_tensor(out=BiasTT[:], in0=BiasTT[:], in1=tmpC[:], op=ALU.add), reads=["tmpC"], writes=["BiasTT"])
            P.barrier()
            with ExitStack() as st:
                Vb = [sbuf(st, "Vb%d" % i, [128, 4, 512], BF16) for i in range(3)]; vbr = Rot("Vb", 3)
                ssb = [sbuf(st, "ssb%d" % i, [128, 4, 64]) for i in range(3)]; ssr2 = Rot("ssb", 3)
                pT = [sbuf(st, "pT%d" % i, [128, 384], BF16) for i in range(3)]; ptr = Rot("pT", 3)
                rden = [sbuf(st, "rden%d" % i, [128, 64]) for i in range(2)]; rdr = Rot("rden", 2)
                ps_ = [psum(st, "psc%d" % i, [128, 512]) for i in range(3)]; psr = Rot("psc", 3)
                po_ = [psum(st, "poc%d" % i, [128, 512]) for i in range(2)]; por = Rot("poc", 2)
                pd_ = [psum(st, "pdc%d" % i, [128, 512]) for i in range(2)]; pdr = Rot("pdc", 2)
                B4 = BiasTT[:].rearrange("p (h j) q -> p h j q", h=8)
                cf = [sbuf(st, "cvf%d" % i, [128, 2048]) for i in range(3)]; cb = [sbuf(st, "cvb%d" % i, [128, 2048], BF16) for i in range(3)]

                def convert_tile(ti):
                    bi = ti % 3
                    P.dma("sp", "cvf%d" % bi, lambda e: e.dma_start(out=cf[bi][:], in_=peer_uv[ti * 128:(ti + 1) * 128, :]), writes=["cvf%d" % bi])
                    P.op("pool", lambda e: e.tensor_copy(out=cb[bi][:], in_=cf[bi][:]), reads=["cvf%d" % bi], writes=["cvb%d" % bi])
                    P.dma("pool", "cvb%d" % bi, lambda e: e.dma_start(out=uvb_d[ti * 128:(ti + 1) * 128, :], in_=cb[bi][:]), reads=["cvb%d" % bi], writes=["uvb_d"])
                def c_scores(r, h, vi):
                    r0 = min(max(r - 4, 0), 56)
                    t = h // 2; po = (h % 2) * 64; psl = slice(po, po + 64)
                    si, skey = psr.next()
                    qsl = slice(r * 64, (r + 1) * 64)
                    for j in range(6):
                        k0 = (CTX + (r0 + 2 * j) * 64) if j < 4 else (j - 4) * 128
                        P.op("pe", (lambda j, k0: lambda e: e.matmul(ps_[si][:, j * 64:(j + 1) * 64], lhsT=kT_sb[psl, t, k0:k0 + 128], rhs=qT_sb[psl, t, qsl], start=True, stop=True))(j, k0),
                             reads=["kT_sb", "qT_sb"], writes=[skey])
                    return (r, h, vi, si, skey)

                def c_part1(state):
                    r, h, vi, si, skey = state
                    r0 = min(max(r - 4, 0), 56); dr0 = r0 - r + 7
                    bi2, bkey2 = ssr2.next()
                    P.op("dve", lambda e: e.scalar_tensor_tensor(out=ssb[bi2][:], in0=ps_[si][:, 0:256].rearrange("p (j q) -> p j q", j=4), scalar=0.125,
                                                                 in1=B4[:, h, dr0:dr0 + 7:2, :], op0=ALU.mult, op1=ALU.add), reads=[skey, "BiasTT"], writes=[bkey2])
                    ti, tkey = ptr.next()
                    P.op("act", lambda e: e.activation(out=pT[ti][:, 0:256], in_=ssb[bi2][:].rearrange("p j q -> p (j q)"), func=AF.Exp), reads=[bkey2], writes=[tkey])
                    P.op("act", lambda e: e.activation(out=pT[ti][:, 256:384], in_=ps_[si][:, 256:384], func=AF.Exp, scale=0.125), reads=[skey], writes=[tkey])
                    return (r, h, vi, ti, tkey)

                def c_part2(state):
                    r, h, vi, ti, tkey = state
                    vkey = "Vb%d" % vi
                    t = h // 2; po = (h % 2) * 64; psl = slice(po, po + 64)
                    qsl = slice(r * 64, (r + 1) * 64)
                    oi, okey = por.next(); di, dkey = pdr.next()
                    hp = (h // 2) * 128
                    for j in range(6):
                        vsrc = (Vb[vi][:, j, hp:hp + 128] if j < 4 else Vctx[:, j - 4, hp:hp + 128])
                        P.op("pe", (lambda j, vsrc: lambda e: e.matmul(po_[oi][:, 0:64], lhsT=vsrc, rhs=pT[ti][:, j * 64:(j + 1) * 64], start=(j == 0), stop=(j == 5)))(j, vsrc),
                             reads=[vkey, "Vctx", tkey], writes=[okey])
                    for j in range(6):
                        P.op("pe", (lambda j: lambda e: e.matmul(pd_[di][:, 0:64], lhsT=ones_b[:], rhs=pT[ti][:, j * 64:(j + 1) * 64], start=(j == 0), stop=(j == 5)))(j),
                             reads=["ones_b", tkey], writes=[dkey])
                    ri, rkey = rdr.next()
                    P.op("dve", lambda e: e.reciprocal(out=rden[ri][psl, :], in_=pd_[di][psl, 0:64]), reads=[dkey], writes=[rkey])
                    P.op("dve", lambda e: e.tensor_tensor(out=attT_sb[psl, t, qsl], in0=po_[oi][psl, 0:64], in1=rden[ri][psl, :], op=ALU.mult), reads=[okey, rkey], writes=["attT_sb"])

                pend1 = None; pend2 = None
                for r in range(64):
                    convert_tile(2 * r); convert_tile(2 * r + 1)
                    r0 = min(max(r - 4, 0), 56)
                    vi, vkey = vbr.next()
                    P.dma("sp", vkey, (lambda vi, r0: lambda e: e.dma_start(out=Vb[vi][:], in_=v_d[CTX + r0 * 64:CTX + (r0 + 8) * 64, :].rearrange("(j p) n -> p j n", p=128)))(vi, r0),
                          reads=["v_d"], writes=[vkey])
                    for h in range(8):
                        stt = c_scores(r, h, vi)
                        nxt2 = c_part1(pend1) if pend1 is not None else None
                        if pend2 is not None:
                            c_part2(pend2)
                        pend2 = nxt2
                        pend1 = stt
                nxt2 = c_part1(pend1)
                if pend2 is not None:
                    c_part2(pend2)
                c_part2(nxt2)
            if debug:
                P.dma("sp", "dbga", lambda e: e.dma_start(out=attT_d.rearrange("(t p) n -> p t n", p=128), in_=attT_sb[:]), reads=["attT_sb"], writes=["attT_d"])
            stC2.close()
            P.barrier()
            with ExitStack() as st:
                wstgC_t = sbuf(st, "wstgC", [128, 4, 1024]); w_bb_sb = sbuf(st, "w_bb_sb", [128, 4, D], BF16)
                g_sb = [sbuf(st, "g_sb%d" % i, [128, 16, 512], BF16) for i in range(2)]
                ba_sb = [sbuf(st, "ba_sb%d" % i, [128, 8, 512], BF16) for i in range(2)]
                t1 = [sbuf(st, "t1c%d" % i, [128, 512]) for i in range(2)]; t1r = Rot("t1c", 2)
                t2 = [sbuf(st, "t2c%d" % i, [128, 512]) for i in range(2)]; t2r = Rot("t2c", 2)
                st_mg = [sbuf(st, "st_mg%d" % i, [128, 8, 512], BF16) for i in range(2)]
                pbb = [psum(st, "pbb%d" % i, [128, 512]) for i in range(3)]; pbr2 = Rot("pbb", 3)
                P.dma("sp", "wc0", lambda e: e.dma_start(out=wstgC_t[:], in_=w_bb.rearrange("(k p) n -> p k n", p=128)), writes=["wstgC"])
                P.op("dve", lambda e: e.tensor_copy(out=w_bb_sb[:], in_=wstgC_t[:]), reads=["wstgC"], writes=["w_bb_sb"])
                for sgi in range(8):
                    b2 = sgi % 2; ssl = slice(sgi * 512, (sgi + 1) * 512)
                    gk, bk, mk = "g_sb%d" % b2, "ba_sb%d" % b2, "st_mg%d" % b2
                    P.dma("sp", gk, (lambda b2, ssl: lambda e: e.dma_start(out=g_sb[b2][:], in_=gT_d.rearrange("(t p) n -> p t n", p=128)[:, :, ssl]))(b2, ssl), reads=["gT_d"], writes=[gk])
                    P.dma("act", bk, (lambda b2, ssl: lambda e: e.dma_start(out=ba_sb[b2][:], in_=baT_d.rearrange("(t p) n -> p t n", p=128)[:, :, ssl]))(b2, ssl), reads=["baT_d"], writes=[bk])
                    for ct2 in range(8):
                        qi, qk = pbr2.next()
                        for k in range(4):
                            P.op("pe", (lambda qi, k, ct2, ssl: lambda e: e.matmul(pbb[qi][:], lhsT=w_bb_sb[:, k, ct2 * 128:(ct2 + 1) * 128], rhs=attT_sb[:, k, ssl],
                                                                                   start=(k == 0), stop=(k == 3)))(qi, k, ct2, ssl),
                                 reads=["w_bb_sb", "attT_sb"], writes=[qk])
                        i1, k1 = t1r.next(); i2, k2 = t2r.next()
                        P.op("dve", (lambda i1, qi, b2, ct2: lambda e: e.tensor_tensor(out=t1[i1][:], in0=pbb[qi][:], in1=g_sb[b2][:, 8 + ct2, :], op=ALU.mult))(i1, qi, b2, ct2),
                             reads=[qk, gk], writes=[k1])
                        P.op("pool", (lambda i2, b2, ct2: lambda e: e.tensor_tensor(out=t2[i2][:], in0=ba_sb[b2][:, ct2, :], in1=g_sb[b2][:, ct2, :], op=ALU.mult))(i2, b2, ct2),
                             reads=[bk, gk], writes=[k2])
                        P.op("dve", (lambda b2, ct2, i1, i2: lambda e: e.tensor_tensor(out=st_mg[b2][:, ct2, :], in0=t1[i1][:], in1=t2[i2][:], op=ALU.add))(b2, ct2, i1, i2),
                             reads=[k1, k2], writes=[mk])
                    P.dma("sp", mk, (lambda b2, ssl: lambda e: e.dma_start(out=mgT_d.rearrange("(t p) n -> p t n", p=128)[:, :, ssl], in_=st_mg[b2][:]))(b2, ssl),
                          reads=[mk], writes=["mgT_d"])
        P.barrier()
        if stage <= 3:
            return finish(nc, P, out)

        x1_d = dscr("x1_d", [SEQ, D], F32) if debug else None
        pf_d = dscr("pf_d", [SEQ, D], F32) if debug else None
        NT = 32 if stage >= 5 else int(stage * 10) % 10 or 1
        if not debug:
            NT = 32
        if "KNT" in os.environ:
            NT = int(os.environ["KNT"])
        gate1B = modB[:, 2 * D:3 * D]; S2B = modB[:, 3 * D:4 * D]; G2B = modB[:, 4 * D:5 * D]; gate2B = modB[:, 5 * D:6 * D]
        with ExitStack() as stD:
            w_out_sb = sbuf(stD, "w_out_sb", [128, 8, D], BF16); w_q_sb = sbuf(stD, "w_q_sb", [128, 8, 2048], BF16)
            skT = sbuf(stD, "skT", [128, 2, 128], BF16); fgB = sbuf(stD, "fgB", [128, D])
            with ExitStack() as st:
                wstgD_t = [sbuf(st, "wstgD%d" % i, [128, 8, 512]) for i in range(2)]
                skf = sbuf(st, "skf", [128, 2, 128]); skb = sbuf(st, "skb", [128, 2, 128], BF16)
                ptk = psum(st, "ptk", [128, 2, 128], BF16)
                for i in range(6):
                    bi = i % 2
                    srcw = (w_out if i < 2 else w_q).rearrange("(k p) n -> p k n", p=128)
                    c0 = (i * 512) if i < 2 else (i - 2) * 512
                    dstw = w_out_sb if i < 2 else w_q_sb
                    P.dma("sp", "wd%d" % bi, (lambda bi, srcw, c0: lambda e: e.dma_start(out=wstgD_t[bi][:], in_=srcw[:, :, c0:c0 + 512]))(bi, srcw, c0), writes=["wstgD%d" % bi])
                    P.op(("dve", "pool")[bi], (lambda bi, dstw, c0: lambda e: e.tensor_copy(out=dstw[:, :, c0:c0 + 512], in_=wstgD_t[bi][:]))(bi, dstw, c0),
                         reads=["wstgD%d" % bi], writes=["wD"])
                P.dma("act", "wd2", lambda e: e.dma_start(out=skf[:], in_=subkeys.rearrange("n k d -> k n d")), writes=["skf"])
                P.dma("act", "wd2", lambda e: e.dma_start(out=fgB[:], in_=final_g.to_broadcast([128, D])), writes=["fgB"])
                P.op("dve", lambda e: e.tensor_copy(out=skb[:], in_=skf[:]), reads=["skf"], writes=["skb"])
                for n_ in range(2):
                    P.op("pe", (lambda n_: lambda e: e.transpose(ptk[:, n_, :], skb[:, n_, :], identb[:]))(n_), reads=["skb", "identb"], writes=["ptk"])
                P.op("dve", lambda e: e.tensor_copy(out=skT[:], in_=ptk[:]), reads=["ptk"], writes=["skT"])
            P.barrier()
            P.barrier()
            if stage == 3.5:
                return finish(nc, P, out)
            with ExitStack() as st:
                NB = 2
                xtD_t = sbuf(st, "xtD", [128, D])
                x1 = [sbuf(st, "x1_%d" % i, [128, D]) for i in range(NB)]
                h2 = [sbuf(st, "h2_%d" % i, [128, D]) for i in range(NB)]
                eid = [sbuf(st, "eid%d" % i, [128, 128], I32) for i in range(NB)]
                gate = [sbuf(st, "gate%d" % i, [128, 128]) for i in range(NB)]
                tmpD = sbuf(st, "tmpD", [128, D]); tmpF = tmpD
                acc = None
                junkD = sbuf(st, "junkD", [128, D], BF16); junkE = sbuf(st, "junkE", [128, D], BF16)
                mg_sb = sbuf(st, "mg_sb", [128, 8, 128], BF16); h2b = sbuf(st, "h2b", [128, D], BF16); h2T = sbuf(st, "h2T", [128, 8, 128], BF16)
                qT_sb2 = sbuf(st, "qT_sb2", [128, 16, 128], BF16); s_sb = sbuf(st, "s_sb", [128, 16, 128]); work = sbuf(st, "workD", [128, 16, 128])
                topv = sbuf(st, "topv", [128, 16, 16]); idxu = sbuf(st, "idxu", [128, 16, 16], U32); idxf = sbuf(st, "idxf", [128, 16, 16])
                cand = sbuf(st, "cand", [128, 8, 256]); cidx = s_sb[:].rearrange("p a b -> p (a b)").rearrange("p (h c) -> p h c", h=8)
                best = sbuf(st, "best", [128, 8, 16]); eg = sbuf(st, "eg", [128, 8, 16]); sm = sbuf(st, "smD", [128, 32]); sm2 = sbuf(st, "smE", [128, 8]); eidf = sbuf(st, "eidf", [128, 128])
                aD = sbuf(st, "aD", [128, 128]); gl_ = sbuf(st, "gl_", [128, 128]); wD = sbuf(st, "wDw", [128, 128])
                NG = 13; GJ = 4
                posI = sbuf(st, "posI", [128, 256], I32); mskI = sbuf(st, "mskI", [128, 1], I32)
                c4I = sbuf(st, "c4I", [128, 1], I32); c15I = sbuf(st, "c15I", [128, 1], I32); iota16 = sbuf(st, "iota16", [128, 16])
                pab = sbuf(st, "pab", [128, 2, 8, 16], I32); pabf = sbuf(st, "pabf", [128, 2, 8, 16]); e3b = sbuf(st, "e3b", [128, 8, 16])
                oh = cand[:].rearrange("p h (k a) -> p h k a", a=16)
                P.op("pool", lambda e: e.iota(c4I[:], pattern=[[0, 1]], base=4, channel_multiplier=0), writes=["posI"])
                P.op("pool", lambda e: e.iota(c15I[:], pattern=[[0, 1]], base=15, channel_multiplier=0), writes=["posI"])
                P.op("pool", lambda e: e.iota(iota16[:], pattern=[[1, 16]], base=0, channel_multiplier=0, allow_small_or_imprecise_dtypes=True), writes=["posI"])
                P.op("pool", lambda e: e.iota(posI[:], pattern=[[1, 256]], base=0, channel_multiplier=0), writes=["posI"])
                P.op("pool", lambda e: e.iota(mskI[:], pattern=[[0, 1]], base=-256, channel_multiplier=0), writes=["posI"])
                UVg = [sbuf(st, "UVg%d" % i, [128, 2, D], BF16) for i in range(NG)]
                dg = [sbuf(st, "dg%d" % i, [128, 128], BF16) for i in range(8)]; dgr = Rot("dg", 8)
                pmo = [psum(st, "pmo%d" % i, [128, 512]) for i in range(2)]
                pacc = [psum(st, "pacc%d" % i, [128, 512]) for i in range(2)]
                tpD = psum(st, "tpD", [128, 8, 128], BF16)
                pq = [psum(st, "pq%d" % i, [128, 4, 128]) for i in range(2)]; pqr = Rot("pq", 2)
                uv_i = [0]

                def rstd_chain(src_ap, srckey, smt, smk, jk, jkey):
                    P.op("act", lambda e: e.activation(out=jk[:], in_=src_ap, func=AF.Square, accum_out=smt[:, 0:1]), reads=[srckey], writes=[smk])
                    P.op("dve", lambda e: e.tensor_scalar(out=smt[:, 1:2], in0=smt[:, 0:1], scalar1=1.0 / D, scalar2=EPS, op0=ALU.mult, op1=ALU.add), reads=[smk], writes=[smk])
                    P.op("act", lambda e: e.sqrt(out=smt[:, 2:3], in_=smt[:, 1:2]), reads=[smk], writes=[smk])
                    P.op("dve", lambda e: e.reciprocal(out=smt[:, 3:4], in_=smt[:, 2:3]), reads=[smk], writes=[smk])

                def front(i):
                    b = i % NB; t0 = i * 128
                    xk, x1k, h2k, ek, gk = "xtD", "x1_%d" % b, "h2_%d" % b, "eid%d" % b, "gate%d" % b
                    P.dma("sp", xk, lambda e: e.dma_start(out=xtD_t[:], in_=x[t0:t0 + 128, :]), writes=[xk])
                    P.dma("sp", "mgl", lambda e: e.dma_start(out=mg_sb[:], in_=mgT_d.rearrange("(k p) n -> p k n", p=128)[:, :, t0:t0 + 128]), reads=["mgT_d"], writes=["mg_sb"])
                    for hf in range(2):
                        for k in range(8):
                            P.op("pe", (lambda hf, k: lambda e: e.matmul(pmo[hf][:], lhsT=mg_sb[:, k, :], rhs=w_out_sb[:, k, hf * 512:(hf + 1) * 512], start=(k == 0), stop=(k == 7)))(hf, k),
                                 reads=["mg_sb", "wD"], writes=["pmo%d" % hf])
                        yield
                        P.op("dve", (lambda hf: lambda e: e.tensor_tensor(out=tmpF[:, hf * 512:(hf + 1) * 512], in0=pmo[hf][:], in1=gate1B[:, hf * 512:(hf + 1) * 512], op=ALU.mult))(hf),
                             reads=["pmo%d" % hf, "modB"], writes=["tmpD"])
                        yield
                    P.op("dve", lambda e: e.tensor_tensor(out=x1[b][:], in0=tmpF[:], in1=xtD_t[:], op=ALU.add), reads=["tmpD", xk], writes=[x1k])
                    yield
                    if debug:
                        P.dma("sp", "dbgx1", lambda e: e.dma_start(out=x1_d[t0:t0 + 128, :], in_=x1[b][:]), reads=[x1k], writes=["x1_d"])
                    rstd_chain(x1[b][:], x1k, sm[:, 0:4], "smD", junkD, "junkD")
                    P.op("dve", lambda e: e.scalar_tensor_tensor(out=tmpF[:], in0=x1[b][:], scalar=sm[:, 3:4], in1=G2B, op0=ALU.mult, op1=ALU.mult), reads=[x1k, "smD", "modB"], writes=["tmpD"])
                    yield
                    P.op("dve", lambda e: e.tensor_tensor(out=h2[b][:], in0=tmpF[:], in1=S2B, op=ALU.add), reads=["tmpD", "modB"], writes=[h2k])
                    yield
                    P.op("act", lambda e: e.copy(out=h2b[:], in_=h2[b][:]), reads=[h2k], writes=["h2b"])
                    for k in range(8):
                        P.op("pe", (lambda k: lambda e: e.transpose(tpD[:, k, :], h2b[:, k * 128:(k + 1) * 128], identb[:]))(k), reads=["h2b", "identb"], writes=["tpD"])
                    P.op("act", lambda e: e.copy(out=h2T[:], in_=tpD[:]), reads=["tpD"], writes=["h2T"])
                    yield
                    for g4 in range(4):
                        qi, qk = pqr.next()
                        for bl in range(4):
                            blk = g4 * 4 + bl
                            for k in range(8):
                                P.op("pe", (lambda qi, bl, blk, k: lambda e: e.matmul(pq[qi][:, bl, :], lhsT=w_q_sb[:, k, blk * 128:(blk + 1) * 128], rhs=h2T[:, k, :], start=(k == 0), stop=(k == 7)))(qi, bl, blk, k),
                                     reads=["wD", "h2T"], writes=[qk])
                        P.op("act", (lambda qi, g4: lambda e: e.copy(out=qT_sb2[:, g4 * 4:(g4 + 1) * 4, :], in_=pq[qi][:]))(qi, g4), reads=[qk], writes=["qT_sb2"])
                        yield
                    for g4 in range(4):
                        si, sk = pqr.next()
                        for bl in range(4):
                            blk = g4 * 4 + bl
                            P.op("pe", (lambda si, bl, blk: lambda e: e.matmul(pq[si][:, bl, :], lhsT=qT_sb2[:, blk, :], rhs=skT[:, blk % 2, :], start=True, stop=True))(si, bl, blk),
                                 reads=["qT_sb2", "skT"], writes=[sk])
                        P.op("act", (lambda si, g4: lambda e: e.copy(out=s_sb[:, g4 * 4:(g4 + 1) * 4, :], in_=pq[si][:]))(si, g4), reads=[sk], writes=["s_sb"])
                        yield
                    TK = ["s_sb", "topk"]
                    for blk in range(16):
                        P.op("dve", (lambda blk: lambda e: e.max(out=topv[:, blk, 0:8], in_=s_sb[:, blk, :]))(blk), reads=TK, writes=["topk"])
                        P.op("dve", (lambda blk: lambda e: e.max_index(out=idxu[:, blk, 0:8], in_max=topv[:, blk, 0:8], in_values=s_sb[:, blk, :]))(blk), reads=TK, writes=["topk"])
                        P.op("dve", (lambda blk: lambda e: e.match_replace(out=work[:, blk, :], in_to_replace=topv[:, blk, 0:8], in_values=s_sb[:, blk, :], imm_value=-1e30))(blk), reads=TK, writes=["topk"])
                        yield
                        P.op("dve", (lambda blk: lambda e: e.max(out=topv[:, blk, 8:16], in_=work[:, blk, :]))(blk), reads=TK, writes=["topk"])
                        P.op("dve", (lambda blk: lambda e: e.max_index(out=idxu[:, blk, 8:16], in_max=topv[:, blk, 8:16], in_values=work[:, blk, :]))(blk), reads=TK, writes=["topk"])
                        yield
                    P.op("dve", lambda e: e.tensor_copy(out=idxf[:], in_=idxu[:]), reads=TK, writes=["topk"])
                    tv4 = topv[:].rearrange("p (h n) a -> p h n a", n=2); ix4 = idxf[:].rearrange("p (h n) a -> p h n a", n=2)
                    c4 = cand[:].rearrange("p h (a b) -> p h a b", a=16); ci4 = cidx.rearrange("p h (a b) -> p h a b", a=16)
                    P.op("dve", lambda e: e.tensor_tensor(out=c4, in0=tv4[:, :, 0, :].unsqueeze(3).to_broadcast([128, 8, 16, 16]),
                                                          in1=tv4[:, :, 1, :].unsqueeze(2).to_broadcast([128, 8, 16, 16]), op=ALU.add), reads=TK, writes=["topk"])
                    yield
                    candI = cand[:].bitcast(I32)
                    P.op("dve", lambda e: e.tensor_scalar(out=candI, in0=candI, scalar1=mskI[:, 0:1], scalar2=None, op0=ALU.bitwise_and), reads=TK + ["posI"], writes=["topk"])
                    P.op("dve", lambda e: e.tensor_tensor(out=candI, in0=candI, in1=posI[:].unsqueeze(1).to_broadcast([128, 8, 256]), op=ALU.bitwise_or), reads=TK + ["posI"], writes=["topk"])
                    P.op("dve", lambda e: e.tensor_single_scalar(out=ix4[:, :, 0, :], in_=ix4[:, :, 0, :], scalar=128.0, op=ALU.mult), reads=TK, writes=["topk"])
                    P.op("dve", lambda e: e.tensor_tensor(out=ci4, in0=ix4[:, :, 0, :].unsqueeze(3).to_broadcast([128, 8, 16, 16]),
                                                          in1=ix4[:, :, 1, :].unsqueeze(2).to_broadcast([128, 8, 16, 16]), op=ALU.add), reads=TK, writes=["topk"])
                    yield
                    w2 = work[:].rearrange("p (h n) k -> p h (n k)", n=2)
                    for h in range(8):
                        P.op("dve", (lambda h: lambda e: e.max(out=best[:, h, 0:8], in_=cand[:, h, :]))(h), reads=TK, writes=["topk"])
                        P.op("dve", (lambda h: lambda e: e.match_replace(out=w2[:, h, :], in_to_replace=best[:, h, 0:8], in_values=cand[:, h, :], imm_value=-1e30))(h), reads=TK, writes=["topk"])
                        P.op("dve", (lambda h: lambda e: e.max(out=best[:, h, 8:16], in_=w2[:, h, :]))(h), reads=TK, writes=["topk"])
                        yield
                    P.op("dve", lambda e: e.tensor_single_scalar(out=sm[:, 8:16], in_=best[:, :, 0], scalar=-1.0, op=ALU.mult), reads=TK + ["smD"], writes=["smD"])
                    for h in range(8):
                        P.op("act", (lambda h: lambda e: e.activation(out=eg[:, h, :], in_=best[:, h, :], func=AF.Exp, bias=sm[:, 8 + h:9 + h], accum_out=sm[:, 16 + h:17 + h]))(h),
                             reads=TK + ["smD"], writes=["eg", "smD"])
                    P.op("dve", lambda e: e.reciprocal(out=sm[:, 24:32], in_=sm[:, 16:24]), reads=["smD"], writes=["smD"])
                    P.op("dve", lambda e: e.tensor_tensor(out=gate[b][:].rearrange("p (h k) -> p h k", h=8), in0=eg[:], in1=sm[:, 24:32].unsqueeze(2).to_broadcast([128, 8, 16]), op=ALU.mult),
                         reads=["eg", "smD"], writes=[gk])
                    yield
                    bestI = best[:].bitcast(I32)
                    P.op("dve", lambda e: e.tensor_scalar(out=pab[:, 0], in0=bestI, scalar1=c4I[:, 0:1], scalar2=None, op0=ALU.arith_shift_right), reads=TK + ["posI"], writes=["topk"])
                    P.op("dve", lambda e: e.tensor_scalar(out=pab[:, 0], in0=pab[:, 0], scalar1=c15I[:, 0:1], scalar2=None, op0=ALU.bitwise_and), reads=TK + ["posI"], writes=["topk"])
                    P.op("dve", lambda e: e.tensor_scalar(out=pab[:, 1], in0=bestI, scalar1=c15I[:, 0:1], scalar2=None, op0=ALU.bitwise_and), reads=TK + ["posI"], writes=["topk"])
                    P.op("dve", lambda e: e.tensor_copy(out=pabf[:], in_=pab[:]), reads=TK, writes=["topk"])
                    yield
                    e3 = eidf[:].rearrange("p (h k) -> p h k", h=8)
                    for n_ in range(2):
                        P.op("dve", (lambda n_: lambda e: e.tensor_tensor(out=oh[:], in0=pabf[:, n_].unsqueeze(3).to_broadcast([128, 8, 16, 16]),
                                                                          in1=iota16[:].unsqueeze(1).unsqueeze(1).to_broadcast([128, 8, 16, 16]), op=ALU.is_equal))(n_), reads=TK + ["posI"], writes=["topk"])
                        P.op("dve", (lambda n_: lambda e: e.tensor_tensor(out=oh[:], in0=oh[:], in1=ix4[:, :, n_, :].unsqueeze(2).to_broadcast([128, 8, 16, 16]), op=ALU.mult))(n_), reads=TK, writes=["topk"])
                        P.op("dve", (lambda n_: lambda e: e.tensor_reduce(out=(e3 if n_ == 0 else e3b[:]), in_=oh[:], axis=AX.X, op=ALU.add))(n_), reads=TK, writes=["topk"])
                        yield
                    P.op("dve", lambda e: e.tensor_tensor(out=e3, in0=e3, in1=e3b[:], op=ALU.add), reads=TK, writes=["topk"])
                    P.op("dve", lambda e: e.tensor_scalar(out=eidf[:], in0=eidf[:], scalar1=0.0, scalar2=16383.0, op0=ALU.max, op1=ALU.min), reads=TK, writes=["topk"])
                    P.op("dve", lambda e: e.tensor_copy(out=eid[b][:], in_=eidf[:]), reads=TK, writes=[ek])
                    yield

                def consume(i, fgen):
                    b = i % NB; t0 = i * 128
                    x1k, h2k, ek, gk = "x1_%d" % b, "h2_%d" % b, "eid%d" % b, "gate%d" % b
                    ngrp = 128 // GJ
                    pend = None

                    def finish_group(g, bufs):
                        j0 = g * GJ
                        if "nofin" in KD:
                            return
                        P.op("act", lambda e: e.activation(out=gl_[:, j0:j0 + GJ], in_=aD[:, j0:j0 + GJ], func=AF.Gelu_apprx_tanh), reads=["aD%d" % g], writes=["gl_%d" % g])
                        P.op("dve", lambda e: e.tensor_tensor(out=wD[:, j0:j0 + GJ], in0=gl_[:, j0:j0 + GJ], in1=gate[b][:, j0:j0 + GJ], op=ALU.mult), reads=["gl_%d" % g, gk], writes=["wDw%d" % g])
                        for jj in range(GJ):
                            j = j0 + jj; gi, ugk = bufs[jj]
                            di, dk = dgr.next()
                            P.op("act", (lambda di, j: lambda e: e.activation(out=dg[di][:], in_=identb[:], func=AF.Copy, scale=wD[:, j:j + 1]))(di, j), reads=["wDw%d" % g, "identb"], writes=[dk])
                            for hf in range(2):
                                if "nov" in KD and j not in (0, 127):
                                    continue
                                P.op("pe", (lambda di, gi, hf, j: lambda e: e.matmul(pacc[hf][:], lhsT=dg[di][:], rhs=UVg[gi][:, 1, hf * 512:(hf + 1) * 512], start=(j == 0), stop=(j == 127)))(di, gi, hf, j),
                                     reads=[dk, ugk], writes=["pacc%d" % hf])

                    for g in range(ngrp):
                        bufs = []
                        for jj in range(GJ):
                            j = g * GJ + jj
                            gi = uv_i[0] % NG; uv_i[0] += 1; ugk = "UVg%d" % gi
                            bufs.append((gi, ugk))
                            if "nog" in KD:
                                P.op("pool", (lambda gi: lambda e: e.memset(UVg[gi][:], 1.0))(gi), reads=[ek], writes=[ugk])
                            else:
                                P.dma("pool", ugk, (lambda gi, j: lambda e: e.indirect_dma_start(out=UVg[gi][:].rearrange("p a d -> p (a d)"), out_offset=None, in_=uvb_d,
                                                                                               in_offset=bass.IndirectOffsetOnAxis(ap=eid[b][:, j:j + 1], axis=0)))(gi, j), reads=[ek], writes=[ugk])
                        for jj in range(GJ):
                            j = g * GJ + jj; gi, ugk = bufs[jj]
                            if "noa" in KD:
                                continue
                            jE = tmpF if "f32junk" in KD else junkE
                            P.op("dve", (lambda gi, j, jE: lambda e: e.scalar_tensor_tensor(out=jE[:], in0=UVg[gi][:, 0, :], scalar=1.0, in1=h2[b][:], op0=ALU.mult, op1=ALU.mult, accum_out=aD[:, j:j + 1]))(gi, j, jE),
                                 reads=[ugk, h2k], writes=["aD%d" % g])
                        if pend is not None:
                            finish_group(*pend)
                        pend = (g, bufs)
                        if fgen is not None:
                            for _ in range(3):
                                next(fgen, None)
                    finish_group(*pend)
                    if fgen is not None:
                        for _ in fgen:
                            pass
                    if "dump23" in KD and i == 23:
                        for nm, src_t in (("d_x1", x1[b]), ("d_wD", wD), ("d_aD", aD), ("d_gate", gate[b]), ("d_h2", h2[b])):
                            dd = nc.dram_tensor(nm, list(src_t[:].shape), F32, kind="ExternalOutput").ap()
                            P.dma("sp", "dump", (lambda dd, src_t: lambda e: e.dma_start(out=dd, in_=src_t[:]))(dd, src_t), reads=[x1k, gk, h2k] + ["wDw%d" % q for q in range(32)] + ["aD%d" % q for q in range(32)], writes=["dumpo"])
                        dd2 = nc.dram_tensor("d_eid", [128, 128], I32, kind="ExternalOutput").ap()
                        P.dma("sp", "dump", lambda e: e.dma_start(out=dd2, in_=eid[b][:]), reads=[ek], writes=["dumpo"])
                    if "notail" in KD:
                        return
                    for hf in range(2):
                        P.op("dve", (lambda hf: lambda e: e.tensor_tensor(out=tmpD[:, hf * 512:(hf + 1) * 512], in0=pacc[hf][:], in1=gate2B[:, hf * 512:(hf + 1) * 512], op=ALU.mult))(hf),
                             reads=["pacc%d" % hf, "modB"], writes=["tmpD"])
                    P.op("dve", lambda e: e.tensor_tensor(out=x1[b][:], in0=tmpD[:], in1=x1[b][:], op=ALU.add), reads=["tmpD", x1k], writes=[x1k])
                    rstd_chain(x1[b][:], x1k, sm2[:, 0:4], "smE", junkD, "junkD")
                    P.op("dve", lambda e: e.scalar_tensor_tensor(out=tmpD[:], in0=x1[b][:], scalar=sm2[:, 3:4], in1=fgB[:], op0=ALU.mult, op1=ALU.mult), reads=[x1k, "smE", "fgB"], writes=["tmpD"])
                    P.dma("sp", "outst", lambda e: e.dma_start(out=out[t0:t0 + 128, :], in_=tmpD[:]), reads=["tmpD"], writes=["out"])

                for _ in front(0):
                    pass
                for i in range(NT):
                    if "noc" in KD:
                        break
                    if "noil" in KD:
                        consume(i, None)
                        if i + 1 < NT:
                            for _ in front(i + 1):
                                pass
                        continue
                    consume(i, front(i + 1) if i + 1 < NT else None)
        return finish(nc, P, out)
    return nc


def finish(nc, P, out):
    P.barrier()
    P.emit()
    return nc


def core_inputs(inp, b):
    f = lambda a: np.ascontiguousarray(a, dtype=np.float32)
    return {
        "x": f(inp["x"][b]), "c": f(inp["c"][b:b + 1]), "ctx": f(inp["ctx"][b]), "c_ctx": f(inp["c_ctx"].reshape(1, D)),
        "w_mod": f(inp["w_mod"][0]), "b_mod": f(inp["b_mod"][0].reshape(1, -1)),
        "norm1_g": f(inp["norm1_g"][0].reshape(1, D)), "norm2_g": f(inp["norm2_g"][0].reshape(1, D)),
        "final_g": f(inp["final_g"].reshape(1, D)), "w_in": f(inp["w_in"][0]),
        "ssm_a_re": f(inp["ssm_a_re"][0]), "ssm_a_im": f(inp["ssm_a_im"][0]), "ssm_log_dt": f(inp["ssm_log_dt"][0]),
        "ssm_b_re": f(inp["ssm_b_re"][0]), "ssm_b_im": f(inp["ssm_b_im"][0]),
        "ssm_c_re": f(inp["ssm_c_re"][0]), "ssm_c_im": f(inp["ssm_c_im"][0]),
        "ssm_d": f(inp["ssm_d"][0].reshape(512, 1)), "w_glu": f(inp["w_glu"][0]), "b_glu": f(inp["b_glu"][0].reshape(512, 1)),
        "w_branch_a": f(inp["w_branch_a"][0]), "w_branch_b": f(inp["w_branch_b"][0]), "na_rpb": f(inp["na_rpb"][0]),
        "w_out": f(inp["w_out"][0]), "peer_w_q": f(inp["peer_w_q"][0]), "peer_subkeys": f(inp["peer_subkeys"][0]),
        "peer_uv": f(np.concatenate([inp["peer_u"][0], inp["peer_v"][0]], axis=1)),
    }


def kernel(**inputs):
    nc = build()
    in_maps = [core_inputs(inputs, b) for b in range(8)]
    res = run_bass_kernel_spmd(nc, in_maps, core_ids=list(range(8)))
    return np.stack([np.asarray(r["out"], dtype=np.float32) for r in res.results], axis=0)
```

```python
import math
import numpy as np
import concourse.bass as bass
import concourse.mybir as mybir
from concourse.bass_utils import run_bass_kernel_spmd

F32 = mybir.dt.float32
BF16 = mybir.dt.bfloat16
I32 = mybir.dt.int32
U32 = mybir.dt.uint32
AF = mybir.ActivationFunctionType
ALU = mybir.AluOpType
AX = mybir.AxisListType


class _Op:
    __slots__ = ("eng", "fn", "deps", "seq", "is_dma", "semkey", "signal", "count", "waits")

    def __init__(self, eng, fn, seq, is_dma=False, semkey=None):
        self.eng = eng
        self.fn = fn
        self.deps = []
        self.seq = seq
        self.is_dma = is_dma
        self.semkey = semkey
        self.signal = is_dma
        self.count = 0
        self.waits = []


class Prog:
    ENGS = ("pe", "dve", "act", "pool", "sp")

    def __init__(self, nc):
        self.nc = nc
        self.ops = []
        self.writer = {}
        self.readers = {}
        import os
        self.same_sync = os.environ.get("KSAME", "1") == "1"

    def _add(self, op, reads, writes):
        deps = []
        for r in reads:
            w = self.writer.get(r)
            if w is not None:
                deps.append(w)
        for w_ in writes:
            w = self.writer.get(w_)
            if w is not None:
                deps.append(w)
            deps.extend(self.readers.get(w_, ()))
        op.deps = [d for d in set(deps) if d is not op]
        for r in reads:
            self.readers.setdefault(r, []).append(op)
        for w_ in writes:
            self.writer[w_] = op
            self.readers[w_] = []
        self.ops.append(op)
        return op

    def op(self, eng, fn, reads=(), writes=()):
        return self._add(_Op(eng, fn, len(self.ops)), reads, writes)

    def dma(self, eng, semkey, fn, reads=(), writes=()):
        return self._add(_Op(eng, fn, len(self.ops), True, semkey), reads, writes)

    def emit(self):
        import bisect
        from contextlib import ExitStack
        nc = self.nc
        if not hasattr(self, "_st"):
            self._st = ExitStack(); self._sem = {}; self._cnt = {}; self._hist = {}
            self._seen = {e: {} for e in self.ENGS}; self._done = 0
        ops = self.ops[self._done:]
        self._done = len(self.ops)
        if not ops:
            return

        def same(d, o):
            return d.eng == o.eng and not o.is_dma and (d.eng == "pe" or not self.same_sync)
        for o in ops:
            for d in o.deps:
                if d.is_dma or same(d, o):
                    continue
                assert d.count == 0 or d.signal, "dependency on an already-emitted non-signalling op"
                d.signal = True
        for o in ops:
            if not o.signal:
                continue
            k = ("dma", o.semkey) if o.is_dma else ("eng", o.eng)
            if k not in self._sem:
                self._sem[k] = self._st.enter_context(nc.semaphore("s%d_%s" % (len(self._sem), str(k[1]).replace(" ", ""))))
                self._cnt[k] = 0
            self._cnt[k] += 1
            o.count = self._cnt[k]
            if o.is_dma:
                self._hist.setdefault(o.semkey, []).append(o.seq)
        for o in ops:
            need = {}
            for d in o.deps:
                if d.is_dma:
                    k = ("dma", d.semkey)
                    v = 16 * bisect.bisect_left(self._hist[d.semkey], o.seq)
                else:
                    if same(d, o):
                        continue
                    k = ("eng", d.eng)
                    v = d.count
                if need.get(k, 0) < v:
                    need[k] = v
            sn = self._seen[o.eng]
            o.waits = []
            for k, v in need.items():
                if sn.get(k, 0) < v:
                    sn[k] = v
                    o.waits.append((k, v))
        self.n_sems = len(self._sem)
        sem = self._sem
        with nc.Block() as block:
            per = {e: [o for o in ops if o.eng == e] for e in self.ENGS}

            def run(engobj, lst):
                for o in lst:
                    for k, v in o.waits:
                        engobj.wait_ge(sem[k], v)
                    ins = o.fn(engobj)
                    if o.signal:
                        k = ("dma", o.semkey) if o.is_dma else ("eng", o.eng)
                        ins.then_inc(sem[k], 16 if o.is_dma else 1)
                    o.fn = None

            @block.tensor
            def _(e):
                run(e, per["pe"])

            @block.vector
            def _(e):
                run(e, per["dve"])

            @block.scalar
            def _(e):
                run(e, per["act"])

            @block.gpsimd
            def _(e):
                run(e, per["pool"])

            @block.sync
            def _(e):
                run(e, per["sp"])

    def close(self):
        self.emit()
        if hasattr(self, "_st"):
            self._st.close()

    def barrier(self, flush=True):
        start = getattr(self, "_done", 0)
        last = {}
        dmas = {}
        for o in self.ops[start:]:
            if o.is_dma:
                dmas[o.semkey] = o
            else:
                last[o.eng] = o
        for e, o in getattr(self, "_bar", {}).items():
            last.setdefault(e, o)
        deps = list(last.values()) + list(dmas.values())
        self._bar = {}
        for e in self.ENGS:
            o = _Op(e, lambda eng: eng.nop(), len(self.ops))
            o.deps = [d for d in deps]
            o.signal = True
            self.ops.append(o)
            self._bar[e] = o
        self.writer = {}
        self.readers = {}
        if flush:
            self.emit()


class Rot:
    def __init__(self, name, n):
        self.name, self.n, self.i = name, n, -1

    def next(self):
        self.i = (self.i + 1) % self.n
        return self.i, "%s%d" % (self.name, self.i)


D = 1024
SEQ = 4096
CTX = 256
NTOK = SEQ + CTX
EPS = 1e-6


def build(stage=99, debug=False):
    import os
    KD = os.environ.get("KDBG", "")
    from contextlib import ExitStack
    nc = bass.Bass("TRN2", target_bir_lowering=False)
    P = Prog(nc)

    def din(name, shape, dt=F32):
        return nc.dram_tensor(name, shape, dt, kind="ExternalInput").ap()

    def dscr(name, shape, dt):
        return nc.dram_tensor(name, shape, dt, kind=("ExternalOutput" if debug else "Internal")).ap()

    x = din("x", [SEQ, D]); c = din("c", [1, D]); ctx = din("ctx", [CTX, D]); c_ctx = din("c_ctx", [1, D])
    w_mod = din("w_mod", [D, 6 * D]); b_mod = din("b_mod", [1, 6 * D])
    norm1_g = din("norm1_g", [1, D]); norm2_g = din("norm2_g", [1, D]); final_g = din("final_g", [1, D])
    w_in = din("w_in", [D, 4096])
    a_re = din("ssm_a_re", [2, 32, 64]); a_im = din("ssm_a_im", [2, 32, 64]); log_dt = din("ssm_log_dt", [2, 32])
    b_re = din("ssm_b_re", [2, 32, 64, 16]); b_im = din("ssm_b_im", [2, 32, 64, 16])
    c_re = din("ssm_c_re", [2, 32, 16, 64]); c_im = din("ssm_c_im", [2, 32, 16, 64])
    ssm_d = din("ssm_d", [512, 1]); w_glu = din("w_glu", [512, 512]); b_glu = din("b_glu", [512, 1])
    w_ba = din("w_branch_a", [512, D]); w_bb = din("w_branch_b", [512, D]); rpb = din("na_rpb", [8, 15, 31])
    w_out = din("w_out", [D, D]); w_q = din("peer_w_q", [D, 2048]); subkeys = din("peer_subkeys", [2, 128, 128])
    peer_uv = din("peer_uv", [16384, 2 * D])
    out = nc.dram_tensor("out", [SEQ, D], F32, kind="ExternalOutput").ap()

    uT_d = dscr("uT_d", [512, NTOK], F32)
    kT_d = dscr("kT_d", [512, NTOK], BF16)
    qT_d = dscr("qT_d", [512, SEQ], BF16)
    v_d = dscr("v_d", [NTOK, 512], BF16)
    gT_d = dscr("gT_d", [2048, SEQ], BF16)
    baT_d = dscr("baT_d", [D, SEQ], BF16)
    mgT_d = dscr("mgT_d", [D, SEQ], BF16)
    uvb_d = nc.dram_tensor("uvb_d", [16384, 2 * D], BF16, kind=("ExternalOutput" if (debug and stage == 3.5) else "Internal")).ap()

    from contextlib import contextmanager

    @contextmanager
    def phase():
        stk = ExitStack()
        try:
            yield stk
            P.barrier()
        finally:
            stk.close()

    top = ExitStack()
    with top:
        def sbuf(st, n, s, d=F32):
            return st.enter_context(nc.sbuf_tensor(n, s, d))

        def psum(st, n, s, d=F32):
            return st.enter_context(nc.psum_tensor(n, s, d))

        identf = sbuf(top, "identf", [128, 128])
        identb = sbuf(top, "identb", [128, 128], BF16)
        modB = sbuf(top, "modB", [128, 6 * D])
        P.op("pool", lambda e: e.iota(identf[:], pattern=[[1, 128]], base=0, channel_multiplier=-1,
                                      allow_small_or_imprecise_dtypes=True), writes=["identf"])
        P.op("dve", lambda e: e.tensor_single_scalar(out=identf[:], in_=identf[:], scalar=0.0, op=ALU.is_equal),
             reads=["identf"], writes=["identf"])
        P.op("dve", lambda e: e.tensor_copy(out=identb[:], in_=identf[:]), reads=["identf"], writes=["identb"])

        with phase() as st:
            modcB = sbuf(st, "modcB", [128, 2 * D])
            wm = [sbuf(st, "wm%d" % i, [128, 8, 512]) for i in range(2)]
            w_in_sb = sbuf(st, "w_in_sb", [128, 8, 4096], BF16)
            st0 = ExitStack()
            cc = sbuf(st0, "cc", [128, 2, 8]); sc = sbuf(st0, "sc", [128, 2, 8]); scB = sbuf(st0, "scB", [128, 2, 8, 128])
            bmB = sbuf(st0, "bmB", [128, 6 * D]); gB = sbuf(st0, "gB", [128, 2, D])
            pmod = [psum(st0, "pmod%d" % i, [128, 512]) for i in range(2)]

            P.dma("sp", "c0", lambda e: e.dma_start(out=cc[:, 0, :], in_=c.rearrange("o (k p) -> p (o k)", p=128),
                                                    allow_slow_non_contiguous=True), writes=["cc"])
            P.dma("sp", "c0", lambda e: e.dma_start(out=cc[:, 1, :], in_=c_ctx.rearrange("o (k p) -> p (o k)", p=128),
                                                    allow_slow_non_contiguous=True), writes=["cc"])
            P.dma("act", "c1", lambda e: e.dma_start(out=bmB[:], in_=b_mod.to_broadcast([128, 6 * D])), writes=["bmB"])
            P.dma("act", "c1", lambda e: e.dma_start(out=gB[:, 0, :], in_=norm1_g.to_broadcast([128, D])), writes=["gB"])
            P.dma("act", "c1", lambda e: e.dma_start(out=gB[:, 1, :], in_=norm2_g.to_broadcast([128, D])), writes=["gB"])
            P.op("act", lambda e: e.activation(out=sc[:], in_=cc[:], func=AF.Silu), reads=["cc"], writes=["sc"])
            P.op("dve", lambda e: e.tensor_copy(out=scB[:], in_=sc[:].unsqueeze(3).to_broadcast([128, 2, 8, 128])),
                 reads=["sc"], writes=["scB"])
            w_mod_v = w_mod.rearrange("(k p) n -> p k n", p=128)
            for cch in range(12):
                bi = cch % 2
                P.dma("sp", "wm%d" % bi, (lambda bi, cch: lambda e: e.dma_start(out=wm[bi][:], in_=w_mod_v[:, :, cch * 512:(cch + 1) * 512]))(bi, cch),
                      writes=["wm%d" % bi])
                for which in range(2 if cch < 4 else 1):
                    for k in range(8):
                        P.op("pe", (lambda bi, which, k: lambda e: e.matmul(pmod[which][:], lhsT=scB[:, which, k, :], rhs=wm[bi][:, k, :],
                                                                            start=(k == 0), stop=(k == 7)))(bi, which, k),
                             reads=["scB", "wm%d" % bi], writes=["pmod%d" % which])
                    dst = modB if which == 0 else modcB
                    P.op("dve", (lambda dst, which, cch: lambda e: e.tensor_tensor(out=dst[:, cch * 512:(cch + 1) * 512], in0=pmod[which][:],
                                                                                   in1=bmB[:, cch * 512:(cch + 1) * 512], op=ALU.add))(dst, which, cch),
                         reads=["pmod%d" % which, "bmB"], writes=["modB" if which == 0 else "modcB"])
            for dst, key, off, gi in ((modB, "modB", D, 0), (modcB, "modcB", D, 0), (modB, "modB", 4 * D, 1)):
                P.op("dve", (lambda dst, off, gi: lambda e: e.scalar_tensor_tensor(out=dst[:, off:off + D], in0=dst[:, off:off + D], scalar=1.0,
                                                                                  in1=gB[:, gi, :], op0=ALU.add, op1=ALU.mult))(dst, off, gi),
                     reads=[key, "gB"], writes=[key])

            P.barrier()
            st0.close()
            w_in_v = w_in.rearrange("(k p) n -> p k n", p=128)
            for cch in range(8):
                bi = cch % 2
                P.dma("sp", "wm%d" % bi, (lambda bi, cch: lambda e: e.dma_start(out=wm[bi][:], in_=w_in_v[:, :, cch * 512:(cch + 1) * 512]))(bi, cch),
                      reads=[], writes=["wm%d" % bi])
                eng = ("pool", "dve")[cch % 2]
                P.op(eng, (lambda bi, cch: lambda e: e.tensor_copy(out=w_in_sb[:, :, cch * 512:(cch + 1) * 512], in_=wm[bi][:]))(bi, cch),
                     reads=["wm%d" % bi], writes=["w_in_sb"])

            xt = [sbuf(st, "xt%d" % i, [128, D]) for i in range(3)]; xr = Rot("xt", 3)
            junk = sbuf(st, "junkA", [128, D]); tmpA = sbuf(st, "tmpA", [128, D])
            ss = [sbuf(st, "ss%d" % i, [128, 4]) for i in range(2)]; ssr = Rot("ss", 2)
            hxb = [sbuf(st, "hxb%d" % i, [128, D], BF16) for i in range(2)]; hr = Rot("hxb", 2)
            hxT = [sbuf(st, "hxT%d" % i, [128, 8, 512], BF16) for i in range(2)]; hTr = Rot("hxT", 2)
            st_u = sbuf(st, "st_u", [128, 4, 512]); st_k = sbuf(st, "st_k", [128, 4, 512], BF16)
            st_q = sbuf(st, "st_q", [128, 4, 512], BF16); st_g = sbuf(st, "st_g", [128, 16, 512], BF16)
            st_v = sbuf(st, "st_v", [128, 4, 512], BF16)
            tp = [psum(st, "tpA%d" % i, [128, 8, 128], BF16) for i in range(2)]; tpr = Rot("tpA", 2)
            pj = [psum(st, "pj%d" % i, [128, 512]) for i in range(4)]; pjr = Rot("pj", 4)
            evac_i = [0]

            def evac(dst_ap, src_ap, reads, writes, func=None):
                if func is not None:
                    P.op("act", lambda e: e.activation(out=dst_ap, in_=src_ap, func=func), reads, writes)
                    return
                evac_i[0] += 1
                if evac_i[0] % 2:
                    P.op("act", lambda e: e.copy(out=dst_ap, in_=src_ap), reads, writes)
                else:
                    P.op("dve", lambda e: e.tensor_copy(out=dst_ap, in_=src_ap), reads, writes)

            chunks = [("ctx", 0, 256)] + [("lat", i * 512, 512) for i in range(8)]
            for kind, t0, n in chunks:
                src = ctx if kind == "ctx" else x
                mB, mkey = (modcB, "modcB") if kind == "ctx" else (modB, "modB")
                col0 = t0 if kind == "ctx" else CTX + t0
                hi, hkey = hTr.next()
                for t in range(n // 128):
                    xi, xkey = xr.next()
                    si, skey = ssr.next()
                    bi, bkey = hr.next()
                    pi, pkey = tpr.next()
                    r0 = t0 + t * 128
                    P.dma("sp", xkey, (lambda xi, r0, src: lambda e: e.dma_start(out=xt[xi][:], in_=src[r0:r0 + 128, :]))(xi, r0, src), writes=[xkey])
                    P.op("act", (lambda xi, si: lambda e: e.activation(out=junk[:], in_=xt[xi][:], func=AF.Square, accum_out=ss[si][:, 0:1]))(xi, si),
                         reads=[xkey], writes=["junkA", skey])
                    P.op("dve", (lambda si: lambda e: e.tensor_scalar(out=ss[si][:, 1:2], in0=ss[si][:, 0:1], scalar1=1.0 / D, scalar2=EPS,
                                                                      op0=ALU.mult, op1=ALU.add))(si), reads=[skey], writes=[skey])
                    P.op("act", (lambda si: lambda e: e.sqrt(out=ss[si][:, 2:3], in_=ss[si][:, 1:2]))(si), reads=[skey], writes=[skey])
                    P.op("dve", (lambda si: lambda e: e.reciprocal(out=ss[si][:, 3:4], in_=ss[si][:, 2:3]))(si), reads=[skey], writes=[skey])
                    P.op("dve", (lambda xi, si, mB: lambda e: e.scalar_tensor_tensor(out=tmpA[:], in0=xt[xi][:], scalar=ss[si][:, 3:4], in1=mB[:, D:2 * D],
                                                                                     op0=ALU.mult, op1=ALU.mult))(xi, si, mB),
                         reads=[xkey, skey, mkey], writes=["tmpA"])
                    P.op("dve", (lambda bi, mB: lambda e: e.tensor_tensor(out=hxb[bi][:], in0=tmpA[:], in1=mB[:, 0:D], op=ALU.add))(bi, mB),
                         reads=["tmpA", mkey], writes=[bkey])
                    for k in range(8):
                        P.op("pe", (lambda pi, bi, k: lambda e: e.transpose(tp[pi][:, k, :], hxb[bi][:, k * 128:(k + 1) * 128], identb[:]))(pi, bi, k),
                             reads=[bkey, "identb"], writes=[pkey])
                    P.op("act", (lambda hi, pi, t: lambda e: e.copy(out=hxT[hi][:, :, t * 128:(t + 1) * 128], in_=tp[pi][:]))(hi, pi, t),
                         reads=[pkey], writes=[hkey])
                cts = list(range(0, 8)) + (list(range(12, 32)) if kind == "lat" else [])
                for ct in cts:
                    qi, qkey = pjr.next()
                    for k in range(8):
                        P.op("pe", (lambda qi, hi, k, ct: lambda e: e.matmul(pj[qi][:, 0:n], lhsT=w_in_sb[:, k, ct * 128:(ct + 1) * 128], rhs=hxT[hi][:, k, 0:n],
                                                                             start=(k == 0), stop=(k == 7)))(qi, hi, k, ct),
                             reads=["w_in_sb", hkey], writes=[qkey])
                    if ct < 4:
                        evac(st_u[:, ct, 0:n], pj[qi][:, 0:n], [qkey], ["st_u"])
                    elif ct < 8:
                        evac(st_k[:, ct - 4, 0:n], pj[qi][:, 0:n], [qkey], ["st_k"])
                    elif ct < 16:
                        evac(st_q[:, ct - 12, 0:n], pj[qi][:, 0:n], [qkey], ["st_q"])
                    else:
                        evac(st_g[:, ct - 16, 0:n], pj[qi][:, 0:n], [qkey], ["st_g"], func=AF.Sigmoid)
                P.dma("sp", "stu", (lambda col0, n: lambda e: e.dma_start(out=uT_d.rearrange("(t p) n -> p t n", p=128)[:, :, col0:col0 + n], in_=st_u[:, :, 0:n]))(col0, n),
                      reads=["st_u"], writes=["uT_d"])
                P.dma("sp", "stk", (lambda col0, n: lambda e: e.dma_start(out=kT_d.rearrange("(t p) n -> p t n", p=128)[:, :, col0:col0 + n], in_=st_k[:, :, 0:n]))(col0, n),
                      reads=["st_k"], writes=["kT_d"])
                if kind == "lat":
                    P.dma("sp", "stq", (lambda t0: lambda e: e.dma_start(out=qT_d.rearrange("(t p) n -> p t n", p=128)[:, :, t0:t0 + 512], in_=st_q[:]))(t0),
                          reads=["st_q"], writes=["qT_d"])
                    P.dma("sp", "stg", (lambda t0: lambda e: e.dma_start(out=gT_d.rearrange("(t p) n -> p t n", p=128)[:, :, t0:t0 + 512], in_=st_g[:]))(t0),
                          reads=["st_g"], writes=["gT_d"])
                for t in range(n // 128):
                    qi, qkey = pjr.next()
                    for k in range(8):
                        P.op("pe", (lambda qi, hi, k, t: lambda e: e.matmul(pj[qi][:], lhsT=hxT[hi][:, k, t * 128:(t + 1) * 128], rhs=w_in_sb[:, k, 1024:1536],
                                                                            start=(k == 0), stop=(k == 7)))(qi, hi, k, t),
                             reads=["w_in_sb", hkey], writes=[qkey])
                    evac(st_v[:, t, :], pj[qi][:], [qkey], ["st_v"])
                nt = n // 128
                P.dma("sp", "stv", (lambda col0, nt: lambda e: e.dma_start(out=v_d[col0:col0 + nt * 128, :].rearrange("(t p) n -> p t n", p=128), in_=st_v[:, 0:nt, :]))(col0, nt),
                      reads=["st_v"], writes=["v_d"])
        P.barrier()
        if stage <= 1:
            return finish(nc, P, out)

        yT_d = dscr("yT_d", [512, SEQ], F32) if debug else None
        TWO_PI = 2.0 * math.pi
        with ExitStack() as stB:
            zT = sbuf(stB, "zT", [128, 4, SEQ], BF16)
            with phase() as st:
                def t32(n):
                    return sbuf(st, n, [128, 32])
                are, aim, ldt = t32("are"), t32("aim"), t32("ldt")
                Bn = [sbuf(st, "Bn%d" % i, [128, 32, 16]) for i in range(2)]
                bb = [sbuf(st, "bb%d" % i, [128, 32, 16]) for i in range(2)]
                tmpb = sbuf(st, "tmpb", [128, 32, 16])
                Cn2 = [sbuf(st, "Cn2%d" % i, [128, 8, 2, 64]) for i in range(2)]
                dsk = sbuf(st, "dsk", [128, 4])
                maskf = sbuf(st, "maskf", [128, 4, 2]); mask2 = sbuf(st, "mask2", [128, 4, 2])
                pwr = sbuf(st, "pwr", [128, 13, 32]); pwi = sbuf(st, "pwi", [128, 13, 32]); npwi = sbuf(st, "npwi", [128, 13, 32])
                kint = sbuf(st, "kint", [128, 32], I32)
                names = ["dt", "er", "th", "mag", "kf", "rr", "half", "sn", "ah", "cq", "sinr", "cosr", "nre", "den", "rden",
                         "fre", "fim", "t1", "t2"]
                T = {n: t32("p_" + n) for n in names}
                uT_sb = [sbuf(st, "uT_sb%d" % i, [128, NTOK]) for i in range(1)]
                PL = [sbuf(st, "PL%d" % i, [128, 2, NTOK]) for i in range(2)]
                yT = sbuf(st, "yT", [128, SEQ])
                Z = [sbuf(st, "Z%d" % i, [128, 2, 128]) for i in range(2)]
                Zc = [sbuf(st, "Zc%d" % i, [128, 2, 128]) for i in range(2)]
                LB = [sbuf(st, "LB%d" % i, [128, 2, 128]) for i in range(2)]
                LC = [sbuf(st, "LC%d" % i, [128, 2, 128]) for i in range(2)]
                pz = [psum(st, "pz%d" % i, [128, 2, 128]) for i in range(2)]; pzr = Rot("pz", 2)
                pb = [psum(st, "pb%d" % i, [128, 512]) for i in range(3)]; pbr = Rot("pb", 3)
                py = [psum(st, "py%d" % i, [128, 512]) for i in range(2)]; pyr = Rot("py", 2)

                for gl in range(2):
                    sl = slice(gl * 64, (gl + 1) * 64)
                    for dst, srcp, key in ((are, a_re, "are"), (aim, a_im, "aim")):
                        P.dma("act", "pb0", (lambda dst, srcp, sl, gl: lambda e: e.dma_start(
                            out=dst[sl, :].rearrange("p (d g) -> p d g", d=2),
                            in_=srcp.rearrange("d (gp gl) p -> gl p d gp", gl=2)[gl], allow_slow_non_contiguous=True))(dst, srcp, sl, gl), writes=[key])
                    P.dma("act", "pb0", (lambda sl, gl: lambda e: e.dma_start(
                        out=ldt[sl, :].rearrange("p (d g) -> p d g", d=2),
                        in_=log_dt.rearrange("d (gp gl) -> gl d gp", gl=2)[gl:gl + 1].to_broadcast([64, 2, 16]), allow_slow_non_contiguous=True))(sl, gl), writes=["ldt"])
                    for i, srcp in enumerate((b_re, b_im)):
                        P.dma("act", "pb0", (lambda i, srcp, sl, gl: lambda e: e.dma_start(
                            out=Bn[i][sl].rearrange("p (d g) h -> p d g h", d=2),
                            in_=srcp.rearrange("d (gp gl) p h -> gl p d gp h", gl=2)[gl]))(i, srcp, sl, gl), writes=["Bn%d" % i])
                for i, srcp in enumerate((c_re, c_im)):
                    for j in range(2):
                        P.dma("act", "pb0", (lambda i, srcp, j: lambda e: e.dma_start(
                            out=Cn2[i][:, :, j, :].rearrange("p (d u) q -> p d u q", d=2),
                            in_=srcp.rearrange("d (ut g8) h p -> (g8 h) d ut p", g8=8)))(i, srcp, j), writes=["Cn2%d" % i])
                P.dma("act", "pb0", lambda e: e.dma_start(out=dsk[:], in_=ssm_d.rearrange("(ut p) o -> p (ut o)", p=128), allow_slow_non_contiguous=True), writes=["dsk"])
                P.op("pool", lambda e: e.iota(maskf[:], pattern=[[-32, 4], [-16, 2]], base=0, channel_multiplier=1, allow_small_or_imprecise_dtypes=True), writes=["maskf"])
                P.op("dve", lambda e: e.tensor_single_scalar(out=mask2[:], in_=maskf[:], scalar=0.0, op=ALU.is_ge), reads=["maskf"], writes=["mask2"])
                P.op("dve", lambda e: e.tensor_single_scalar(out=maskf[:], in_=maskf[:], scalar=16.0, op=ALU.is_lt), reads=["maskf", "mask2"], writes=["maskf"])
                P.op("dve", lambda e: e.tensor_tensor(out=maskf[:], in0=maskf[:], in1=mask2[:], op=ALU.mult), reads=["maskf", "mask2"], writes=["maskf"])

                PK = ["are", "aim", "ldt", "Bn0", "Bn1", "prm"]

                def dve(fn):
                    P.op("dve", fn, reads=PK, writes=["prm"])

                def act(fn):
                    P.op("act", fn, reads=PK, writes=["prm"])
                act(lambda e: e.activation(out=T["dt"][:], in_=ldt[:], func=AF.Exp))
                dve(lambda e: e.tensor_tensor(out=T["er"][:], in0=are[:], in1=T["dt"][:], op=ALU.mult))
                dve(lambda e: e.tensor_tensor(out=T["th"][:], in0=aim[:], in1=T["dt"][:], op=ALU.mult))
                act(lambda e: e.activation(out=T["mag"][:], in_=T["er"][:], func=AF.Exp))
                dve(lambda e: e.tensor_single_scalar(out=T["kf"][:], in_=T["th"][:], scalar=1.0 / TWO_PI, op=ALU.mult))
                dve(lambda e: e.tensor_copy(out=kint[:], in_=T["kf"][:]))
                dve(lambda e: e.tensor_copy(out=T["kf"][:], in_=kint[:]))
                dve(lambda e: e.scalar_tensor_tensor(out=T["rr"][:], in0=T["kf"][:], scalar=-TWO_PI, in1=T["th"][:], op0=ALU.mult, op1=ALU.add))
                dve(lambda e: e.tensor_single_scalar(out=T["half"][:], in_=T["rr"][:], scalar=0.5, op=ALU.mult))
                act(lambda e: e.activation(out=T["ah"][:], in_=T["half"][:], func=AF.Abs))
                dve(lambda e: e.tensor_scalar(out=T["t1"][:], in0=T["ah"][:], scalar1=-1.0, scalar2=math.pi / 2, op0=ALU.mult, op1=ALU.add))
                act(lambda e: e.activation(out=T["sn"][:], in_=T["half"][:], func=AF.Sin))
                act(lambda e: e.activation(out=T["cq"][:], in_=T["t1"][:], func=AF.Sin))
                dve(lambda e: e.scalar_tensor_tensor(out=T["sinr"][:], in0=T["sn"][:], scalar=2.0, in1=T["cq"][:], op0=ALU.mult, op1=ALU.mult))
                dve(lambda e: e.scalar_tensor_tensor(out=T["t2"][:], in0=T["sn"][:], scalar=-2.0, in1=T["sn"][:], op0=ALU.mult, op1=ALU.mult))
                dve(lambda e: e.tensor_single_scalar(out=T["cosr"][:], in_=T["t2"][:], scalar=1.0, op=ALU.add))
                dve(lambda e: e.tensor_tensor(out=pwr[:, 0, :], in0=T["mag"][:], in1=T["cosr"][:], op=ALU.mult))
                dve(lambda e: e.tensor_tensor(out=pwi[:, 0, :], in0=T["mag"][:], in1=T["sinr"][:], op=ALU.mult))
                dve(lambda e: e.tensor_single_scalar(out=T["nre"][:], in_=pwr[:, 0, :], scalar=-1.0, op=ALU.add))
                dve(lambda e: e.tensor_tensor(out=T["den"][:], in0=are[:], in1=are[:], op=ALU.mult))
                dve(lambda e: e.tensor_tensor(out=T["t1"][:], in0=aim[:], in1=aim[:], op=ALU.mult))
                dve(lambda e: e.tensor_tensor(out=T["den"][:], in0=T["den"][:], in1=T["t1"][:], op=ALU.add))
                dve(lambda e: e.reciprocal(out=T["rden"][:], in_=T["den"][:]))
                dve(lambda e: e.tensor_tensor(out=T["t1"][:], in0=T["nre"][:], in1=are[:], op=ALU.mult))
                dve(lambda e: e.tensor_tensor(out=T["t2"][:], in0=pwi[:, 0, :], in1=aim[:], op=ALU.mult))
                dve(lambda e: e.tensor_tensor(out=T["t1"][:], in0=T["t1"][:], in1=T["t2"][:], op=ALU.add))
                dve(lambda e: e.tensor_tensor(out=T["fre"][:], in0=T["t1"][:], in1=T["rden"][:], op=ALU.mult))
                dve(lambda e: e.tensor_tensor(out=T["t1"][:], in0=pwi[:, 0, :], in1=are[:], op=ALU.mult))
                dve(lambda e: e.tensor_tensor(out=T["t2"][:], in0=T["nre"][:], in1=aim[:], op=ALU.mult))
                dve(lambda e: e.tensor_tensor(out=T["t1"][:], in0=T["t1"][:], in1=T["t2"][:], op=ALU.subtract))
                dve(lambda e: e.tensor_tensor(out=T["fim"][:], in0=T["t1"][:], in1=T["rden"][:], op=ALU.mult))
                fr = T["fre"][:].unsqueeze(2).to_broadcast([128, 32, 16]); fi = T["fim"][:].unsqueeze(2).to_broadcast([128, 32, 16])
                dve(lambda e: e.tensor_tensor(out=bb[0][:], in0=Bn[0][:], in1=fr, op=ALU.mult))
                dve(lambda e: e.tensor_tensor(out=tmpb[:], in0=Bn[1][:], in1=fi, op=ALU.mult))
                dve(lambda e: e.tensor_tensor(out=bb[0][:], in0=bb[0][:], in1=tmpb[:], op=ALU.subtract))
                dve(lambda e: e.tensor_tensor(out=bb[1][:], in0=Bn[1][:], in1=fr, op=ALU.mult))
                dve(lambda e: e.tensor_tensor(out=tmpb[:], in0=Bn[0][:], in1=fi, op=ALU.mult))
                dve(lambda e: e.tensor_tensor(out=bb[1][:], in0=bb[1][:], in1=tmpb[:], op=ALU.add))
                for k in range(12):
                    dve((lambda k: lambda e: e.tensor_tensor(out=T["t1"][:], in0=pwr[:, k, :], in1=pwr[:, k, :], op=ALU.mult))(k))
                    dve((lambda k: lambda e: e.tensor_tensor(out=T["t2"][:], in0=pwi[:, k, :], in1=pwi[:, k, :], op=ALU.mult))(k))
                    dve((lambda k: lambda e: e.tensor_tensor(out=pwr[:, k + 1, :], in0=T["t1"][:], in1=T["t2"][:], op=ALU.subtract))(k))
                    dve((lambda k: lambda e: e.scalar_tensor_tensor(out=pwi[:, k + 1, :], in0=pwr[:, k, :], scalar=2.0, in1=pwi[:, k, :], op0=ALU.mult, op1=ALU.mult))(k))
                dve(lambda e: e.tensor_single_scalar(out=npwi[:], in_=pwi[:], scalar=-1.0, op=ALU.mult))

                def cmul_acc(hi_re, hi_im, lo_re, lo_im, k, u, key):
                    sr = pwr[:, k, u:u + 1]; si = pwi[:, k, u:u + 1]; nsi = npwi[:, k, u:u + 1]
                    for o_, a_, s_ in ((hi_re, lo_re, sr), (hi_re, lo_im, nsi), (hi_im, lo_re, si), (hi_im, lo_im, sr)):
                        P.op("dve", (lambda o_, a_, s_: lambda e: e.scalar_tensor_tensor(out=o_, in0=a_, scalar=s_, in1=o_, op0=ALU.mult, op1=ALU.add))(o_, a_, s_),
                             reads=[key, "prm"], writes=[key])

                def bk_scan(pl, c0, n, rev, u, key):
                    L = n.bit_length() - 1
                    re = pl[:, 0, c0:c0 + n]; im = pl[:, 1, c0:c0 + n]
                    for k in range(L):
                        s_ = 2 << k; h_ = 1 << k
                        vr = re.rearrange("p (m s) -> p m s", s=s_); vi = im.rearrange("p (m s) -> p m s", s=s_)
                        if not rev:
                            cmul_acc(vr[:, :, s_ - 1], vi[:, :, s_ - 1], vr[:, :, h_ - 1], vi[:, :, h_ - 1], k, u, key)
                        else:
                            cmul_acc(vr[:, :, 0], vi[:, :, 0], vr[:, :, h_], vi[:, :, h_], k, u, key)
                    for k in range(L - 2, -1, -1):
                        s_ = 2 << k; h_ = 1 << k
                        vr = re.rearrange("p (m s) -> p m s", s=s_); vi = im.rearrange("p (m s) -> p m s", s=s_)
                        if not rev:
                            cmul_acc(vr[:, 1:, h_ - 1], vi[:, 1:, h_ - 1], vr[:, :-1, s_ - 1], vi[:, :-1, s_ - 1], k, u, key)
                        else:
                            cmul_acc(vr[:, :-1, h_], vi[:, :-1, h_], vr[:, 1:, 0], vi[:, 1:, 0], k, u, key)

                segs = [(0, 256)] + [(CTX + i * 512, 512) for i in range(8)]
                units = [(ut, d_, gpl) for ut in range(4) for d_ in range(2) for gpl in range(4)]

                def stA(ix):
                    ut, d_, gpl = units[ix]
                    u = d_ * 16 + ut * 4 + gpl
                    bi = ix % 2; ub = 0; ukey = "uT_sb0"
                    zk, zck, lbk, lck, plk = "Z%d" % bi, "Zc%d" % bi, "LB%d" % bi, "LC%d" % bi, "PL%d" % bi
                    if ix % 8 == 0:
                        P.dma("sp", ukey, lambda e: e.dma_start(out=uT_sb[ub][:], in_=uT_d[ut * 128:(ut + 1) * 128, :]), reads=["uT_d"], writes=[ukey])
                    P.op("pool", lambda e: e.memset(Z[bi][:], 0.0), writes=[zk])
                    for j in range(2):
                        for gl in range(2):
                            cs = (2 * gpl + gl) * 16
                            P.op("pool", (lambda j, gl, cs: lambda e: e.tensor_copy(out=Z[bi][gl * 64:(gl + 1) * 64, j, cs:cs + 16], in_=bb[j][gl * 64:(gl + 1) * 64, u, :]))(j, gl, cs),
                                 reads=["prm"], writes=[zk])
                    zi, zkey = pzr.next()
                    for j in range(2):
                        P.op("pe", (lambda zi, j: lambda e: e.matmul(pz[zi][:, j, :], lhsT=Z[bi][:, j, :], rhs=identf[:], start=True, stop=True))(zi, j), reads=[zk, "identf"], writes=[zkey])
                    P.op("act", (lambda zi: lambda e: e.copy(out=LB[bi][:], in_=pz[zi][:]))(zi), reads=[zkey], writes=[lbk])
                    for j in range(2):
                        P.op("pool", (lambda j: lambda e: e.tensor_tensor(out=Zc[bi][:, j, :].rearrange("p (g q) -> p g q", g=2), in0=Cn2[j][:, d_ * 4 + ut, :, :],
                                                                         in1=maskf[:, gpl, :].unsqueeze(2).to_broadcast([128, 2, 64]), op=ALU.mult))(j),
                             reads=["Cn2%d" % j, "maskf"], writes=[zck])
                    zi2, zkey2 = pzr.next()
                    for j in range(2):
                        P.op("pe", (lambda zi2, j: lambda e: e.matmul(pz[zi2][:, j, :], lhsT=Zc[bi][:, j, :], rhs=identf[:], start=True, stop=True))(zi2, j), reads=[zck, "identf"], writes=[zkey2])
                    P.op("act", lambda e: e.copy(out=LC[bi][:, 0, :], in_=pz[zi2][:, 0, :]), reads=[zkey2], writes=[lck])
                    P.op("act", lambda e: e.mul(out=LC[bi][:, 1, :], in_=pz[zi2][:, 1, :], mul=-1.0), reads=[zkey2], writes=[lck])
                    for (c0, n) in segs:
                        for j in range(2):
                            qi, qkey = pbr.next()
                            P.op("pe", (lambda qi, j, c0, n: lambda e: e.matmul(pb[qi][:, 0:n], lhsT=LB[bi][:, j, :], rhs=uT_sb[ub][:, c0:c0 + n], start=True, stop=True))(qi, j, c0, n),
                                 reads=[lbk, ukey], writes=[qkey])
                            P.op("act", (lambda qi, j, c0, n: lambda e: e.copy(out=PL[bi][:, j, c0:c0 + n], in_=pb[qi][:, 0:n]))(qi, j, c0, n), reads=[qkey], writes=[plk])

                def stB(ix):
                    ut, d_, gpl = units[ix]
                    u = d_ * 16 + ut * 4 + gpl
                    bi = ix % 2; plk = "PL%d" % bi
                    rev = (d_ == 1)
                    bk_scan(PL[bi], 0, CTX, rev, u, plk)
                    if not rev:
                        cmul_acc(PL[bi][:, 0, CTX:CTX + 1], PL[bi][:, 1, CTX:CTX + 1], PL[bi][:, 0, CTX - 1:CTX], PL[bi][:, 1, CTX - 1:CTX], 0, u, plk)
                    else:
                        cmul_acc(PL[bi][:, 0, NTOK - 1:NTOK], PL[bi][:, 1, NTOK - 1:NTOK], PL[bi][:, 0, 0:1], PL[bi][:, 1, 0:1], 0, u, plk)
                    bk_scan(PL[bi], CTX, SEQ, rev, u, plk)

                def stC(ix):
                    ut, d_, gpl = units[ix]
                    bi = ix % 2; ub = 0; ukey = "uT_sb0"; lck, plk = "LC%d" % bi, "PL%d" % bi
                    first = (ix % 8 == 0)
                    for sgi in range(8):
                        c0 = CTX + sgi * 512
                        yi, ykey = pyr.next()
                        for j in range(2):
                            P.op("pe", (lambda yi, j, c0: lambda e: e.matmul(py[yi][:], lhsT=LC[bi][:, j, :], rhs=PL[bi][:, j, c0:c0 + 512], start=(j == 0), stop=(j == 1)))(yi, j, c0),
                                 reads=[lck, plk], writes=[ykey])
                        ysl = slice(sgi * 512, (sgi + 1) * 512)
                        if first:
                            P.op("dve", (lambda yi, c0, ysl: lambda e: e.scalar_tensor_tensor(out=yT[:, ysl], in0=uT_sb[ub][:, c0:c0 + 512], scalar=dsk[:, ut:ut + 1],
                                                                                              in1=py[yi][:], op0=ALU.mult, op1=ALU.add))(yi, c0, ysl),
                                 reads=[ykey, ukey, "dsk"], writes=["yT"])
                        else:
                            P.op("dve", (lambda yi, ysl: lambda e: e.tensor_tensor(out=yT[:, ysl], in0=yT[:, ysl], in1=py[yi][:], op=ALU.add))(yi, ysl), reads=[ykey], writes=["yT"])
                    if ix % 8 == 7:
                        if debug:
                            P.dma("sp", "dbgy", lambda e: e.dma_start(out=yT_d[ut * 128:(ut + 1) * 128, :], in_=yT[:]), reads=["yT"], writes=["yT_d"])
                        P.op("act", lambda e: e.activation(out=zT[:, ut, :], in_=yT[:], func=AF.Gelu_apprx_tanh), reads=["yT"], writes=["zT"])

                stA(0)
                for ix in range(32):
                    if ix + 1 < 32:
                        stA(ix + 1)
                    stB(ix)
                    stC(ix)
            P.barrier()
            with phase() as st:
                wstgB_t = sbuf(st, "wstgB", [128, 4, 1024])
                w_glu_sb = sbuf(st, "w_glu_sb", [128, 4, 512], BF16); w_ba_sb = sbuf(st, "w_ba_sb", [128, 4, D], BF16)
                bglu = sbuf(st, "bglu", [128, 4])
                sg = [sbuf(st, "sg%d" % i, [128, 512], BF16) for i in range(2)]; sgr = Rot("sg", 2)
                glu = [sbuf(st, "glu%d" % i, [128, 4, 512], BF16) for i in range(2)]
                st_ba = [sbuf(st, "st_ba%d" % i, [128, 8, 512], BF16) for i in range(2)]
                pg = [psum(st, "pg%d" % i, [128, 512]) for i in range(3)]; pgr = Rot("pg", 3)
                pa = [psum(st, "pa%d" % i, [128, 512]) for i in range(3)]; par = Rot("pa", 3)
                P.dma("sp", "wl0", lambda e: e.dma_start(out=wstgB_t[:, :, 0:512], in_=w_glu.rearrange("(k p) n -> p k n", p=128)), writes=["wstgB"])
                P.op("dve", lambda e: e.tensor_copy(out=w_glu_sb[:], in_=wstgB_t[:, :, 0:512]), reads=["wstgB"], writes=["w_glu_sb"])
                P.dma("sp", "wl0", lambda e: e.dma_start(out=wstgB_t[:], in_=w_ba.rearrange("(k p) n -> p k n", p=128)), reads=["wstgB"], writes=["wstgB"])
                P.op("dve", lambda e: e.tensor_copy(out=w_ba_sb[:], in_=wstgB_t[:]), reads=["wstgB"], writes=["w_ba_sb"])
                P.dma("act", "wl1", lambda e: e.dma_start(out=bglu[:], in_=b_glu.rearrange("(k p) o -> p (k o)", p=128), allow_slow_non_contiguous=True), writes=["bglu"])
                for sgi in range(8):
                    gb_ = sgi % 2; gkey = "glu%d" % gb_; bakey = "st_ba%d" % gb_
                    ssl = slice(sgi * 512, (sgi + 1) * 512)
                    for ct in range(4):
                        gi, gk = pgr.next()
                        for k in range(4):
                            P.op("pe", (lambda gi, k, ct, ssl: lambda e: e.matmul(pg[gi][:], lhsT=w_glu_sb[:, k, ct * 128:(ct + 1) * 128], rhs=zT[:, k, ssl],
                                                                                  start=(k == 0), stop=(k == 3)))(gi, k, ct, ssl),
                                 reads=["w_glu_sb", "zT"], writes=[gk])
                        si_, sk_ = sgr.next()
                        P.op("act", (lambda si_, gi, ct: lambda e: e.activation(out=sg[si_][:], in_=pg[gi][:], func=AF.Sigmoid, bias=bglu[:, ct:ct + 1]))(si_, gi, ct),
                             reads=[gk, "bglu"], writes=[sk_])
                        P.op("dve", (lambda gb_, ct, si_, ssl: lambda e: e.tensor_tensor(out=glu[gb_][:, ct, :], in0=sg[si_][:], in1=zT[:, ct, ssl], op=ALU.mult))(gb_, ct, si_, ssl),
                             reads=[sk_, "zT"], writes=[gkey])
                    for ct2 in range(8):
                        ai, ak = par.next()
                        for k in range(4):
                            P.op("pe", (lambda ai, k, ct2, gb_: lambda e: e.matmul(pa[ai][:], lhsT=w_ba_sb[:, k, ct2 * 128:(ct2 + 1) * 128], rhs=glu[gb_][:, k, :],
                                                                                   start=(k == 0), stop=(k == 3)))(ai, k, ct2, gb_),
                                 reads=["w_ba_sb", gkey], writes=[ak])
                        if ct2 % 2:
                            P.op("act", (lambda gb_, ct2, ai: lambda e: e.copy(out=st_ba[gb_][:, ct2, :], in_=pa[ai][:]))(gb_, ct2, ai), reads=[ak], writes=[bakey])
                        else:
                            P.op("dve", (lambda gb_, ct2, ai: lambda e: e.tensor_copy(out=st_ba[gb_][:, ct2, :], in_=pa[ai][:]))(gb_, ct2, ai), reads=[ak], writes=[bakey])
                    P.dma("sp", bakey, (lambda gb_, ssl: lambda e: e.dma_start(out=baT_d.rearrange("(t p) n -> p t n", p=128)[:, :, ssl], in_=st_ba[gb_][:]))(gb_, ssl),
                          reads=[bakey], writes=["baT_d"])
        P.barrier()
        if stage <= 2:
            return finish(nc, P, out)

        attT_d = dscr("attT_d", [512, SEQ], BF16) if debug else None
        NEG = -30000.0
        with ExitStack() as stC:
            attT_sb = sbuf(stC, "attT_sb", [128, 4, SEQ], BF16)
            stC2 = ExitStack()
            kT_sb = sbuf(stC2, "kT_sb", [128, 4, NTOK], BF16); qT_sb = sbuf(stC2, "qT_sb", [128, 4, SEQ], BF16)
            BiasTT = sbuf(stC2, "BiasTT", [128, 8 * 14, 64])
            Vctx = sbuf(stC2, "Vctx", [128, 2, 512], BF16)
            ones_b = sbuf(stC2, "ones_b", [128, 128], BF16)
            P.dma("sp", "lc0", lambda e: e.dma_start(out=kT_sb[:], in_=kT_d.rearrange("(t p) n -> p t n", p=128)), reads=["kT_d"], writes=["kT_sb"])
            P.dma("act", "lc1", lambda e: e.dma_start(out=qT_sb[:], in_=qT_d.rearrange("(t p) n -> p t n", p=128)), reads=["qT_d"], writes=["qT_sb"])
            P.dma("act", "lc1", lambda e: e.dma_start(out=Vctx[:], in_=v_d[0:CTX, :].rearrange("(t p) n -> p t n", p=128)), reads=["v_d"], writes=["Vctx"])
            P.op("pool", lambda e: e.memset(ones_b[:], 1.0), writes=["ones_b"])
            with phase() as st:
                rpbB = sbuf(st, "rpbB", [128, 8 * 14, 31]); tmpC = sbuf(st, "tmpC", [128, 8 * 14, 64])
                Dm = sbuf(st, "Dm", [128, 64]); eqm = [sbuf(st, "eqm%d" % i, [128, 64]) for i in range(2)]
                c0t = sbuf(st, "c0t", [128, 64]); kcv = sbuf(st, "kcv", [128, 64]); m2 = sbuf(st, "m2c", [128, 64])
                for half in range(2):
                    sl = slice(half * 64, (half + 1) * 64)
                    P.dma("sp", "lc2", (lambda sl, half: lambda e: e.dma_start(out=rpbB[sl].rearrange("p (h j) m -> p h (j m)", h=8),
                                                                              in_=rpb[:, half:half + 14, :].rearrange("h j m -> h (j m)").unsqueeze(0).to_broadcast([64, 8, 14 * 31])))(sl, half),
                          writes=["rpbB"])
                    P.op("pool", (lambda sl: lambda e: e.iota(Dm[sl], pattern=[[-1, 64]], base=15, channel_multiplier=1, allow_small_or_imprecise_dtypes=True))(sl), writes=["Dm"])
                    P.op("pool", (lambda sl: lambda e: e.iota(kcv[sl], pattern=[[0, 64]], base=0, channel_multiplier=1, allow_small_or_imprecise_dtypes=True))(sl), writes=["kcv"])
                P.op("pool", lambda e: e.iota(c0t[:], pattern=[[1, 64]], base=-8, channel_multiplier=0, allow_small_or_imprecise_dtypes=True), writes=["c0t"])
                P.op("dve", lambda e: e.tensor_scalar(out=c0t[:], in0=c0t[:], scalar1=0.0, scalar2=48.0, op0=ALU.max, op1=ALU.min), reads=["c0t"], writes=["c0t"])
                P.op("dve", lambda e: e.tensor_tensor(out=kcv[:], in0=kcv[:], in1=c0t[:], op=ALU.subtract), reads=["kcv", "c0t"], writes=["kcv"])
                P.op("dve", lambda e: e.tensor_single_scalar(out=m2[:], in_=kcv[:], scalar=0.0, op=ALU.is_ge), reads=["kcv"], writes=["m2c"])
                P.op("dve", lambda e: e.tensor_single_scalar(out=kcv[:], in_=kcv[:], scalar=15.0, op=ALU.is_le), reads=["kcv", "m2c"], writes=["kcv"])
                P.op("dve", lambda e: e.tensor_tensor(out=m2[:], in0=m2[:], in1=kcv[:], op=ALU.mult), reads=["kcv", "m2c"], writes=["m2c"])
                P.op("dve", lambda e: e.tensor_scalar(out=m2[:], in0=m2[:], scalar1=-1.0, scalar2=-NEG, op0=ALU.add, op1=ALU.mult), reads=["m2c"], writes=["m2c"])
                P.op("dve", lambda e: e.tensor_copy(out=BiasTT[:], in_=m2[:].unsqueeze(1).to_broadcast([128, 112, 64])), reads=["m2c"], writes=["BiasTT"])
                for m in range(31):
                    ei = m % 2; ek = "eqm%d" % ei
                    P.op("dve", (lambda ei, m: lambda e: e.tensor_single_scalar(out=eqm[ei][:], in_=Dm[:], scalar=float(m), op=ALU.is_equal))(ei, m), reads=["Dm"], writes=[ek])
                    P.op("pool", (lambda ei, m: lambda e: e.tensor_tensor(out=tmpC[:], in0=eqm[ei][:].unsqueeze(1).to_broadcast([128, 112, 64]),
                                                                          in1=rpbB[:, :, m:m + 1].to_broadcast([128, 112, 64]), op=ALU.mult))(ei, m),
                         reads=[ek, "rpbB"], writes=["tmpC"])
                    P.op("dve", lambda e: e.tensor_tensor(out=BiasTT[:], in0=BiasTT[:], in1=tmpC[:], op=ALU.add), reads=["tmpC"], writes=["BiasTT"])
            P.barrier()
            with phase() as st:
                Vb = [sbuf(st, "Vb%d" % i, [128, 4, 512], BF16) for i in range(3)]; vbr = Rot("Vb", 3)
                ssb = [sbuf(st, "ssb%d" % i, [128, 4, 64]) for i in range(3)]; ssr2 = Rot("ssb", 3)
                pT = [sbuf(st, "pT%d" % i, [128, 384], BF16) for i in range(3)]; ptr = Rot("pT", 3)
                rden = [sbuf(st, "rden%d" % i, [128, 64]) for i in range(2)]; rdr = Rot("rden", 2)
                ps_ = [psum(st, "psc%d" % i, [128, 512]) for i in range(3)]; psr = Rot("psc", 3)
                po_ = [psum(st, "poc%d" % i, [128, 512]) for i in range(2)]; por = Rot("poc", 2)
                pd_ = [psum(st, "pdc%d" % i, [128, 512]) for i in range(2)]; pdr = Rot("pdc", 2)
                B4 = BiasTT[:].rearrange("p (h j) q -> p h j q", h=8)
                cf = [sbuf(st, "cvf%d" % i, [128, 2048]) for i in range(3)]; cb = [sbuf(st, "cvb%d" % i, [128, 2048], BF16) for i in range(3)]

                def convert_tile(ti):
                    bi = ti % 3
                    P.dma("sp", "cvf%d" % bi, lambda e: e.dma_start(out=cf[bi][:], in_=peer_uv[ti * 128:(ti + 1) * 128, :]), writes=["cvf%d" % bi])
                    P.op("pool", lambda e: e.tensor_copy(out=cb[bi][:], in_=cf[bi][:]), reads=["cvf%d" % bi], writes=["cvb%d" % bi])
                    P.dma("pool", "cvb%d" % bi, lambda e: e.dma_start(out=uvb_d[ti * 128:(ti + 1) * 128, :], in_=cb[bi][:]), reads=["cvb%d" % bi], writes=["uvb_d"])
                def c_scores(r, h, vi):
                    r0 = min(max(r - 4, 0), 56)
                    t = h // 2; po = (h % 2) * 64; psl = slice(po, po + 64)
                    si, skey = psr.next()
                    qsl = slice(r * 64, (r + 1) * 64)
                    for j in range(6):
                        k0 = (CTX + (r0 + 2 * j) * 64) if j < 4 else (j - 4) * 128
                        P.op("pe", (lambda j, k0: lambda e: e.matmul(ps_[si][:, j * 64:(j + 1) * 64], lhsT=kT_sb[psl, t, k0:k0 + 128], rhs=qT_sb[psl, t, qsl], start=True, stop=True))(j, k0),
                             reads=["kT_sb", "qT_sb"], writes=[skey])
                    return (r, h, vi, si, skey)

                def c_part1(state):
                    r, h, vi, si, skey = state
                    r0 = min(max(r - 4, 0), 56); dr0 = r0 - r + 7
                    bi2, bkey2 = ssr2.next()
                    P.op("dve", lambda e: e.scalar_tensor_tensor(out=ssb[bi2][:], in0=ps_[si][:, 0:256].rearrange("p (j q) -> p j q", j=4), scalar=0.125,
                                                                 in1=B4[:, h, dr0:dr0 + 7:2, :], op0=ALU.mult, op1=ALU.add), reads=[skey, "BiasTT"], writes=[bkey2])
                    ti, tkey = ptr.next()
                    P.op("act", lambda e: e.activation(out=pT[ti][:, 0:256], in_=ssb[bi2][:].rearrange("p j q -> p (j q)"), func=AF.Exp), reads=[bkey2], writes=[tkey])
                    P.op("act", lambda e: e.activation(out=pT[ti][:, 256:384], in_=ps_[si][:, 256:384], func=AF.Exp, scale=0.125), reads=[skey], writes=[tkey])
                    return (r, h, vi, ti, tkey)

                def c_part2(state):
                    r, h, vi, ti, tkey = state
                    vkey = "Vb%d" % vi
                    t = h // 2; po = (h % 2) * 64; psl = slice(po, po + 64)
                    qsl = slice(r * 64, (r + 1) * 64)
                    oi, okey = por.next(); di, dkey = pdr.next()
                    hp = (h // 2) * 128
                    for j in range(6):
                        vsrc = (Vb[vi][:, j, hp:hp + 128] if j < 4 else Vctx[:, j - 4, hp:hp + 128])
                        P.op("pe", (lambda j, vsrc: lambda e: e.matmul(po_[oi][:, 0:64], lhsT=vsrc, rhs=pT[ti][:, j * 64:(j + 1) * 64], start=(j == 0), stop=(j == 5)))(j, vsrc),
                             reads=[vkey, "Vctx", tkey], writes=[okey])
                    for j in range(6):
                        P.op("pe", (lambda j: lambda e: e.matmul(pd_[di][:, 0:64], lhsT=ones_b[:], rhs=pT[ti][:, j * 64:(j + 1) * 64], start=(j == 0), stop=(j == 5)))(j),
                             reads=["ones_b", tkey], writes=[dkey])
                    ri, rkey = rdr.next()
                    P.op("dve", lambda e: e.reciprocal(out=rden[ri][psl, :], in_=pd_[di][psl, 0:64]), reads=[dkey], writes=[rkey])
                    P.op("dve", lambda e: e.tensor_tensor(out=attT_sb[psl, t, qsl], in0=po_[oi][psl, 0:64], in1=rden[ri][psl, :], op=ALU.mult), reads=[okey, rkey], writes=["attT_sb"])

                pend1 = None; pend2 = None
                for r in range(64):
                    convert_tile(2 * r); convert_tile(2 * r + 1)
                    r0 = min(max(r - 4, 0), 56)
                    vi, vkey = vbr.next()
                    P.dma("sp", vkey, (lambda vi, r0: lambda e: e.dma_start(out=Vb[vi][:], in_=v_d[CTX + r0 * 64:CTX + (r0 + 8) * 64, :].rearrange("(j p) n -> p j n", p=128)))(vi, r0),
                          reads=["v_d"], writes=[vkey])
                    for h in range(8):
                        stt = c_scores(r, h, vi)
                        nxt2 = c_part1(pend1) if pend1 is not None else None
                        if pend2 is not None:
                            c_part2(pend2)
                        pend2 = nxt2
                        pend1 = stt
                nxt2 = c_part1(pend1)
                if pend2 is not None:
                    c_part2(pend2)
                c_part2(nxt2)
            if debug:
                P.dma("sp", "dbga", lambda e: e.dma_start(out=attT_d.rearrange("(t p) n -> p t n", p=128), in_=attT_sb[:]), reads=["attT_sb"], writes=["attT_d"])
            P.barrier()
            stC2.close()
            with phase() as st:
                wstgC_t = sbuf(st, "wstgC", [128, 4, 1024]); w_bb_sb = sbuf(st, "w_bb_sb", [128, 4, D], BF16)
                g_sb = [sbuf(st, "g_sb%d" % i, [128, 16, 512], BF16) for i in range(2)]
                ba_sb = [sbuf(st, "ba_sb%d" % i, [128, 8, 512], BF16) for i in range(2)]
                t1 = [sbuf(st, "t1c%d" % i, [128, 512]) for i in range(2)]; t1r = Rot("t1c", 2)
                t2 = [sbuf(st, "t2c%d" % i, [128, 512]) for i in range(2)]; t2r = Rot("t2c", 2)
                st_mg = [sbuf(st, "st_mg%d" % i, [128, 8, 512], BF16) for i in range(2)]
                pbb = [psum(st, "pbb%d" % i, [128, 512]) for i in range(3)]; pbr2 = Rot("pbb", 3)
                P.dma("sp", "wc0", lambda e: e.dma_start(out=wstgC_t[:], in_=w_bb.rearrange("(k p) n -> p k n", p=128)), writes=["wstgC"])
                P.op("dve", lambda e: e.tensor_copy(out=w_bb_sb[:], in_=wstgC_t[:]), reads=["wstgC"], writes=["w_bb_sb"])
                for sgi in range(8):
                    b2 = sgi % 2; ssl = slice(sgi * 512, (sgi + 1) * 512)
                    gk, bk, mk = "g_sb%d" % b2, "ba_sb%d" % b2, "st_mg%d" % b2
                    P.dma("sp", gk, (lambda b2, ssl: lambda e: e.dma_start(out=g_sb[b2][:], in_=gT_d.rearrange("(t p) n -> p t n", p=128)[:, :, ssl]))(b2, ssl), reads=["gT_d"], writes=[gk])
                    P.dma("act", bk, (lambda b2, ssl: lambda e: e.dma_start(out=ba_sb[b2][:], in_=baT_d.rearrange("(t p) n -> p t n", p=128)[:, :, ssl]))(b2, ssl), reads=["baT_d"], writes=[bk])
                    for ct2 in range(8):
                        qi, qk = pbr2.next()
                        for k in range(4):
                            P.op("pe", (lambda qi, k, ct2, ssl: lambda e: e.matmul(pbb[qi][:], lhsT=w_bb_sb[:, k, ct2 * 128:(ct2 + 1) * 128], rhs=attT_sb[:, k, ssl],
                                                                                   start=(k == 0), stop=(k == 3)))(qi, k, ct2, ssl),
                                 reads=["w_bb_sb", "attT_sb"], writes=[qk])
                        i1, k1 = t1r.next(); i2, k2 = t2r.next()
                        P.op("dve", (lambda i1, qi, b2, ct2: lambda e: e.tensor_tensor(out=t1[i1][:], in0=pbb[qi][:], in1=g_sb[b2][:, 8 + ct2, :], op=ALU.mult))(i1, qi, b2, ct2),
                             reads=[qk, gk], writes=[k1])
                        P.op("pool", (lambda i2, b2, ct2: lambda e: e.tensor_tensor(out=t2[i2][:], in0=ba_sb[b2][:, ct2, :], in1=g_sb[b2][:, ct2, :], op=ALU.mult))(i2, b2, ct2),
                             reads=[bk, gk], writes=[k2])
                        P.op("dve", (lambda b2, ct2, i1, i2: lambda e: e.tensor_tensor(out=st_mg[b2][:, ct2, :], in0=t1[i1][:], in1=t2[i2][:], op=ALU.add))(b2, ct2, i1, i2),
                             reads=[k1, k2], writes=[mk])
                    P.dma("sp", mk, (lambda b2, ssl: lambda e: e.dma_start(out=mgT_d.rearrange("(t p) n -> p t n", p=128)[:, :, ssl], in_=st_mg[b2][:]))(b2, ssl),
                          reads=[mk], writes=["mgT_d"])
        P.barrier()
        if stage <= 3:
            return finish(nc, P, out)

        x1_d = dscr("x1_d", [SEQ, D], F32) if debug else None
        pf_d = dscr("pf_d", [SEQ, D], F32) if debug else None
        NT = 32 if stage >= 5 else int(stage * 10) % 10 or 1
        if not debug:
            NT = 32
        if "KNT" in os.environ:
            NT = int(os.environ["KNT"])
        gate1B = modB[:, 2 * D:3 * D]; S2B = modB[:, 3 * D:4 * D]; G2B = modB[:, 4 * D:5 * D]; gate2B = modB[:, 5 * D:6 * D]
        with ExitStack() as stD:
            w_out_sb = sbuf(stD, "w_out_sb", [128, 8, D], BF16); w_q_sb = sbuf(stD, "w_q_sb", [128, 8, 2048], BF16)
            skT = sbuf(stD, "skT", [128, 2, 128], BF16); fgB = sbuf(stD, "fgB", [128, D])
            with phase() as st:
                wstgD_t = [sbuf(st, "wstgD%d" % i, [128, 8, 512]) for i in range(2)]
                skf = sbuf(st, "skf", [128, 2, 128]); skb = sbuf(st, "skb", [128, 2, 128], BF16)
                ptk = psum(st, "ptk", [128, 2, 128], BF16)
                for i in range(6):
                    bi = i % 2
                    srcw = (w_out if i < 2 else w_q).rearrange("(k p) n -> p k n", p=128)
                    c0 = (i * 512) if i < 2 else (i - 2) * 512
                    dstw = w_out_sb if i < 2 else w_q_sb
                    P.dma("sp", "wd%d" % bi, (lambda bi, srcw, c0: lambda e: e.dma_start(out=wstgD_t[bi][:], in_=srcw[:, :, c0:c0 + 512]))(bi, srcw, c0), writes=["wstgD%d" % bi])
                    P.op(("dve", "pool")[bi], (lambda bi, dstw, c0: lambda e: e.tensor_copy(out=dstw[:, :, c0:c0 + 512], in_=wstgD_t[bi][:]))(bi, dstw, c0),
                         reads=["wstgD%d" % bi], writes=["wD"])
                P.dma("act", "wd2", lambda e: e.dma_start(out=skf[:], in_=subkeys.rearrange("n k d -> k n d")), writes=["skf"])
                P.dma("act", "wd2", lambda e: e.dma_start(out=fgB[:], in_=final_g.to_broadcast([128, D])), writes=["fgB"])
                P.op("dve", lambda e: e.tensor_copy(out=skb[:], in_=skf[:]), reads=["skf"], writes=["skb"])
                for n_ in range(2):
                    P.op("pe", (lambda n_: lambda e: e.transpose(ptk[:, n_, :], skb[:, n_, :], identb[:]))(n_), reads=["skb", "identb"], writes=["ptk"])
                P.op("dve", lambda e: e.tensor_copy(out=skT[:], in_=ptk[:]), reads=["ptk"], writes=["skT"])
            P.barrier()
            P.barrier()
            if stage == 3.5:
                return finish(nc, P, out)
            with phase() as st:
                NB = 2
                xtD_t = sbuf(st, "xtD", [128, D])
                x1 = [sbuf(st, "x1_%d" % i, [128, D]) for i in range(NB)]
                h2 = [sbuf(st, "h2_%d" % i, [128, D]) for i in range(NB)]
                eid = [sbuf(st, "eid%d" % i, [128, 128], I32) for i in range(NB)]
                gate = [sbuf(st, "gate%d" % i, [128, 128]) for i in range(NB)]
                tmpD = sbuf(st, "tmpD", [128, D]); tmpF = tmpD
                acc = None
                junkD = sbuf(st, "junkD", [128, D], BF16); junkE = sbuf(st, "junkE", [128, D], BF16)
                mg_sb = sbuf(st, "mg_sb", [128, 8, 128], BF16); h2b = sbuf(st, "h2b", [128, D], BF16); h2T = sbuf(st, "h2T", [128, 8, 128], BF16)
                qT_sb2 = sbuf(st, "qT_sb2", [128, 16, 128], BF16); s_sb = sbuf(st, "s_sb", [128, 16, 128]); work = sbuf(st, "workD", [128, 16, 128])
                topv = sbuf(st, "topv", [128, 16, 16]); idxu = sbuf(st, "idxu", [128, 16, 16], U32); idxf = sbuf(st, "idxf", [128, 16, 16])
                cand = sbuf(st, "cand", [128, 8, 256]); cidx = s_sb[:].rearrange("p a b -> p (a b)").rearrange("p (h c) -> p h c", h=8)
                best = sbuf(st, "best", [128, 8, 16]); eg = sbuf(st, "eg", [128, 8, 16]); sm = sbuf(st, "smD", [128, 32]); sm2 = sbuf(st, "smE", [128, 8]); eidf = sbuf(st, "eidf", [128, 128])
                aD = sbuf(st, "aD", [128, 128]); gl_ = sbuf(st, "gl_", [128, 128]); wD = sbuf(st, "wDw", [128, 128])
                NG = 13; GJ = 4
                posI = sbuf(st, "posI", [128, 256], I32); mskI = sbuf(st, "mskI", [128, 1], I32)
                c4I = sbuf(st, "c4I", [128, 1], I32); c15I = sbuf(st, "c15I", [128, 1], I32); iota16 = sbuf(st, "iota16", [128, 16])
                pab = sbuf(st, "pab", [128, 2, 8, 16], I32); pabf = sbuf(st, "pabf", [128, 2, 8, 16]); e3b = sbuf(st, "e3b", [128, 8, 16])
                oh = cand[:].rearrange("p h (k a) -> p h k a", a=16)
                P.op("pool", lambda e: e.iota(c4I[:], pattern=[[0, 1]], base=4, channel_multiplier=0), writes=["posI"])
                P.op("pool", lambda e: e.iota(c15I[:], pattern=[[0, 1]], base=15, channel_multiplier=0), writes=["posI"])
                P.op("pool", lambda e: e.iota(iota16[:], pattern=[[1, 16]], base=0, channel_multiplier=0, allow_small_or_imprecise_dtypes=True), writes=["posI"])
                P.op("pool", lambda e: e.iota(posI[:], pattern=[[1, 256]], base=0, channel_multiplier=0), writes=["posI"])
                P.op("pool", lambda e: e.iota(mskI[:], pattern=[[0, 1]], base=-256, channel_multiplier=0), writes=["posI"])
                UVg = [sbuf(st, "UVg%d" % i, [128, 2, D], BF16) for i in range(NG)]
                dg = [sbuf(st, "dg%d" % i, [128, 128], BF16) for i in range(8)]; dgr = Rot("dg", 8)
                pmo = [psum(st, "pmo%d" % i, [128, 512]) for i in range(2)]
                pacc = [psum(st, "pacc%d" % i, [128, 512]) for i in range(2)]
                tpD = psum(st, "tpD", [128, 8, 128], BF16)
                pq = [psum(st, "pq%d" % i, [128, 4, 128]) for i in range(2)]; pqr = Rot("pq", 2)
                uv_i = [0]

                def rstd_chain(src_ap, srckey, smt, smk, jk, jkey):
                    P.op("act", lambda e: e.activation(out=jk[:], in_=src_ap, func=AF.Square, accum_out=smt[:, 0:1]), reads=[srckey], writes=[smk])
                    P.op("dve", lambda e: e.tensor_scalar(out=smt[:, 1:2], in0=smt[:, 0:1], scalar1=1.0 / D, scalar2=EPS, op0=ALU.mult, op1=ALU.add), reads=[smk], writes=[smk])
                    P.op("act", lambda e: e.sqrt(out=smt[:, 2:3], in_=smt[:, 1:2]), reads=[smk], writes=[smk])
                    P.op("dve", lambda e: e.reciprocal(out=smt[:, 3:4], in_=smt[:, 2:3]), reads=[smk], writes=[smk])

                def front(i):
                    b = i % NB; t0 = i * 128
                    xk, x1k, h2k, ek, gk = "xtD", "x1_%d" % b, "h2_%d" % b, "eid%d" % b, "gate%d" % b
                    P.dma("sp", xk, lambda e: e.dma_start(out=xtD_t[:], in_=x[t0:t0 + 128, :]), writes=[xk])
                    P.dma("sp", "mgl", lambda e: e.dma_start(out=mg_sb[:], in_=mgT_d.rearrange("(k p) n -> p k n", p=128)[:, :, t0:t0 + 128]), reads=["mgT_d"], writes=["mg_sb"])
                    for hf in range(2):
                        for k in range(8):
                            P.op("pe", (lambda hf, k: lambda e: e.matmul(pmo[hf][:], lhsT=mg_sb[:, k, :], rhs=w_out_sb[:, k, hf * 512:(hf + 1) * 512], start=(k == 0), stop=(k == 7)))(hf, k),
                                 reads=["mg_sb", "wD"], writes=["pmo%d" % hf])
                        yield
                        P.op("dve", (lambda hf: lambda e: e.tensor_tensor(out=tmpF[:, hf * 512:(hf + 1) * 512], in0=pmo[hf][:], in1=gate1B[:, hf * 512:(hf + 1) * 512], op=ALU.mult))(hf),
                             reads=["pmo%d" % hf, "modB"], writes=["tmpD"])
                        yield
                    P.op("dve", lambda e: e.tensor_tensor(out=x1[b][:], in0=tmpF[:], in1=xtD_t[:], op=ALU.add), reads=["tmpD", xk], writes=[x1k])
                    yield
                    if debug:
                        P.dma("sp", "dbgx1", lambda e: e.dma_start(out=x1_d[t0:t0 + 128, :], in_=x1[b][:]), reads=[x1k], writes=["x1_d"])
                    rstd_chain(x1[b][:], x1k, sm[:, 0:4], "smD", junkD, "junkD")
                    P.op("dve", lambda e: e.scalar_tensor_tensor(out=tmpF[:], in0=x1[b][:], scalar=sm[:, 3:4], in1=G2B, op0=ALU.mult, op1=ALU.mult), reads=[x1k, "smD", "modB"], writes=["tmpD"])
                    yield
                    P.op("dve", lambda e: e.tensor_tensor(out=h2[b][:], in0=tmpF[:], in1=S2B, op=ALU.add), reads=["tmpD", "modB"], writes=[h2k])
                    yield
                    P.op("act", lambda e: e.copy(out=h2b[:], in_=h2[b][:]), reads=[h2k], writes=["h2b"])
                    for k in range(8):
                        P.op("pe", (lambda k: lambda e: e.transpose(tpD[:, k, :], h2b[:, k * 128:(k + 1) * 128], identb[:]))(k), reads=["h2b", "identb"], writes=["tpD"])
                    P.op("act", lambda e: e.copy(out=h2T[:], in_=tpD[:]), reads=["tpD"], writes=["h2T"])
                    yield
                    for g4 in range(4):
                        qi, qk = pqr.next()
                        for bl in range(4):
                            blk = g4 * 4 + bl
                            for k in range(8):
                                P.op("pe", (lambda qi, bl, blk, k: lambda e: e.matmul(pq[qi][:, bl, :], lhsT=w_q_sb[:, k, blk * 128:(blk + 1) * 128], rhs=h2T[:, k, :], start=(k == 0), stop=(k == 7)))(qi, bl, blk, k),
                                     reads=["wD", "h2T"], writes=[qk])
                        P.op("act", (lambda qi, g4: lambda e: e.copy(out=qT_sb2[:, g4 * 4:(g4 + 1) * 4, :], in_=pq[qi][:]))(qi, g4), reads=[qk], writes=["qT_sb2"])
                        yield
                    for g4 in range(4):
                        si, sk = pqr.next()
                        for bl in range(4):
                            blk = g4 * 4 + bl
                            P.op("pe", (lambda si, bl, blk: lambda e: e.matmul(pq[si][:, bl, :], lhsT=qT_sb2[:, blk, :], rhs=skT[:, blk % 2, :], start=True, stop=True))(si, bl, blk),
                                 reads=["qT_sb2", "skT"], writes=[sk])
                        P.op("act", (lambda si, g4: lambda e: e.copy(out=s_sb[:, g4 * 4:(g4 + 1) * 4, :], in_=pq[si][:]))(si, g4), reads=[sk], writes=["s_sb"])
                        yield
                    TK = ["s_sb", "topk"]
                    BK = ["tk%d" % q for q in range(16)]
                    for blk in range(16):
                        P.op("dve", (lambda blk: lambda e: e.max(out=topv[:, blk, 0:8], in_=s_sb[:, blk, :]))(blk), reads=TK, writes=[BK[blk]])
                    yield
                    for blk in range(16):
                        P.op("dve", (lambda blk: lambda e: e.max_index(out=idxu[:, blk, 0:8], in_max=topv[:, blk, 0:8], in_values=s_sb[:, blk, :]))(blk), reads=["s_sb", BK[blk]], writes=[BK[blk]])
                        if blk % 8 == 7:
                            yield
                    for blk in range(16):
                        P.op("dve", (lambda blk: lambda e: e.match_replace(out=work[:, blk, :], in_to_replace=topv[:, blk, 0:8], in_values=s_sb[:, blk, :], imm_value=-1e30))(blk), reads=["s_sb", BK[blk]], writes=[BK[blk]])
                        if blk % 8 == 7:
                            yield
                    for blk in range(16):
                        P.op("dve", (lambda blk: lambda e: e.max(out=topv[:, blk, 8:16], in_=work[:, blk, :]))(blk), reads=[BK[blk]], writes=[BK[blk]])
                    yield
                    for blk in range(16):
                        P.op("dve", (lambda blk: lambda e: e.max_index(out=idxu[:, blk, 8:16], in_max=topv[:, blk, 8:16], in_values=work[:, blk, :]))(blk), reads=[BK[blk]], writes=[BK[blk]])
                        if blk % 8 == 7:
                            yield
                    TK = TK + BK
                    P.op("dve", lambda e: e.tensor_copy(out=idxf[:], in_=idxu[:]), reads=TK, writes=["topk"])
                    tv4 = topv[:].rearrange("p (h n) a -> p h n a", n=2); ix4 = idxf[:].rearrange("p (h n) a -> p h n a", n=2)
                    c4 = cand[:].rearrange("p h (a b) -> p h a b", a=16); ci4 = cidx.rearrange("p h (a b) -> p h a b", a=16)
                    P.op("dve", lambda e: e.tensor_tensor(out=c4, in0=tv4[:, :, 0, :].unsqueeze(3).to_broadcast([128, 8, 16, 16]),
                                                          in1=tv4[:, :, 1, :].unsqueeze(2).to_broadcast([128, 8, 16, 16]), op=ALU.add), reads=TK, writes=["topk"])
                    yield
                    candI = cand[:].bitcast(I32)
                    P.op("dve", lambda e: e.tensor_scalar(out=candI, in0=candI, scalar1=mskI[:, 0:1], scalar2=None, op0=ALU.bitwise_and), reads=TK + ["posI"], writes=["topk"])
                    P.op("dve", lambda e: e.tensor_tensor(out=candI, in0=candI, in1=posI[:].unsqueeze(1).to_broadcast([128, 8, 256]), op=ALU.bitwise_or), reads=TK + ["posI"], writes=["topk"])
                    P.op("dve", lambda e: e.tensor_single_scalar(out=ix4[:, :, 0, :], in_=ix4[:, :, 0, :], scalar=128.0, op=ALU.mult), reads=TK, writes=["topk"])
                    P.op("dve", lambda e: e.tensor_tensor(out=ci4, in0=ix4[:, :, 0, :].unsqueeze(3).to_broadcast([128, 8, 16, 16]),
                                                          in1=ix4[:, :, 1, :].unsqueeze(2).to_broadcast([128, 8, 16, 16]), op=ALU.add), reads=TK, writes=["topk"])
                    yield
                    w2 = work[:].rearrange("p (h n) k -> p h (n k)", n=2)
                    HK = ["hk%d" % q for q in range(8)]
                    for h in range(8):
                        P.op("dve", (lambda h: lambda e: e.max(out=best[:, h, 0:8], in_=cand[:, h, :]))(h), reads=TK, writes=[HK[h]])
                    yield
                    for h in range(8):
                        P.op("dve", (lambda h: lambda e: e.match_replace(out=w2[:, h, :], in_to_replace=best[:, h, 0:8], in_values=cand[:, h, :], imm_value=-1e30))(h), reads=TK + [HK[h]], writes=[HK[h]])
                    yield
                    for h in range(8):
                        P.op("dve", (lambda h: lambda e: e.max(out=best[:, h, 8:16], in_=w2[:, h, :]))(h), reads=[HK[h]], writes=[HK[h]])
                    yield
                    TK = TK + HK
                    P.op("dve", lambda e: e.tensor_single_scalar(out=sm[:, 8:16], in_=best[:, :, 0], scalar=-1.0, op=ALU.mult), reads=TK + ["smD"], writes=["smD"])
                    for h in range(8):
                        P.op("act", (lambda h: lambda e: e.activation(out=eg[:, h, :], in_=best[:, h, :], func=AF.Exp, bias=sm[:, 8 + h:9 + h], accum_out=sm[:, 16 + h:17 + h]))(h),
                             reads=TK + ["smD"], writes=["eg", "smD"])
                    P.op("dve", lambda e: e.reciprocal(out=sm[:, 24:32], in_=sm[:, 16:24]), reads=["smD"], writes=["smD"])
                    P.op("dve", lambda e: e.tensor_tensor(out=gate[b][:].rearrange("p (h k) -> p h k", h=8), in0=eg[:], in1=sm[:, 24:32].unsqueeze(2).to_broadcast([128, 8, 16]), op=ALU.mult),
                         reads=["eg", "smD"], writes=[gk])
                    yield
                    bestI = best[:].bitcast(I32)
                    P.op("dve", lambda e: e.tensor_scalar(out=pab[:, 0], in0=bestI, scalar1=c4I[:, 0:1], scalar2=None, op0=ALU.arith_shift_right), reads=TK + ["posI"], writes=["topk"])
                    P.op("dve", lambda e: e.tensor_scalar(out=pab[:, 0], in0=pab[:, 0], scalar1=c15I[:, 0:1], scalar2=None, op0=ALU.bitwise_and), reads=TK + ["posI"], writes=["topk"])
                    P.op("dve", lambda e: e.tensor_scalar(out=pab[:, 1], in0=bestI, scalar1=c15I[:, 0:1], scalar2=None, op0=ALU.bitwise_and), reads=TK + ["posI"], writes=["topk"])
                    P.op("dve", lambda e: e.tensor_copy(out=pabf[:], in_=pab[:]), reads=TK, writes=["topk"])
                    yield
                    e3 = eidf[:].rearrange("p (h k) -> p h k", h=8)
                    for n_ in range(2):
                        P.op("dve", (lambda n_: lambda e: e.tensor_tensor(out=oh[:], in0=pabf[:, n_].unsqueeze(3).to_broadcast([128, 8, 16, 16]),
                                                                          in1=iota16[:].unsqueeze(1).unsqueeze(1).to_broadcast([128, 8, 16, 16]), op=ALU.is_equal))(n_), reads=TK + ["posI"], writes=["topk"])
                        P.op("dve", (lambda n_: lambda e: e.tensor_tensor(out=oh[:], in0=oh[:], in1=ix4[:, :, n_, :].unsqueeze(2).to_broadcast([128, 8, 16, 16]), op=ALU.mult))(n_), reads=TK, writes=["topk"])
                        P.op("dve", (lambda n_: lambda e: e.tensor_reduce(out=(e3 if n_ == 0 else e3b[:]), in_=oh[:], axis=AX.X, op=ALU.add))(n_), reads=TK, writes=["topk"])
                        yield
                    P.op("dve", lambda e: e.tensor_tensor(out=e3, in0=e3, in1=e3b[:], op=ALU.add), reads=TK, writes=["topk"])
                    P.op("dve", lambda e: e.tensor_scalar(out=eidf[:], in0=eidf[:], scalar1=0.0, scalar2=16383.0, op0=ALU.max, op1=ALU.min), reads=TK, writes=["topk"])
                    P.op("dve", lambda e: e.tensor_copy(out=eid[b][:], in_=eidf[:]), reads=TK, writes=[ek])
                    yield

                def consume(i, fgen):
                    b = i % NB; t0 = i * 128
                    x1k, h2k, ek, gk = "x1_%d" % b, "h2_%d" % b, "eid%d" % b, "gate%d" % b
                    ngrp = 128 // GJ
                    pend = None

                    def finish_group(g, bufs):
                        j0 = g * GJ
                        if "nofin" in KD:
                            return
                        P.op("act", lambda e: e.activation(out=gl_[:, j0:j0 + GJ], in_=aD[:, j0:j0 + GJ], func=AF.Gelu_apprx_tanh), reads=["aD%d" % g], writes=["gl_%d" % g])
                        for jj in range(GJ):
                            P.op("act", (lambda jj: lambda e: e.mul(out=wD[:, j0 + jj:j0 + jj + 1], in_=gl_[:, j0 + jj:j0 + jj + 1], mul=gate[b][:, j0 + jj:j0 + jj + 1]))(jj),
                                 reads=["gl_%d" % g, gk], writes=["wDw%d" % g])
                        for jj in range(GJ):
                            j = j0 + jj; gi, ugk = bufs[jj]
                            di, dk = dgr.next()
                            P.op("act", (lambda di, j: lambda e: e.activation(out=dg[di][:], in_=identb[:], func=AF.Copy, scale=wD[:, j:j + 1]))(di, j), reads=["wDw%d" % g, "identb"], writes=[dk])
                            for hf in range(2):
                                if "nov" in KD and j not in (0, 127):
                                    continue
                                P.op("pe", (lambda di, gi, hf, j: lambda e: e.matmul(pacc[hf][:], lhsT=dg[di][:], rhs=UVg[gi][:, 1, hf * 512:(hf + 1) * 512], start=(j == 0), stop=(j == 127)))(di, gi, hf, j),
                                     reads=[dk, ugk], writes=["pacc%d" % hf])

                    for g in range(ngrp):
                        bufs = []
                        for jj in range(GJ):
                            j = g * GJ + jj
                            gi = uv_i[0] % NG; uv_i[0] += 1; ugk = "UVg%d" % gi
                            bufs.append((gi, ugk))
                            if "nog" in KD:
                                P.op("pool", (lambda gi: lambda e: e.memset(UVg[gi][:], 1.0))(gi), reads=[ek], writes=[ugk])
                            else:
                                P.dma("pool", ugk, (lambda gi, j: lambda e: e.indirect_dma_start(out=UVg[gi][:].rearrange("p a d -> p (a d)"), out_offset=None, in_=uvb_d,
                                                                                               in_offset=bass.IndirectOffsetOnAxis(ap=eid[b][:, j:j + 1], axis=0)))(gi, j), reads=[ek], writes=[ugk])
                        for jj in range(GJ):
                            j = g * GJ + jj; gi, ugk = bufs[jj]
                            if "noa" in KD:
                                continue
                            jE = tmpF if "f32junk" in KD else junkE
                            P.op("dve", (lambda gi, j, jE: lambda e: e.scalar_tensor_tensor(out=jE[:], in0=UVg[gi][:, 0, :], scalar=1.0, in1=h2[b][:], op0=ALU.mult, op1=ALU.mult, accum_out=aD[:, j:j + 1]))(gi, j, jE),
                                 reads=[ugk, h2k], writes=["aD%d" % g])
                        finish_group(g, bufs)
                        if fgen is not None:
                            for _ in range(3):
                                next(fgen, None)
                    if fgen is not None:
                        for _ in fgen:
                            pass
                    if "dump23" in KD and i == 23:
                        for nm, src_t in (("d_x1", x1[b]), ("d_wD", wD), ("d_aD", aD), ("d_gate", gate[b]), ("d_h2", h2[b])):
                            dd = nc.dram_tensor(nm, list(src_t[:].shape), F32, kind="ExternalOutput").ap()
                            P.dma("sp", "dump", (lambda dd, src_t: lambda e: e.dma_start(out=dd, in_=src_t[:]))(dd, src_t), reads=[x1k, gk, h2k] + ["wDw%d" % q for q in range(32)] + ["aD%d" % q for q in range(32)], writes=["dumpo"])
                        dd2 = nc.dram_tensor("d_eid", [128, 128], I32, kind="ExternalOutput").ap()
                        P.dma("sp", "dump", lambda e: e.dma_start(out=dd2, in_=eid[b][:]), reads=[ek], writes=["dumpo"])
                    if "notail" in KD:
                        return
                    for hf in range(2):
                        P.op("dve", (lambda hf: lambda e: e.tensor_tensor(out=tmpD[:, hf * 512:(hf + 1) * 512], in0=pacc[hf][:], in1=gate2B[:, hf * 512:(hf + 1) * 512], op=ALU.mult))(hf),
                             reads=["pacc%d" % hf, "modB"], writes=["tmpD"])
                    P.op("dve", lambda e: e.tensor_tensor(out=x1[b][:], in0=tmpD[:], in1=x1[b][:], op=ALU.add), reads=["tmpD", x1k], writes=[x1k])
                    rstd_chain(x1[b][:], x1k, sm2[:, 0:4], "smE", junkD, "junkD")
                    P.op("dve", lambda e: e.scalar_tensor_tensor(out=tmpD[:], in0=x1[b][:], scalar=sm2[:, 3:4], in1=fgB[:], op0=ALU.mult, op1=ALU.mult), reads=[x1k, "smE", "fgB"], writes=["tmpD"])
                    P.dma("sp", "outst", lambda e: e.dma_start(out=out[t0:t0 + 128, :], in_=tmpD[:]), reads=["tmpD"], writes=["out"])

                for _ in front(0):
                    pass
                for i in range(NT):
                    if "noc" in KD:
                        break
                    if "noil" in KD:
                        consume(i, None)
                        if i + 1 < NT:
                            for _ in front(i + 1):
                                pass
                        continue
                    consume(i, front(i + 1) if i + 1 < NT else None)
        return finish(nc, P, out)
    return nc


def finish(nc, P, out):
    P.barrier()
    P.close()
    return nc


def core_inputs(inp, b):
    f = lambda a: np.ascontiguousarray(a, dtype=np.float32)
    return {
        "x": f(inp["x"][b]), "c": f(inp["c"][b:b + 1]), "ctx": f(inp["ctx"][b]), "c_ctx": f(inp["c_ctx"].reshape(1, D)),
        "w_mod": f(inp["w_mod"][0]), "b_mod": f(inp["b_mod"][0].reshape(1, -1)),
        "norm1_g": f(inp["norm1_g"][0].reshape(1, D)), "norm2_g": f(inp["norm2_g"][0].reshape(1, D)),
        "final_g": f(inp["final_g"].reshape(1, D)), "w_in": f(inp["w_in"][0]),
        "ssm_a_re": f(inp["ssm_a_re"][0]), "ssm_a_im": f(inp["ssm_a_im"][0]), "ssm_log_dt": f(inp["ssm_log_dt"][0]),
        "ssm_b_re": f(inp["ssm_b_re"][0]), "ssm_b_im": f(inp["ssm_b_im"][0]),
        "ssm_c_re": f(inp["ssm_c_re"][0]), "ssm_c_im": f(inp["ssm_c_im"][0]),
        "ssm_d": f(inp["ssm_d"][0].reshape(512, 1)), "w_glu": f(inp["w_glu"][0]), "b_glu": f(inp["b_glu"][0].reshape(512, 1)),
        "w_branch_a": f(inp["w_branch_a"][0]), "w_branch_b": f(inp["w_branch_b"][0]), "na_rpb": f(inp["na_rpb"][0]),
        "w_out": f(inp["w_out"][0]), "peer_w_q": f(inp["peer_w_q"][0]), "peer_subkeys": f(inp["peer_subkeys"][0]),
        "peer_uv": f(np.concatenate([inp["peer_u"][0], inp["peer_v"][0]], axis=1)),
    }


def kernel(**inputs):
    nc = build()
    in_maps = [core_inputs(inputs, b) for b in range(8)]
    res = run_bass_kernel_spmd(nc, in_maps, core_ids=list(range(8)))
    return np.stack([np.asarray(r["out"], dtype=np.float32) for r in res.results], axis=0)
```

```python
import math
import numpy as np
import concourse.bass as bass
import concourse.mybir as mybir
from concourse.bass_utils import run_bass_kernel_spmd

F32 = mybir.dt.float32
BF16 = mybir.dt.bfloat16
I32 = mybir.dt.int32
U32 = mybir.dt.uint32
AF = mybir.ActivationFunctionType
ALU = mybir.AluOpType
AX = mybir.AxisListType


class _Op:
    __slots__ = ("eng", "fn", "deps", "seq", "is_dma", "semkey", "signal", "count", "waits")

    def __init__(self, eng, fn, seq, is_dma=False, semkey=None):
        self.eng = eng
        self.fn = fn
        self.deps = []
        self.seq = seq
        self.is_dma = is_dma
        self.semkey = semkey
        self.signal = is_dma
        self.count = 0
        self.waits = []


class Prog:
    ENGS = ("pe", "dve", "act", "pool", "sp")

    def __init__(self, nc):
        self.nc = nc
        self.ops = []
        self.writer = {}
        self.readers = {}
        import os
        self.same_sync = os.environ.get("KSAME", "1") == "1"

    def _add(self, op, reads, writes):
        deps = []
        for r in reads:
            w = self.writer.get(r)
            if w is not None:
                deps.append(w)
        for w_ in writes:
            w = self.writer.get(w_)
            if w is not None:
                deps.append(w)
            deps.extend(self.readers.get(w_, ()))
        op.deps = [d for d in set(deps) if d is not op]
        for r in reads:
            self.readers.setdefault(r, []).append(op)
        for w_ in writes:
            self.writer[w_] = op
            self.readers[w_] = []
        self.ops.append(op)
        return op

    def op(self, eng, fn, reads=(), writes=()):
        return self._add(_Op(eng, fn, len(self.ops)), reads, writes)

    def dma(self, eng, semkey, fn, reads=(), writes=()):
        return self._add(_Op(eng, fn, len(self.ops), True, semkey), reads, writes)

    def emit(self):
        import bisect
        from contextlib import ExitStack
        nc = self.nc
        if not hasattr(self, "_st"):
            self._st = ExitStack(); self._sem = {}; self._cnt = {}; self._hist = {}
            self._seen = {e: {} for e in self.ENGS}; self._done = 0
        ops = self.ops[self._done:]
        self._done = len(self.ops)
        if not ops:
            return

        def same(d, o):
            return d.eng == o.eng and not o.is_dma and (d.eng == "pe" or not self.same_sync)
        for o in ops:
            for d in o.deps:
                if d.is_dma or same(d, o):
                    continue
                assert d.count == 0 or d.signal, "dependency on an already-emitted non-signalling op"
                d.signal = True
        for o in ops:
            if not o.signal:
                continue
            k = ("dma", o.semkey) if o.is_dma else ("eng", o.eng)
            if k not in self._sem:
                self._sem[k] = self._st.enter_context(nc.semaphore("s%d_%s" % (len(self._sem), str(k[1]).replace(" ", ""))))
                self._cnt[k] = 0
            self._cnt[k] += 1
            o.count = self._cnt[k]
            if o.is_dma:
                self._hist.setdefault(o.semkey, []).append(o.seq)
        for o in ops:
            need = {}
            for d in o.deps:
                if d.is_dma:
                    k = ("dma", d.semkey)
                    v = 16 * bisect.bisect_left(self._hist[d.semkey], o.seq)
                else:
                    if same(d, o):
                        continue
                    k = ("eng", d.eng)
                    v = d.count
                if need.get(k, 0) < v:
                    need[k] = v
            sn = self._seen[o.eng]
            o.waits = []
            for k, v in need.items():
                if sn.get(k, 0) < v:
                    sn[k] = v
                    o.waits.append((k, v))
        self.n_sems = len(self._sem)
        sem = self._sem
        with nc.Block() as block:
            per = {e: [o for o in ops if o.eng == e] for e in self.ENGS}

            def run(engobj, lst):
                for o in lst:
                    for k, v in o.waits:
                        engobj.wait_ge(sem[k], v)
                    ins = o.fn(engobj)
                    if o.signal:
                        k = ("dma", o.semkey) if o.is_dma else ("eng", o.eng)
                        ins.then_inc(sem[k], 16 if o.is_dma else 1)
                    o.fn = None

            @block.tensor
            def _(e):
                run(e, per["pe"])

            @block.vector
            def _(e):
                run(e, per["dve"])

            @block.scalar
            def _(e):
                run(e, per["act"])

            @block.gpsimd
            def _(e):
                run(e, per["pool"])

            @block.sync
            def _(e):
                run(e, per["sp"])

    def close(self):
        self.emit()
        if hasattr(self, "_st"):
            self._st.close()

    def barrier(self, flush=True):
        start = getattr(self, "_done", 0)
        last = {}
        dmas = {}
        for o in self.ops[start:]:
            if o.is_dma:
                dmas[o.semkey] = o
            else:
                last[o.eng] = o
        for e, o in getattr(self, "_bar", {}).items():
            last.setdefault(e, o)
        deps = list(last.values()) + list(dmas.values())
        self._bar = {}
        for e in self.ENGS:
            o = _Op(e, lambda eng: eng.nop(), len(self.ops))
            o.deps = [d for d in deps]
            o.signal = True
            self.ops.append(o)
            self._bar[e] = o
        self.writer = {}
        self.readers = {}
        if flush:
            self.emit()


class Rot:
    def __init__(self, name, n):
        self.name, self.n, self.i = name, n, -1

    def next(self):
        self.i = (self.i + 1) % self.n
        return self.i, "%s%d" % (self.name, self.i)


D = 1024
SEQ = 4096
CTX = 256
NTOK = SEQ + CTX
EPS = 1e-6


def build(stage=99, debug=False):
    import os
    KD = os.environ.get("KDBG", "")
    from contextlib import ExitStack
    nc = bass.Bass("TRN2", target_bir_lowering=False)
    P = Prog(nc)

    def din(name, shape, dt=F32):
        return nc.dram_tensor(name, shape, dt, kind="ExternalInput").ap()

    def dscr(name, shape, dt):
        return nc.dram_tensor(name, shape, dt, kind=("ExternalOutput" if debug else "Internal")).ap()

    x = din("x", [SEQ, D]); c = din("c", [1, D]); ctx = din("ctx", [CTX, D]); c_ctx = din("c_ctx", [1, D])
    w_mod = din("w_mod", [D, 6 * D]); b_mod = din("b_mod", [1, 6 * D])
    norm1_g = din("norm1_g", [1, D]); norm2_g = din("norm2_g", [1, D]); final_g = din("final_g", [1, D])
    w_in = din("w_in", [D, 4096])
    a_re = din("ssm_a_re", [2, 32, 64]); a_im = din("ssm_a_im", [2, 32, 64]); log_dt = din("ssm_log_dt", [2, 32])
    b_re = din("ssm_b_re", [2, 32, 64, 16]); b_im = din("ssm_b_im", [2, 32, 64, 16])
    c_re = din("ssm_c_re", [2, 32, 16, 64]); c_im = din("ssm_c_im", [2, 32, 16, 64])
    ssm_d = din("ssm_d", [512, 1]); w_glu = din("w_glu", [512, 512]); b_glu = din("b_glu", [512, 1])
    w_ba = din("w_branch_a", [512, D]); w_bb = din("w_branch_b", [512, D]); rpb = din("na_rpb", [8, 15, 31])
    w_out = din("w_out", [D, D]); w_q = din("peer_w_q", [D, 2048]); subkeys = din("peer_subkeys", [2, 128, 128])
    peer_uv = din("peer_uv", [16384, 2 * D])
    out = nc.dram_tensor("out", [SEQ, D], F32, kind="ExternalOutput").ap()

    uT_d = dscr("uT_d", [512, NTOK], F32)
    kT_d = dscr("kT_d", [512, NTOK], BF16)
    qT_d = dscr("qT_d", [512, SEQ], BF16)
    v_d = dscr("v_d", [NTOK, 512], BF16)
    gT_d = dscr("gT_d", [2048, SEQ], BF16)
    baT_d = dscr("baT_d", [D, SEQ], BF16)
    mgT_d = dscr("mgT_d", [D, SEQ], BF16)
    uvb_d = nc.dram_tensor("uvb_d", [16384, 2 * D], BF16, kind=("ExternalOutput" if (debug and stage == 3.5) else "Internal")).ap()

    from contextlib import contextmanager

    @contextmanager
    def phase():
        stk = ExitStack()
        try:
            yield stk
            P.barrier()
        finally:
            stk.close()

    top = ExitStack()
    with top:
        def sbuf(st, n, s, d=F32):
            return st.enter_context(nc.sbuf_tensor(n, s, d))

        def psum(st, n, s, d=F32):
            return st.enter_context(nc.psum_tensor(n, s, d))

        identf = sbuf(top, "identf", [128, 128])
        identb = sbuf(top, "identb", [128, 128], BF16)
        modB = sbuf(top, "modB", [128, 6 * D])
        P.op("pool", lambda e: e.iota(identf[:], pattern=[[1, 128]], base=0, channel_multiplier=-1,
                                      allow_small_or_imprecise_dtypes=True), writes=["identf"])
        P.op("dve", lambda e: e.tensor_single_scalar(out=identf[:], in_=identf[:], scalar=0.0, op=ALU.is_equal),
             reads=["identf"], writes=["identf"])
        P.op("dve", lambda e: e.tensor_copy(out=identb[:], in_=identf[:]), reads=["identf"], writes=["identb"])

        with phase() as st:
            modcB = sbuf(st, "modcB", [128, 2 * D])
            wm = [sbuf(st, "wm%d" % i, [128, 8, 512]) for i in range(2)]
            w_in_sb = sbuf(st, "w_in_sb", [128, 8, 4096], BF16)
            st0 = ExitStack()
            cc = sbuf(st0, "cc", [128, 2, 8]); sc = sbuf(st0, "sc", [128, 2, 8]); scB = sbuf(st0, "scB", [128, 2, 8, 128])
            bmB = sbuf(st0, "bmB", [128, 6 * D]); gB = sbuf(st0, "gB", [128, 2, D])
            pmod = [psum(st0, "pmod%d" % i, [128, 512]) for i in range(2)]

            P.dma("sp", "c0", lambda e: e.dma_start(out=cc[:, 0, :], in_=c.rearrange("o (k p) -> p (o k)", p=128),
                                                    allow_slow_non_contiguous=True), writes=["cc"])
            P.dma("sp", "c0", lambda e: e.dma_start(out=cc[:, 1, :], in_=c_ctx.rearrange("o (k p) -> p (o k)", p=128),
                                                    allow_slow_non_contiguous=True), writes=["cc"])
            P.dma("act", "c1", lambda e: e.dma_start(out=bmB[:], in_=b_mod.to_broadcast([128, 6 * D])), writes=["bmB"])
            P.dma("act", "c1", lambda e: e.dma_start(out=gB[:, 0, :], in_=norm1_g.to_broadcast([128, D])), writes=["gB"])
            P.dma("act", "c1", lambda e: e.dma_start(out=gB[:, 1, :], in_=norm2_g.to_broadcast([128, D])), writes=["gB"])
            P.op("act", lambda e: e.activation(out=sc[:], in_=cc[:], func=AF.Silu), reads=["cc"], writes=["sc"])
            P.op("dve", lambda e: e.tensor_copy(out=scB[:], in_=sc[:].unsqueeze(3).to_broadcast([128, 2, 8, 128])),
                 reads=["sc"], writes=["scB"])
            w_mod_v = w_mod.rearrange("(k p) n -> p k n", p=128)
            for cch in range(12):
                bi = cch % 2
                P.dma("sp", "wm%d" % bi, (lambda bi, cch: lambda e: e.dma_start(out=wm[bi][:], in_=w_mod_v[:, :, cch * 512:(cch + 1) * 512]))(bi, cch),
                      writes=["wm%d" % bi])
                for which in range(2 if cch < 4 else 1):
                    for k in range(8):
                        P.op("pe", (lambda bi, which, k: lambda e: e.matmul(pmod[which][:], lhsT=scB[:, which, k, :], rhs=wm[bi][:, k, :],
                                                                            start=(k == 0), stop=(k == 7)))(bi, which, k),
                             reads=["scB", "wm%d" % bi], writes=["pmod%d" % which])
                    dst = modB if which == 0 else modcB
                    P.op("dve", (lambda dst, which, cch: lambda e: e.tensor_tensor(out=dst[:, cch * 512:(cch + 1) * 512], in0=pmod[which][:],
                                                                                   in1=bmB[:, cch * 512:(cch + 1) * 512], op=ALU.add))(dst, which, cch),
                         reads=["pmod%d" % which, "bmB"], writes=["modB" if which == 0 else "modcB"])
            for dst, key, off, gi in ((modB, "modB", D, 0), (modcB, "modcB", D, 0), (modB, "modB", 4 * D, 1)):
                P.op("dve", (lambda dst, off, gi: lambda e: e.scalar_tensor_tensor(out=dst[:, off:off + D], in0=dst[:, off:off + D], scalar=1.0,
                                                                                  in1=gB[:, gi, :], op0=ALU.add, op1=ALU.mult))(dst, off, gi),
                     reads=[key, "gB"], writes=[key])

            P.barrier()
            st0.close()
            w_in_v = w_in.rearrange("(k p) n -> p k n", p=128)
            for cch in range(8):
                bi = cch % 2
                P.dma("sp", "wm%d" % bi, (lambda bi, cch: lambda e: e.dma_start(out=wm[bi][:], in_=w_in_v[:, :, cch * 512:(cch + 1) * 512]))(bi, cch),
                      reads=[], writes=["wm%d" % bi])
                eng = ("pool", "dve")[cch % 2]
                P.op(eng, (lambda bi, cch: lambda e: e.tensor_copy(out=w_in_sb[:, :, cch * 512:(cch + 1) * 512], in_=wm[bi][:]))(bi, cch),
                     reads=["wm%d" % bi], writes=["w_in_sb"])

            xt = [sbuf(st, "xt%d" % i, [128, D]) for i in range(3)]; xr = Rot("xt", 3)
            junk = sbuf(st, "junkA", [128, D]); tmpA = sbuf(st, "tmpA", [128, D])
            ss = [sbuf(st, "ss%d" % i, [128, 4]) for i in range(2)]; ssr = Rot("ss", 2)
            hxb = [sbuf(st, "hxb%d" % i, [128, D], BF16) for i in range(2)]; hr = Rot("hxb", 2)
            hxT = [sbuf(st, "hxT%d" % i, [128, 8, 512], BF16) for i in range(2)]; hTr = Rot("hxT", 2)
            st_u = sbuf(st, "st_u", [128, 4, 512]); st_k = sbuf(st, "st_k", [128, 4, 512], BF16)
            st_q = sbuf(st, "st_q", [128, 4, 512], BF16); st_g = sbuf(st, "st_g", [128, 16, 512], BF16)
            st_v = sbuf(st, "st_v", [128, 4, 512], BF16)
            tp = [psum(st, "tpA%d" % i, [128, 8, 128], BF16) for i in range(2)]; tpr = Rot("tpA", 2)
            pj = [psum(st, "pj%d" % i, [128, 512]) for i in range(4)]; pjr = Rot("pj", 4)
            evac_i = [0]

            def evac(dst_ap, src_ap, reads, writes, func=None):
                if func is not None:
                    P.op("act", lambda e: e.activation(out=dst_ap, in_=src_ap, func=func), reads, writes)
                    return
                evac_i[0] += 1
                if evac_i[0] % 2:
                    P.op("act", lambda e: e.copy(out=dst_ap, in_=src_ap), reads, writes)
                else:
                    P.op("dve", lambda e: e.tensor_copy(out=dst_ap, in_=src_ap), reads, writes)

            for i_ in range(2):
                P.op("pool", (lambda i_: lambda e: e.memset(hxT[i_][:], 0.0))(i_), writes=["hxT%d" % i_])
            chunks = [("ctx", 0, 256)] + [("lat", i * 512, 512) for i in range(8)]
            for kind, t0, n in chunks:
                src = ctx if kind == "ctx" else x
                mB, mkey = (modcB, "modcB") if kind == "ctx" else (modB, "modB")
                col0 = t0 if kind == "ctx" else CTX + t0
                hi, hkey = hTr.next()
                for t in range(n // 128):
                    xi, xkey = xr.next()
                    si, skey = ssr.next()
                    bi, bkey = hr.next()
                    pi, pkey = tpr.next()
                    r0 = t0 + t * 128
                    P.dma("sp", xkey, (lambda xi, r0, src: lambda e: e.dma_start(out=xt[xi][:], in_=src[r0:r0 + 128, :]))(xi, r0, src), writes=[xkey])
                    P.op("act", (lambda xi, si: lambda e: e.activation(out=junk[:], in_=xt[xi][:], func=AF.Square, accum_out=ss[si][:, 0:1]))(xi, si),
                         reads=[xkey], writes=["junkA", skey])
                    P.op("dve", (lambda si: lambda e: e.tensor_scalar(out=ss[si][:, 1:2], in0=ss[si][:, 0:1], scalar1=1.0 / D, scalar2=EPS,
                                                                      op0=ALU.mult, op1=ALU.add))(si), reads=[skey], writes=[skey])
                    P.op("act", (lambda si: lambda e: e.sqrt(out=ss[si][:, 2:3], in_=ss[si][:, 1:2]))(si), reads=[skey], writes=[skey])
                    P.op("dve", (lambda si: lambda e: e.reciprocal(out=ss[si][:, 3:4], in_=ss[si][:, 2:3]))(si), reads=[skey], writes=[skey])
                    P.op("dve", (lambda xi, si, mB: lambda e: e.scalar_tensor_tensor(out=tmpA[:], in0=xt[xi][:], scalar=ss[si][:, 3:4], in1=mB[:, D:2 * D],
                                                                                     op0=ALU.mult, op1=ALU.mult))(xi, si, mB),
                         reads=[xkey, skey, mkey], writes=["tmpA"])
                    P.op("dve", (lambda bi, mB: lambda e: e.tensor_tensor(out=hxb[bi][:], in0=tmpA[:], in1=mB[:, 0:D], op=ALU.add))(bi, mB),
                         reads=["tmpA", mkey], writes=[bkey])
                    for k in range(8):
                        P.op("pe", (lambda pi, bi, k: lambda e: e.transpose(tp[pi][:, k, :], hxb[bi][:, k * 128:(k + 1) * 128], identb[:]))(pi, bi, k),
                             reads=[bkey, "identb"], writes=[pkey])
                    P.op("act", (lambda hi, pi, t: lambda e: e.copy(out=hxT[hi][:, :, t * 128:(t + 1) * 128], in_=tp[pi][:]))(hi, pi, t),
                         reads=[pkey], writes=[hkey])
                cts = list(range(0, 8)) + (list(range(12, 32)) if kind == "lat" else [])
                for ct in cts:
                    qi, qkey = pjr.next()
                    for k in range(8):
                        P.op("pe", (lambda qi, hi, k, ct: lambda e: e.matmul(pj[qi][:, 0:n], lhsT=w_in_sb[:, k, ct * 128:(ct + 1) * 128], rhs=hxT[hi][:, k, 0:n],
                                                                             start=(k == 0), stop=(k == 7)))(qi, hi, k, ct),
                             reads=["w_in_sb", hkey], writes=[qkey])
                    if ct < 4:
                        evac(st_u[:, ct, 0:n], pj[qi][:, 0:n], [qkey], ["st_u"])
                    elif ct < 8:
                        evac(st_k[:, ct - 4, 0:n], pj[qi][:, 0:n], [qkey], ["st_k"])
                    elif ct < 16:
                        evac(st_q[:, ct - 12, 0:n], pj[qi][:, 0:n], [qkey], ["st_q"])
                    else:
                        evac(st_g[:, ct - 16, 0:n], pj[qi][:, 0:n], [qkey], ["st_g"], func=AF.Sigmoid)
                P.dma("sp", "stu", (lambda col0, n: lambda e: e.dma_start(out=uT_d.rearrange("(t p) n -> p t n", p=128)[:, :, col0:col0 + n], in_=st_u[:, :, 0:n]))(col0, n),
                      reads=["st_u"], writes=["uT_d"])
                P.dma("sp", "stk", (lambda col0, n: lambda e: e.dma_start(out=kT_d.rearrange("(t p) n -> p t n", p=128)[:, :, col0:col0 + n], in_=st_k[:, :, 0:n]))(col0, n),
                      reads=["st_k"], writes=["kT_d"])
                if kind == "lat":
                    P.dma("sp", "stq", (lambda t0: lambda e: e.dma_start(out=qT_d.rearrange("(t p) n -> p t n", p=128)[:, :, t0:t0 + 512], in_=st_q[:]))(t0),
                          reads=["st_q"], writes=["qT_d"])
                    P.dma("sp", "stg", (lambda t0: lambda e: e.dma_start(out=gT_d.rearrange("(t p) n -> p t n", p=128)[:, :, t0:t0 + 512], in_=st_g[:]))(t0),
                          reads=["st_g"], writes=["gT_d"])
                for t in range(n // 128):
                    qi, qkey = pjr.next()
                    for k in range(8):
                        P.op("pe", (lambda qi, hi, k, t: lambda e: e.matmul(pj[qi][:], lhsT=hxT[hi][:, k, t * 128:(t + 1) * 128], rhs=w_in_sb[:, k, 1024:1536],
                                                                            start=(k == 0), stop=(k == 7)))(qi, hi, k, t),
                             reads=["w_in_sb", hkey], writes=[qkey])
                    evac(st_v[:, t, :], pj[qi][:], [qkey], ["st_v"])
                nt = n // 128
                P.dma("sp", "stv", (lambda col0, nt: lambda e: e.dma_start(out=v_d[col0:col0 + nt * 128, :].rearrange("(t p) n -> p t n", p=128), in_=st_v[:, 0:nt, :]))(col0, nt),
                      reads=["st_v"], writes=["v_d"])
        P.barrier()
        if stage <= 1:
            return finish(nc, P, out)

        yT_d = dscr("yT_d", [512, SEQ], F32) if debug else None
        TWO_PI = 2.0 * math.pi
        with ExitStack() as stB:
            zT = sbuf(stB, "zT", [128, 4, SEQ], BF16)
            with phase() as st:
                def t32(n):
                    return sbuf(st, n, [128, 32])
                are, aim, ldt = t32("are"), t32("aim"), t32("ldt")
                Bn = [sbuf(st, "Bn%d" % i, [128, 32, 16]) for i in range(2)]
                bb = [sbuf(st, "bb%d" % i, [128, 32, 16]) for i in range(2)]
                tmpb = sbuf(st, "tmpb", [128, 32, 16])
                Cn2 = [sbuf(st, "Cn2%d" % i, [128, 8, 2, 64]) for i in range(2)]
                dsk = sbuf(st, "dsk", [128, 4])
                maskf = sbuf(st, "maskf", [128, 4, 2]); mask2 = sbuf(st, "mask2", [128, 4, 2])
                pwr = sbuf(st, "pwr", [128, 13, 32]); pwi = sbuf(st, "pwi", [128, 13, 32]); npwi = sbuf(st, "npwi", [128, 13, 32])
                kint = sbuf(st, "kint", [128, 32], I32)
                names = ["dt", "er", "th", "mag", "kf", "rr", "half", "sn", "ah", "cq", "sinr", "cosr", "nre", "den", "rden",
                         "fre", "fim", "t1", "t2"]
                T = {n: t32("p_" + n) for n in names}
                uT_sb = [sbuf(st, "uT_sb%d" % i, [128, NTOK]) for i in range(1)]
                PL = [sbuf(st, "PL%d" % i, [128, 2, NTOK]) for i in range(2)]
                yT = sbuf(st, "yT", [128, SEQ])
                Z = [sbuf(st, "Z%d" % i, [128, 2, 128]) for i in range(2)]
                Zc = [sbuf(st, "Zc%d" % i, [128, 2, 128]) for i in range(2)]
                LB = [sbuf(st, "LB%d" % i, [128, 2, 128]) for i in range(2)]
                LC = [sbuf(st, "LC%d" % i, [128, 2, 128]) for i in range(2)]
                pz = [psum(st, "pz%d" % i, [128, 2, 128]) for i in range(2)]; pzr = Rot("pz", 2)
                pb = [psum(st, "pb%d" % i, [128, 512]) for i in range(3)]; pbr = Rot("pb", 3)
                py = [psum(st, "py%d" % i, [128, 512]) for i in range(2)]; pyr = Rot("py", 2)

                for gl in range(2):
                    sl = slice(gl * 64, (gl + 1) * 64)
                    for dst, srcp, key in ((are, a_re, "are"), (aim, a_im, "aim")):
                        P.dma("act", "pb0", (lambda dst, srcp, sl, gl: lambda e: e.dma_start(
                            out=dst[sl, :].rearrange("p (d g) -> p d g", d=2),
                            in_=srcp.rearrange("d (gp gl) p -> gl p d gp", gl=2)[gl], allow_slow_non_contiguous=True))(dst, srcp, sl, gl), writes=[key])
                    P.dma("act", "pb0", (lambda sl, gl: lambda e: e.dma_start(
                        out=ldt[sl, :].rearrange("p (d g) -> p d g", d=2),
                        in_=log_dt.rearrange("d (gp gl) -> gl d gp", gl=2)[gl:gl + 1].to_broadcast([64, 2, 16]), allow_slow_non_contiguous=True))(sl, gl), writes=["ldt"])
                    for i, srcp in enumerate((b_re, b_im)):
                        P.dma("act", "pb0", (lambda i, srcp, sl, gl: lambda e: e.dma_start(
                            out=Bn[i][sl].rearrange("p (d g) h -> p d g h", d=2),
                            in_=srcp.rearrange("d (gp gl) p h -> gl p d gp h", gl=2)[gl]))(i, srcp, sl, gl), writes=["Bn%d" % i])
                for i, srcp in enumerate((c_re, c_im)):
                    for j in range(2):
                        P.dma("act", "pb0", (lambda i, srcp, j: lambda e: e.dma_start(
                            out=Cn2[i][:, :, j, :].rearrange("p (d u) q -> p d u q", d=2),
                            in_=srcp.rearrange("d (ut g8) h p -> (g8 h) d ut p", g8=8)))(i, srcp, j), writes=["Cn2%d" % i])
                P.dma("act", "pb0", lambda e: e.dma_start(out=dsk[:], in_=ssm_d.rearrange("(ut p) o -> p (ut o)", p=128), allow_slow_non_contiguous=True), writes=["dsk"])
                P.op("pool", lambda e: e.iota(maskf[:], pattern=[[-32, 4], [-16, 2]], base=0, channel_multiplier=1, allow_small_or_imprecise_dtypes=True), writes=["maskf"])
                P.op("dve", lambda e: e.tensor_single_scalar(out=mask2[:], in_=maskf[:], scalar=0.0, op=ALU.is_ge), reads=["maskf"], writes=["mask2"])
                P.op("dve", lambda e: e.tensor_single_scalar(out=maskf[:], in_=maskf[:], scalar=16.0, op=ALU.is_lt), reads=["maskf", "mask2"], writes=["maskf"])
                P.op("dve", lambda e: e.tensor_tensor(out=maskf[:], in0=maskf[:], in1=mask2[:], op=ALU.mult), reads=["maskf", "mask2"], writes=["maskf"])

                PK = ["are", "aim", "ldt", "Bn0", "Bn1", "prm"]

                def dve(fn):
                    P.op("dve", fn, reads=PK, writes=["prm"])

                def act(fn):
                    P.op("act", fn, reads=PK, writes=["prm"])
                act(lambda e: e.activation(out=T["dt"][:], in_=ldt[:], func=AF.Exp))
                dve(lambda e: e.tensor_tensor(out=T["er"][:], in0=are[:], in1=T["dt"][:], op=ALU.mult))
                dve(lambda e: e.tensor_tensor(out=T["th"][:], in0=aim[:], in1=T["dt"][:], op=ALU.mult))
                act(lambda e: e.activation(out=T["mag"][:], in_=T["er"][:], func=AF.Exp))
                dve(lambda e: e.tensor_single_scalar(out=T["kf"][:], in_=T["th"][:], scalar=1.0 / TWO_PI, op=ALU.mult))
                dve(lambda e: e.tensor_copy(out=kint[:], in_=T["kf"][:]))
                dve(lambda e: e.tensor_copy(out=T["kf"][:], in_=kint[:]))
                dve(lambda e: e.scalar_tensor_tensor(out=T["rr"][:], in0=T["kf"][:], scalar=-TWO_PI, in1=T["th"][:], op0=ALU.mult, op1=ALU.add))
                dve(lambda e: e.tensor_single_scalar(out=T["half"][:], in_=T["rr"][:], scalar=0.5, op=ALU.mult))
                act(lambda e: e.activation(out=T["ah"][:], in_=T["half"][:], func=AF.Abs))
                dve(lambda e: e.tensor_scalar(out=T["t1"][:], in0=T["ah"][:], scalar1=-1.0, scalar2=math.pi / 2, op0=ALU.mult, op1=ALU.add))
                act(lambda e: e.activation(out=T["sn"][:], in_=T["half"][:], func=AF.Sin))
                act(lambda e: e.activation(out=T["cq"][:], in_=T["t1"][:], func=AF.Sin))
                dve(lambda e: e.scalar_tensor_tensor(out=T["sinr"][:], in0=T["sn"][:], scalar=2.0, in1=T["cq"][:], op0=ALU.mult, op1=ALU.mult))
                dve(lambda e: e.scalar_tensor_tensor(out=T["t2"][:], in0=T["sn"][:], scalar=-2.0, in1=T["sn"][:], op0=ALU.mult, op1=ALU.mult))
                dve(lambda e: e.tensor_single_scalar(out=T["cosr"][:], in_=T["t2"][:], scalar=1.0, op=ALU.add))
                dve(lambda e: e.tensor_tensor(out=pwr[:, 0, :], in0=T["mag"][:], in1=T["cosr"][:], op=ALU.mult))
                dve(lambda e: e.tensor_tensor(out=pwi[:, 0, :], in0=T["mag"][:], in1=T["sinr"][:], op=ALU.mult))
                dve(lambda e: e.tensor_single_scalar(out=T["nre"][:], in_=pwr[:, 0, :], scalar=-1.0, op=ALU.add))
                dve(lambda e: e.tensor_tensor(out=T["den"][:], in0=are[:], in1=are[:], op=ALU.mult))
                dve(lambda e: e.tensor_tensor(out=T["t1"][:], in0=aim[:], in1=aim[:], op=ALU.mult))
                dve(lambda e: e.tensor_tensor(out=T["den"][:], in0=T["den"][:], in1=T["t1"][:], op=ALU.add))
                dve(lambda e: e.reciprocal(out=T["rden"][:], in_=T["den"][:]))
                dve(lambda e: e.tensor_tensor(out=T["t1"][:], in0=T["nre"][:], in1=are[:], op=ALU.mult))
                dve(lambda e: e.tensor_tensor(out=T["t2"][:], in0=pwi[:, 0, :], in1=aim[:], op=ALU.mult))
                dve(lambda e: e.tensor_tensor(out=T["t1"][:], in0=T["t1"][:], in1=T["t2"][:], op=ALU.add))
                dve(lambda e: e.tensor_tensor(out=T["fre"][:], in0=T["t1"][:], in1=T["rden"][:], op=ALU.mult))
                dve(lambda e: e.tensor_tensor(out=T["t1"][:], in0=pwi[:, 0, :], in1=are[:], op=ALU.mult))
                dve(lambda e: e.tensor_tensor(out=T["t2"][:], in0=T["nre"][:], in1=aim[:], op=ALU.mult))
                dve(lambda e: e.tensor_tensor(out=T["t1"][:], in0=T["t1"][:], in1=T["t2"][:], op=ALU.subtract))
                dve(lambda e: e.tensor_tensor(out=T["fim"][:], in0=T["t1"][:], in1=T["rden"][:], op=ALU.mult))
                fr = T["fre"][:].unsqueeze(2).to_broadcast([128, 32, 16]); fi = T["fim"][:].unsqueeze(2).to_broadcast([128, 32, 16])
                dve(lambda e: e.tensor_tensor(out=bb[0][:], in0=Bn[0][:], in1=fr, op=ALU.mult))
                dve(lambda e: e.tensor_tensor(out=tmpb[:], in0=Bn[1][:], in1=fi, op=ALU.mult))
                dve(lambda e: e.tensor_tensor(out=bb[0][:], in0=bb[0][:], in1=tmpb[:], op=ALU.subtract))
                dve(lambda e: e.tensor_tensor(out=bb[1][:], in0=Bn[1][:], in1=fr, op=ALU.mult))
                dve(lambda e: e.tensor_tensor(out=tmpb[:], in0=Bn[0][:], in1=fi, op=ALU.mult))
                dve(lambda e: e.tensor_tensor(out=bb[1][:], in0=bb[1][:], in1=tmpb[:], op=ALU.add))
                for k in range(12):
                    dve((lambda k: lambda e: e.tensor_tensor(out=T["t1"][:], in0=pwr[:, k, :], in1=pwr[:, k, :], op=ALU.mult))(k))
                    dve((lambda k: lambda e: e.tensor_tensor(out=T["t2"][:], in0=pwi[:, k, :], in1=pwi[:, k, :], op=ALU.mult))(k))
                    dve((lambda k: lambda e: e.tensor_tensor(out=pwr[:, k + 1, :], in0=T["t1"][:], in1=T["t2"][:], op=ALU.subtract))(k))
                    dve((lambda k: lambda e: e.scalar_tensor_tensor(out=pwi[:, k + 1, :], in0=pwr[:, k, :], scalar=2.0, in1=pwi[:, k, :], op0=ALU.mult, op1=ALU.mult))(k))
                dve(lambda e: e.tensor_single_scalar(out=npwi[:], in_=pwi[:], scalar=-1.0, op=ALU.mult))

                chain = {}

                def cmul_acc(hi_re, hi_im, lo_re, lo_im, k, u, key):
                    sr = pwr[:, k, u:u + 1]; si = pwi[:, k, u:u + 1]; nsi = npwi[:, k, u:u + 1]
                    prev = chain.get(key)
                    if prev is None:
                        prev = [w for w in (P.writer.get(key), P.writer.get("prm")) if w is not None]
                    ops_ = []
                    for n_, (o_, a_, s_) in enumerate(((hi_re, lo_re, sr), (hi_im, lo_re, si), (hi_re, lo_im, nsi), (hi_im, lo_im, sr))):
                        op = _Op("dve", (lambda o_, a_, s_: lambda e: e.scalar_tensor_tensor(out=o_, in0=a_, scalar=s_, in1=o_, op0=ALU.mult, op1=ALU.add))(o_, a_, s_), len(P.ops))
                        op.deps = list(prev) if n_ < 2 else [ops_[n_ - 2]]
                        P.ops.append(op)
                        ops_.append(op)
                    chain[key] = [ops_[3]]

                def scan_done(key):
                    P.writer[key] = chain.pop(key)[0]
                    P.readers[key] = []

                def bk_scan(pl, c0, n, rev, u, key, up_only=False):
                    L = n.bit_length() - 1
                    re = pl[:, 0, c0:c0 + n]; im = pl[:, 1, c0:c0 + n]
                    for k in range(L):
                        s_ = 2 << k; h_ = 1 << k
                        vr = re.rearrange("p (m s) -> p m s", s=s_); vi = im.rearrange("p (m s) -> p m s", s=s_)
                        if not rev:
                            cmul_acc(vr[:, :, s_ - 1], vi[:, :, s_ - 1], vr[:, :, h_ - 1], vi[:, :, h_ - 1], k, u, key)
                        else:
                            cmul_acc(vr[:, :, 0], vi[:, :, 0], vr[:, :, h_], vi[:, :, h_], k, u, key)
                    for k in (range(L - 2, -1, -1) if not up_only else ()):
                        s_ = 2 << k; h_ = 1 << k
                        vr = re.rearrange("p (m s) -> p m s", s=s_); vi = im.rearrange("p (m s) -> p m s", s=s_)
                        if not rev:
                            cmul_acc(vr[:, 1:, h_ - 1], vi[:, 1:, h_ - 1], vr[:, :-1, s_ - 1], vi[:, :-1, s_ - 1], k, u, key)
                        else:
                            cmul_acc(vr[:, :-1, h_], vi[:, :-1, h_], vr[:, 1:, 0], vi[:, 1:, 0], k, u, key)

                segs = [(0, 256)] + [(CTX + i * 512, 512) for i in range(8)]
                units = [(ut, d_, gpl) for ut in range(4) for d_ in range(2) for gpl in range(4)]

                def stA(ix):
                    ut, d_, gpl = units[ix]
                    u = d_ * 16 + ut * 4 + gpl
                    bi = ix % 2; ub = 0; ukey = "uT_sb0"
                    zk, zck, lbk, lck, plk = "Z%d" % bi, "Zc%d" % bi, "LB%d" % bi, "LC%d" % bi, "PL%d" % bi
                    if ix % 8 == 0:
                        P.dma("sp", ukey, lambda e: e.dma_start(out=uT_sb[ub][:], in_=uT_d[ut * 128:(ut + 1) * 128, :]), reads=["uT_d"], writes=[ukey])
                    P.op("pool", lambda e: e.memset(Z[bi][:], 0.0), writes=[zk])
                    for j in range(2):
                        for gl in range(2):
                            cs = (2 * gpl + gl) * 16
                            P.op("pool", (lambda j, gl, cs: lambda e: e.tensor_copy(out=Z[bi][gl * 64:(gl + 1) * 64, j, cs:cs + 16], in_=bb[j][gl * 64:(gl + 1) * 64, u, :]))(j, gl, cs),
                                 reads=["prm"], writes=[zk])
                    zi, zkey = pzr.next()
                    for j in range(2):
                        P.op("pe", (lambda zi, j: lambda e: e.matmul(pz[zi][:, j, :], lhsT=Z[bi][:, j, :], rhs=identf[:], start=True, stop=True))(zi, j), reads=[zk, "identf"], writes=[zkey])
                    P.op("act", (lambda zi: lambda e: e.copy(out=LB[bi][:], in_=pz[zi][:]))(zi), reads=[zkey], writes=[lbk])
                    for j in range(2):
                        P.op("pool", (lambda j: lambda e: e.tensor_tensor(out=Zc[bi][:, j, :].rearrange("p (g q) -> p g q", g=2), in0=Cn2[j][:, d_ * 4 + ut, :, :],
                                                                         in1=maskf[:, gpl, :].unsqueeze(2).to_broadcast([128, 2, 64]), op=ALU.mult))(j),
                             reads=["Cn2%d" % j, "maskf"], writes=[zck])
                    zi2, zkey2 = pzr.next()
                    for j in range(2):
                        P.op("pe", (lambda zi2, j: lambda e: e.matmul(pz[zi2][:, j, :], lhsT=Zc[bi][:, j, :], rhs=identf[:], start=True, stop=True))(zi2, j), reads=[zck, "identf"], writes=[zkey2])
                    P.op("act", lambda e: e.copy(out=LC[bi][:, 0, :], in_=pz[zi2][:, 0, :]), reads=[zkey2], writes=[lck])
                    P.op("act", lambda e: e.mul(out=LC[bi][:, 1, :], in_=pz[zi2][:, 1, :], mul=-1.0), reads=[zkey2], writes=[lck])
                    for (c0, n) in segs:
                        for j in range(2):
                            qi, qkey = pbr.next()
                            P.op("pe", (lambda qi, j, c0, n: lambda e: e.matmul(pb[qi][:, 0:n], lhsT=LB[bi][:, j, :], rhs=uT_sb[ub][:, c0:c0 + n], start=True, stop=True))(qi, j, c0, n),
                                 reads=[lbk, ukey], writes=[qkey])
                            P.op("act", (lambda qi, j, c0, n: lambda e: e.copy(out=PL[bi][:, j, c0:c0 + n], in_=pb[qi][:, 0:n]))(qi, j, c0, n), reads=[qkey], writes=[plk])

                def stB(ix):
                    ut, d_, gpl = units[ix]
                    u = d_ * 16 + ut * 4 + gpl
                    bi = ix % 2; plk = "PL%d" % bi
                    rev = (d_ == 1)
                    bk_scan(PL[bi], 0, CTX, rev, u, plk, up_only=True)
                    if not rev:
                        cmul_acc(PL[bi][:, 0, CTX:CTX + 1], PL[bi][:, 1, CTX:CTX + 1], PL[bi][:, 0, CTX - 1:CTX], PL[bi][:, 1, CTX - 1:CTX], 0, u, plk)
                    else:
                        cmul_acc(PL[bi][:, 0, NTOK - 1:NTOK], PL[bi][:, 1, NTOK - 1:NTOK], PL[bi][:, 0, 0:1], PL[bi][:, 1, 0:1], 0, u, plk)
                    bk_scan(PL[bi], CTX, SEQ, rev, u, plk)
                    scan_done(plk)

                def stC(ix):
                    ut, d_, gpl = units[ix]
                    bi = ix % 2; ub = 0; ukey = "uT_sb0"; lck, plk = "LC%d" % bi, "PL%d" % bi
                    first = (ix % 8 == 0)
                    for sgi in range(8):
                        c0 = CTX + sgi * 512
                        yi, ykey = pyr.next()
                        for j in range(2):
                            P.op("pe", (lambda yi, j, c0: lambda e: e.matmul(py[yi][:], lhsT=LC[bi][:, j, :], rhs=PL[bi][:, j, c0:c0 + 512], start=(j == 0), stop=(j == 1)))(yi, j, c0),
                                 reads=[lck, plk], writes=[ykey])
                        ysl = slice(sgi * 512, (sgi + 1) * 512)
                        if first:
                            P.op("dve", (lambda yi, c0, ysl: lambda e: e.scalar_tensor_tensor(out=yT[:, ysl], in0=uT_sb[ub][:, c0:c0 + 512], scalar=dsk[:, ut:ut + 1],
                                                                                              in1=py[yi][:], op0=ALU.mult, op1=ALU.add))(yi, c0, ysl),
                                 reads=[ykey, ukey, "dsk"], writes=["yT"])
                        else:
                            P.op("dve", (lambda yi, ysl: lambda e: e.tensor_tensor(out=yT[:, ysl], in0=yT[:, ysl], in1=py[yi][:], op=ALU.add))(yi, ysl), reads=[ykey], writes=["yT"])
                    if ix % 8 == 7:
                        if debug:
                            P.dma("sp", "dbgy", lambda e: e.dma_start(out=yT_d[ut * 128:(ut + 1) * 128, :], in_=yT[:]), reads=["yT"], writes=["yT_d"])
                        P.op("act", lambda e: e.activation(out=zT[:, ut, :], in_=yT[:], func=AF.Gelu_apprx_tanh), reads=["yT"], writes=["zT"])

                stA(0)
                for ix in range(32):
                    if ix + 1 < 32:
                        stA(ix + 1)
                    stB(ix)
                    stC(ix)
            P.barrier()
            with phase() as st:
                wstgB_t = sbuf(st, "wstgB", [128, 4, 1024])
                w_glu_sb = sbuf(st, "w_glu_sb", [128, 4, 512], BF16); w_ba_sb = sbuf(st, "w_ba_sb", [128, 4, D], BF16)
                bglu = sbuf(st, "bglu", [128, 4])
                sg = [sbuf(st, "sg%d" % i, [128, 512], BF16) for i in range(2)]; sgr = Rot("sg", 2)
                glu = [sbuf(st, "glu%d" % i, [128, 4, 512], BF16) for i in range(2)]
                st_ba = [sbuf(st, "st_ba%d" % i, [128, 8, 512], BF16) for i in range(2)]
                pg = [psum(st, "pg%d" % i, [128, 512]) for i in range(3)]; pgr = Rot("pg", 3)
                pa = [psum(st, "pa%d" % i, [128, 512]) for i in range(3)]; par = Rot("pa", 3)
                P.dma("sp", "wl0", lambda e: e.dma_start(out=wstgB_t[:, :, 0:512], in_=w_glu.rearrange("(k p) n -> p k n", p=128)), writes=["wstgB"])
                P.op("dve", lambda e: e.tensor_copy(out=w_glu_sb[:], in_=wstgB_t[:, :, 0:512]), reads=["wstgB"], writes=["w_glu_sb"])
                P.dma("sp", "wl0", lambda e: e.dma_start(out=wstgB_t[:], in_=w_ba.rearrange("(k p) n -> p k n", p=128)), reads=["wstgB"], writes=["wstgB"])
                P.op("dve", lambda e: e.tensor_copy(out=w_ba_sb[:], in_=wstgB_t[:]), reads=["wstgB"], writes=["w_ba_sb"])
                P.dma("act", "wl1", lambda e: e.dma_start(out=bglu[:], in_=b_glu.rearrange("(k p) o -> p (k o)", p=128), allow_slow_non_contiguous=True), writes=["bglu"])
                for sgi in range(8):
                    gb_ = sgi % 2; gkey = "glu%d" % gb_; bakey = "st_ba%d" % gb_
                    ssl = slice(sgi * 512, (sgi + 1) * 512)
                    for ct in range(4):
                        gi, gk = pgr.next()
                        for k in range(4):
                            P.op("pe", (lambda gi, k, ct, ssl: lambda e: e.matmul(pg[gi][:], lhsT=w_glu_sb[:, k, ct * 128:(ct + 1) * 128], rhs=zT[:, k, ssl],
                                                                                  start=(k == 0), stop=(k == 3)))(gi, k, ct, ssl),
                                 reads=["w_glu_sb", "zT"], writes=[gk])
                        si_, sk_ = sgr.next()
                        P.op("act", (lambda si_, gi, ct: lambda e: e.activation(out=sg[si_][:], in_=pg[gi][:], func=AF.Sigmoid, bias=bglu[:, ct:ct + 1]))(si_, gi, ct),
                             reads=[gk, "bglu"], writes=[sk_])
                        P.op("dve", (lambda gb_, ct, si_, ssl: lambda e: e.tensor_tensor(out=glu[gb_][:, ct, :], in0=sg[si_][:], in1=zT[:, ct, ssl], op=ALU.mult))(gb_, ct, si_, ssl),
                             reads=[sk_, "zT"], writes=[gkey])
                    for ct2 in range(8):
                        ai, ak = par.next()
                        for k in range(4):
                            P.op("pe", (lambda ai, k, ct2, gb_: lambda e: e.matmul(pa[ai][:], lhsT=w_ba_sb[:, k, ct2 * 128:(ct2 + 1) * 128], rhs=glu[gb_][:, k, :],
                                                                                   start=(k == 0), stop=(k == 3)))(ai, k, ct2, gb_),
                                 reads=["w_ba_sb", gkey], writes=[ak])
                        if ct2 % 2:
                            P.op("act", (lambda gb_, ct2, ai: lambda e: e.copy(out=st_ba[gb_][:, ct2, :], in_=pa[ai][:]))(gb_, ct2, ai), reads=[ak], writes=[bakey])
                        else:
                            P.op("dve", (lambda gb_, ct2, ai: lambda e: e.tensor_copy(out=st_ba[gb_][:, ct2, :], in_=pa[ai][:]))(gb_, ct2, ai), reads=[ak], writes=[bakey])
                    P.dma("sp", bakey, (lambda gb_, ssl: lambda e: e.dma_start(out=baT_d.rearrange("(t p) n -> p t n", p=128)[:, :, ssl], in_=st_ba[gb_][:]))(gb_, ssl),
                          reads=[bakey], writes=["baT_d"])
        P.barrier()
        if stage <= 2:
            return finish(nc, P, out)

        attT_d = dscr("attT_d", [512, SEQ], BF16) if debug else None
        NEG = -30000.0
        with ExitStack() as stC:
            attT_sb = sbuf(stC, "attT_sb", [128, 4, SEQ], BF16)
            stC2 = ExitStack()
            kT_sb = sbuf(stC2, "kT_sb", [128, 4, NTOK], BF16); qT_sb = sbuf(stC2, "qT_sb", [128, 4, SEQ], BF16)
            BiasTT = sbuf(stC2, "BiasTT", [128, 8 * 14, 64])
            Vctx = sbuf(stC2, "Vctx", [128, 2, 512], BF16)
            ones_b = sbuf(stC2, "ones_b", [128, 128], BF16)
            P.dma("sp", "lc0", lambda e: e.dma_start(out=kT_sb[:], in_=kT_d.rearrange("(t p) n -> p t n", p=128)), reads=["kT_d"], writes=["kT_sb"])
            P.dma("act", "lc1", lambda e: e.dma_start(out=qT_sb[:], in_=qT_d.rearrange("(t p) n -> p t n", p=128)), reads=["qT_d"], writes=["qT_sb"])
            P.dma("act", "lc1", lambda e: e.dma_start(out=Vctx[:], in_=v_d[0:CTX, :].rearrange("(t p) n -> p t n", p=128)), reads=["v_d"], writes=["Vctx"])
            P.op("pool", lambda e: e.memset(ones_b[:], 1.0), writes=["ones_b"])
            with phase() as st:
                rpbB = sbuf(st, "rpbB", [128, 8 * 14, 31]); tmpC = sbuf(st, "tmpC", [128, 8 * 14, 64])
                Dm = sbuf(st, "Dm", [128, 64]); eqm = [sbuf(st, "eqm%d" % i, [128, 64]) for i in range(2)]
                c0t = sbuf(st, "c0t", [128, 64]); kcv = sbuf(st, "kcv", [128, 64]); m2 = sbuf(st, "m2c", [128, 64])
                for half in range(2):
                    sl = slice(half * 64, (half + 1) * 64)
                    P.dma("sp", "lc2", (lambda sl, half: lambda e: e.dma_start(out=rpbB[sl].rearrange("p (h j) m -> p h (j m)", h=8),
                                                                              in_=rpb[:, half:half + 14, :].rearrange("h j m -> h (j m)").unsqueeze(0).to_broadcast([64, 8, 14 * 31])))(sl, half),
                          writes=["rpbB"])
                    P.op("pool", (lambda sl: lambda e: e.iota(Dm[sl], pattern=[[-1, 64]], base=15, channel_multiplier=1, allow_small_or_imprecise_dtypes=True))(sl), writes=["Dm"])
                    P.op("pool", (lambda sl: lambda e: e.iota(kcv[sl], pattern=[[0, 64]], base=0, channel_multiplier=1, allow_small_or_imprecise_dtypes=True))(sl), writes=["kcv"])
                P.op("pool", lambda e: e.iota(c0t[:], pattern=[[1, 64]], base=-8, channel_multiplier=0, allow_small_or_imprecise_dtypes=True), writes=["c0t"])
                P.op("dve", lambda e: e.tensor_scalar(out=c0t[:], in0=c0t[:], scalar1=0.0, scalar2=48.0, op0=ALU.max, op1=ALU.min), reads=["c0t"], writes=["c0t"])
                P.op("dve", lambda e: e.tensor_tensor(out=kcv[:], in0=kcv[:], in1=c0t[:], op=ALU.subtract), reads=["kcv", "c0t"], writes=["kcv"])
                P.op("dve", lambda e: e.tensor_single_scalar(out=m2[:], in_=kcv[:], scalar=0.0, op=ALU.is_ge), reads=["kcv"], writes=["m2c"])
                P.op("dve", lambda e: e.tensor_single_scalar(out=kcv[:], in_=kcv[:], scalar=15.0, op=ALU.is_le), reads=["kcv", "m2c"], writes=["kcv"])
                P.op("dve", lambda e: e.tensor_tensor(out=m2[:], in0=m2[:], in1=kcv[:], op=ALU.mult), reads=["kcv", "m2c"], writes=["m2c"])
                P.op("dve", lambda e: e.tensor_scalar(out=m2[:], in0=m2[:], scalar1=-1.0, scalar2=-NEG, op0=ALU.add, op1=ALU.mult), reads=["m2c"], writes=["m2c"])
                P.op("dve", lambda e: e.tensor_copy(out=BiasTT[:], in_=m2[:].unsqueeze(1).to_broadcast([128, 112, 64])), reads=["m2c"], writes=["BiasTT"])
                for m in range(31):
                    ei = m % 2; ek = "eqm%d" % ei
                    P.op("dve", (lambda ei, m: lambda e: e.tensor_single_scalar(out=eqm[ei][:], in_=Dm[:], scalar=float(m), op=ALU.is_equal))(ei, m), reads=["Dm"], writes=[ek])
                    P.op("pool", (lambda ei, m: lambda e: e.tensor_tensor(out=tmpC[:], in0=eqm[ei][:].unsqueeze(1).to_broadcast([128, 112, 64]),
                                                                          in1=rpbB[:, :, m:m + 1].to_broadcast([128, 112, 64]), op=ALU.mult))(ei, m),
                         reads=[ek, "rpbB"], writes=["tmpC"])
                    P.op("dve", lambda e: e.tensor_tensor(out=BiasTT[:], in0=BiasTT[:], in1=tmpC[:], op=ALU.add), reads=["tmpC"], writes=["BiasTT"])
            P.barrier()
            with phase() as st:
                Vb = [sbuf(st, "Vb%d" % i, [128, 4, 512], BF16) for i in range(3)]; vbr = Rot("Vb", 3)
                ssb = [sbuf(st, "ssb%d" % i, [128, 4, 64]) for i in range(3)]; ssr2 = Rot("ssb", 3)
                pT = [sbuf(st, "pT%d" % i, [128, 384], BF16) for i in range(3)]; ptr = Rot("pT", 3)
                rden = [sbuf(st, "rden%d" % i, [128, 64]) for i in range(2)]; rdr = Rot("rden", 2)
                ps_ = [psum(st, "psc%d" % i, [128, 512]) for i in range(3)]; psr = Rot("psc", 3)
                po_ = [psum(st, "poc%d" % i, [128, 512]) for i in range(2)]; por = Rot("poc", 2)
                pd_ = [psum(st, "pdc%d" % i, [128, 512]) for i in range(2)]; pdr = Rot("pdc", 2)
                B4 = BiasTT[:].rearrange("p (h j) q -> p h j q", h=8)
                cf = [sbuf(st, "cvf%d" % i, [128, 2048]) for i in range(3)]; cb = [sbuf(st, "cvb%d" % i, [128, 2048], BF16) for i in range(3)]

                def convert_tile(ti):
                    bi = ti % 3
                    P.dma("sp", "cvf%d" % bi, lambda e: e.dma_start(out=cf[bi][:], in_=peer_uv[ti * 128:(ti + 1) * 128, :]), writes=["cvf%d" % bi])
                    P.op("pool", lambda e: e.tensor_copy(out=cb[bi][:], in_=cf[bi][:]), reads=["cvf%d" % bi], writes=["cvb%d" % bi])
                    P.dma("pool", "cvb%d" % bi, lambda e: e.dma_start(out=uvb_d[ti * 128:(ti + 1) * 128, :], in_=cb[bi][:]), reads=["cvb%d" % bi], writes=["uvb_d"])
                def c_scores(r, h, vi):
                    r0 = min(max(r - 4, 0), 56)
                    t = h // 2; po = (h % 2) * 64; psl = slice(po, po + 64)
                    si, skey = psr.next()
                    qsl = slice(r * 64, (r + 1) * 64)
                    for j in range(6):
                        k0 = (CTX + (r0 + 2 * j) * 64) if j < 4 else (j - 4) * 128
                        P.op("pe", (lambda j, k0: lambda e: e.matmul(ps_[si][:, j * 64:(j + 1) * 64], lhsT=kT_sb[psl, t, k0:k0 + 128], rhs=qT_sb[psl, t, qsl], start=True, stop=True))(j, k0),
                             reads=["kT_sb", "qT_sb"], writes=[skey])
                    return (r, h, vi, si, skey)

                def c_part1(state):
                    r, h, vi, si, skey = state
                    r0 = min(max(r - 4, 0), 56); dr0 = r0 - r + 7
                    bi2, bkey2 = ssr2.next()
                    P.op("dve", lambda e: e.scalar_tensor_tensor(out=ssb[bi2][:], in0=ps_[si][:, 0:256].rearrange("p (j q) -> p j q", j=4), scalar=0.125,
                                                                 in1=B4[:, h, dr0:dr0 + 7:2, :], op0=ALU.mult, op1=ALU.add), reads=[skey, "BiasTT"], writes=[bkey2])
                    ti, tkey = ptr.next()
                    P.op("act", lambda e: e.activation(out=pT[ti][:, 0:256], in_=ssb[bi2][:].rearrange("p j q -> p (j q)"), func=AF.Exp), reads=[bkey2], writes=[tkey])
                    P.op("act", lambda e: e.activation(out=pT[ti][:, 256:384], in_=ps_[si][:, 256:384], func=AF.Exp, scale=0.125), reads=[skey], writes=[tkey])
                    return (r, h, vi, ti, tkey)

                def c_part2(state):
                    r, h, vi, ti, tkey = state
                    vkey = "Vb%d" % vi
                    t = h // 2; po = (h % 2) * 64; psl = slice(po, po + 64)
                    qsl = slice(r * 64, (r + 1) * 64)
                    oi, okey = por.next(); di, dkey = pdr.next()
                    hp = (h // 2) * 128
                    for j in range(6):
                        vsrc = (Vb[vi][:, j, hp:hp + 128] if j < 4 else Vctx[:, j - 4, hp:hp + 128])
                        P.op("pe", (lambda j, vsrc: lambda e: e.matmul(po_[oi][:, 0:64], lhsT=vsrc, rhs=pT[ti][:, j * 64:(j + 1) * 64], start=(j == 0), stop=(j == 5)))(j, vsrc),
                             reads=[vkey, "Vctx", tkey], writes=[okey])
                    for j in range(6):
                        P.op("pe", (lambda j: lambda e: e.matmul(pd_[di][:, 0:64], lhsT=ones_b[:], rhs=pT[ti][:, j * 64:(j + 1) * 64], start=(j == 0), stop=(j == 5)))(j),
                             reads=["ones_b", tkey], writes=[dkey])
                    ri, rkey = rdr.next()
                    P.op("dve", lambda e: e.reciprocal(out=rden[ri][psl, :], in_=pd_[di][psl, 0:64]), reads=[dkey], writes=[rkey])
                    P.op("dve", lambda e: e.tensor_tensor(out=attT_sb[psl, t, qsl], in0=po_[oi][psl, 0:64], in1=rden[ri][psl, :], op=ALU.mult), reads=[okey, rkey], writes=["attT_sb"])

                pend1 = None; pend2 = None
                for r in range(64):
                    convert_tile(2 * r); convert_tile(2 * r + 1)
                    r0 = min(max(r - 4, 0), 56)
                    vi, vkey = vbr.next()
                    P.dma("sp", vkey, (lambda vi, r0: lambda e: e.dma_start(out=Vb[vi][:], in_=v_d[CTX + r0 * 64:CTX + (r0 + 8) * 64, :].rearrange("(j p) n -> p j n", p=128)))(vi, r0),
                          reads=["v_d"], writes=[vkey])
                    for h in range(8):
                        stt = c_scores(r, h, vi)
                        nxt2 = c_part1(pend1) if pend1 is not None else None
                        if pend2 is not None:
                            c_part2(pend2)
                        pend2 = nxt2
                        pend1 = stt
                nxt2 = c_part1(pend1)
                if pend2 is not None:
                    c_part2(pend2)
                c_part2(nxt2)
            if debug:
                P.dma("sp", "dbga", lambda e: e.dma_start(out=attT_d.rearrange("(t p) n -> p t n", p=128), in_=attT_sb[:]), reads=["attT_sb"], writes=["attT_d"])
            P.barrier()
            stC2.close()
            with phase() as st:
                wstgC_t = sbuf(st, "wstgC", [128, 4, 1024]); w_bb_sb = sbuf(st, "w_bb_sb", [128, 4, D], BF16)
                g_sb = [sbuf(st, "g_sb%d" % i, [128, 16, 512], BF16) for i in range(2)]
                ba_sb = [sbuf(st, "ba_sb%d" % i, [128, 8, 512], BF16) for i in range(2)]
                t1 = [sbuf(st, "t1c%d" % i, [128, 512]) for i in range(2)]; t1r = Rot("t1c", 2)
                t2 = [sbuf(st, "t2c%d" % i, [128, 512]) for i in range(2)]; t2r = Rot("t2c", 2)
                st_mg = [sbuf(st, "st_mg%d" % i, [128, 8, 512], BF16) for i in range(2)]
                pbb = [psum(st, "pbb%d" % i, [128, 512]) for i in range(3)]; pbr2 = Rot("pbb", 3)
                P.dma("sp", "wc0", lambda e: e.dma_start(out=wstgC_t[:], in_=w_bb.rearrange("(k p) n -> p k n", p=128)), writes=["wstgC"])
                P.op("dve", lambda e: e.tensor_copy(out=w_bb_sb[:], in_=wstgC_t[:]), reads=["wstgC"], writes=["w_bb_sb"])
                for sgi in range(8):
                    b2 = sgi % 2; ssl = slice(sgi * 512, (sgi + 1) * 512)
                    gk, bk, mk = "g_sb%d" % b2, "ba_sb%d" % b2, "st_mg%d" % b2
                    P.dma("sp", gk, (lambda b2, ssl: lambda e: e.dma_start(out=g_sb[b2][:], in_=gT_d.rearrange("(t p) n -> p t n", p=128)[:, :, ssl]))(b2, ssl), reads=["gT_d"], writes=[gk])
                    P.dma("act", bk, (lambda b2, ssl: lambda e: e.dma_start(out=ba_sb[b2][:], in_=baT_d.rearrange("(t p) n -> p t n", p=128)[:, :, ssl]))(b2, ssl), reads=["baT_d"], writes=[bk])
                    for ct2 in range(8):
                        qi, qk = pbr2.next()
                        for k in range(4):
                            P.op("pe", (lambda qi, k, ct2, ssl: lambda e: e.matmul(pbb[qi][:], lhsT=w_bb_sb[:, k, ct2 * 128:(ct2 + 1) * 128], rhs=attT_sb[:, k, ssl],
                                                                                   start=(k == 0), stop=(k == 3)))(qi, k, ct2, ssl),
                                 reads=["w_bb_sb", "attT_sb"], writes=[qk])
                        i1, k1 = t1r.next(); i2, k2 = t2r.next()
                        P.op("dve", (lambda i1, qi, b2, ct2: lambda e: e.tensor_tensor(out=t1[i1][:], in0=pbb[qi][:], in1=g_sb[b2][:, 8 + ct2, :], op=ALU.mult))(i1, qi, b2, ct2),
                             reads=[qk, gk], writes=[k1])
                        P.op("pool", (lambda i2, b2, ct2: lambda e: e.tensor_tensor(out=t2[i2][:], in0=ba_sb[b2][:, ct2, :], in1=g_sb[b2][:, ct2, :], op=ALU.mult))(i2, b2, ct2),
                             reads=[bk, gk], writes=[k2])
                        P.op("dve", (lambda b2, ct2, i1, i2: lambda e: e.tensor_tensor(out=st_mg[b2][:, ct2, :], in0=t1[i1][:], in1=t2[i2][:], op=ALU.add))(b2, ct2, i1, i2),
                             reads=[k1, k2], writes=[mk])
                    P.dma("sp", mk, (lambda b2, ssl: lambda e: e.dma_start(out=mgT_d.rearrange("(t p) n -> p t n", p=128)[:, :, ssl], in_=st_mg[b2][:]))(b2, ssl),
                          reads=[mk], writes=["mgT_d"])
        P.barrier()
        if stage <= 3:
            return finish(nc, P, out)

        x1_d = dscr("x1_d", [SEQ, D], F32) if debug else None
        pf_d = dscr("pf_d", [SEQ, D], F32) if debug else None
        NT = 32 if stage >= 5 else int(stage * 10) % 10 or 1
        if not debug:
            NT = 32
        if "KNT" in os.environ:
            NT = int(os.environ["KNT"])
        gate1B = modB[:, 2 * D:3 * D]; S2B = modB[:, 3 * D:4 * D]; G2B = modB[:, 4 * D:5 * D]; gate2B = modB[:, 5 * D:6 * D]
        with ExitStack() as stD:
            w_out_sb = sbuf(stD, "w_out_sb", [128, 8, D], BF16); w_q_sb = sbuf(stD, "w_q_sb", [128, 8, 2048], BF16)
            skT = sbuf(stD, "skT", [128, 2, 128], BF16); fgB = sbuf(stD, "fgB", [128, D])
            with phase() as st:
                wstgD_t = [sbuf(st, "wstgD%d" % i, [128, 8, 512]) for i in range(2)]
                skf = sbuf(st, "skf", [128, 2, 128]); skb = sbuf(st, "skb", [128, 2, 128], BF16)
                ptk = psum(st, "ptk", [128, 2, 128], BF16)
                for i in range(6):
                    bi = i % 2
                    srcw = (w_out if i < 2 else w_q).rearrange("(k p) n -> p k n", p=128)
                    c0 = (i * 512) if i < 2 else (i - 2) * 512
                    dstw = w_out_sb if i < 2 else w_q_sb
                    P.dma("sp", "wd%d" % bi, (lambda bi, srcw, c0: lambda e: e.dma_start(out=wstgD_t[bi][:], in_=srcw[:, :, c0:c0 + 512]))(bi, srcw, c0), writes=["wstgD%d" % bi])
                    P.op(("dve", "pool")[bi], (lambda bi, dstw, c0: lambda e: e.tensor_copy(out=dstw[:, :, c0:c0 + 512], in_=wstgD_t[bi][:]))(bi, dstw, c0),
                         reads=["wstgD%d" % bi], writes=["wD"])
                P.dma("act", "wd2", lambda e: e.dma_start(out=skf[:], in_=subkeys.rearrange("n k d -> k n d")), writes=["skf"])
                P.dma("act", "wd2", lambda e: e.dma_start(out=fgB[:], in_=final_g.to_broadcast([128, D])), writes=["fgB"])
                P.op("dve", lambda e: e.tensor_copy(out=skb[:], in_=skf[:]), reads=["skf"], writes=["skb"])
                for n_ in range(2):
                    P.op("pe", (lambda n_: lambda e: e.transpose(ptk[:, n_, :], skb[:, n_, :], identb[:]))(n_), reads=["skb", "identb"], writes=["ptk"])
                P.op("dve", lambda e: e.tensor_copy(out=skT[:], in_=ptk[:]), reads=["ptk"], writes=["skT"])
            P.barrier()
            P.barrier()
            if stage == 3.5:
                return finish(nc, P, out)
            with phase() as st:
                NB = 2
                xtD_t = sbuf(st, "xtD", [128, D])
                x1 = [sbuf(st, "x1_%d" % i, [128, D]) for i in range(NB)]
                h2 = [sbuf(st, "h2_%d" % i, [128, D]) for i in range(NB)]
                eid = [sbuf(st, "eid%d" % i, [128, 128], I32) for i in range(NB)]
                gate = [sbuf(st, "gate%d" % i, [128, 128]) for i in range(NB)]
                tmpD = sbuf(st, "tmpD", [128, D]); tmpF = tmpD
                acc = None
                junkD = sbuf(st, "junkD", [128, D], BF16); junkE3 = [sbuf(st, "junkE%d" % i, [128, D], BF16) for i in range(3)]; jer = Rot("junkE", 3)
                mg_sb = sbuf(st, "mg_sb", [128, 8, 128], BF16); h2b = sbuf(st, "h2b", [128, D], BF16); h2T = sbuf(st, "h2T", [128, 8, 128], BF16)
                qT_sb2 = sbuf(st, "qT_sb2", [128, 16, 128], BF16); s_sb = sbuf(st, "s_sb", [128, 16, 128]); work = sbuf(st, "workD", [128, 16, 128])
                topv = sbuf(st, "topv", [128, 16, 16]); idxu = sbuf(st, "idxu", [128, 16, 16], U32); idxf = sbuf(st, "idxf", [128, 16, 16])
                cand = sbuf(st, "cand", [128, 8, 256]); cidx = s_sb[:].rearrange("p a b -> p (a b)").rearrange("p (h c) -> p h c", h=8)
                best = sbuf(st, "best", [128, 8, 16]); eg = sbuf(st, "eg", [128, 8, 16]); sm = sbuf(st, "smD", [128, 32]); sm2 = sbuf(st, "smE", [128, 8]); eidf = sbuf(st, "eidf", [128, 128])
                aD = sbuf(st, "aD", [128, 128]); gl_ = sbuf(st, "gl_", [128, 128]); wD = sbuf(st, "wDw", [128, 128])
                NG = int(os.environ.get("KNG", "12")); GJ = int(os.environ.get("KGJ", "4"))
                FY = int(os.environ.get("KFY", "1"))
                posI = sbuf(st, "posI", [128, 256], I32); mskI = sbuf(st, "mskI", [128, 1], I32)
                c4I = sbuf(st, "c4I", [128, 1], I32); c15I = sbuf(st, "c15I", [128, 1], I32); iota16 = sbuf(st, "iota16", [128, 16])
                pab = sbuf(st, "pab", [128, 2, 8, 16], I32); pabf = sbuf(st, "pabf", [128, 2, 8, 16]); e3b = sbuf(st, "e3b", [128, 8, 16])
                oh = cand[:].rearrange("p h (k a) -> p h k a", a=16)
                P.op("pool", lambda e: e.iota(c4I[:], pattern=[[0, 1]], base=4, channel_multiplier=0), writes=["posI"])
                P.op("pool", lambda e: e.iota(c15I[:], pattern=[[0, 1]], base=15, channel_multiplier=0), writes=["posI"])
                P.op("pool", lambda e: e.iota(iota16[:], pattern=[[1, 16]], base=0, channel_multiplier=0, allow_small_or_imprecise_dtypes=True), writes=["posI"])
                P.op("pool", lambda e: e.iota(posI[:], pattern=[[1, 256]], base=0, channel_multiplier=0), writes=["posI"])
                P.op("pool", lambda e: e.iota(mskI[:], pattern=[[0, 1]], base=-256, channel_multiplier=0), writes=["posI"])
                UVg = [sbuf(st, "UVg%d" % i, [128, 2, D], BF16) for i in range(NG)]
                dg = [sbuf(st, "dg%d" % i, [128, 128], BF16) for i in range(8)]; dgr = Rot("dg", 8)
                pmo = [psum(st, "pmo%d" % i, [128, 512]) for i in range(2)]
                pacc = [psum(st, "pacc%d" % i, [128, 512]) for i in range(2)]
                tpD = psum(st, "tpD", [128, 8, 128], BF16)
                pq = [psum(st, "pq%d" % i, [128, 4, 128]) for i in range(2)]; pqr = Rot("pq", 2)
                uv_i = [0]

                def rstd_chain(src_ap, srckey, smt, smk, jk, jkey):
                    P.op("act", lambda e: e.activation(out=jk[:], in_=src_ap, func=AF.Square, accum_out=smt[:, 0:1]), reads=[srckey], writes=[smk, jkey])
                    P.op("dve", lambda e: e.tensor_scalar(out=smt[:, 1:2], in0=smt[:, 0:1], scalar1=1.0 / D, scalar2=EPS, op0=ALU.mult, op1=ALU.add), reads=[smk], writes=[smk])
                    P.op("act", lambda e: e.sqrt(out=smt[:, 2:3], in_=smt[:, 1:2]), reads=[smk], writes=[smk])
                    P.op("dve", lambda e: e.reciprocal(out=smt[:, 3:4], in_=smt[:, 2:3]), reads=[smk], writes=[smk])

                def front(i):
                    b = i % NB; t0 = i * 128
                    xk, x1k, h2k, ek, gk = "xtD", "x1_%d" % b, "h2_%d" % b, "eid%d" % b, "gate%d" % b
                    P.dma("sp", xk, lambda e: e.dma_start(out=xtD_t[:], in_=x[t0:t0 + 128, :]), writes=[xk])
                    P.dma("sp", "mgl", lambda e: e.dma_start(out=mg_sb[:], in_=mgT_d.rearrange("(k p) n -> p k n", p=128)[:, :, t0:t0 + 128]), reads=["mgT_d"], writes=["mg_sb"])
                    for hf in range(2):
                        for k in range(8):
                            P.op("pe", (lambda hf, k: lambda e: e.matmul(pmo[hf][:], lhsT=mg_sb[:, k, :], rhs=w_out_sb[:, k, hf * 512:(hf + 1) * 512], start=(k == 0), stop=(k == 7)))(hf, k),
                                 reads=["mg_sb", "wD"], writes=["pmo%d" % hf])
                        yield
                        P.op("dve", (lambda hf: lambda e: e.tensor_tensor(out=tmpF[:, hf * 512:(hf + 1) * 512], in0=pmo[hf][:], in1=gate1B[:, hf * 512:(hf + 1) * 512], op=ALU.mult))(hf),
                             reads=["pmo%d" % hf, "modB"], writes=["tmpD"])
                        yield
                    P.op("dve", lambda e: e.tensor_tensor(out=x1[b][:], in0=tmpF[:], in1=xtD_t[:], op=ALU.add), reads=["tmpD", xk], writes=[x1k])
                    yield
                    if debug:
                        P.dma("sp", "dbgx1", lambda e: e.dma_start(out=x1_d[t0:t0 + 128, :], in_=x1[b][:]), reads=[x1k], writes=["x1_d"])
                    rstd_chain(x1[b][:], x1k, sm[:, 0:4], "smD", junkD, "junkD")
                    P.op("dve", lambda e: e.scalar_tensor_tensor(out=tmpF[:], in0=x1[b][:], scalar=sm[:, 3:4], in1=G2B, op0=ALU.mult, op1=ALU.mult), reads=[x1k, "smD", "modB"], writes=["tmpD"])
                    yield
                    P.op("dve", lambda e: e.tensor_tensor(out=h2[b][:], in0=tmpF[:], in1=S2B, op=ALU.add), reads=["tmpD", "modB"], writes=[h2k])
                    yield
                    P.op("act", lambda e: e.copy(out=h2b[:], in_=h2[b][:]), reads=[h2k], writes=["h2b"])
                    for k in range(8):
                        P.op("pe", (lambda k: lambda e: e.transpose(tpD[:, k, :], h2b[:, k * 128:(k + 1) * 128], identb[:]))(k), reads=["h2b", "identb"], writes=["tpD"])
                    P.op("act", lambda e: e.copy(out=h2T[:], in_=tpD[:]), reads=["tpD"], writes=["h2T"])
                    yield
                    for g4 in range(4):
                        qi, qk = pqr.next()
                        for bl in range(4):
                            blk = g4 * 4 + bl
                            for k in range(8):
                                P.op("pe", (lambda qi, bl, blk, k: lambda e: e.matmul(pq[qi][:, bl, :], lhsT=w_q_sb[:, k, blk * 128:(blk + 1) * 128], rhs=h2T[:, k, :], start=(k == 0), stop=(k == 7)))(qi, bl, blk, k),
                                     reads=["wD", "h2T"], writes=[qk])
                        P.op("act", (lambda qi, g4: lambda e: e.copy(out=qT_sb2[:, g4 * 4:(g4 + 1) * 4, :], in_=pq[qi][:]))(qi, g4), reads=[qk], writes=["qT_sb2"])
                        yield
                    for g4 in range(4):
                        si, sk = pqr.next()
                        for bl in range(4):
                            blk = g4 * 4 + bl
                            P.op("pe", (lambda si, bl, blk: lambda e: e.matmul(pq[si][:, bl, :], lhsT=qT_sb2[:, blk, :], rhs=skT[:, blk % 2, :], start=True, stop=True))(si, bl, blk),
                                 reads=["qT_sb2", "skT"], writes=[sk])
                        P.op("act", (lambda si, g4: lambda e: e.copy(out=s_sb[:, g4 * 4:(g4 + 1) * 4, :], in_=pq[si][:]))(si, g4), reads=[sk], writes=["s_sb"])
                        yield
                    TK = ["s_sb", "topk"]
                    BK = ["tk%d" % q for q in range(16)]
                    for blk in range(16):
                        P.op("dve", (lambda blk: lambda e: e.max(out=topv[:, blk, 0:8], in_=s_sb[:, blk, :]))(blk), reads=TK, writes=[BK[blk]])
                    yield
                    for blk in range(16):
                        P.op("dve", (lambda blk: lambda e: e.max_index(out=idxu[:, blk, 0:8], in_max=topv[:, blk, 0:8], in_values=s_sb[:, blk, :]))(blk), reads=["s_sb", BK[blk]], writes=[BK[blk]])
                        if blk % 8 == 7:
                            yield
                    for blk in range(16):
                        P.op("dve", (lambda blk: lambda e: e.match_replace(out=work[:, blk, :], in_to_replace=topv[:, blk, 0:8], in_values=s_sb[:, blk, :], imm_value=-1e30))(blk), reads=["s_sb", BK[blk]], writes=[BK[blk]])
                        if blk % 8 == 7:
                            yield
                    for blk in range(16):
                        P.op("dve", (lambda blk: lambda e: e.max(out=topv[:, blk, 8:16], in_=work[:, blk, :]))(blk), reads=[BK[blk]], writes=[BK[blk]])
                    yield
                    for blk in range(16):
                        P.op("dve", (lambda blk: lambda e: e.max_index(out=idxu[:, blk, 8:16], in_max=topv[:, blk, 8:16], in_values=work[:, blk, :]))(blk), reads=[BK[blk]], writes=[BK[blk]])
                        if blk % 8 == 7:
                            yield
                    TK = TK + BK
                    P.op("dve", lambda e: e.tensor_copy(out=idxf[:], in_=idxu[:]), reads=TK, writes=["topk"])
                    tv4 = topv[:].rearrange("p (h n) a -> p h n a", n=2); ix4 = idxf[:].rearrange("p (h n) a -> p h n a", n=2)
                    c4 = cand[:].rearrange("p h (a b) -> p h a b", a=16); ci4 = cidx.rearrange("p h (a b) -> p h a b", a=16)
                    P.op("dve", lambda e: e.tensor_tensor(out=c4, in0=tv4[:, :, 0, :].unsqueeze(3).to_broadcast([128, 8, 16, 16]),
                                                          in1=tv4[:, :, 1, :].unsqueeze(2).to_broadcast([128, 8, 16, 16]), op=ALU.add), reads=TK, writes=["topk"])
                    yield
                    candI = cand[:].bitcast(I32)
                    P.op("dve", lambda e: e.tensor_scalar(out=candI, in0=candI, scalar1=mskI[:, 0:1], scalar2=None, op0=ALU.bitwise_and), reads=TK + ["posI"], writes=["topk"])
                    P.op("dve", lambda e: e.tensor_tensor(out=candI, in0=candI, in1=posI[:].unsqueeze(1).to_broadcast([128, 8, 256]), op=ALU.bitwise_or), reads=TK + ["posI"], writes=["topk"])
                    P.op("dve", lambda e: e.tensor_single_scalar(out=ix4[:, :, 0, :], in_=ix4[:, :, 0, :], scalar=128.0, op=ALU.mult), reads=TK, writes=["topk"])
                    yield
                    w2 = work[:].rearrange("p (h n) k -> p h (n k)", n=2)
                    HK = ["hk%d" % q for q in range(8)]
                    for h in range(8):
                        P.op("dve", (lambda h: lambda e: e.max(out=best[:, h, 0:8], in_=cand[:, h, :]))(h), reads=TK, writes=[HK[h]])
                    yield
                    for h in range(8):
                        P.op("dve", (lambda h: lambda e: e.match_replace(out=w2[:, h, :], in_to_replace=best[:, h, 0:8], in_values=cand[:, h, :], imm_value=-1e30))(h), reads=TK + [HK[h]], writes=[HK[h]])
                    yield
                    for h in range(8):
                        P.op("dve", (lambda h: lambda e: e.max(out=best[:, h, 8:16], in_=w2[:, h, :]))(h), reads=[HK[h]], writes=[HK[h]])
                    yield
                    TK = TK + HK
                    P.op("dve", lambda e: e.tensor_single_scalar(out=sm[:, 8:16], in_=best[:, :, 0], scalar=-1.0, op=ALU.mult), reads=TK + ["smD"], writes=["smD"])
                    for h in range(8):
                        P.op("act", (lambda h: lambda e: e.activation(out=eg[:, h, :], in_=best[:, h, :], func=AF.Exp, bias=sm[:, 8 + h:9 + h], accum_out=sm[:, 16 + h:17 + h]))(h),
                             reads=TK + ["smD"], writes=["eg", "smD"])
                    P.op("dve", lambda e: e.reciprocal(out=sm[:, 24:32], in_=sm[:, 16:24]), reads=["smD"], writes=["smD"])
                    P.op("dve", lambda e: e.tensor_tensor(out=gate[b][:].rearrange("p (h k) -> p h k", h=8), in0=eg[:], in1=sm[:, 24:32].unsqueeze(2).to_broadcast([128, 8, 16]), op=ALU.mult),
                         reads=["eg", "smD"], writes=[gk])
                    yield
                    bestI = best[:].bitcast(I32)
                    P.op("dve", lambda e: e.tensor_scalar(out=pab[:, 0], in0=bestI, scalar1=c4I[:, 0:1], scalar2=None, op0=ALU.arith_shift_right), reads=TK + ["posI"], writes=["topk"])
                    P.op("dve", lambda e: e.tensor_scalar(out=pab[:, 0], in0=pab[:, 0], scalar1=c15I[:, 0:1], scalar2=None, op0=ALU.bitwise_and), reads=TK + ["posI"], writes=["topk"])
                    P.op("dve", lambda e: e.tensor_scalar(out=pab[:, 1], in0=bestI, scalar1=c15I[:, 0:1], scalar2=None, op0=ALU.bitwise_and), reads=TK + ["posI"], writes=["topk"])
                    P.op("dve", lambda e: e.tensor_copy(out=pabf[:], in_=pab[:]), reads=TK, writes=["topk"])
                    yield
                    e3 = eidf[:].rearrange("p (h k) -> p h k", h=8)
                    for n_ in range(2):
                        P.op("dve", (lambda n_: lambda e: e.tensor_tensor(out=oh[:], in0=pabf[:, n_].unsqueeze(3).to_broadcast([128, 8, 16, 16]),
                                                                          in1=iota16[:].unsqueeze(1).unsqueeze(1).to_broadcast([128, 8, 16, 16]), op=ALU.is_equal))(n_), reads=TK + ["posI"], writes=["topk"])
                        P.op("dve", (lambda n_: lambda e: e.tensor_tensor(out=oh[:], in0=oh[:], in1=ix4[:, :, n_, :].unsqueeze(2).to_broadcast([128, 8, 16, 16]), op=ALU.mult))(n_), reads=TK, writes=["topk"])
                        P.op("dve", (lambda n_: lambda e: e.tensor_reduce(out=(e3 if n_ == 0 else e3b[:]), in_=oh[:], axis=AX.X, op=ALU.add))(n_), reads=TK, writes=["topk"])
                        yield
                    P.op("dve", lambda e: e.tensor_tensor(out=e3, in0=e3, in1=e3b[:], op=ALU.add), reads=TK, writes=["topk"])
                    P.op("dve", lambda e: e.tensor_scalar(out=eidf[:], in0=eidf[:], scalar1=0.0, scalar2=16383.0, op0=ALU.max, op1=ALU.min), reads=TK, writes=["topk"])
                    P.op("dve", lambda e: e.tensor_copy(out=eid[b][:], in_=eidf[:]), reads=TK, writes=[ek])
                    yield

                def consume(i, fgen):
                    b = i % NB; t0 = i * 128
                    x1k, h2k, ek, gk = "x1_%d" % b, "h2_%d" % b, "eid%d" % b, "gate%d" % b
                    ngrp = 128 // GJ
                    pend = None

                    def finish_group(g, bufs):
                        j0 = g * GJ
                        if "nofin" in KD:
                            return
                        P.op("act", lambda e: e.activation(out=gl_[:, j0:j0 + GJ], in_=aD[:, j0:j0 + GJ], func=AF.Gelu_apprx_tanh), reads=["aD%d" % g], writes=["gl_%d" % g])
                        for jj in range(GJ):
                            P.op("act", (lambda jj: lambda e: e.mul(out=wD[:, j0 + jj:j0 + jj + 1], in_=gl_[:, j0 + jj:j0 + jj + 1], mul=gate[b][:, j0 + jj:j0 + jj + 1]))(jj),
                                 reads=["gl_%d" % g, gk], writes=["wDw%d" % g])
                        for jj in range(GJ):
                            j = j0 + jj; gi, ugk = bufs[jj]
                            di, dk = dgr.next()
                            P.op("act", (lambda di, j: lambda e: e.activation(out=dg[di][:], in_=identb[:], func=AF.Copy, scale=wD[:, j:j + 1]))(di, j), reads=["wDw%d" % g, "identb"], writes=[dk])
                            for hf in range(2):
                                if "nov" in KD and j not in (0, 127):
                                    continue
                                P.op("pe", (lambda di, gi, hf, j: lambda e: e.matmul(pacc[hf][:], lhsT=dg[di][:], rhs=UVg[gi][:, 1, hf * 512:(hf + 1) * 512], start=(j == 0), stop=(j == 127)))(di, gi, hf, j),
                                     reads=[dk, ugk], writes=["pacc%d" % hf])

                    for g in range(ngrp):
                        bufs = []
                        for jj in range(GJ):
                            j = g * GJ + jj
                            gi = uv_i[0] % NG; uv_i[0] += 1; ugk = "UVg%d" % gi
                            bufs.append((gi, ugk))
                            if "nog" in KD:
                                P.op("pool", (lambda gi: lambda e: e.memset(UVg[gi][:], 1.0))(gi), reads=[ek], writes=[ugk])
                            else:
                                P.dma("pool", ugk, (lambda gi, j: lambda e: e.indirect_dma_start(out=UVg[gi][:].rearrange("p a d -> p (a d)"), out_offset=None, in_=uvb_d,
                                                                                               in_offset=bass.IndirectOffsetOnAxis(ap=eid[b][:, j:j + 1], axis=0)))(gi, j), reads=[ek], writes=[ugk])
                        for jj in range(GJ):
                            j = g * GJ + jj; gi, ugk = bufs[jj]
                            if "noa" in KD:
                                continue
                            ji, jkey_ = jer.next()
                            jE = junkE3[ji]
                            P.op("dve", (lambda gi, j, jE: lambda e: e.scalar_tensor_tensor(out=jE[:], in0=UVg[gi][:, 0, :], scalar=1.0, in1=h2[b][:], op0=ALU.mult, op1=ALU.mult, accum_out=aD[:, j:j + 1]))(gi, j, jE),
                                 reads=[ugk, h2k], writes=["aD%d" % g, jkey_])
                        finish_group(g, bufs)
                        if fgen is not None:
                            for _ in range(FY):
                                next(fgen, None)
                    if fgen is not None:
                        for _ in fgen:
                            pass
                    if "dump23" in KD and i == 23:
                        for nm, src_t in (("d_x1", x1[b]), ("d_wD", wD), ("d_aD", aD), ("d_gate", gate[b]), ("d_h2", h2[b])):
                            dd = nc.dram_tensor(nm, list(src_t[:].shape), F32, kind="ExternalOutput").ap()
                            P.dma("sp", "dump", (lambda dd, src_t: lambda e: e.dma_start(out=dd, in_=src_t[:]))(dd, src_t), reads=[x1k, gk, h2k] + ["wDw%d" % q for q in range(32)] + ["aD%d" % q for q in range(32)], writes=["dumpo"])
                        dd2 = nc.dram_tensor("d_eid", [128, 128], I32, kind="ExternalOutput").ap()
                        P.dma("sp", "dump", lambda e: e.dma_start(out=dd2, in_=eid[b][:]), reads=[ek], writes=["dumpo"])
                    if "notail" in KD:
                        return
                    for hf in range(2):
                        P.op("dve", (lambda hf: lambda e: e.tensor_tensor(out=tmpD[:, hf * 512:(hf + 1) * 512], in0=pacc[hf][:], in1=gate2B[:, hf * 512:(hf + 1) * 512], op=ALU.mult))(hf),
                             reads=["pacc%d" % hf, "modB"], writes=["tmpD"])
                    P.op("dve", lambda e: e.tensor_tensor(out=x1[b][:], in0=tmpD[:], in1=x1[b][:], op=ALU.add), reads=["tmpD", x1k], writes=[x1k])
                    rstd_chain(x1[b][:], x1k, sm2[:, 0:4], "smE", junkD, "junkD")
                    P.op("dve", lambda e: e.scalar_tensor_tensor(out=tmpD[:], in0=x1[b][:], scalar=sm2[:, 3:4], in1=fgB[:], op0=ALU.mult, op1=ALU.mult), reads=[x1k, "smE", "fgB"], writes=["tmpD"])
                    P.dma("sp", "outst", lambda e: e.dma_start(out=out[t0:t0 + 128, :], in_=tmpD[:]), reads=["tmpD"], writes=["out"])

                for _ in front(0):
                    pass
                for i in range(NT):
                    if "noc" in KD:
                        break
                    if "noil" in KD:
                        consume(i, None)
                        if i + 1 < NT:
                            for _ in front(i + 1):
                                pass
                        continue
                    consume(i, front(i + 1) if i + 1 < NT else None)
        return finish(nc, P, out)
    return nc


def finish(nc, P, out):
    P.barrier()
    P.close()
    return nc


def core_inputs(inp, b):
    f = lambda a: np.ascontiguousarray(a, dtype=np.float32)
    return {
        "x": f(inp["x"][b]), "c": f(inp["c"][b:b + 1]), "ctx": f(inp["ctx"][b]), "c_ctx": f(inp["c_ctx"].reshape(1, D)),
        "w_mod": f(inp["w_mod"][0]), "b_mod": f(inp["b_mod"][0].reshape(1, -1)),
        "norm1_g": f(inp["norm1_g"][0].reshape(1, D)), "norm2_g": f(inp["norm2_g"][0].reshape(1, D)),
        "final_g": f(inp["final_g"].reshape(1, D)), "w_in": f(inp["w_in"][0]),
        "ssm_a_re": f(inp["ssm_a_re"][0]), "ssm_a_im": f(inp["ssm_a_im"][0]), "ssm_log_dt": f(inp["ssm_log_dt"][0]),
        "ssm_b_re": f(inp["ssm_b_re"][0]), "ssm_b_im": f(inp["ssm_b_im"][0]),
        "ssm_c_re": f(inp["ssm_c_re"][0]), "ssm_c_im": f(inp["ssm_c_im"][0]),
        "ssm_d": f(inp["ssm_d"][0].reshape(512, 1)), "w_glu": f(inp["w_glu"][0]), "b_glu": f(inp["b_glu"][0].reshape(512, 1)),
        "w_branch_a": f(inp["w_branch_a"][0]), "w_branch_b": f(inp["w_branch_b"][0]), "na_rpb": f(inp["na_rpb"][0]),
        "w_out": f(inp["w_out"][0]), "peer_w_q": f(inp["peer_w_q"][0]), "peer_subkeys": f(inp["peer_subkeys"][0]),
        "peer_uv": f(np.concatenate([inp["peer_u"][0], inp["peer_v"][0]], axis=1)),
    }


def kernel(**inputs):
    nc = build()
    in_maps = [core_inputs(inputs, b) for b in range(8)]
    res = run_bass_kernel_spmd(nc, in_maps, core_ids=list(range(8)))
    return np.stack([np.asarray(r["out"], dtype=np.float32) for r in res.results], axis=0)
```

```python
import math
import numpy as np
import concourse.bass as bass
import concourse.mybir as mybir
from concourse.bass_utils import run_bass_kernel_spmd

F32 = mybir.dt.float32
BF16 = mybir.dt.bfloat16
I32 = mybir.dt.int32
U32 = mybir.dt.uint32
AF = mybir.ActivationFunctionType
ALU = mybir.AluOpType
AX = mybir.AxisListType


class _Op:
    __slots__ = ("eng", "fn", "deps", "seq", "is_dma", "semkey", "signal", "count", "waits")

    def __init__(self, eng, fn, seq, is_dma=False, semkey=None):
        self.eng = eng
        self.fn = fn
        self.deps = []
        self.seq = seq
        self.is_dma = is_dma
        self.semkey = semkey
        self.signal = is_dma
        self.count = 0
        self.waits = []


class Prog:
    ENGS = ("pe", "dve", "act", "pool", "sp")

    def __init__(self, nc):
        self.nc = nc
        self.ops = []
        self.writer = {}
        self.readers = {}
        import os
        self.same_sync = os.environ.get("KSAME", "1") == "1"

    def _add(self, op, reads, writes):
        deps = []
        for r in reads:
            w = self.writer.get(r)
            if w is not None:
                deps.append(w)
        for w_ in writes:
            w = self.writer.get(w_)
            if w is not None:
                deps.append(w)
            deps.extend(self.readers.get(w_, ()))
        op.deps = [d for d in set(deps) if d is not op]
        for r in reads:
            self.readers.setdefault(r, []).append(op)
        for w_ in writes:
            self.writer[w_] = op
            self.readers[w_] = []
        self.ops.append(op)
        return op

    def op(self, eng, fn, reads=(), writes=()):
        return self._add(_Op(eng, fn, len(self.ops)), reads, writes)

    def dma(self, eng, semkey, fn, reads=(), writes=()):
        return self._add(_Op(eng, fn, len(self.ops), True, semkey), reads, writes)

    def emit(self):
        import bisect
        from contextlib import ExitStack
        nc = self.nc
        if not hasattr(self, "_st"):
            self._st = ExitStack(); self._sem = {}; self._cnt = {}; self._hist = {}
            self._seen = {e: {} for e in self.ENGS}; self._done = 0
        ops = self.ops[self._done:]
        self._done = len(self.ops)
        if not ops:
            return

        def same(d, o):
            return d.eng == o.eng and not o.is_dma and (d.eng == "pe" or not self.same_sync)
        for o in ops:
            for d in o.deps:
                if d.is_dma or same(d, o):
                    continue
                assert d.count == 0 or d.signal, "dependency on an already-emitted non-signalling op"
                d.signal = True
        for o in ops:
            if not o.signal:
                continue
            k = ("dma", o.semkey) if o.is_dma else ("eng", o.eng)
            if k not in self._sem:
                self._sem[k] = self._st.enter_context(nc.semaphore("s%d_%s" % (len(self._sem), str(k[1]).replace(" ", ""))))
                self._cnt[k] = 0
            self._cnt[k] += 1
            o.count = self._cnt[k]
            if o.is_dma:
                self._hist.setdefault(o.semkey, []).append(o.seq)
        for o in ops:
            need = {}
            for d in o.deps:
                if d.is_dma:
                    k = ("dma", d.semkey)
                    v = 16 * bisect.bisect_left(self._hist[d.semkey], o.seq)
                else:
                    if same(d, o):
                        continue
                    k = ("eng", d.eng)
                    v = d.count
                if need.get(k, 0) < v:
                    need[k] = v
            sn = self._seen[o.eng]
            o.waits = []
            for k, v in need.items():
                if sn.get(k, 0) < v:
                    sn[k] = v
                    o.waits.append((k, v))
        self.n_sems = len(self._sem)
        sem = self._sem
        with nc.Block() as block:
            per = {e: [o for o in ops if o.eng == e] for e in self.ENGS}

            def run(engobj, lst):
                for o in lst:
                    for k, v in o.waits:
                        engobj.wait_ge(sem[k], v)
                    ins = o.fn(engobj)
                    if o.signal:
                        k = ("dma", o.semkey) if o.is_dma else ("eng", o.eng)
                        ins.then_inc(sem[k], 16 if o.is_dma else 1)
                    o.fn = None

            @block.tensor
            def _(e):
                run(e, per["pe"])

            @block.vector
            def _(e):
                run(e, per["dve"])

            @block.scalar
            def _(e):
                run(e, per["act"])

            @block.gpsimd
            def _(e):
                run(e, per["pool"])

            @block.sync
            def _(e):
                run(e, per["sp"])

    def close(self):
        self.emit()
        if hasattr(self, "_st"):
            self._st.close()

    def barrier(self, flush=True):
        start = getattr(self, "_done", 0)
        last = {}
        dmas = {}
        for o in self.ops[start:]:
            if o.is_dma:
                dmas[o.semkey] = o
            else:
                last[o.eng] = o
        for e, o in getattr(self, "_bar", {}).items():
            last.setdefault(e, o)
        deps = list(last.values()) + list(dmas.values())
        self._bar = {}
        for e in self.ENGS:
            o = _Op(e, lambda eng: eng.nop(), len(self.ops))
            o.deps = [d for d in deps]
            o.signal = True
            self.ops.append(o)
            self._bar[e] = o
        self.writer = {}
        self.readers = {}
        if flush:
            self.emit()


class Rot:
    def __init__(self, name, n):
        self.name, self.n, self.i = name, n, -1

    def next(self):
        self.i = (self.i + 1) % self.n
        return self.i, "%s%d" % (self.name, self.i)


D = 1024
SEQ = 4096
CTX = 256
NTOK = SEQ + CTX
EPS = 1e-6


def build(stage=99, debug=False):
    import os
    KD = os.environ.get("KDBG", "")
    from contextlib import ExitStack
    nc = bass.Bass("TRN2", target_bir_lowering=False)
    P = Prog(nc)

    def din(name, shape, dt=F32):
        return nc.dram_tensor(name, shape, dt, kind="ExternalInput").ap()

    def dscr(name, shape, dt):
        return nc.dram_tensor(name, shape, dt, kind=("ExternalOutput" if debug else "Internal")).ap()

    x = din("x", [SEQ, D]); c = din("c", [1, D]); ctx = din("ctx", [CTX, D]); c_ctx = din("c_ctx", [1, D])
    w_mod = din("w_mod", [D, 6 * D]); b_mod = din("b_mod", [1, 6 * D])
    norm1_g = din("norm1_g", [1, D]); norm2_g = din("norm2_g", [1, D]); final_g = din("final_g", [1, D])
    w_in = din("w_in", [D, 4096])
    a_re = din("ssm_a_re", [2, 32, 64]); a_im = din("ssm_a_im", [2, 32, 64]); log_dt = din("ssm_log_dt", [2, 32])
    b_re = din("ssm_b_re", [2, 32, 64, 16]); b_im = din("ssm_b_im", [2, 32, 64, 16])
    c_re = din("ssm_c_re", [2, 32, 16, 64]); c_im = din("ssm_c_im", [2, 32, 16, 64])
    ssm_d = din("ssm_d", [512, 1]); w_glu = din("w_glu", [512, 512]); b_glu = din("b_glu", [512, 1])
    w_ba = din("w_branch_a", [512, D]); w_bb = din("w_branch_b", [512, D]); rpb = din("na_rpb", [8, 15, 31])
    w_out = din("w_out", [D, D]); w_q = din("peer_w_q", [D, 2048]); subkeys = din("peer_subkeys", [2, 128, 128])
    peer_uv = din("peer_uv", [16384, 2 * D])
    out = nc.dram_tensor("out", [SEQ, D], F32, kind="ExternalOutput").ap()

    uT_d = dscr("uT_d", [512, NTOK], F32)
    kT_d = dscr("kT_d", [512, NTOK], BF16)
    qT_d = dscr("qT_d", [512, SEQ], BF16)
    v_d = dscr("v_d", [NTOK, 512], BF16)
    gT_d = dscr("gT_d", [2048, SEQ], BF16)
    baT_d = dscr("baT_d", [D, SEQ], BF16)
    mgT_d = dscr("mgT_d", [D, SEQ], BF16)
    uvb_d = nc.dram_tensor("uvb_d", [16384, 2 * D], BF16, kind=("ExternalOutput" if (debug and stage == 3.5) else "Internal")).ap()

    from contextlib import contextmanager

    @contextmanager
    def phase():
        stk = ExitStack()
        try:
            yield stk
            P.barrier()
        finally:
            stk.close()

    top = ExitStack()
    with top:
        def sbuf(st, n, s, d=F32):
            return st.enter_context(nc.sbuf_tensor(n, s, d))

        def psum(st, n, s, d=F32):
            return st.enter_context(nc.psum_tensor(n, s, d))

        identf = sbuf(top, "identf", [128, 128])
        identb = sbuf(top, "identb", [128, 128], BF16)
        modB = sbuf(top, "modB", [128, 6 * D])
        P.op("pool", lambda e: e.iota(identf[:], pattern=[[1, 128]], base=0, channel_multiplier=-1,
                                      allow_small_or_imprecise_dtypes=True), writes=["identf"])
        P.op("dve", lambda e: e.tensor_single_scalar(out=identf[:], in_=identf[:], scalar=0.0, op=ALU.is_equal),
             reads=["identf"], writes=["identf"])
        P.op("dve", lambda e: e.tensor_copy(out=identb[:], in_=identf[:]), reads=["identf"], writes=["identb"])

        with phase() as st:
            modcB = sbuf(st, "modcB", [128, 2 * D])
            wm = [sbuf(st, "wm%d" % i, [128, 8, 512]) for i in range(2)]
            w_in_sb = sbuf(st, "w_in_sb", [128, 8, 4096], BF16)
            st0 = ExitStack()
            cc = sbuf(st0, "cc", [128, 2, 8]); sc = sbuf(st0, "sc", [128, 2, 8]); scB = sbuf(st0, "scB", [128, 2, 8, 128])
            bmB = sbuf(st0, "bmB", [128, 6 * D]); gB = sbuf(st0, "gB", [128, 2, D])
            pmod = [psum(st0, "pmod%d" % i, [128, 512]) for i in range(2)]

            P.dma("sp", "c0", lambda e: e.dma_start(out=cc[:, 0, :], in_=c.rearrange("o (k p) -> p (o k)", p=128),
                                                    allow_slow_non_contiguous=True), writes=["cc"])
            P.dma("sp", "c0", lambda e: e.dma_start(out=cc[:, 1, :], in_=c_ctx.rearrange("o (k p) -> p (o k)", p=128),
                                                    allow_slow_non_contiguous=True), writes=["cc"])
            P.dma("act", "c1", lambda e: e.dma_start(out=bmB[:], in_=b_mod.to_broadcast([128, 6 * D])), writes=["bmB"])
            P.dma("act", "c1", lambda e: e.dma_start(out=gB[:, 0, :], in_=norm1_g.to_broadcast([128, D])), writes=["gB"])
            P.dma("act", "c1", lambda e: e.dma_start(out=gB[:, 1, :], in_=norm2_g.to_broadcast([128, D])), writes=["gB"])
            P.op("act", lambda e: e.activation(out=sc[:], in_=cc[:], func=AF.Silu), reads=["cc"], writes=["sc"])
            P.op("dve", lambda e: e.tensor_copy(out=scB[:], in_=sc[:].unsqueeze(3).to_broadcast([128, 2, 8, 128])),
                 reads=["sc"], writes=["scB"])
            w_mod_v = w_mod.rearrange("(k p) n -> p k n", p=128)
            for cch in range(12):
                bi = cch % 2
                P.dma("sp", "wm%d" % bi, (lambda bi, cch: lambda e: e.dma_start(out=wm[bi][:], in_=w_mod_v[:, :, cch * 512:(cch + 1) * 512]))(bi, cch),
                      writes=["wm%d" % bi])
                for which in range(2 if cch < 4 else 1):
                    for k in range(8):
                        P.op("pe", (lambda bi, which, k: lambda e: e.matmul(pmod[which][:], lhsT=scB[:, which, k, :], rhs=wm[bi][:, k, :],
                                                                            start=(k == 0), stop=(k == 7)))(bi, which, k),
                             reads=["scB", "wm%d" % bi], writes=["pmod%d" % which])
                    dst = modB if which == 0 else modcB
                    P.op("dve", (lambda dst, which, cch: lambda e: e.tensor_tensor(out=dst[:, cch * 512:(cch + 1) * 512], in0=pmod[which][:],
                                                                                   in1=bmB[:, cch * 512:(cch + 1) * 512], op=ALU.add))(dst, which, cch),
                         reads=["pmod%d" % which, "bmB"], writes=["modB" if which == 0 else "modcB"])
            for dst, key, off, gi in ((modB, "modB", D, 0), (modcB, "modcB", D, 0), (modB, "modB", 4 * D, 1)):
                P.op("dve", (lambda dst, off, gi: lambda e: e.scalar_tensor_tensor(out=dst[:, off:off + D], in0=dst[:, off:off + D], scalar=1.0,
                                                                                  in1=gB[:, gi, :], op0=ALU.add, op1=ALU.mult))(dst, off, gi),
                     reads=[key, "gB"], writes=[key])

            P.barrier()
            st0.close()
            w_in_v = w_in.rearrange("(k p) n -> p k n", p=128)
            for cch in range(8):
                bi = cch % 2
                P.dma("sp", "wm%d" % bi, (lambda bi, cch: lambda e: e.dma_start(out=wm[bi][:], in_=w_in_v[:, :, cch * 512:(cch + 1) * 512]))(bi, cch),
                      reads=[], writes=["wm%d" % bi])
                eng = ("pool", "dve")[cch % 2]
                P.op(eng, (lambda bi, cch: lambda e: e.tensor_copy(out=w_in_sb[:, :, cch * 512:(cch + 1) * 512], in_=wm[bi][:]))(bi, cch),
                     reads=["wm%d" % bi], writes=["w_in_sb"])

            xt = [sbuf(st, "xt%d" % i, [128, D]) for i in range(3)]; xr = Rot("xt", 3)
            junk = sbuf(st, "junkA", [128, D]); tmpA = sbuf(st, "tmpA", [128, D])
            ss = [sbuf(st, "ss%d" % i, [128, 4]) for i in range(2)]; ssr = Rot("ss", 2)
            hxb = [sbuf(st, "hxb%d" % i, [128, D], BF16) for i in range(2)]; hr = Rot("hxb", 2)
            hxT = [sbuf(st, "hxT%d" % i, [128, 8, 512], BF16) for i in range(2)]; hTr = Rot("hxT", 2)
            st_u = sbuf(st, "st_u", [128, 4, 512]); st_k = sbuf(st, "st_k", [128, 4, 512], BF16)
            st_q = sbuf(st, "st_q", [128, 4, 512], BF16); st_g = sbuf(st, "st_g", [128, 16, 512], BF16)
            st_v = sbuf(st, "st_v", [128, 4, 512], BF16)
            tp = [psum(st, "tpA%d" % i, [128, 8, 128], BF16) for i in range(2)]; tpr = Rot("tpA", 2)
            pj = [psum(st, "pj%d" % i, [128, 512]) for i in range(4)]; pjr = Rot("pj", 4)
            evac_i = [0]

            def evac(dst_ap, src_ap, reads, writes, func=None):
                if func is not None:
                    P.op("act", lambda e: e.activation(out=dst_ap, in_=src_ap, func=func), reads, writes)
                    return
                evac_i[0] += 1
                if evac_i[0] % 2:
                    P.op("act", lambda e: e.copy(out=dst_ap, in_=src_ap), reads, writes)
                else:
                    P.op("dve", lambda e: e.tensor_copy(out=dst_ap, in_=src_ap), reads, writes)

            for i_ in range(2):
                P.op("pool", (lambda i_: lambda e: e.memset(hxT[i_][:], 0.0))(i_), writes=["hxT%d" % i_])
            chunks = [("ctx", 0, 256)] + [("lat", i * 512, 512) for i in range(8)]
            for kind, t0, n in chunks:
                src = ctx if kind == "ctx" else x
                mB, mkey = (modcB, "modcB") if kind == "ctx" else (modB, "modB")
                col0 = t0 if kind == "ctx" else CTX + t0
                hi, hkey = hTr.next()
                for t in range(n // 128):
                    xi, xkey = xr.next()
                    si, skey = ssr.next()
                    bi, bkey = hr.next()
                    pi, pkey = tpr.next()
                    r0 = t0 + t * 128
                    P.dma("sp", xkey, (lambda xi, r0, src: lambda e: e.dma_start(out=xt[xi][:], in_=src[r0:r0 + 128, :]))(xi, r0, src), writes=[xkey])
                    P.op("act", (lambda xi, si: lambda e: e.activation(out=junk[:], in_=xt[xi][:], func=AF.Square, accum_out=ss[si][:, 0:1]))(xi, si),
                         reads=[xkey], writes=["junkA", skey])
                    P.op("dve", (lambda si: lambda e: e.tensor_scalar(out=ss[si][:, 1:2], in0=ss[si][:, 0:1], scalar1=1.0 / D, scalar2=EPS,
                                                                      op0=ALU.mult, op1=ALU.add))(si), reads=[skey], writes=[skey])
                    P.op("act", (lambda si: lambda e: e.sqrt(out=ss[si][:, 2:3], in_=ss[si][:, 1:2]))(si), reads=[skey], writes=[skey])
                    P.op("dve", (lambda si: lambda e: e.reciprocal(out=ss[si][:, 3:4], in_=ss[si][:, 2:3]))(si), reads=[skey], writes=[skey])
                    P.op("dve", (lambda xi, si, mB: lambda e: e.scalar_tensor_tensor(out=tmpA[:], in0=xt[xi][:], scalar=ss[si][:, 3:4], in1=mB[:, D:2 * D],
                                                                                     op0=ALU.mult, op1=ALU.mult))(xi, si, mB),
                         reads=[xkey, skey, mkey], writes=["tmpA"])
                    P.op("dve", (lambda bi, mB: lambda e: e.tensor_tensor(out=hxb[bi][:], in0=tmpA[:], in1=mB[:, 0:D], op=ALU.add))(bi, mB),
                         reads=["tmpA", mkey], writes=[bkey])
                    for k in range(8):
                        P.op("pe", (lambda pi, bi, k: lambda e: e.transpose(tp[pi][:, k, :], hxb[bi][:, k * 128:(k + 1) * 128], identb[:]))(pi, bi, k),
                             reads=[bkey, "identb"], writes=[pkey])
                    P.op("act", (lambda hi, pi, t: lambda e: e.copy(out=hxT[hi][:, :, t * 128:(t + 1) * 128], in_=tp[pi][:]))(hi, pi, t),
                         reads=[pkey], writes=[hkey])
                cts = list(range(0, 8)) + (list(range(12, 32)) if kind == "lat" else [])
                for ct in cts:
                    qi, qkey = pjr.next()
                    for k in range(8):
                        P.op("pe", (lambda qi, hi, k, ct: lambda e: e.matmul(pj[qi][:, 0:n], lhsT=w_in_sb[:, k, ct * 128:(ct + 1) * 128], rhs=hxT[hi][:, k, 0:n],
                                                                             start=(k == 0), stop=(k == 7)))(qi, hi, k, ct),
                             reads=["w_in_sb", hkey], writes=[qkey])
                    if ct < 4:
                        evac(st_u[:, ct, 0:n], pj[qi][:, 0:n], [qkey], ["st_u"])
                    elif ct < 8:
                        evac(st_k[:, ct - 4, 0:n], pj[qi][:, 0:n], [qkey], ["st_k"])
                    elif ct < 16:
                        evac(st_q[:, ct - 12, 0:n], pj[qi][:, 0:n], [qkey], ["st_q"])
                    else:
                        evac(st_g[:, ct - 16, 0:n], pj[qi][:, 0:n], [qkey], ["st_g"], func=AF.Sigmoid)
                P.dma("sp", "stu", (lambda col0, n: lambda e: e.dma_start(out=uT_d.rearrange("(t p) n -> p t n", p=128)[:, :, col0:col0 + n], in_=st_u[:, :, 0:n]))(col0, n),
                      reads=["st_u"], writes=["uT_d"])
                P.dma("sp", "stk", (lambda col0, n: lambda e: e.dma_start(out=kT_d.rearrange("(t p) n -> p t n", p=128)[:, :, col0:col0 + n], in_=st_k[:, :, 0:n]))(col0, n),
                      reads=["st_k"], writes=["kT_d"])
                if kind == "lat":
                    P.dma("sp", "stq", (lambda t0: lambda e: e.dma_start(out=qT_d.rearrange("(t p) n -> p t n", p=128)[:, :, t0:t0 + 512], in_=st_q[:]))(t0),
                          reads=["st_q"], writes=["qT_d"])
                    P.dma("sp", "stg", (lambda t0: lambda e: e.dma_start(out=gT_d.rearrange("(t p) n -> p t n", p=128)[:, :, t0:t0 + 512], in_=st_g[:]))(t0),
                          reads=["st_g"], writes=["gT_d"])
                for t in range(n // 128):
                    qi, qkey = pjr.next()
                    for k in range(8):
                        P.op("pe", (lambda qi, hi, k, t: lambda e: e.matmul(pj[qi][:], lhsT=hxT[hi][:, k, t * 128:(t + 1) * 128], rhs=w_in_sb[:, k, 1024:1536],
                                                                            start=(k == 0), stop=(k == 7)))(qi, hi, k, t),
                             reads=["w_in_sb", hkey], writes=[qkey])
                    evac(st_v[:, t, :], pj[qi][:], [qkey], ["st_v"])
                nt = n // 128
                P.dma("sp", "stv", (lambda col0, nt: lambda e: e.dma_start(out=v_d[col0:col0 + nt * 128, :].rearrange("(t p) n -> p t n", p=128), in_=st_v[:, 0:nt, :]))(col0, nt),
                      reads=["st_v"], writes=["v_d"])
        P.barrier()
        if stage <= 1:
            return finish(nc, P, out)

        yT_d = dscr("yT_d", [512, SEQ], F32) if debug else None
        TWO_PI = 2.0 * math.pi
        with ExitStack() as stB:
            zT = sbuf(stB, "zT", [128, 4, SEQ], BF16)
            with phase() as st:
                def t32(n):
                    return sbuf(st, n, [128, 32])
                are, aim, ldt = t32("are"), t32("aim"), t32("ldt")
                Bn = [sbuf(st, "Bn%d" % i, [128, 32, 16]) for i in range(2)]
                bb = [sbuf(st, "bb%d" % i, [128, 32, 16]) for i in range(2)]
                tmpb = sbuf(st, "tmpb", [128, 32, 16])
                Cn2 = [sbuf(st, "Cn2%d" % i, [128, 8, 2, 64]) for i in range(2)]
                dsk = sbuf(st, "dsk", [128, 4])
                maskf = sbuf(st, "maskf", [128, 4, 2]); mask2 = sbuf(st, "mask2", [128, 4, 2])
                pwr = sbuf(st, "pwr", [128, 13, 32]); pwi = sbuf(st, "pwi", [128, 13, 32]); npwi = sbuf(st, "npwi", [128, 13, 32])
                kint = sbuf(st, "kint", [128, 32], I32)
                names = ["dt", "er", "th", "mag", "kf", "rr", "half", "sn", "ah", "cq", "sinr", "cosr", "nre", "den", "rden",
                         "fre", "fim", "t1", "t2"]
                T = {n: t32("p_" + n) for n in names}
                uT_sb = [sbuf(st, "uT_sb%d" % i, [128, NTOK]) for i in range(1)]
                PL = [sbuf(st, "PL%d" % i, [128, 2, NTOK]) for i in range(2)]
                yT = sbuf(st, "yT", [128, SEQ])
                Z = [sbuf(st, "Z%d" % i, [128, 2, 128]) for i in range(2)]
                Zc = [sbuf(st, "Zc%d" % i, [128, 2, 128]) for i in range(2)]
                LB = [sbuf(st, "LB%d" % i, [128, 2, 128]) for i in range(2)]
                LC = [sbuf(st, "LC%d" % i, [128, 2, 128]) for i in range(2)]
                pz = [psum(st, "pz%d" % i, [128, 2, 128]) for i in range(2)]; pzr = Rot("pz", 2)
                pb = [psum(st, "pb%d" % i, [128, 512]) for i in range(3)]; pbr = Rot("pb", 3)
                py = [psum(st, "py%d" % i, [128, 512]) for i in range(2)]; pyr = Rot("py", 2)

                for gl in range(2):
                    sl = slice(gl * 64, (gl + 1) * 64)
                    for dst, srcp, key in ((are, a_re, "are"), (aim, a_im, "aim")):
                        P.dma("act", "pb0", (lambda dst, srcp, sl, gl: lambda e: e.dma_start(
                            out=dst[sl, :].rearrange("p (d g) -> p d g", d=2),
                            in_=srcp.rearrange("d (gp gl) p -> gl p d gp", gl=2)[gl], allow_slow_non_contiguous=True))(dst, srcp, sl, gl), writes=[key])
                    P.dma("act", "pb0", (lambda sl, gl: lambda e: e.dma_start(
                        out=ldt[sl, :].rearrange("p (d g) -> p d g", d=2),
                        in_=log_dt.rearrange("d (gp gl) -> gl d gp", gl=2)[gl:gl + 1].to_broadcast([64, 2, 16]), allow_slow_non_contiguous=True))(sl, gl), writes=["ldt"])
                    for i, srcp in enumerate((b_re, b_im)):
                        P.dma("act", "pb0", (lambda i, srcp, sl, gl: lambda e: e.dma_start(
                            out=Bn[i][sl].rearrange("p (d g) h -> p d g h", d=2),
                            in_=srcp.rearrange("d (gp gl) p h -> gl p d gp h", gl=2)[gl]))(i, srcp, sl, gl), writes=["Bn%d" % i])
                for i, srcp in enumerate((c_re, c_im)):
                    for j in range(2):
                        P.dma("act", "pb0", (lambda i, srcp, j: lambda e: e.dma_start(
                            out=Cn2[i][:, :, j, :].rearrange("p (d u) q -> p d u q", d=2),
                            in_=srcp.rearrange("d (ut g8) h p -> (g8 h) d ut p", g8=8)))(i, srcp, j), writes=["Cn2%d" % i])
                P.dma("act", "pb0", lambda e: e.dma_start(out=dsk[:], in_=ssm_d.rearrange("(ut p) o -> p (ut o)", p=128), allow_slow_non_contiguous=True), writes=["dsk"])
                P.op("pool", lambda e: e.iota(maskf[:], pattern=[[-32, 4], [-16, 2]], base=0, channel_multiplier=1, allow_small_or_imprecise_dtypes=True), writes=["maskf"])
                P.op("dve", lambda e: e.tensor_single_scalar(out=mask2[:], in_=maskf[:], scalar=0.0, op=ALU.is_ge), reads=["maskf"], writes=["mask2"])
                P.op("dve", lambda e: e.tensor_single_scalar(out=maskf[:], in_=maskf[:], scalar=16.0, op=ALU.is_lt), reads=["maskf", "mask2"], writes=["maskf"])
                P.op("dve", lambda e: e.tensor_tensor(out=maskf[:], in0=maskf[:], in1=mask2[:], op=ALU.mult), reads=["maskf", "mask2"], writes=["maskf"])

                PK = ["are", "aim", "ldt", "Bn0", "Bn1", "prm"]

                def dve(fn):
                    P.op("dve", fn, reads=PK, writes=["prm"])

                def act(fn):
                    P.op("act", fn, reads=PK, writes=["prm"])
                act(lambda e: e.activation(out=T["dt"][:], in_=ldt[:], func=AF.Exp))
                dve(lambda e: e.tensor_tensor(out=T["er"][:], in0=are[:], in1=T["dt"][:], op=ALU.mult))
                dve(lambda e: e.tensor_tensor(out=T["th"][:], in0=aim[:], in1=T["dt"][:], op=ALU.mult))
                act(lambda e: e.activation(out=T["mag"][:], in_=T["er"][:], func=AF.Exp))
                dve(lambda e: e.tensor_single_scalar(out=T["kf"][:], in_=T["th"][:], scalar=1.0 / TWO_PI, op=ALU.mult))
                dve(lambda e: e.tensor_copy(out=kint[:], in_=T["kf"][:]))
                dve(lambda e: e.tensor_copy(out=T["kf"][:], in_=kint[:]))
                dve(lambda e: e.scalar_tensor_tensor(out=T["rr"][:], in0=T["kf"][:], scalar=-TWO_PI, in1=T["th"][:], op0=ALU.mult, op1=ALU.add))
                dve(lambda e: e.tensor_single_scalar(out=T["half"][:], in_=T["rr"][:], scalar=0.5, op=ALU.mult))
                act(lambda e: e.activation(out=T["ah"][:], in_=T["half"][:], func=AF.Abs))
                dve(lambda e: e.tensor_scalar(out=T["t1"][:], in0=T["ah"][:], scalar1=-1.0, scalar2=math.pi / 2, op0=ALU.mult, op1=ALU.add))
                act(lambda e: e.activation(out=T["sn"][:], in_=T["half"][:], func=AF.Sin))
                act(lambda e: e.activation(out=T["cq"][:], in_=T["t1"][:], func=AF.Sin))
                dve(lambda e: e.scalar_tensor_tensor(out=T["sinr"][:], in0=T["sn"][:], scalar=2.0, in1=T["cq"][:], op0=ALU.mult, op1=ALU.mult))
                dve(lambda e: e.scalar_tensor_tensor(out=T["t2"][:], in0=T["sn"][:], scalar=-2.0, in1=T["sn"][:], op0=ALU.mult, op1=ALU.mult))
                dve(lambda e: e.tensor_single_scalar(out=T["cosr"][:], in_=T["t2"][:], scalar=1.0, op=ALU.add))
                dve(lambda e: e.tensor_tensor(out=pwr[:, 0, :], in0=T["mag"][:], in1=T["cosr"][:], op=ALU.mult))
                dve(lambda e: e.tensor_tensor(out=pwi[:, 0, :], in0=T["mag"][:], in1=T["sinr"][:], op=ALU.mult))
                dve(lambda e: e.tensor_single_scalar(out=T["nre"][:], in_=pwr[:, 0, :], scalar=-1.0, op=ALU.add))
                dve(lambda e: e.tensor_tensor(out=T["den"][:], in0=are[:], in1=are[:], op=ALU.mult))
                dve(lambda e: e.tensor_tensor(out=T["t1"][:], in0=aim[:], in1=aim[:], op=ALU.mult))
                dve(lambda e: e.tensor_tensor(out=T["den"][:], in0=T["den"][:], in1=T["t1"][:], op=ALU.add))
                dve(lambda e: e.reciprocal(out=T["rden"][:], in_=T["den"][:]))
                dve(lambda e: e.tensor_tensor(out=T["t1"][:], in0=T["nre"][:], in1=are[:], op=ALU.mult))
                dve(lambda e: e.tensor_tensor(out=T["t2"][:], in0=pwi[:, 0, :], in1=aim[:], op=ALU.mult))
                dve(lambda e: e.tensor_tensor(out=T["t1"][:], in0=T["t1"][:], in1=T["t2"][:], op=ALU.add))
                dve(lambda e: e.tensor_tensor(out=T["fre"][:], in0=T["t1"][:], in1=T["rden"][:], op=ALU.mult))
                dve(lambda e: e.tensor_tensor(out=T["t1"][:], in0=pwi[:, 0, :], in1=are[:], op=ALU.mult))
                dve(lambda e: e.tensor_tensor(out=T["t2"][:], in0=T["nre"][:], in1=aim[:], op=ALU.mult))
                dve(lambda e: e.tensor_tensor(out=T["t1"][:], in0=T["t1"][:], in1=T["t2"][:], op=ALU.subtract))
                dve(lambda e: e.tensor_tensor(out=T["fim"][:], in0=T["t1"][:], in1=T["rden"][:], op=ALU.mult))
                fr = T["fre"][:].unsqueeze(2).to_broadcast([128, 32, 16]); fi = T["fim"][:].unsqueeze(2).to_broadcast([128, 32, 16])
                dve(lambda e: e.tensor_tensor(out=bb[0][:], in0=Bn[0][:], in1=fr, op=ALU.mult))
                dve(lambda e: e.tensor_tensor(out=tmpb[:], in0=Bn[1][:], in1=fi, op=ALU.mult))
                dve(lambda e: e.tensor_tensor(out=bb[0][:], in0=bb[0][:], in1=tmpb[:], op=ALU.subtract))
                dve(lambda e: e.tensor_tensor(out=bb[1][:], in0=Bn[1][:], in1=fr, op=ALU.mult))
                dve(lambda e: e.tensor_tensor(out=tmpb[:], in0=Bn[0][:], in1=fi, op=ALU.mult))
                dve(lambda e: e.tensor_tensor(out=bb[1][:], in0=bb[1][:], in1=tmpb[:], op=ALU.add))
                for k in range(12):
                    dve((lambda k: lambda e: e.tensor_tensor(out=T["t1"][:], in0=pwr[:, k, :], in1=pwr[:, k, :], op=ALU.mult))(k))
                    dve((lambda k: lambda e: e.tensor_tensor(out=T["t2"][:], in0=pwi[:, k, :], in1=pwi[:, k, :], op=ALU.mult))(k))
                    dve((lambda k: lambda e: e.tensor_tensor(out=pwr[:, k + 1, :], in0=T["t1"][:], in1=T["t2"][:], op=ALU.subtract))(k))
                    dve((lambda k: lambda e: e.scalar_tensor_tensor(out=pwi[:, k + 1, :], in0=pwr[:, k, :], scalar=2.0, in1=pwi[:, k, :], op0=ALU.mult, op1=ALU.mult))(k))
                dve(lambda e: e.tensor_single_scalar(out=npwi[:], in_=pwi[:], scalar=-1.0, op=ALU.mult))

                chain = {}

                def cmul_acc(hi_re, hi_im, lo_re, lo_im, k, u, key):
                    sr = pwr[:, k, u:u + 1]; si = pwi[:, k, u:u + 1]; nsi = npwi[:, k, u:u + 1]
                    prev = chain.get(key)
                    if prev is None:
                        prev = [w for w in (P.writer.get(key), P.writer.get("prm")) if w is not None]
                    ops_ = []
                    for n_, (o_, a_, s_) in enumerate(((hi_re, lo_re, sr), (hi_im, lo_re, si), (hi_re, lo_im, nsi), (hi_im, lo_im, sr))):
                        op = _Op("dve", (lambda o_, a_, s_: lambda e: e.scalar_tensor_tensor(out=o_, in0=a_, scalar=s_, in1=o_, op0=ALU.mult, op1=ALU.add))(o_, a_, s_), len(P.ops))
                        op.deps = list(prev) if n_ < 2 else [ops_[n_ - 2]]
                        P.ops.append(op)
                        ops_.append(op)
                    chain[key] = [ops_[3]]

                def scan_done(key):
                    P.writer[key] = chain.pop(key)[0]
                    P.readers[key] = []

                def bk_scan(pl, c0, n, rev, u, key, up_only=False):
                    L = n.bit_length() - 1
                    re = pl[:, 0, c0:c0 + n]; im = pl[:, 1, c0:c0 + n]
                    for k in range(L):
                        s_ = 2 << k; h_ = 1 << k
                        vr = re.rearrange("p (m s) -> p m s", s=s_); vi = im.rearrange("p (m s) -> p m s", s=s_)
                        if not rev:
                            cmul_acc(vr[:, :, s_ - 1], vi[:, :, s_ - 1], vr[:, :, h_ - 1], vi[:, :, h_ - 1], k, u, key)
                        else:
                            cmul_acc(vr[:, :, 0], vi[:, :, 0], vr[:, :, h_], vi[:, :, h_], k, u, key)
                    for k in (range(L - 2, -1, -1) if not up_only else ()):
                        s_ = 2 << k; h_ = 1 << k
                        vr = re.rearrange("p (m s) -> p m s", s=s_); vi = im.rearrange("p (m s) -> p m s", s=s_)
                        if not rev:
                            cmul_acc(vr[:, 1:, h_ - 1], vi[:, 1:, h_ - 1], vr[:, :-1, s_ - 1], vi[:, :-1, s_ - 1], k, u, key)
                        else:
                            cmul_acc(vr[:, :-1, h_], vi[:, :-1, h_], vr[:, 1:, 0], vi[:, 1:, 0], k, u, key)

                segs = [(0, 256)] + [(CTX + i * 512, 512) for i in range(8)]
                units = [(ut, d_, gpl) for ut in range(4) for d_ in range(2) for gpl in range(4)]

                def stA(ix):
                    ut, d_, gpl = units[ix]
                    u = d_ * 16 + ut * 4 + gpl
                    bi = ix % 2; ub = 0; ukey = "uT_sb0"
                    zk, zck, lbk, lck, plk = "Z%d" % bi, "Zc%d" % bi, "LB%d" % bi, "LC%d" % bi, "PL%d" % bi
                    if ix % 8 == 0:
                        P.dma("sp", ukey, lambda e: e.dma_start(out=uT_sb[ub][:], in_=uT_d[ut * 128:(ut + 1) * 128, :]), reads=["uT_d"], writes=[ukey])
                    P.op("pool", lambda e: e.memset(Z[bi][:], 0.0), writes=[zk])
                    for j in range(2):
                        for gl in range(2):
                            cs = (2 * gpl + gl) * 16
                            P.op("pool", (lambda j, gl, cs: lambda e: e.tensor_copy(out=Z[bi][gl * 64:(gl + 1) * 64, j, cs:cs + 16], in_=bb[j][gl * 64:(gl + 1) * 64, u, :]))(j, gl, cs),
                                 reads=["prm"], writes=[zk])
                    zi, zkey = pzr.next()
                    for j in range(2):
                        P.op("pe", (lambda zi, j: lambda e: e.matmul(pz[zi][:, j, :], lhsT=Z[bi][:, j, :], rhs=identf[:], start=True, stop=True))(zi, j), reads=[zk, "identf"], writes=[zkey])
                    P.op("act", (lambda zi: lambda e: e.copy(out=LB[bi][:], in_=pz[zi][:]))(zi), reads=[zkey], writes=[lbk])
                    for j in range(2):
                        P.op("pool", (lambda j: lambda e: e.tensor_tensor(out=Zc[bi][:, j, :].rearrange("p (g q) -> p g q", g=2), in0=Cn2[j][:, d_ * 4 + ut, :, :],
                                                                         in1=maskf[:, gpl, :].unsqueeze(2).to_broadcast([128, 2, 64]), op=ALU.mult))(j),
                             reads=["Cn2%d" % j, "maskf"], writes=[zck])
                    zi2, zkey2 = pzr.next()
                    for j in range(2):
                        P.op("pe", (lambda zi2, j: lambda e: e.matmul(pz[zi2][:, j, :], lhsT=Zc[bi][:, j, :], rhs=identf[:], start=True, stop=True))(zi2, j), reads=[zck, "identf"], writes=[zkey2])
                    P.op("act", lambda e: e.copy(out=LC[bi][:, 0, :], in_=pz[zi2][:, 0, :]), reads=[zkey2], writes=[lck])
                    P.op("act", lambda e: e.mul(out=LC[bi][:, 1, :], in_=pz[zi2][:, 1, :], mul=-1.0), reads=[zkey2], writes=[lck])
                    for (c0, n) in segs:
                        for j in range(2):
                            qi, qkey = pbr.next()
                            P.op("pe", (lambda qi, j, c0, n: lambda e: e.matmul(pb[qi][:, 0:n], lhsT=LB[bi][:, j, :], rhs=uT_sb[ub][:, c0:c0 + n], start=True, stop=True))(qi, j, c0, n),
                                 reads=[lbk, ukey], writes=[qkey])
                            P.op("act", (lambda qi, j, c0, n: lambda e: e.copy(out=PL[bi][:, j, c0:c0 + n], in_=pb[qi][:, 0:n]))(qi, j, c0, n), reads=[qkey], writes=[plk])

                def stB(ix):
                    ut, d_, gpl = units[ix]
                    u = d_ * 16 + ut * 4 + gpl
                    bi = ix % 2; plk = "PL%d" % bi
                    rev = (d_ == 1)
                    bk_scan(PL[bi], 0, CTX, rev, u, plk, up_only=True)
                    if not rev:
                        cmul_acc(PL[bi][:, 0, CTX:CTX + 1], PL[bi][:, 1, CTX:CTX + 1], PL[bi][:, 0, CTX - 1:CTX], PL[bi][:, 1, CTX - 1:CTX], 0, u, plk)
                    else:
                        cmul_acc(PL[bi][:, 0, NTOK - 1:NTOK], PL[bi][:, 1, NTOK - 1:NTOK], PL[bi][:, 0, 0:1], PL[bi][:, 1, 0:1], 0, u, plk)
                    bk_scan(PL[bi], CTX, SEQ, rev, u, plk)
                    scan_done(plk)

                def stC(ix):
                    ut, d_, gpl = units[ix]
                    bi = ix % 2; ub = 0; ukey = "uT_sb0"; lck, plk = "LC%d" % bi, "PL%d" % bi
                    first = (ix % 8 == 0)
                    for sgi in range(8):
                        c0 = CTX + sgi * 512
                        yi, ykey = pyr.next()
                        for j in range(2):
                            P.op("pe", (lambda yi, j, c0: lambda e: e.matmul(py[yi][:], lhsT=LC[bi][:, j, :], rhs=PL[bi][:, j, c0:c0 + 512], start=(j == 0), stop=(j == 1)))(yi, j, c0),
                                 reads=[lck, plk], writes=[ykey])
                        ysl = slice(sgi * 512, (sgi + 1) * 512)
                        if first:
                            P.op("dve", (lambda yi, c0, ysl: lambda e: e.scalar_tensor_tensor(out=yT[:, ysl], in0=uT_sb[ub][:, c0:c0 + 512], scalar=dsk[:, ut:ut + 1],
                                                                                              in1=py[yi][:], op0=ALU.mult, op1=ALU.add))(yi, c0, ysl),
                                 reads=[ykey, ukey, "dsk"], writes=["yT"])
                        else:
                            P.op("dve", (lambda yi, ysl: lambda e: e.tensor_tensor(out=yT[:, ysl], in0=yT[:, ysl], in1=py[yi][:], op=ALU.add))(yi, ysl), reads=[ykey], writes=["yT"])
                    if ix % 8 == 7:
                        if debug:
                            P.dma("sp", "dbgy", lambda e: e.dma_start(out=yT_d[ut * 128:(ut + 1) * 128, :], in_=yT[:]), reads=["yT"], writes=["yT_d"])
                        P.op("act", lambda e: e.activation(out=zT[:, ut, :], in_=yT[:], func=AF.Gelu_apprx_tanh), reads=["yT"], writes=["zT"])

                stA(0)
                for ix in range(32):
                    if ix + 1 < 32:
                        stA(ix + 1)
                    stB(ix)
                    stC(ix)
            P.barrier()
            with phase() as st:
                wstgB_t = sbuf(st, "wstgB", [128, 4, 1024])
                w_glu_sb = sbuf(st, "w_glu_sb", [128, 4, 512], BF16); w_ba_sb = sbuf(st, "w_ba_sb", [128, 4, D], BF16)
                bglu = sbuf(st, "bglu", [128, 4])
                sg = [sbuf(st, "sg%d" % i, [128, 512], BF16) for i in range(2)]; sgr = Rot("sg", 2)
                glu = [sbuf(st, "glu%d" % i, [128, 4, 512], BF16) for i in range(2)]
                st_ba = [sbuf(st, "st_ba%d" % i, [128, 8, 512], BF16) for i in range(2)]
                pg = [psum(st, "pg%d" % i, [128, 512]) for i in range(3)]; pgr = Rot("pg", 3)
                pa = [psum(st, "pa%d" % i, [128, 512]) for i in range(3)]; par = Rot("pa", 3)
                P.dma("sp", "wl0", lambda e: e.dma_start(out=wstgB_t[:, :, 0:512], in_=w_glu.rearrange("(k p) n -> p k n", p=128)), writes=["wstgB"])
                P.op("dve", lambda e: e.tensor_copy(out=w_glu_sb[:], in_=wstgB_t[:, :, 0:512]), reads=["wstgB"], writes=["w_glu_sb"])
                P.dma("sp", "wl0", lambda e: e.dma_start(out=wstgB_t[:], in_=w_ba.rearrange("(k p) n -> p k n", p=128)), reads=["wstgB"], writes=["wstgB"])
                P.op("dve", lambda e: e.tensor_copy(out=w_ba_sb[:], in_=wstgB_t[:]), reads=["wstgB"], writes=["w_ba_sb"])
                P.dma("act", "wl1", lambda e: e.dma_start(out=bglu[:], in_=b_glu.rearrange("(k p) o -> p (k o)", p=128), allow_slow_non_contiguous=True), writes=["bglu"])
                for sgi in range(8):
                    gb_ = sgi % 2; gkey = "glu%d" % gb_; bakey = "st_ba%d" % gb_
                    ssl = slice(sgi * 512, (sgi + 1) * 512)
                    for ct in range(4):
                        gi, gk = pgr.next()
                        for k in range(4):
                            P.op("pe", (lambda gi, k, ct, ssl: lambda e: e.matmul(pg[gi][:], lhsT=w_glu_sb[:, k, ct * 128:(ct + 1) * 128], rhs=zT[:, k, ssl],
                                                                                  start=(k == 0), stop=(k == 3)))(gi, k, ct, ssl),
                                 reads=["w_glu_sb", "zT"], writes=[gk])
                        si_, sk_ = sgr.next()
                        P.op("act", (lambda si_, gi, ct: lambda e: e.activation(out=sg[si_][:], in_=pg[gi][:], func=AF.Sigmoid, bias=bglu[:, ct:ct + 1]))(si_, gi, ct),
                             reads=[gk, "bglu"], writes=[sk_])
                        P.op("dve", (lambda gb_, ct, si_, ssl: lambda e: e.tensor_tensor(out=glu[gb_][:, ct, :], in0=sg[si_][:], in1=zT[:, ct, ssl], op=ALU.mult))(gb_, ct, si_, ssl),
                             reads=[sk_, "zT"], writes=[gkey])
                    for ct2 in range(8):
                        ai, ak = par.next()
                        for k in range(4):
                            P.op("pe", (lambda ai, k, ct2, gb_: lambda e: e.matmul(pa[ai][:], lhsT=w_ba_sb[:, k, ct2 * 128:(ct2 + 1) * 128], rhs=glu[gb_][:, k, :],
                                                                                   start=(k == 0), stop=(k == 3)))(ai, k, ct2, gb_),
                                 reads=["w_ba_sb", gkey], writes=[ak])
                        if ct2 % 2:
                            P.op("act", (lambda gb_, ct2, ai: lambda e: e.copy(out=st_ba[gb_][:, ct2, :], in_=pa[ai][:]))(gb_, ct2, ai), reads=[ak], writes=[bakey])
                        else:
                            P.op("dve", (lambda gb_, ct2, ai: lambda e: e.tensor_copy(out=st_ba[gb_][:, ct2, :], in_=pa[ai][:]))(gb_, ct2, ai), reads=[ak], writes=[bakey])
                    P.dma("sp", bakey, (lambda gb_, ssl: lambda e: e.dma_start(out=baT_d.rearrange("(t p) n -> p t n", p=128)[:, :, ssl], in_=st_ba[gb_][:]))(gb_, ssl),
                          reads=[bakey], writes=["baT_d"])
        P.barrier()
        if stage <= 2:
            return finish(nc, P, out)

        attT_d = dscr("attT_d", [512, SEQ], BF16) if debug else None
        NEG = -30000.0
        with ExitStack() as stC:
            attT_sb = sbuf(stC, "attT_sb", [128, 4, SEQ], BF16)
            stC2 = ExitStack()
            kT_sb = sbuf(stC2, "kT_sb", [128, 4, NTOK], BF16); qT_sb = sbuf(stC2, "qT_sb", [128, 4, SEQ], BF16)
            BiasTT = sbuf(stC2, "BiasTT", [128, 8 * 14, 64])
            Vctx = sbuf(stC2, "Vctx", [128, 2, 512], BF16)
            ones_b = sbuf(stC2, "ones_b", [128, 128], BF16)
            P.dma("sp", "lc0", lambda e: e.dma_start(out=kT_sb[:], in_=kT_d.rearrange("(t p) n -> p t n", p=128)), reads=["kT_d"], writes=["kT_sb"])
            P.dma("act", "lc1", lambda e: e.dma_start(out=qT_sb[:], in_=qT_d.rearrange("(t p) n -> p t n", p=128)), reads=["qT_d"], writes=["qT_sb"])
            P.dma("act", "lc1", lambda e: e.dma_start(out=Vctx[:], in_=v_d[0:CTX, :].rearrange("(t p) n -> p t n", p=128)), reads=["v_d"], writes=["Vctx"])
            P.op("pool", lambda e: e.memset(ones_b[:], 1.0), writes=["ones_b"])
            with phase() as st:
                rpbB = sbuf(st, "rpbB", [128, 8 * 14, 31]); tmpC = sbuf(st, "tmpC", [128, 8 * 14, 64])
                Dm = sbuf(st, "Dm", [128, 64]); eqm = [sbuf(st, "eqm%d" % i, [128, 64]) for i in range(2)]
                c0t = sbuf(st, "c0t", [128, 64]); kcv = sbuf(st, "kcv", [128, 64]); m2 = sbuf(st, "m2c", [128, 64])
                for half in range(2):
                    sl = slice(half * 64, (half + 1) * 64)
                    P.dma("sp", "lc2", (lambda sl, half: lambda e: e.dma_start(out=rpbB[sl].rearrange("p (h j) m -> p h (j m)", h=8),
                                                                              in_=rpb[:, half:half + 14, :].rearrange("h j m -> h (j m)").unsqueeze(0).to_broadcast([64, 8, 14 * 31])))(sl, half),
                          writes=["rpbB"])
                    P.op("pool", (lambda sl: lambda e: e.iota(Dm[sl], pattern=[[-1, 64]], base=15, channel_multiplier=1, allow_small_or_imprecise_dtypes=True))(sl), writes=["Dm"])
                    P.op("pool", (lambda sl: lambda e: e.iota(kcv[sl], pattern=[[0, 64]], base=0, channel_multiplier=1, allow_small_or_imprecise_dtypes=True))(sl), writes=["kcv"])
                P.op("pool", lambda e: e.iota(c0t[:], pattern=[[1, 64]], base=-8, channel_multiplier=0, allow_small_or_imprecise_dtypes=True), writes=["c0t"])
                P.op("dve", lambda e: e.tensor_scalar(out=c0t[:], in0=c0t[:], scalar1=0.0, scalar2=48.0, op0=ALU.max, op1=ALU.min), reads=["c0t"], writes=["c0t"])
                P.op("dve", lambda e: e.tensor_tensor(out=kcv[:], in0=kcv[:], in1=c0t[:], op=ALU.subtract), reads=["kcv", "c0t"], writes=["kcv"])
                P.op("dve", lambda e: e.tensor_single_scalar(out=m2[:], in_=kcv[:], scalar=0.0, op=ALU.is_ge), reads=["kcv"], writes=["m2c"])
                P.op("dve", lambda e: e.tensor_single_scalar(out=kcv[:], in_=kcv[:], scalar=15.0, op=ALU.is_le), reads=["kcv", "m2c"], writes=["kcv"])
                P.op("dve", lambda e: e.tensor_tensor(out=m2[:], in0=m2[:], in1=kcv[:], op=ALU.mult), reads=["kcv", "m2c"], writes=["m2c"])
                P.op("dve", lambda e: e.tensor_scalar(out=m2[:], in0=m2[:], scalar1=-1.0, scalar2=-NEG, op0=ALU.add, op1=ALU.mult), reads=["m2c"], writes=["m2c"])
                P.op("dve", lambda e: e.tensor_copy(out=BiasTT[:], in_=m2[:].unsqueeze(1).to_broadcast([128, 112, 64])), reads=["m2c"], writes=["BiasTT"])
                for m in range(31):
                    ei = m % 2; ek = "eqm%d" % ei
                    P.op("dve", (lambda ei, m: lambda e: e.tensor_single_scalar(out=eqm[ei][:], in_=Dm[:], scalar=float(m), op=ALU.is_equal))(ei, m), reads=["Dm"], writes=[ek])
                    P.op("pool", (lambda ei, m: lambda e: e.tensor_tensor(out=tmpC[:], in0=eqm[ei][:].unsqueeze(1).to_broadcast([128, 112, 64]),
                                                                          in1=rpbB[:, :, m:m + 1].to_broadcast([128, 112, 64]), op=ALU.mult))(ei, m),
                         reads=[ek, "rpbB"], writes=["tmpC"])
                    P.op("dve", lambda e: e.tensor_tensor(out=BiasTT[:], in0=BiasTT[:], in1=tmpC[:], op=ALU.add), reads=["tmpC"], writes=["BiasTT"])
            P.barrier()
            with phase() as st:
                Vb = [sbuf(st, "Vb%d" % i, [128, 4, 512], BF16) for i in range(3)]; vbr = Rot("Vb", 3)
                ssb = [sbuf(st, "ssb%d" % i, [128, 4, 64]) for i in range(3)]; ssr2 = Rot("ssb", 3)
                pT = [sbuf(st, "pT%d" % i, [128, 384], BF16) for i in range(3)]; ptr = Rot("pT", 3)
                rden = [sbuf(st, "rden%d" % i, [128, 64]) for i in range(2)]; rdr = Rot("rden", 2)
                ps_ = [psum(st, "psc%d" % i, [128, 512]) for i in range(3)]; psr = Rot("psc", 3)
                po_ = [psum(st, "poc%d" % i, [128, 512]) for i in range(2)]; por = Rot("poc", 2)
                pd_ = [psum(st, "pdc%d" % i, [128, 512]) for i in range(2)]; pdr = Rot("pdc", 2)
                B4 = BiasTT[:].rearrange("p (h j) q -> p h j q", h=8)
                cf = [sbuf(st, "cvf%d" % i, [128, 2048]) for i in range(3)]; cb = [sbuf(st, "cvb%d" % i, [128, 2048], BF16) for i in range(3)]

                def convert_tile(ti):
                    bi = ti % 3
                    P.dma("sp", "cvf%d" % bi, lambda e: e.dma_start(out=cf[bi][:], in_=peer_uv[ti * 128:(ti + 1) * 128, :]), writes=["cvf%d" % bi])
                    P.op("pool", lambda e: e.tensor_copy(out=cb[bi][:], in_=cf[bi][:]), reads=["cvf%d" % bi], writes=["cvb%d" % bi])
                    P.dma("pool", "cvb%d" % bi, lambda e: e.dma_start(out=uvb_d[ti * 128:(ti + 1) * 128, :], in_=cb[bi][:]), reads=["cvb%d" % bi], writes=["uvb_d"])
                def c_scores(r, h, vi):
                    r0 = min(max(r - 4, 0), 56)
                    t = h // 2; po = (h % 2) * 64; psl = slice(po, po + 64)
                    si, skey = psr.next()
                    qsl = slice(r * 64, (r + 1) * 64)
                    for j in range(6):
                        k0 = (CTX + (r0 + 2 * j) * 64) if j < 4 else (j - 4) * 128
                        P.op("pe", (lambda j, k0: lambda e: e.matmul(ps_[si][:, j * 64:(j + 1) * 64], lhsT=kT_sb[psl, t, k0:k0 + 128], rhs=qT_sb[psl, t, qsl], start=True, stop=True))(j, k0),
                             reads=["kT_sb", "qT_sb"], writes=[skey])
                    return (r, h, vi, si, skey)

                def c_part1(state):
                    r, h, vi, si, skey = state
                    r0 = min(max(r - 4, 0), 56); dr0 = r0 - r + 7
                    bi2, bkey2 = ssr2.next()
                    P.op("dve", lambda e: e.scalar_tensor_tensor(out=ssb[bi2][:], in0=ps_[si][:, 0:256].rearrange("p (j q) -> p j q", j=4), scalar=0.125,
                                                                 in1=B4[:, h, dr0:dr0 + 7:2, :], op0=ALU.mult, op1=ALU.add), reads=[skey, "BiasTT"], writes=[bkey2])
                    ti, tkey = ptr.next()
                    P.op("act", lambda e: e.activation(out=pT[ti][:, 0:256], in_=ssb[bi2][:].rearrange("p j q -> p (j q)"), func=AF.Exp), reads=[bkey2], writes=[tkey])
                    P.op("act", lambda e: e.activation(out=pT[ti][:, 256:384], in_=ps_[si][:, 256:384], func=AF.Exp, scale=0.125), reads=[skey], writes=[tkey])
                    return (r, h, vi, ti, tkey)

                def c_part2(state):
                    r, h, vi, ti, tkey = state
                    vkey = "Vb%d" % vi
                    t = h // 2; po = (h % 2) * 64; psl = slice(po, po + 64)
                    qsl = slice(r * 64, (r + 1) * 64)
                    oi, okey = por.next(); di, dkey = pdr.next()
                    hp = (h // 2) * 128
                    for j in range(6):
                        vsrc = (Vb[vi][:, j, hp:hp + 128] if j < 4 else Vctx[:, j - 4, hp:hp + 128])
                        P.op("pe", (lambda j, vsrc: lambda e: e.matmul(po_[oi][:, 0:64], lhsT=vsrc, rhs=pT[ti][:, j * 64:(j + 1) * 64], start=(j == 0), stop=(j == 5)))(j, vsrc),
                             reads=[vkey, "Vctx", tkey], writes=[okey])
                    for j in range(6):
                        P.op("pe", (lambda j: lambda e: e.matmul(pd_[di][:, 0:64], lhsT=ones_b[:], rhs=pT[ti][:, j * 64:(j + 1) * 64], start=(j == 0), stop=(j == 5)))(j),
                             reads=["ones_b", tkey], writes=[dkey])
                    ri, rkey = rdr.next()
                    P.op("dve", lambda e: e.reciprocal(out=rden[ri][psl, :], in_=pd_[di][psl, 0:64]), reads=[dkey], writes=[rkey])
                    P.op("dve", lambda e: e.tensor_tensor(out=attT_sb[psl, t, qsl], in0=po_[oi][psl, 0:64], in1=rden[ri][psl, :], op=ALU.mult), reads=[okey, rkey], writes=["attT_sb"])

                pend1 = None; pend2 = None
                for r in range(64):
                    convert_tile(2 * r); convert_tile(2 * r + 1)
                    r0 = min(max(r - 4, 0), 56)
                    vi, vkey = vbr.next()
                    P.dma("sp", vkey, (lambda vi, r0: lambda e: e.dma_start(out=Vb[vi][:], in_=v_d[CTX + r0 * 64:CTX + (r0 + 8) * 64, :].rearrange("(j p) n -> p j n", p=128)))(vi, r0),
                          reads=["v_d"], writes=[vkey])
                    for h in range(8):
                        stt = c_scores(r, h, vi)
                        nxt2 = c_part1(pend1) if pend1 is not None else None
                        if pend2 is not None:
                            c_part2(pend2)
                        pend2 = nxt2
                        pend1 = stt
                nxt2 = c_part1(pend1)
                if pend2 is not None:
                    c_part2(pend2)
                c_part2(nxt2)
            if debug:
                P.dma("sp", "dbga", lambda e: e.dma_start(out=attT_d.rearrange("(t p) n -> p t n", p=128), in_=attT_sb[:]), reads=["attT_sb"], writes=["attT_d"])
            P.barrier()
            stC2.close()
            with phase() as st:
                wstgC_t = sbuf(st, "wstgC", [128, 4, 1024]); w_bb_sb = sbuf(st, "w_bb_sb", [128, 4, D], BF16)
                g_sb = [sbuf(st, "g_sb%d" % i, [128, 16, 512], BF16) for i in range(2)]
                ba_sb = [sbuf(st, "ba_sb%d" % i, [128, 8, 512], BF16) for i in range(2)]
                t1 = [sbuf(st, "t1c%d" % i, [128, 512]) for i in range(2)]; t1r = Rot("t1c", 2)
                t2 = [sbuf(st, "t2c%d" % i, [128, 512]) for i in range(2)]; t2r = Rot("t2c", 2)
                st_mg = [sbuf(st, "st_mg%d" % i, [128, 8, 512], BF16) for i in range(2)]
                pbb = [psum(st, "pbb%d" % i, [128, 512]) for i in range(3)]; pbr2 = Rot("pbb", 3)
                P.dma("sp", "wc0", lambda e: e.dma_start(out=wstgC_t[:], in_=w_bb.rearrange("(k p) n -> p k n", p=128)), writes=["wstgC"])
                P.op("dve", lambda e: e.tensor_copy(out=w_bb_sb[:], in_=wstgC_t[:]), reads=["wstgC"], writes=["w_bb_sb"])
                for sgi in range(8):
                    b2 = sgi % 2; ssl = slice(sgi * 512, (sgi + 1) * 512)
                    gk, bk, mk = "g_sb%d" % b2, "ba_sb%d" % b2, "st_mg%d" % b2
                    P.dma("sp", gk, (lambda b2, ssl: lambda e: e.dma_start(out=g_sb[b2][:], in_=gT_d.rearrange("(t p) n -> p t n", p=128)[:, :, ssl]))(b2, ssl), reads=["gT_d"], writes=[gk])
                    P.dma("act", bk, (lambda b2, ssl: lambda e: e.dma_start(out=ba_sb[b2][:], in_=baT_d.rearrange("(t p) n -> p t n", p=128)[:, :, ssl]))(b2, ssl), reads=["baT_d"], writes=[bk])
                    for ct2 in range(8):
                        qi, qk = pbr2.next()
                        for k in range(4):
                            P.op("pe", (lambda qi, k, ct2, ssl: lambda e: e.matmul(pbb[qi][:], lhsT=w_bb_sb[:, k, ct2 * 128:(ct2 + 1) * 128], rhs=attT_sb[:, k, ssl],
                                                                                   start=(k == 0), stop=(k == 3)))(qi, k, ct2, ssl),
                                 reads=["w_bb_sb", "attT_sb"], writes=[qk])
                        i1, k1 = t1r.next(); i2, k2 = t2r.next()
                        P.op("dve", (lambda i1, qi, b2, ct2: lambda e: e.tensor_tensor(out=t1[i1][:], in0=pbb[qi][:], in1=g_sb[b2][:, 8 + ct2, :], op=ALU.mult))(i1, qi, b2, ct2),
                             reads=[qk, gk], writes=[k1])
                        P.op("pool", (lambda i2, b2, ct2: lambda e: e.tensor_tensor(out=t2[i2][:], in0=ba_sb[b2][:, ct2, :], in1=g_sb[b2][:, ct2, :], op=ALU.mult))(i2, b2, ct2),
                             reads=[bk, gk], writes=[k2])
                        P.op("dve", (lambda b2, ct2, i1, i2: lambda e: e.tensor_tensor(out=st_mg[b2][:, ct2, :], in0=t1[i1][:], in1=t2[i2][:], op=ALU.add))(b2, ct2, i1, i2),
                             reads=[k1, k2], writes=[mk])
                    P.dma("sp", mk, (lambda b2, ssl: lambda e: e.dma_start(out=mgT_d.rearrange("(t p) n -> p t n", p=128)[:, :, ssl], in_=st_mg[b2][:]))(b2, ssl),
                          reads=[mk], writes=["mgT_d"])
        P.barrier()
        if stage <= 3:
            return finish(nc, P, out)

        x1_d = dscr("x1_d", [SEQ, D], F32) if debug else None
        pf_d = dscr("pf_d", [SEQ, D], F32) if debug else None
        NT = 32 if stage >= 5 else int(stage * 10) % 10 or 1
        if not debug:
            NT = 32
        if "KNT" in os.environ:
            NT = int(os.environ["KNT"])
        gate1B = modB[:, 2 * D:3 * D]; S2B = modB[:, 3 * D:4 * D]; G2B = modB[:, 4 * D:5 * D]; gate2B = modB[:, 5 * D:6 * D]
        with ExitStack() as stD:
            w_out_sb = sbuf(stD, "w_out_sb", [128, 8, D], BF16); w_q_sb = sbuf(stD, "w_q_sb", [128, 8, 2048], BF16)
            skT = sbuf(stD, "skT", [128, 2, 128], BF16); fgB = sbuf(stD, "fgB", [128, D])
            with phase() as st:
                wstgD_t = [sbuf(st, "wstgD%d" % i, [128, 8, 512]) for i in range(2)]
                skf = sbuf(st, "skf", [128, 2, 128]); skb = sbuf(st, "skb", [128, 2, 128], BF16)
                ptk = psum(st, "ptk", [128, 2, 128], BF16)
                for i in range(6):
                    bi = i % 2
                    srcw = (w_out if i < 2 else w_q).rearrange("(k p) n -> p k n", p=128)
                    c0 = (i * 512) if i < 2 else (i - 2) * 512
                    dstw = w_out_sb if i < 2 else w_q_sb
                    P.dma("sp", "wd%d" % bi, (lambda bi, srcw, c0: lambda e: e.dma_start(out=wstgD_t[bi][:], in_=srcw[:, :, c0:c0 + 512]))(bi, srcw, c0), writes=["wstgD%d" % bi])
                    P.op(("dve", "pool")[bi], (lambda bi, dstw, c0: lambda e: e.tensor_copy(out=dstw[:, :, c0:c0 + 512], in_=wstgD_t[bi][:]))(bi, dstw, c0),
                         reads=["wstgD%d" % bi], writes=["wD"])
                P.dma("act", "wd2", lambda e: e.dma_start(out=skf[:], in_=subkeys.rearrange("n k d -> k n d")), writes=["skf"])
                P.dma("act", "wd2", lambda e: e.dma_start(out=fgB[:], in_=final_g.to_broadcast([128, D])), writes=["fgB"])
                P.op("dve", lambda e: e.tensor_copy(out=skb[:], in_=skf[:]), reads=["skf"], writes=["skb"])
                for n_ in range(2):
                    P.op("pe", (lambda n_: lambda e: e.transpose(ptk[:, n_, :], skb[:, n_, :], identb[:]))(n_), reads=["skb", "identb"], writes=["ptk"])
                P.op("dve", lambda e: e.tensor_copy(out=skT[:], in_=ptk[:]), reads=["ptk"], writes=["skT"])
            P.barrier()
            P.barrier()
            if stage == 3.5:
                return finish(nc, P, out)
            with phase() as st:
                NB = 2
                xtD_t = sbuf(st, "xtD", [128, D])
                x1 = [sbuf(st, "x1_%d" % i, [128, D]) for i in range(NB)]
                h2 = [sbuf(st, "h2_%d" % i, [128, D]) for i in range(NB)]
                eid = [sbuf(st, "eid%d" % i, [128, 128], I32) for i in range(NB)]
                gate = [sbuf(st, "gate%d" % i, [128, 128]) for i in range(NB)]
                tmpD = sbuf(st, "tmpD", [128, D]); tmpF = tmpD
                acc = None
                junkD = sbuf(st, "junkD", [128, D], BF16); NJ = int(os.environ.get("KJ", "2"))
                junkE3 = [sbuf(st, "junkE%d" % i, [128, D], BF16) for i in range(NJ)]; jer = Rot("junkE", NJ)
                mg_sb = sbuf(st, "mg_sb", [128, 8, 128], BF16); h2b2 = [sbuf(st, "h2b%d" % i, [128, D], BF16) for i in range(NB)]; h2T = sbuf(st, "h2T", [128, 8, 128], BF16)
                qT_sb2 = sbuf(st, "qT_sb2", [128, 16, 128], BF16); s_sb = sbuf(st, "s_sb", [128, 16, 128]); work = sbuf(st, "workD", [128, 16, 128])
                topv = sbuf(st, "topv", [128, 16, 16]); idxu = sbuf(st, "idxu", [128, 16, 16], U32); idxf = sbuf(st, "idxf", [128, 16, 16])
                cand = sbuf(st, "cand", [128, 8, 256]); cidx = s_sb[:].rearrange("p a b -> p (a b)").rearrange("p (h c) -> p h c", h=8)
                best = sbuf(st, "best", [128, 8, 16]); eg = sbuf(st, "eg", [128, 8, 16]); sm = sbuf(st, "smD", [128, 32]); sm2 = sbuf(st, "smE", [128, 8]); eidf = sbuf(st, "eidf", [128, 128])
                aD = sbuf(st, "aD", [128, 128]); gl_ = sbuf(st, "gl_", [128, 128]); wD = sbuf(st, "wDw", [128, 128])
                NG = int(os.environ.get("KNG", "12")); GJ = int(os.environ.get("KGJ", "4"))
                FY = int(os.environ.get("KFY", "1")); KAR = int(os.environ.get("KAR", "0"))
                posI = sbuf(st, "posI", [128, 256], I32); mskI = sbuf(st, "mskI", [128, 1], I32)
                c4I = sbuf(st, "c4I", [128, 1], I32); c15I = sbuf(st, "c15I", [128, 1], I32); iota16 = sbuf(st, "iota16", [128, 16])
                pab = sbuf(st, "pab", [128, 2, 8, 16], I32); pabf = sbuf(st, "pabf", [128, 2, 8, 16]); e3b = sbuf(st, "e3b", [128, 8, 16])
                oh = cand[:].rearrange("p h (k a) -> p h k a", a=16)
                P.op("pool", lambda e: e.iota(c4I[:], pattern=[[0, 1]], base=4, channel_multiplier=0), writes=["posI"])
                P.op("pool", lambda e: e.iota(c15I[:], pattern=[[0, 1]], base=15, channel_multiplier=0), writes=["posI"])
                P.op("pool", lambda e: e.iota(iota16[:], pattern=[[1, 16]], base=0, channel_multiplier=0, allow_small_or_imprecise_dtypes=True), writes=["posI"])
                P.op("pool", lambda e: e.iota(posI[:], pattern=[[1, 256]], base=0, channel_multiplier=0), writes=["posI"])
                P.op("pool", lambda e: e.iota(mskI[:], pattern=[[0, 1]], base=-256, channel_multiplier=0), writes=["posI"])
                UVg = [sbuf(st, "UVg%d" % i, [128, 2, D], BF16) for i in range(NG)]
                dg = [sbuf(st, "dg%d" % i, [128, 128], BF16) for i in range(8)]; dgr = Rot("dg", 8)
                pmo = [psum(st, "pmo%d" % i, [128, 512]) for i in range(2)]
                pacc = [psum(st, "pacc%d" % i, [128, 512]) for i in range(2)]
                tpD = psum(st, "tpD", [128, 8, 128], BF16)
                pq = [psum(st, "pq%d" % i, [128, 4, 128]) for i in range(2)]; pqr = Rot("pq", 2)
                uv_i = [0]

                def rstd_chain(src_ap, srckey, smt, smk, jk, jkey):
                    P.op("act", lambda e: e.activation(out=jk[:], in_=src_ap, func=AF.Square, accum_out=smt[:, 0:1]), reads=[srckey], writes=[smk, jkey])
                    P.op("dve", lambda e: e.tensor_scalar(out=smt[:, 1:2], in0=smt[:, 0:1], scalar1=1.0 / D, scalar2=EPS, op0=ALU.mult, op1=ALU.add), reads=[smk], writes=[smk])
                    P.op("act", lambda e: e.sqrt(out=smt[:, 2:3], in_=smt[:, 1:2]), reads=[smk], writes=[smk])
                    P.op("dve", lambda e: e.reciprocal(out=smt[:, 3:4], in_=smt[:, 2:3]), reads=[smk], writes=[smk])

                def front(i):
                    b = i % NB; t0 = i * 128
                    xk, x1k, h2k, ek, gk = "xtD", "x1_%d" % b, "h2_%d" % b, "eid%d" % b, "gate%d" % b
                    P.dma("sp", xk, lambda e: e.dma_start(out=xtD_t[:], in_=x[t0:t0 + 128, :]), writes=[xk])
                    P.dma("sp", "mgl", lambda e: e.dma_start(out=mg_sb[:], in_=mgT_d.rearrange("(k p) n -> p k n", p=128)[:, :, t0:t0 + 128]), reads=["mgT_d"], writes=["mg_sb"])
                    for hf in range(2):
                        for k in range(8):
                            P.op("pe", (lambda hf, k: lambda e: e.matmul(pmo[hf][:], lhsT=mg_sb[:, k, :], rhs=w_out_sb[:, k, hf * 512:(hf + 1) * 512], start=(k == 0), stop=(k == 7)))(hf, k),
                                 reads=["mg_sb", "wD"], writes=["pmo%d" % hf])
                        yield
                        P.op("dve", (lambda hf: lambda e: e.tensor_tensor(out=tmpF[:, hf * 512:(hf + 1) * 512], in0=pmo[hf][:], in1=gate1B[:, hf * 512:(hf + 1) * 512], op=ALU.mult))(hf),
                             reads=["pmo%d" % hf, "modB"], writes=["tmpD"])
                        yield
                    P.op("dve", lambda e: e.tensor_tensor(out=x1[b][:], in0=tmpF[:], in1=xtD_t[:], op=ALU.add), reads=["tmpD", xk], writes=[x1k])
                    yield
                    if debug:
                        P.dma("sp", "dbgx1", lambda e: e.dma_start(out=x1_d[t0:t0 + 128, :], in_=x1[b][:]), reads=[x1k], writes=["x1_d"])
                    rstd_chain(x1[b][:], x1k, sm[:, 0:4], "smD", junkD, "junkD")
                    P.op("dve", lambda e: e.scalar_tensor_tensor(out=tmpF[:], in0=x1[b][:], scalar=sm[:, 3:4], in1=G2B, op0=ALU.mult, op1=ALU.mult), reads=[x1k, "smD", "modB"], writes=["tmpD"])
                    yield
                    P.op("dve", lambda e: e.tensor_tensor(out=h2[b][:], in0=tmpF[:], in1=S2B, op=ALU.add), reads=["tmpD", "modB"], writes=[h2k])
                    yield
                    P.op("act", lambda e: e.copy(out=h2b2[b][:], in_=h2[b][:]), reads=[h2k], writes=["h2b%d" % b])
                    for k in range(8):
                        P.op("pe", (lambda k: lambda e: e.transpose(tpD[:, k, :], h2b2[b][:, k * 128:(k + 1) * 128], identb[:]))(k), reads=["h2b%d" % b, "identb"], writes=["tpD"])
                    P.op("act", lambda e: e.copy(out=h2T[:], in_=tpD[:]), reads=["tpD"], writes=["h2T"])
                    yield
                    for g4 in range(4):
                        qi, qk = pqr.next()
                        for bl in range(4):
                            blk = g4 * 4 + bl
                            for k in range(8):
                                P.op("pe", (lambda qi, bl, blk, k: lambda e: e.matmul(pq[qi][:, bl, :], lhsT=w_q_sb[:, k, blk * 128:(blk + 1) * 128], rhs=h2T[:, k, :], start=(k == 0), stop=(k == 7)))(qi, bl, blk, k),
                                     reads=["wD", "h2T"], writes=[qk])
                        P.op("act", (lambda qi, g4: lambda e: e.copy(out=qT_sb2[:, g4 * 4:(g4 + 1) * 4, :], in_=pq[qi][:]))(qi, g4), reads=[qk], writes=["qT_sb2"])
                        yield
                    for g4 in range(4):
                        si, sk = pqr.next()
                        for bl in range(4):
                            blk = g4 * 4 + bl
                            P.op("pe", (lambda si, bl, blk: lambda e: e.matmul(pq[si][:, bl, :], lhsT=qT_sb2[:, blk, :], rhs=skT[:, blk % 2, :], start=True, stop=True))(si, bl, blk),
                                 reads=["qT_sb2", "skT"], writes=[sk])
                        P.op("act", (lambda si, g4: lambda e: e.copy(out=s_sb[:, g4 * 4:(g4 + 1) * 4, :], in_=pq[si][:]))(si, g4), reads=[sk], writes=["s_sb"])
                        yield
                    TK = ["s_sb", "topk"]
                    BK = ["tk%d" % q for q in range(16)]
                    for blk in range(16):
                        P.op("dve", (lambda blk: lambda e: e.max(out=topv[:, blk, 0:8], in_=s_sb[:, blk, :]))(blk), reads=TK, writes=[BK[blk]])
                    yield
                    for blk in range(16):
                        P.op("dve", (lambda blk: lambda e: e.max_index(out=idxu[:, blk, 0:8], in_max=topv[:, blk, 0:8], in_values=s_sb[:, blk, :]))(blk), reads=["s_sb", BK[blk]], writes=[BK[blk]])
                        if blk % 8 == 7:
                            yield
                    for blk in range(16):
                        P.op("dve", (lambda blk: lambda e: e.match_replace(out=work[:, blk, :], in_to_replace=topv[:, blk, 0:8], in_values=s_sb[:, blk, :], imm_value=-1e30))(blk), reads=["s_sb", BK[blk]], writes=[BK[blk]])
                        if blk % 8 == 7:
                            yield
                    for blk in range(16):
                        P.op("dve", (lambda blk: lambda e: e.max(out=topv[:, blk, 8:16], in_=work[:, blk, :]))(blk), reads=[BK[blk]], writes=[BK[blk]])
                    yield
                    for blk in range(16):
                        P.op("dve", (lambda blk: lambda e: e.max_index(out=idxu[:, blk, 8:16], in_max=topv[:, blk, 8:16], in_values=work[:, blk, :]))(blk), reads=[BK[blk]], writes=[BK[blk]])
                        if blk % 8 == 7:
                            yield
                    TK = TK + BK
                    P.op("dve", lambda e: e.tensor_copy(out=idxf[:], in_=idxu[:]), reads=TK, writes=["topk"])
                    tv4 = topv[:].rearrange("p (h n) a -> p h n a", n=2); ix4 = idxf[:].rearrange("p (h n) a -> p h n a", n=2)
                    c4 = cand[:].rearrange("p h (a b) -> p h a b", a=16); ci4 = cidx.rearrange("p h (a b) -> p h a b", a=16)
                    P.op("dve", lambda e: e.tensor_tensor(out=c4, in0=tv4[:, :, 0, :].unsqueeze(3).to_broadcast([128, 8, 16, 16]),
                                                          in1=tv4[:, :, 1, :].unsqueeze(2).to_broadcast([128, 8, 16, 16]), op=ALU.add), reads=TK, writes=["topk"])
                    yield
                    candI = cand[:].bitcast(I32)
                    P.op("dve", lambda e: e.tensor_scalar(out=candI, in0=candI, scalar1=mskI[:, 0:1], scalar2=None, op0=ALU.bitwise_and), reads=TK + ["posI"], writes=["topk"])
                    P.op("dve", lambda e: e.tensor_tensor(out=candI, in0=candI, in1=posI[:].unsqueeze(1).to_broadcast([128, 8, 256]), op=ALU.bitwise_or), reads=TK + ["posI"], writes=["topk"])
                    P.op("dve", lambda e: e.tensor_single_scalar(out=ix4[:, :, 0, :], in_=ix4[:, :, 0, :], scalar=128.0, op=ALU.mult), reads=TK, writes=["topk"])
                    yield
                    w2 = work[:].rearrange("p (h n) k -> p h (n k)", n=2)
                    HK = ["hk%d" % q for q in range(8)]
                    for h in range(8):
                        P.op("dve", (lambda h: lambda e: e.max(out=best[:, h, 0:8], in_=cand[:, h, :]))(h), reads=TK, writes=[HK[h]])
                    yield
                    for h in range(8):
                        P.op("dve", (lambda h: lambda e: e.match_replace(out=w2[:, h, :], in_to_replace=best[:, h, 0:8], in_values=cand[:, h, :], imm_value=-1e30))(h), reads=TK + [HK[h]], writes=[HK[h]])
                    yield
                    for h in range(8):
                        P.op("dve", (lambda h: lambda e: e.max(out=best[:, h, 8:16], in_=w2[:, h, :]))(h), reads=[HK[h]], writes=[HK[h]])
                    yield
                    TK = TK + HK
                    P.op("dve", lambda e: e.tensor_single_scalar(out=sm[:, 8:16], in_=best[:, :, 0], scalar=-1.0, op=ALU.mult), reads=TK + ["smD"], writes=["smD"])
                    for h in range(8):
                        P.op("act", (lambda h: lambda e: e.activation(out=eg[:, h, :], in_=best[:, h, :], func=AF.Exp, bias=sm[:, 8 + h:9 + h], accum_out=sm[:, 16 + h:17 + h]))(h),
                             reads=TK + ["smD"], writes=["eg", "smD"])
                    P.op("dve", lambda e: e.reciprocal(out=sm[:, 24:32], in_=sm[:, 16:24]), reads=["smD"], writes=["smD"])
                    P.op("dve", lambda e: e.tensor_tensor(out=gate[b][:].rearrange("p (h k) -> p h k", h=8), in0=eg[:], in1=sm[:, 24:32].unsqueeze(2).to_broadcast([128, 8, 16]), op=ALU.mult),
                         reads=["eg", "smD"], writes=[gk])
                    yield
                    bestI = best[:].bitcast(I32)
                    P.op("dve", lambda e: e.tensor_scalar(out=pab[:, 0], in0=bestI, scalar1=c4I[:, 0:1], scalar2=None, op0=ALU.arith_shift_right), reads=TK + ["posI"], writes=["topk"])
                    P.op("dve", lambda e: e.tensor_scalar(out=pab[:, 0], in0=pab[:, 0], scalar1=c15I[:, 0:1], scalar2=None, op0=ALU.bitwise_and), reads=TK + ["posI"], writes=["topk"])
                    P.op("dve", lambda e: e.tensor_scalar(out=pab[:, 1], in0=bestI, scalar1=c15I[:, 0:1], scalar2=None, op0=ALU.bitwise_and), reads=TK + ["posI"], writes=["topk"])
                    P.op("dve", lambda e: e.tensor_copy(out=pabf[:], in_=pab[:]), reads=TK, writes=["topk"])
                    yield
                    e3 = eidf[:].rearrange("p (h k) -> p h k", h=8)
                    for n_ in range(2):
                        P.op("dve", (lambda n_: lambda e: e.tensor_tensor(out=oh[:], in0=pabf[:, n_].unsqueeze(3).to_broadcast([128, 8, 16, 16]),
                                                                          in1=iota16[:].unsqueeze(1).unsqueeze(1).to_broadcast([128, 8, 16, 16]), op=ALU.is_equal))(n_), reads=TK + ["posI"], writes=["topk"])
                        P.op("dve", (lambda n_: lambda e: e.tensor_tensor(out=oh[:], in0=oh[:], in1=ix4[:, :, n_, :].unsqueeze(2).to_broadcast([128, 8, 16, 16]), op=ALU.mult))(n_), reads=TK, writes=["topk"])
                        P.op("dve", (lambda n_: lambda e: e.tensor_reduce(out=(e3 if n_ == 0 else e3b[:]), in_=oh[:], axis=AX.X, op=ALU.add))(n_), reads=TK, writes=["topk"])
                        yield
                    P.op("dve", lambda e: e.tensor_tensor(out=e3, in0=e3, in1=e3b[:], op=ALU.add), reads=TK, writes=["topk"])
                    P.op("dve", lambda e: e.tensor_scalar(out=eidf[:], in0=eidf[:], scalar1=0.0, scalar2=16383.0, op0=ALU.max, op1=ALU.min), reads=TK, writes=["topk"])
                    P.op("dve", lambda e: e.tensor_copy(out=eid[b][:], in_=eidf[:]), reads=TK, writes=[ek])
                    yield

                def consume(i, fgen):
                    b = i % NB; t0 = i * 128
                    x1k, h2k, ek, gk = "x1_%d" % b, "h2_%d" % b, "eid%d" % b, "gate%d" % b
                    ngrp = 128 // GJ
                    pend = None

                    def finish_group(g, bufs):
                        j0 = g * GJ
                        if "nofin" in KD:
                            return
                        P.op("act", lambda e: e.activation(out=gl_[:, j0:j0 + GJ], in_=aD[:, j0:j0 + GJ], func=AF.Gelu_apprx_tanh), reads=["aD%d_%d" % (g, q) for q in range(GJ)], writes=["gl_%d" % g])
                        for jj in range(GJ):
                            P.op("act", (lambda jj: lambda e: e.mul(out=wD[:, j0 + jj:j0 + jj + 1], in_=gl_[:, j0 + jj:j0 + jj + 1], mul=gate[b][:, j0 + jj:j0 + jj + 1]))(jj),
                                 reads=["gl_%d" % g, gk], writes=["wDw%d" % g])
                        for jj in range(GJ):
                            j = j0 + jj; gi, ugk = bufs[jj]
                            di, dk = dgr.next()
                            P.op("act", (lambda di, j: lambda e: e.activation(out=dg[di][:], in_=identb[:], func=AF.Copy, scale=wD[:, j:j + 1]))(di, j), reads=["wDw%d" % g, "identb"], writes=[dk])
                            for hf in range(2):
                                if "nov" in KD and j not in (0, 127):
                                    continue
                                P.op("pe", (lambda di, gi, hf, j: lambda e: e.matmul(pacc[hf][:], lhsT=dg[di][:], rhs=UVg[gi][:, 1, hf * 512:(hf + 1) * 512], start=(j == 0), stop=(j == 127)))(di, gi, hf, j),
                                     reads=[dk, ugk], writes=["pacc%d" % hf])

                    for g in range(ngrp):
                        bufs = []
                        for jj in range(GJ):
                            j = g * GJ + jj
                            gi = uv_i[0] % NG; uv_i[0] += 1; ugk = "UVg%d" % gi
                            bufs.append((gi, ugk))
                            if "nog" in KD:
                                P.op("pool", (lambda gi: lambda e: e.memset(UVg[gi][:], 1.0))(gi), reads=[ek], writes=[ugk])
                            else:
                                P.dma("pool", ugk, (lambda gi, j: lambda e: e.indirect_dma_start(out=UVg[gi][:].rearrange("p a d -> p (a d)"), out_offset=None, in_=uvb_d,
                                                                                               in_offset=bass.IndirectOffsetOnAxis(ap=eid[b][:, j:j + 1], axis=0)))(gi, j), reads=[ek], writes=[ugk])
                        for jj in range(GJ):
                            j = g * GJ + jj; gi, ugk = bufs[jj]
                            if "noa" in KD:
                                continue
                            ji, jkey_ = jer.next()
                            jE = junkE3[ji]
                            if jj < KAR:
                                P.op("dve", (lambda gi, jE: lambda e: e.tensor_tensor(out=jE[:], in0=UVg[gi][:, 0, :], in1=h2b2[b][:], op=ALU.mult))(gi, jE),
                                     reads=[ugk, "h2b%d" % b], writes=[jkey_])
                                P.op("act", (lambda j, jE: lambda e: e.activation(out=jE[:], in_=jE[:], func=AF.Copy, accum_out=aD[:, j:j + 1]))(j, jE),
                                     reads=[jkey_], writes=[jkey_, "aD%d_%d" % (g, jj)])
                            else:
                                P.op("dve", (lambda gi, j, jE: lambda e: e.scalar_tensor_tensor(out=jE[:], in0=UVg[gi][:, 0, :], scalar=1.0, in1=h2[b][:], op0=ALU.mult, op1=ALU.mult, accum_out=aD[:, j:j + 1]))(gi, j, jE),
                                     reads=[ugk, h2k], writes=["aD%d_%d" % (g, jj), jkey_])
                        finish_group(g, bufs)
                        if fgen is not None:
                            for _ in range(FY):
                                next(fgen, None)
                    if fgen is not None:
                        for _ in fgen:
                            pass
                    if "dump23" in KD and i == 23:
                        for nm, src_t in (("d_x1", x1[b]), ("d_wD", wD), ("d_aD", aD), ("d_gate", gate[b]), ("d_h2", h2[b])):
                            dd = nc.dram_tensor(nm, list(src_t[:].shape), F32, kind="ExternalOutput").ap()
                            P.dma("sp", "dump", (lambda dd, src_t: lambda e: e.dma_start(out=dd, in_=src_t[:]))(dd, src_t), reads=[x1k, gk, h2k] + ["wDw%d" % q for q in range(32)] + ["aD%d" % q for q in range(32)], writes=["dumpo"])
                        dd2 = nc.dram_tensor("d_eid", [128, 128], I32, kind="ExternalOutput").ap()
                        P.dma("sp", "dump", lambda e: e.dma_start(out=dd2, in_=eid[b][:]), reads=[ek], writes=["dumpo"])
                    if "notail" in KD:
                        return
                    for hf in range(2):
                        P.op("dve", (lambda hf: lambda e: e.tensor_tensor(out=tmpD[:, hf * 512:(hf + 1) * 512], in0=pacc[hf][:], in1=gate2B[:, hf * 512:(hf + 1) * 512], op=ALU.mult))(hf),
                             reads=["pacc%d" % hf, "modB"], writes=["tmpD"])
                    P.op("dve", lambda e: e.tensor_tensor(out=x1[b][:], in0=tmpD[:], in1=x1[b][:], op=ALU.add), reads=["tmpD", x1k], writes=[x1k])
                    rstd_chain(x1[b][:], x1k, sm2[:, 0:4], "smE", junkD, "junkD")
                    P.op("dve", lambda e: e.scalar_tensor_tensor(out=tmpD[:], in0=x1[b][:], scalar=sm2[:, 3:4], in1=fgB[:], op0=ALU.mult, op1=ALU.mult), reads=[x1k, "smE", "fgB"], writes=["tmpD"])
                    P.dma("sp", "outst", lambda e: e.dma_start(out=out[t0:t0 + 128, :], in_=tmpD[:]), reads=["tmpD"], writes=["out"])

                for _ in front(0):
                    pass
                for i in range(NT):
                    if "noc" in KD:
                        break
                    if "noil" in KD:
                        consume(i, None)
                        if i + 1 < NT:
                            for _ in front(i + 1):
                                pass
                        continue
                    consume(i, front(i + 1) if i + 1 < NT else None)
        return finish(nc, P, out)
    return nc


def finish(nc, P, out):
    P.barrier()
    P.close()
    return nc


def core_inputs(inp, b):
    f = lambda a: np.ascontiguousarray(a, dtype=np.float32)
    return {
        "x": f(inp["x"][b]), "c": f(inp["c"][b:b + 1]), "ctx": f(inp["ctx"][b]), "c_ctx": f(inp["c_ctx"].reshape(1, D)),
        "w_mod": f(inp["w_mod"][0]), "b_mod": f(inp["b_mod"][0].reshape(1, -1)),
        "norm1_g": f(inp["norm1_g"][0].reshape(1, D)), "norm2_g": f(inp["norm2_g"][0].reshape(1, D)),
        "final_g": f(inp["final_g"].reshape(1, D)), "w_in": f(inp["w_in"][0]),
        "ssm_a_re": f(inp["ssm_a_re"][0]), "ssm_a_im": f(inp["ssm_a_im"][0]), "ssm_log_dt": f(inp["ssm_log_dt"][0]),
        "ssm_b_re": f(inp["ssm_b_re"][0]), "ssm_b_im": f(inp["ssm_b_im"][0]),
        "ssm_c_re": f(inp["ssm_c_re"][0]), "ssm_c_im": f(inp["ssm_c_im"][0]),
        "ssm_d": f(inp["ssm_d"][0].reshape(512, 1)), "w_glu": f(inp["w_glu"][0]), "b_glu": f(inp["b_glu"][0].reshape(512, 1)),
        "w_branch_a": f(inp["w_branch_a"][0]), "w_branch_b": f(inp["w_branch_b"][0]), "na_rpb": f(inp["na_rpb"][0]),
        "w_out": f(inp["w_out"][0]), "peer_w_q": f(inp["peer_w_q"][0]), "peer_subkeys": f(inp["peer_subkeys"][0]),
        "peer_uv": f(np.concatenate([inp["peer_u"][0], inp["peer_v"][0]], axis=1)),
    }


def kernel(**inputs):
    nc = build()
    in_maps = [core_inputs(inputs, b) for b in range(8)]
    res = run_bass_kernel_spmd(nc, in_maps, core_ids=list(range(8)))
    return np.stack([np.asarray(r["out"], dtype=np.float32) for r in res.results], axis=0)
```

```python
import math
import numpy as np
import concourse.bass as bass
import concourse.mybir as mybir
from concourse.bass_utils import run_bass_kernel_spmd

F32 = mybir.dt.float32
BF16 = mybir.dt.bfloat16
I32 = mybir.dt.int32
U32 = mybir.dt.uint32
AF = mybir.ActivationFunctionType
ALU = mybir.AluOpType
AX = mybir.AxisListType


class _Op:
    __slots__ = ("eng", "fn", "deps", "seq", "is_dma", "semkey", "signal", "count", "waits")

    def __init__(self, eng, fn, seq, is_dma=False, semkey=None):
        self.eng = eng
        self.fn = fn
        self.deps = []
        self.seq = seq
        self.is_dma = is_dma
        self.semkey = semkey
        self.signal = is_dma
        self.count = 0
        self.waits = []


class Prog:
    ENGS = ("pe", "dve", "act", "pool", "sp")

    def __init__(self, nc):
        self.nc = nc
        self.ops = []
        self.writer = {}
        self.readers = {}
        import os
        self.same_sync = os.environ.get("KSAME", "1") == "1"

    def _add(self, op, reads, writes):
        deps = []
        for r in reads:
            w = self.writer.get(r)
            if w is not None:
                deps.append(w)
        for w_ in writes:
            w = self.writer.get(w_)
            if w is not None:
                deps.append(w)
            deps.extend(self.readers.get(w_, ()))
        op.deps = [d for d in set(deps) if d is not op]
        for r in reads:
            self.readers.setdefault(r, []).append(op)
        for w_ in writes:
            self.writer[w_] = op
            self.readers[w_] = []
        self.ops.append(op)
        return op

    def op(self, eng, fn, reads=(), writes=()):
        return self._add(_Op(eng, fn, len(self.ops)), reads, writes)

    def dma(self, eng, semkey, fn, reads=(), writes=()):
        return self._add(_Op(eng, fn, len(self.ops), True, semkey), reads, writes)

    def emit(self):
        import bisect
        from contextlib import ExitStack
        nc = self.nc
        if not hasattr(self, "_st"):
            self._st = ExitStack(); self._sem = {}; self._cnt = {}; self._hist = {}
            self._seen = {e: {} for e in self.ENGS}; self._done = 0
        ops = self.ops[self._done:]
        self._done = len(self.ops)
        if not ops:
            return

        def same(d, o):
            return d.eng == o.eng and not o.is_dma and (d.eng == "pe" or not self.same_sync)
        for o in ops:
            for d in o.deps:
                if d.is_dma or same(d, o):
                    continue
                assert d.count == 0 or d.signal, "dependency on an already-emitted non-signalling op"
                d.signal = True
        for o in ops:
            if not o.signal:
                continue
            k = ("dma", o.semkey) if o.is_dma else ("eng", o.eng)
            if k not in self._sem:
                self._sem[k] = self._st.enter_context(nc.semaphore("s%d_%s" % (len(self._sem), str(k[1]).replace(" ", ""))))
                self._cnt[k] = 0
            self._cnt[k] += 1
            o.count = self._cnt[k]
            if o.is_dma:
                self._hist.setdefault(o.semkey, []).append(o.seq)
        for o in ops:
            need = {}
            for d in o.deps:
                if d.is_dma:
                    k = ("dma", d.semkey)
                    v = 16 * bisect.bisect_left(self._hist[d.semkey], o.seq)
                else:
                    if same(d, o):
                        continue
                    k = ("eng", d.eng)
                    v = d.count
                if need.get(k, 0) < v:
                    need[k] = v
            sn = self._seen[o.eng]
            o.waits = []
            for k, v in need.items():
                if sn.get(k, 0) < v:
                    sn[k] = v
                    o.waits.append((k, v))
        self.n_sems = len(self._sem)
        sem = self._sem
        with nc.Block() as block:
            per = {e: [o for o in ops if o.eng == e] for e in self.ENGS}

            def run(engobj, lst):
                for o in lst:
                    for k, v in o.waits:
                        engobj.wait_ge(sem[k], v)
                    ins = o.fn(engobj)
                    if o.signal:
                        k = ("dma", o.semkey) if o.is_dma else ("eng", o.eng)
                        ins.then_inc(sem[k], 16 if o.is_dma else 1)
                    o.fn = None

            @block.tensor
            def _(e):
                run(e, per["pe"])

            @block.vector
            def _(e):
                run(e, per["dve"])

            @block.scalar
            def _(e):
                run(e, per["act"])

            @block.gpsimd
            def _(e):
                run(e, per["pool"])

            @block.sync
            def _(e):
                run(e, per["sp"])

    def close(self):
        self.emit()
        if hasattr(self, "_st"):
            self._st.close()

    def barrier(self, flush=True):
        start = getattr(self, "_done", 0)
        last = {}
        dmas = {}
        for o in self.ops[start:]:
            if o.is_dma:
                dmas[o.semkey] = o
            else:
                last[o.eng] = o
        for e, o in getattr(self, "_bar", {}).items():
            last.setdefault(e, o)
        deps = list(last.values()) + list(dmas.values())
        self._bar = {}
        for e in self.ENGS:
            o = _Op(e, lambda eng: eng.nop(), len(self.ops))
            o.deps = [d for d in deps]
            o.signal = True
            self.ops.append(o)
            self._bar[e] = o
        self.writer = {}
        self.readers = {}
        if flush:
            self.emit()


class Rot:
    def __init__(self, name, n):
        self.name, self.n, self.i = name, n, -1

    def next(self):
        self.i = (self.i + 1) % self.n
        return self.i, "%s%d" % (self.name, self.i)


D = 1024
SEQ = 4096
CTX = 256
NTOK = SEQ + CTX
EPS = 1e-6


def build(stage=99, debug=False):
    import os
    KD = os.environ.get("KDBG", "")
    from contextlib import ExitStack
    nc = bass.Bass("TRN2", target_bir_lowering=False)
    P = Prog(nc)

    def din(name, shape, dt=F32):
        return nc.dram_tensor(name, shape, dt, kind="ExternalInput").ap()

    def dscr(name, shape, dt):
        return nc.dram_tensor(name, shape, dt, kind=("ExternalOutput" if debug else "Internal")).ap()

    x = din("x", [SEQ, D]); c = din("c", [1, D]); ctx = din("ctx", [CTX, D]); c_ctx = din("c_ctx", [1, D])
    w_mod = din("w_mod", [D, 6 * D]); b_mod = din("b_mod", [1, 6 * D])
    norm1_g = din("norm1_g", [1, D]); norm2_g = din("norm2_g", [1, D]); final_g = din("final_g", [1, D])
    w_in = din("w_in", [D, 4096])
    a_re = din("ssm_a_re", [2, 32, 64]); a_im = din("ssm_a_im", [2, 32, 64]); log_dt = din("ssm_log_dt", [2, 32])
    b_re = din("ssm_b_re", [2, 32, 64, 16]); b_im = din("ssm_b_im", [2, 32, 64, 16])
    c_re = din("ssm_c_re", [2, 32, 16, 64]); c_im = din("ssm_c_im", [2, 32, 16, 64])
    ssm_d = din("ssm_d", [512, 1]); w_glu = din("w_glu", [512, 512]); b_glu = din("b_glu", [512, 1])
    w_ba = din("w_branch_a", [512, D]); w_bb = din("w_branch_b", [512, D]); rpb = din("na_rpb", [8, 15, 31])
    w_out = din("w_out", [D, D]); w_q = din("peer_w_q", [D, 2048]); subkeys = din("peer_subkeys", [2, 128, 128])
    peer_uv = din("peer_uv", [16384, 2 * D])
    out = nc.dram_tensor("out", [SEQ, D], F32, kind="ExternalOutput").ap()

    uT_d = dscr("uT_d", [512, NTOK], F32)
    kT_d = dscr("kT_d", [512, NTOK], BF16)
    qT_d = dscr("qT_d", [512, SEQ], BF16)
    v_d = dscr("v_d", [NTOK, 512], BF16)
    gT_d = dscr("gT_d", [2048, SEQ], BF16)
    baT_d = dscr("baT_d", [D, SEQ], BF16)
    mgT_d = dscr("mgT_d", [D, SEQ], BF16)
    uvb_d = nc.dram_tensor("uvb_d", [16384, 2 * D], BF16, kind=("ExternalOutput" if (debug and stage == 3.5) else "Internal")).ap()

    from contextlib import contextmanager

    @contextmanager
    def phase():
        stk = ExitStack()
        try:
            yield stk
            P.barrier()
        finally:
            stk.close()

    top = ExitStack()
    with top:
        def sbuf(st, n, s, d=F32):
            return st.enter_context(nc.sbuf_tensor(n, s, d))

        def psum(st, n, s, d=F32):
            return st.enter_context(nc.psum_tensor(n, s, d))

        identf = sbuf(top, "identf", [128, 128])
        identb = sbuf(top, "identb", [128, 128], BF16)
        modB = sbuf(top, "modB", [128, 6 * D])
        P.op("pool", lambda e: e.iota(identf[:], pattern=[[1, 128]], base=0, channel_multiplier=-1,
                                      allow_small_or_imprecise_dtypes=True), writes=["identf"])
        P.op("dve", lambda e: e.tensor_single_scalar(out=identf[:], in_=identf[:], scalar=0.0, op=ALU.is_equal),
             reads=["identf"], writes=["identf"])
        P.op("dve", lambda e: e.tensor_copy(out=identb[:], in_=identf[:]), reads=["identf"], writes=["identb"])

        with phase() as st:
            modcB = sbuf(st, "modcB", [128, 2 * D])
            wm = [sbuf(st, "wm%d" % i, [128, 8, 512]) for i in range(2)]
            w_in_sb = sbuf(st, "w_in_sb", [128, 8, 4096], BF16)
            st0 = ExitStack()
            cc = sbuf(st0, "cc", [128, 2, 8]); sc = sbuf(st0, "sc", [128, 2, 8]); scB = sbuf(st0, "scB", [128, 2, 8, 128])
            bmB = sbuf(st0, "bmB", [128, 6 * D]); gB = sbuf(st0, "gB", [128, 2, D])
            pmod = [psum(st0, "pmod%d" % i, [128, 512]) for i in range(2)]

            P.dma("sp", "c0", lambda e: e.dma_start(out=cc[:, 0, :], in_=c.rearrange("o (k p) -> p (o k)", p=128),
                                                    allow_slow_non_contiguous=True), writes=["cc"])
            P.dma("sp", "c0", lambda e: e.dma_start(out=cc[:, 1, :], in_=c_ctx.rearrange("o (k p) -> p (o k)", p=128),
                                                    allow_slow_non_contiguous=True), writes=["cc"])
            P.dma("act", "c1", lambda e: e.dma_start(out=bmB[:], in_=b_mod.to_broadcast([128, 6 * D])), writes=["bmB"])
            P.dma("act", "c1", lambda e: e.dma_start(out=gB[:, 0, :], in_=norm1_g.to_broadcast([128, D])), writes=["gB"])
            P.dma("act", "c1", lambda e: e.dma_start(out=gB[:, 1, :], in_=norm2_g.to_broadcast([128, D])), writes=["gB"])
            P.op("act", lambda e: e.activation(out=sc[:], in_=cc[:], func=AF.Silu), reads=["cc"], writes=["sc"])
            P.op("dve", lambda e: e.tensor_copy(out=scB[:], in_=sc[:].unsqueeze(3).to_broadcast([128, 2, 8, 128])),
                 reads=["sc"], writes=["scB"])
            w_mod_v = w_mod.rearrange("(k p) n -> p k n", p=128)
            for cch in range(12):
                bi = cch % 2
                P.dma("sp", "wm%d" % bi, (lambda bi, cch: lambda e: e.dma_start(out=wm[bi][:], in_=w_mod_v[:, :, cch * 512:(cch + 1) * 512]))(bi, cch),
                      writes=["wm%d" % bi])
                for which in range(2 if cch < 4 else 1):
                    for k in range(8):
                        P.op("pe", (lambda bi, which, k: lambda e: e.matmul(pmod[which][:], lhsT=scB[:, which, k, :], rhs=wm[bi][:, k, :],
                                                                            start=(k == 0), stop=(k == 7)))(bi, which, k),
                             reads=["scB", "wm%d" % bi], writes=["pmod%d" % which])
                    dst = modB if which == 0 else modcB
                    P.op("dve", (lambda dst, which, cch: lambda e: e.tensor_tensor(out=dst[:, cch * 512:(cch + 1) * 512], in0=pmod[which][:],
                                                                                   in1=bmB[:, cch * 512:(cch + 1) * 512], op=ALU.add))(dst, which, cch),
                         reads=["pmod%d" % which, "bmB"], writes=["modB" if which == 0 else "modcB"])
            for dst, key, off, gi in ((modB, "modB", D, 0), (modcB, "modcB", D, 0), (modB, "modB", 4 * D, 1)):
                P.op("dve", (lambda dst, off, gi: lambda e: e.scalar_tensor_tensor(out=dst[:, off:off + D], in0=dst[:, off:off + D], scalar=1.0,
                                                                                  in1=gB[:, gi, :], op0=ALU.add, op1=ALU.mult))(dst, off, gi),
                     reads=[key, "gB"], writes=[key])

            P.barrier()
            st0.close()
            w_in_v = w_in.rearrange("(k p) n -> p k n", p=128)
            for cch in range(8):
                bi = cch % 2
                P.dma("sp", "wm%d" % bi, (lambda bi, cch: lambda e: e.dma_start(out=wm[bi][:], in_=w_in_v[:, :, cch * 512:(cch + 1) * 512]))(bi, cch),
                      reads=[], writes=["wm%d" % bi])
                eng = ("pool", "dve")[cch % 2]
                P.op(eng, (lambda bi, cch: lambda e: e.tensor_copy(out=w_in_sb[:, :, cch * 512:(cch + 1) * 512], in_=wm[bi][:]))(bi, cch),
                     reads=["wm%d" % bi], writes=["w_in_sb"])

            xt = [sbuf(st, "xt%d" % i, [128, D]) for i in range(3)]; xr = Rot("xt", 3)
            junk = sbuf(st, "junkA", [128, D]); tmpA = sbuf(st, "tmpA", [128, D])
            ss = [sbuf(st, "ss%d" % i, [128, 4]) for i in range(2)]; ssr = Rot("ss", 2)
            hxb = [sbuf(st, "hxb%d" % i, [128, D], BF16) for i in range(2)]; hr = Rot("hxb", 2)
            hxT = [sbuf(st, "hxT%d" % i, [128, 8, 512], BF16) for i in range(2)]; hTr = Rot("hxT", 2)
            st_u = sbuf(st, "st_u", [128, 4, 512]); st_k = sbuf(st, "st_k", [128, 4, 512], BF16)
            st_q = sbuf(st, "st_q", [128, 4, 512], BF16); st_g = sbuf(st, "st_g", [128, 16, 512], BF16)
            st_v = sbuf(st, "st_v", [128, 4, 512], BF16)
            tp = [psum(st, "tpA%d" % i, [128, 8, 128], BF16) for i in range(2)]; tpr = Rot("tpA", 2)
            pj = [psum(st, "pj%d" % i, [128, 512]) for i in range(4)]; pjr = Rot("pj", 4)
            evac_i = [0]

            def evac(dst_ap, src_ap, reads, writes, func=None):
                if func is not None:
                    P.op("act", lambda e: e.activation(out=dst_ap, in_=src_ap, func=func), reads, writes)
                    return
                evac_i[0] += 1
                if evac_i[0] % 2:
                    P.op("act", lambda e: e.copy(out=dst_ap, in_=src_ap), reads, writes)
                else:
                    P.op("dve", lambda e: e.tensor_copy(out=dst_ap, in_=src_ap), reads, writes)

            for i_ in range(2):
                P.op("pool", (lambda i_: lambda e: e.memset(hxT[i_][:], 0.0))(i_), writes=["hxT%d" % i_])
            chunks = [("ctx", 0, 256)] + [("lat", i * 512, 512) for i in range(8)]
            for kind, t0, n in chunks:
                src = ctx if kind == "ctx" else x
                mB, mkey = (modcB, "modcB") if kind == "ctx" else (modB, "modB")
                col0 = t0 if kind == "ctx" else CTX + t0
                hi, hkey = hTr.next()
                for t in range(n // 128):
                    xi, xkey = xr.next()
                    si, skey = ssr.next()
                    bi, bkey = hr.next()
                    pi, pkey = tpr.next()
                    r0 = t0 + t * 128
                    P.dma("sp", xkey, (lambda xi, r0, src: lambda e: e.dma_start(out=xt[xi][:], in_=src[r0:r0 + 128, :]))(xi, r0, src), writes=[xkey])
                    P.op("act", (lambda xi, si: lambda e: e.activation(out=junk[:], in_=xt[xi][:], func=AF.Square, accum_out=ss[si][:, 0:1]))(xi, si),
                         reads=[xkey], writes=["junkA", skey])
                    P.op("dve", (lambda si: lambda e: e.tensor_scalar(out=ss[si][:, 1:2], in0=ss[si][:, 0:1], scalar1=1.0 / D, scalar2=EPS,
                                                                      op0=ALU.mult, op1=ALU.add))(si), reads=[skey], writes=[skey])
                    P.op("act", (lambda si: lambda e: e.sqrt(out=ss[si][:, 2:3], in_=ss[si][:, 1:2]))(si), reads=[skey], writes=[skey])
                    P.op("dve", (lambda si: lambda e: e.reciprocal(out=ss[si][:, 3:4], in_=ss[si][:, 2:3]))(si), reads=[skey], writes=[skey])
                    P.op("dve", (lambda xi, si, mB: lambda e: e.scalar_tensor_tensor(out=tmpA[:], in0=xt[xi][:], scalar=ss[si][:, 3:4], in1=mB[:, D:2 * D],
                                                                                     op0=ALU.mult, op1=ALU.mult))(xi, si, mB),
                         reads=[xkey, skey, mkey], writes=["tmpA"])
                    P.op("dve", (lambda bi, mB: lambda e: e.tensor_tensor(out=hxb[bi][:], in0=tmpA[:], in1=mB[:, 0:D], op=ALU.add))(bi, mB),
                         reads=["tmpA", mkey], writes=[bkey])
                    for k in range(8):
                        P.op("pe", (lambda pi, bi, k: lambda e: e.transpose(tp[pi][:, k, :], hxb[bi][:, k * 128:(k + 1) * 128], identb[:]))(pi, bi, k),
                             reads=[bkey, "identb"], writes=[pkey])
                    P.op("act", (lambda hi, pi, t: lambda e: e.copy(out=hxT[hi][:, :, t * 128:(t + 1) * 128], in_=tp[pi][:]))(hi, pi, t),
                         reads=[pkey], writes=[hkey])
                cts = list(range(0, 8)) + (list(range(12, 32)) if kind == "lat" else [])
                for ct in cts:
                    qi, qkey = pjr.next()
                    for k in range(8):
                        P.op("pe", (lambda qi, hi, k, ct: lambda e: e.matmul(pj[qi][:, 0:n], lhsT=w_in_sb[:, k, ct * 128:(ct + 1) * 128], rhs=hxT[hi][:, k, 0:n],
                                                                             start=(k == 0), stop=(k == 7)))(qi, hi, k, ct),
                             reads=["w_in_sb", hkey], writes=[qkey])
                    if ct < 4:
                        evac(st_u[:, ct, 0:n], pj[qi][:, 0:n], [qkey], ["st_u"])
                    elif ct < 8:
                        evac(st_k[:, ct - 4, 0:n], pj[qi][:, 0:n], [qkey], ["st_k"])
                    elif ct < 16:
                        evac(st_q[:, ct - 12, 0:n], pj[qi][:, 0:n], [qkey], ["st_q"])
                    else:
                        evac(st_g[:, ct - 16, 0:n], pj[qi][:, 0:n], [qkey], ["st_g"], func=AF.Sigmoid)
                P.dma("sp", "stu", (lambda col0, n: lambda e: e.dma_start(out=uT_d.rearrange("(t p) n -> p t n", p=128)[:, :, col0:col0 + n], in_=st_u[:, :, 0:n]))(col0, n),
                      reads=["st_u"], writes=["uT_d"])
                P.dma("sp", "stk", (lambda col0, n: lambda e: e.dma_start(out=kT_d.rearrange("(t p) n -> p t n", p=128)[:, :, col0:col0 + n], in_=st_k[:, :, 0:n]))(col0, n),
                      reads=["st_k"], writes=["kT_d"])
                if kind == "lat":
                    P.dma("sp", "stq", (lambda t0: lambda e: e.dma_start(out=qT_d.rearrange("(t p) n -> p t n", p=128)[:, :, t0:t0 + 512], in_=st_q[:]))(t0),
                          reads=["st_q"], writes=["qT_d"])
                    P.dma("sp", "stg", (lambda t0: lambda e: e.dma_start(out=gT_d.rearrange("(t p) n -> p t n", p=128)[:, :, t0:t0 + 512], in_=st_g[:]))(t0),
                          reads=["st_g"], writes=["gT_d"])
                for t in range(n // 128):
                    qi, qkey = pjr.next()
                    for k in range(8):
                        P.op("pe", (lambda qi, hi, k, t: lambda e: e.matmul(pj[qi][:], lhsT=hxT[hi][:, k, t * 128:(t + 1) * 128], rhs=w_in_sb[:, k, 1024:1536],
                                                                            start=(k == 0), stop=(k == 7)))(qi, hi, k, t),
                             reads=["w_in_sb", hkey], writes=[qkey])
                    evac(st_v[:, t, :], pj[qi][:], [qkey], ["st_v"])
                nt = n // 128
                P.dma("sp", "stv", (lambda col0, nt: lambda e: e.dma_start(out=v_d[col0:col0 + nt * 128, :].rearrange("(t p) n -> p t n", p=128), in_=st_v[:, 0:nt, :]))(col0, nt),
                      reads=["st_v"], writes=["v_d"])
        P.barrier()
        if stage <= 1:
            return finish(nc, P, out)

        yT_d = dscr("yT_d", [512, SEQ], F32) if debug else None
        TWO_PI = 2.0 * math.pi
        with ExitStack() as stB:
            zT = sbuf(stB, "zT", [128, 4, SEQ], BF16)
            with phase() as st:
                def t32(n):
                    return sbuf(st, n, [128, 32])
                are, aim, ldt = t32("are"), t32("aim"), t32("ldt")
                Bn = [sbuf(st, "Bn%d" % i, [128, 32, 16]) for i in range(2)]
                bb = [sbuf(st, "bb%d" % i, [128, 32, 16]) for i in range(2)]
                tmpb = sbuf(st, "tmpb", [128, 32, 16])
                Cn2 = [sbuf(st, "Cn2%d" % i, [128, 8, 2, 64]) for i in range(2)]
                dsk = sbuf(st, "dsk", [128, 4])
                maskf = sbuf(st, "maskf", [128, 4, 2]); mask2 = sbuf(st, "mask2", [128, 4, 2])
                pwr = sbuf(st, "pwr", [128, 13, 32]); pwi = sbuf(st, "pwi", [128, 13, 32]); npwi = sbuf(st, "npwi", [128, 13, 32])
                kint = sbuf(st, "kint", [128, 32], I32)
                names = ["dt", "er", "th", "mag", "kf", "rr", "half", "sn", "ah", "cq", "sinr", "cosr", "nre", "den", "rden",
                         "fre", "fim", "t1", "t2"]
                T = {n: t32("p_" + n) for n in names}
                uT_sb = [sbuf(st, "uT_sb%d" % i, [128, NTOK]) for i in range(1)]
                PL = [sbuf(st, "PL%d" % i, [128, 2, NTOK]) for i in range(2)]
                yT = sbuf(st, "yT", [128, SEQ])
                Z = [sbuf(st, "Z%d" % i, [128, 2, 128]) for i in range(2)]
                Zc = [sbuf(st, "Zc%d" % i, [128, 2, 128]) for i in range(2)]
                LB = [sbuf(st, "LB%d" % i, [128, 2, 128]) for i in range(2)]
                LC = [sbuf(st, "LC%d" % i, [128, 2, 128]) for i in range(2)]
                pz = [psum(st, "pz%d" % i, [128, 2, 128]) for i in range(2)]; pzr = Rot("pz", 2)
                pb = [psum(st, "pb%d" % i, [128, 512]) for i in range(3)]; pbr = Rot("pb", 3)
                py = [psum(st, "py%d" % i, [128, 512]) for i in range(2)]; pyr = Rot("py", 2)

                for gl in range(2):
                    sl = slice(gl * 64, (gl + 1) * 64)
                    for dst, srcp, key in ((are, a_re, "are"), (aim, a_im, "aim")):
                        P.dma("act", "pb0", (lambda dst, srcp, sl, gl: lambda e: e.dma_start(
                            out=dst[sl, :].rearrange("p (d g) -> p d g", d=2),
                            in_=srcp.rearrange("d (gp gl) p -> gl p d gp", gl=2)[gl], allow_slow_non_contiguous=True))(dst, srcp, sl, gl), writes=[key])
                    P.dma("act", "pb0", (lambda sl, gl: lambda e: e.dma_start(
                        out=ldt[sl, :].rearrange("p (d g) -> p d g", d=2),
                        in_=log_dt.rearrange("d (gp gl) -> gl d gp", gl=2)[gl:gl + 1].to_broadcast([64, 2, 16]), allow_slow_non_contiguous=True))(sl, gl), writes=["ldt"])
                    for i, srcp in enumerate((b_re, b_im)):
                        P.dma("act", "pb0", (lambda i, srcp, sl, gl: lambda e: e.dma_start(
                            out=Bn[i][sl].rearrange("p (d g) h -> p d g h", d=2),
                            in_=srcp.rearrange("d (gp gl) p h -> gl p d gp h", gl=2)[gl]))(i, srcp, sl, gl), writes=["Bn%d" % i])
                for i, srcp in enumerate((c_re, c_im)):
                    for j in range(2):
                        P.dma("act", "pb0", (lambda i, srcp, j: lambda e: e.dma_start(
                            out=Cn2[i][:, :, j, :].rearrange("p (d u) q -> p d u q", d=2),
                            in_=srcp.rearrange("d (ut g8) h p -> (g8 h) d ut p", g8=8)))(i, srcp, j), writes=["Cn2%d" % i])
                P.dma("act", "pb0", lambda e: e.dma_start(out=dsk[:], in_=ssm_d.rearrange("(ut p) o -> p (ut o)", p=128), allow_slow_non_contiguous=True), writes=["dsk"])
                P.op("pool", lambda e: e.iota(maskf[:], pattern=[[-32, 4], [-16, 2]], base=0, channel_multiplier=1, allow_small_or_imprecise_dtypes=True), writes=["maskf"])
                P.op("dve", lambda e: e.tensor_single_scalar(out=mask2[:], in_=maskf[:], scalar=0.0, op=ALU.is_ge), reads=["maskf"], writes=["mask2"])
                P.op("dve", lambda e: e.tensor_single_scalar(out=maskf[:], in_=maskf[:], scalar=16.0, op=ALU.is_lt), reads=["maskf", "mask2"], writes=["maskf"])
                P.op("dve", lambda e: e.tensor_tensor(out=maskf[:], in0=maskf[:], in1=mask2[:], op=ALU.mult), reads=["maskf", "mask2"], writes=["maskf"])

                PK = ["are", "aim", "ldt", "Bn0", "Bn1", "prm"]

                def dve(fn):
                    P.op("dve", fn, reads=PK, writes=["prm"])

                def act(fn):
                    P.op("act", fn, reads=PK, writes=["prm"])
                act(lambda e: e.activation(out=T["dt"][:], in_=ldt[:], func=AF.Exp))
                dve(lambda e: e.tensor_tensor(out=T["er"][:], in0=are[:], in1=T["dt"][:], op=ALU.mult))
                dve(lambda e: e.tensor_tensor(out=T["th"][:], in0=aim[:], in1=T["dt"][:], op=ALU.mult))
                act(lambda e: e.activation(out=T["mag"][:], in_=T["er"][:], func=AF.Exp))
                dve(lambda e: e.tensor_single_scalar(out=T["kf"][:], in_=T["th"][:], scalar=1.0 / TWO_PI, op=ALU.mult))
                dve(lambda e: e.tensor_copy(out=kint[:], in_=T["kf"][:]))
                dve(lambda e: e.tensor_copy(out=T["kf"][:], in_=kint[:]))
                dve(lambda e: e.scalar_tensor_tensor(out=T["rr"][:], in0=T["kf"][:], scalar=-TWO_PI, in1=T["th"][:], op0=ALU.mult, op1=ALU.add))
                dve(lambda e: e.tensor_single_scalar(out=T["half"][:], in_=T["rr"][:], scalar=0.5, op=ALU.mult))
                act(lambda e: e.activation(out=T["ah"][:], in_=T["half"][:], func=AF.Abs))
                dve(lambda e: e.tensor_scalar(out=T["t1"][:], in0=T["ah"][:], scalar1=-1.0, scalar2=math.pi / 2, op0=ALU.mult, op1=ALU.add))
                act(lambda e: e.activation(out=T["sn"][:], in_=T["half"][:], func=AF.Sin))
                act(lambda e: e.activation(out=T["cq"][:], in_=T["t1"][:], func=AF.Sin))
                dve(lambda e: e.scalar_tensor_tensor(out=T["sinr"][:], in0=T["sn"][:], scalar=2.0, in1=T["cq"][:], op0=ALU.mult, op1=ALU.mult))
                dve(lambda e: e.scalar_tensor_tensor(out=T["t2"][:], in0=T["sn"][:], scalar=-2.0, in1=T["sn"][:], op0=ALU.mult, op1=ALU.mult))
                dve(lambda e: e.tensor_single_scalar(out=T["cosr"][:], in_=T["t2"][:], scalar=1.0, op=ALU.add))
                dve(lambda e: e.tensor_tensor(out=pwr[:, 0, :], in0=T["mag"][:], in1=T["cosr"][:], op=ALU.mult))
                dve(lambda e: e.tensor_tensor(out=pwi[:, 0, :], in0=T["mag"][:], in1=T["sinr"][:], op=ALU.mult))
                dve(lambda e: e.tensor_single_scalar(out=T["nre"][:], in_=pwr[:, 0, :], scalar=-1.0, op=ALU.add))
                dve(lambda e: e.tensor_tensor(out=T["den"][:], in0=are[:], in1=are[:], op=ALU.mult))
                dve(lambda e: e.tensor_tensor(out=T["t1"][:], in0=aim[:], in1=aim[:], op=ALU.mult))
                dve(lambda e: e.tensor_tensor(out=T["den"][:], in0=T["den"][:], in1=T["t1"][:], op=ALU.add))
                dve(lambda e: e.reciprocal(out=T["rden"][:], in_=T["den"][:]))
                dve(lambda e: e.tensor_tensor(out=T["t1"][:], in0=T["nre"][:], in1=are[:], op=ALU.mult))
                dve(lambda e: e.tensor_tensor(out=T["t2"][:], in0=pwi[:, 0, :], in1=aim[:], op=ALU.mult))
                dve(lambda e: e.tensor_tensor(out=T["t1"][:], in0=T["t1"][:], in1=T["t2"][:], op=ALU.add))
                dve(lambda e: e.tensor_tensor(out=T["fre"][:], in0=T["t1"][:], in1=T["rden"][:], op=ALU.mult))
                dve(lambda e: e.tensor_tensor(out=T["t1"][:], in0=pwi[:, 0, :], in1=are[:], op=ALU.mult))
                dve(lambda e: e.tensor_tensor(out=T["t2"][:], in0=T["nre"][:], in1=aim[:], op=ALU.mult))
                dve(lambda e: e.tensor_tensor(out=T["t1"][:], in0=T["t1"][:], in1=T["t2"][:], op=ALU.subtract))
                dve(lambda e: e.tensor_tensor(out=T["fim"][:], in0=T["t1"][:], in1=T["rden"][:], op=ALU.mult))
                fr = T["fre"][:].unsqueeze(2).to_broadcast([128, 32, 16]); fi = T["fim"][:].unsqueeze(2).to_broadcast([128, 32, 16])
                dve(lambda e: e.tensor_tensor(out=bb[0][:], in0=Bn[0][:], in1=fr, op=ALU.mult))
                dve(lambda e: e.tensor_tensor(out=tmpb[:], in0=Bn[1][:], in1=fi, op=ALU.mult))
                dve(lambda e: e.tensor_tensor(out=bb[0][:], in0=bb[0][:], in1=tmpb[:], op=ALU.subtract))
                dve(lambda e: e.tensor_tensor(out=bb[1][:], in0=Bn[1][:], in1=fr, op=ALU.mult))
                dve(lambda e: e.tensor_tensor(out=tmpb[:], in0=Bn[0][:], in1=fi, op=ALU.mult))
                dve(lambda e: e.tensor_tensor(out=bb[1][:], in0=bb[1][:], in1=tmpb[:], op=ALU.add))
                for k in range(12):
                    dve((lambda k: lambda e: e.tensor_tensor(out=T["t1"][:], in0=pwr[:, k, :], in1=pwr[:, k, :], op=ALU.mult))(k))
                    dve((lambda k: lambda e: e.tensor_tensor(out=T["t2"][:], in0=pwi[:, k, :], in1=pwi[:, k, :], op=ALU.mult))(k))
                    dve((lambda k: lambda e: e.tensor_tensor(out=pwr[:, k + 1, :], in0=T["t1"][:], in1=T["t2"][:], op=ALU.subtract))(k))
                    dve((lambda k: lambda e: e.scalar_tensor_tensor(out=pwi[:, k + 1, :], in0=pwr[:, k, :], scalar=2.0, in1=pwi[:, k, :], op0=ALU.mult, op1=ALU.mult))(k))
                dve(lambda e: e.tensor_single_scalar(out=npwi[:], in_=pwi[:], scalar=-1.0, op=ALU.mult))

                chain = {}

                def cmul_acc(hi_re, hi_im, lo_re, lo_im, k, u, key):
                    sr = pwr[:, k, u:u + 1]; si = pwi[:, k, u:u + 1]; nsi = npwi[:, k, u:u + 1]
                    prev = chain.get(key)
                    if prev is None:
                        prev = [w for w in (P.writer.get(key), P.writer.get("prm")) if w is not None]
                    ops_ = []
                    for n_, (o_, a_, s_) in enumerate(((hi_re, lo_re, sr), (hi_im, lo_re, si), (hi_re, lo_im, nsi), (hi_im, lo_im, sr))):
                        op = _Op("dve", (lambda o_, a_, s_: lambda e: e.scalar_tensor_tensor(out=o_, in0=a_, scalar=s_, in1=o_, op0=ALU.mult, op1=ALU.add))(o_, a_, s_), len(P.ops))
                        op.deps = list(prev) if n_ < 2 else [ops_[n_ - 2]]
                        P.ops.append(op)
                        ops_.append(op)
                    chain[key] = [ops_[3]]

                def scan_done(key):
                    P.writer[key] = chain.pop(key)[0]
                    P.readers[key] = []

                def bk_scan(pl, c0, n, rev, u, key, up_only=False):
                    L = n.bit_length() - 1
                    re = pl[:, 0, c0:c0 + n]; im = pl[:, 1, c0:c0 + n]
                    for k in range(L):
                        s_ = 2 << k; h_ = 1 << k
                        vr = re.rearrange("p (m s) -> p m s", s=s_); vi = im.rearrange("p (m s) -> p m s", s=s_)
                        if not rev:
                            cmul_acc(vr[:, :, s_ - 1], vi[:, :, s_ - 1], vr[:, :, h_ - 1], vi[:, :, h_ - 1], k, u, key)
                        else:
                            cmul_acc(vr[:, :, 0], vi[:, :, 0], vr[:, :, h_], vi[:, :, h_], k, u, key)
                    for k in (range(L - 2, -1, -1) if not up_only else ()):
                        s_ = 2 << k; h_ = 1 << k
                        vr = re.rearrange("p (m s) -> p m s", s=s_); vi = im.rearrange("p (m s) -> p m s", s=s_)
                        if not rev:
                            cmul_acc(vr[:, 1:, h_ - 1], vi[:, 1:, h_ - 1], vr[:, :-1, s_ - 1], vi[:, :-1, s_ - 1], k, u, key)
                        else:
                            cmul_acc(vr[:, :-1, h_], vi[:, :-1, h_], vr[:, 1:, 0], vi[:, 1:, 0], k, u, key)

                segs = [(0, 256)] + [(CTX + i * 512, 512) for i in range(8)]
                units = [(ut, d_, gpl) for ut in range(4) for d_ in range(2) for gpl in range(4)]

                def stA(ix):
                    ut, d_, gpl = units[ix]
                    u = d_ * 16 + ut * 4 + gpl
                    bi = ix % 2; ub = 0; ukey = "uT_sb0"
                    zk, zck, lbk, lck, plk = "Z%d" % bi, "Zc%d" % bi, "LB%d" % bi, "LC%d" % bi, "PL%d" % bi
                    if ix % 8 == 0:
                        P.dma("sp", ukey, lambda e: e.dma_start(out=uT_sb[ub][:], in_=uT_d[ut * 128:(ut + 1) * 128, :]), reads=["uT_d"], writes=[ukey])
                    P.op("pool", lambda e: e.memset(Z[bi][:], 0.0), writes=[zk])
                    for j in range(2):
                        for gl in range(2):
                            cs = (2 * gpl + gl) * 16
                            P.op("pool", (lambda j, gl, cs: lambda e: e.tensor_copy(out=Z[bi][gl * 64:(gl + 1) * 64, j, cs:cs + 16], in_=bb[j][gl * 64:(gl + 1) * 64, u, :]))(j, gl, cs),
                                 reads=["prm"], writes=[zk])
                    zi, zkey = pzr.next()
                    for j in range(2):
                        P.op("pe", (lambda zi, j: lambda e: e.matmul(pz[zi][:, j, :], lhsT=Z[bi][:, j, :], rhs=identf[:], start=True, stop=True))(zi, j), reads=[zk, "identf"], writes=[zkey])
                    P.op("act", (lambda zi: lambda e: e.copy(out=LB[bi][:], in_=pz[zi][:]))(zi), reads=[zkey], writes=[lbk])
                    for j in range(2):
                        P.op("pool", (lambda j: lambda e: e.tensor_tensor(out=Zc[bi][:, j, :].rearrange("p (g q) -> p g q", g=2), in0=Cn2[j][:, d_ * 4 + ut, :, :],
                                                                         in1=maskf[:, gpl, :].unsqueeze(2).to_broadcast([128, 2, 64]), op=ALU.mult))(j),
                             reads=["Cn2%d" % j, "maskf"], writes=[zck])
                    zi2, zkey2 = pzr.next()
                    for j in range(2):
                        P.op("pe", (lambda zi2, j: lambda e: e.matmul(pz[zi2][:, j, :], lhsT=Zc[bi][:, j, :], rhs=identf[:], start=True, stop=True))(zi2, j), reads=[zck, "identf"], writes=[zkey2])
                    P.op("act", lambda e: e.copy(out=LC[bi][:, 0, :], in_=pz[zi2][:, 0, :]), reads=[zkey2], writes=[lck])
                    P.op("act", lambda e: e.mul(out=LC[bi][:, 1, :], in_=pz[zi2][:, 1, :], mul=-1.0), reads=[zkey2], writes=[lck])
                    for (c0, n) in segs:
                        for j in range(2):
                            qi, qkey = pbr.next()
                            P.op("pe", (lambda qi, j, c0, n: lambda e: e.matmul(pb[qi][:, 0:n], lhsT=LB[bi][:, j, :], rhs=uT_sb[ub][:, c0:c0 + n], start=True, stop=True))(qi, j, c0, n),
                                 reads=[lbk, ukey], writes=[qkey])
                            P.op("act", (lambda qi, j, c0, n: lambda e: e.copy(out=PL[bi][:, j, c0:c0 + n], in_=pb[qi][:, 0:n]))(qi, j, c0, n), reads=[qkey], writes=[plk])

                def stB(ix):
                    ut, d_, gpl = units[ix]
                    u = d_ * 16 + ut * 4 + gpl
                    bi = ix % 2; plk = "PL%d" % bi
                    rev = (d_ == 1)
                    bk_scan(PL[bi], 0, CTX, rev, u, plk, up_only=True)
                    if not rev:
                        cmul_acc(PL[bi][:, 0, CTX:CTX + 1], PL[bi][:, 1, CTX:CTX + 1], PL[bi][:, 0, CTX - 1:CTX], PL[bi][:, 1, CTX - 1:CTX], 0, u, plk)
                    else:
                        cmul_acc(PL[bi][:, 0, NTOK - 1:NTOK], PL[bi][:, 1, NTOK - 1:NTOK], PL[bi][:, 0, 0:1], PL[bi][:, 1, 0:1], 0, u, plk)
                    bk_scan(PL[bi], CTX, SEQ, rev, u, plk)
                    scan_done(plk)

                def stC(ix):
                    ut, d_, gpl = units[ix]
                    bi = ix % 2; ub = 0; ukey = "uT_sb0"; lck, plk = "LC%d" % bi, "PL%d" % bi
                    first = (ix % 8 == 0)
                    for sgi in range(8):
                        c0 = CTX + sgi * 512
                        yi, ykey = pyr.next()
                        for j in range(2):
                            P.op("pe", (lambda yi, j, c0: lambda e: e.matmul(py[yi][:], lhsT=LC[bi][:, j, :], rhs=PL[bi][:, j, c0:c0 + 512], start=(j == 0), stop=(j == 1)))(yi, j, c0),
                                 reads=[lck, plk], writes=[ykey])
                        ysl = slice(sgi * 512, (sgi + 1) * 512)
                        if first:
                            P.op("dve", (lambda yi, c0, ysl: lambda e: e.scalar_tensor_tensor(out=yT[:, ysl], in0=uT_sb[ub][:, c0:c0 + 512], scalar=dsk[:, ut:ut + 1],
                                                                                              in1=py[yi][:], op0=ALU.mult, op1=ALU.add))(yi, c0, ysl),
                                 reads=[ykey, ukey, "dsk"], writes=["yT"])
                        else:
                            P.op("dve", (lambda yi, ysl: lambda e: e.tensor_tensor(out=yT[:, ysl], in0=yT[:, ysl], in1=py[yi][:], op=ALU.add))(yi, ysl), reads=[ykey], writes=["yT"])
                    if ix % 8 == 7:
                        if debug:
                            P.dma("sp", "dbgy", lambda e: e.dma_start(out=yT_d[ut * 128:(ut + 1) * 128, :], in_=yT[:]), reads=["yT"], writes=["yT_d"])
                        P.op("act", lambda e: e.activation(out=zT[:, ut, :], in_=yT[:], func=AF.Gelu_apprx_tanh), reads=["yT"], writes=["zT"])

                stA(0)
                for ix in range(32):
                    if ix + 1 < 32:
                        stA(ix + 1)
                    stB(ix)
                    stC(ix)
            P.barrier()
            with phase() as st:
                wstgB_t = sbuf(st, "wstgB", [128, 4, 1024])
                w_glu_sb = sbuf(st, "w_glu_sb", [128, 4, 512], BF16); w_ba_sb = sbuf(st, "w_ba_sb", [128, 4, D], BF16)
                bglu = sbuf(st, "bglu", [128, 4])
                sg = [sbuf(st, "sg%d" % i, [128, 512], BF16) for i in range(2)]; sgr = Rot("sg", 2)
                glu = [sbuf(st, "glu%d" % i, [128, 4, 512], BF16) for i in range(2)]
                st_ba = [sbuf(st, "st_ba%d" % i, [128, 8, 512], BF16) for i in range(2)]
                pg = [psum(st, "pg%d" % i, [128, 512]) for i in range(3)]; pgr = Rot("pg", 3)
                pa = [psum(st, "pa%d" % i, [128, 512]) for i in range(3)]; par = Rot("pa", 3)
                P.dma("sp", "wl0", lambda e: e.dma_start(out=wstgB_t[:, :, 0:512], in_=w_glu.rearrange("(k p) n -> p k n", p=128)), writes=["wstgB"])
                P.op("dve", lambda e: e.tensor_copy(out=w_glu_sb[:], in_=wstgB_t[:, :, 0:512]), reads=["wstgB"], writes=["w_glu_sb"])
                P.dma("sp", "wl0", lambda e: e.dma_start(out=wstgB_t[:], in_=w_ba.rearrange("(k p) n -> p k n", p=128)), reads=["wstgB"], writes=["wstgB"])
                P.op("dve", lambda e: e.tensor_copy(out=w_ba_sb[:], in_=wstgB_t[:]), reads=["wstgB"], writes=["w_ba_sb"])
                P.dma("act", "wl1", lambda e: e.dma_start(out=bglu[:], in_=b_glu.rearrange("(k p) o -> p (k o)", p=128), allow_slow_non_contiguous=True), writes=["bglu"])
                for sgi in range(8):
                    gb_ = sgi % 2; gkey = "glu%d" % gb_; bakey = "st_ba%d" % gb_
                    ssl = slice(sgi * 512, (sgi + 1) * 512)
                    for ct in range(4):
                        gi, gk = pgr.next()
                        for k in range(4):
                            P.op("pe", (lambda gi, k, ct, ssl: lambda e: e.matmul(pg[gi][:], lhsT=w_glu_sb[:, k, ct * 128:(ct + 1) * 128], rhs=zT[:, k, ssl],
                                                                                  start=(k == 0), stop=(k == 3)))(gi, k, ct, ssl),
                                 reads=["w_glu_sb", "zT"], writes=[gk])
                        si_, sk_ = sgr.next()
                        P.op("act", (lambda si_, gi, ct: lambda e: e.activation(out=sg[si_][:], in_=pg[gi][:], func=AF.Sigmoid, bias=bglu[:, ct:ct + 1]))(si_, gi, ct),
                             reads=[gk, "bglu"], writes=[sk_])
                        P.op("dve", (lambda gb_, ct, si_, ssl: lambda e: e.tensor_tensor(out=glu[gb_][:, ct, :], in0=sg[si_][:], in1=zT[:, ct, ssl], op=ALU.mult))(gb_, ct, si_, ssl),
                             reads=[sk_, "zT"], writes=[gkey])
                    for ct2 in range(8):
                        ai, ak = par.next()
                        for k in range(4):
                            P.op("pe", (lambda ai, k, ct2, gb_: lambda e: e.matmul(pa[ai][:], lhsT=w_ba_sb[:, k, ct2 * 128:(ct2 + 1) * 128], rhs=glu[gb_][:, k, :],
                                                                                   start=(k == 0), stop=(k == 3)))(ai, k, ct2, gb_),
                                 reads=["w_ba_sb", gkey], writes=[ak])
                        if ct2 % 2:
                            P.op("act", (lambda gb_, ct2, ai: lambda e: e.copy(out=st_ba[gb_][:, ct2, :], in_=pa[ai][:]))(gb_, ct2, ai), reads=[ak], writes=[bakey])
                        else:
                            P.op("dve", (lambda gb_, ct2, ai: lambda e: e.tensor_copy(out=st_ba[gb_][:, ct2, :], in_=pa[ai][:]))(gb_, ct2, ai), reads=[ak], writes=[bakey])
                    P.dma("sp", bakey, (lambda gb_, ssl: lambda e: e.dma_start(out=baT_d.rearrange("(t p) n -> p t n", p=128)[:, :, ssl], in_=st_ba[gb_][:]))(gb_, ssl),
                          reads=[bakey], writes=["baT_d"])
        P.barrier()
        if stage <= 2:
            return finish(nc, P, out)

        attT_d = dscr("attT_d", [512, SEQ], BF16) if debug else None
        NEG = -30000.0
        with ExitStack() as stC:
            attT_sb = sbuf(stC, "attT_sb", [128, 4, SEQ], BF16)
            stC2 = ExitStack()
            kT_sb = sbuf(stC2, "kT_sb", [128, 4, NTOK], BF16); qT_sb = sbuf(stC2, "qT_sb", [128, 4, SEQ], BF16)
            BiasTT = sbuf(stC2, "BiasTT", [128, 8 * 14, 64])
            Vctx = sbuf(stC2, "Vctx", [128, 2, 512], BF16)
            ones_b = sbuf(stC2, "ones_b", [128, 128], BF16)
            P.dma("sp", "lc0", lambda e: e.dma_start(out=kT_sb[:], in_=kT_d.rearrange("(t p) n -> p t n", p=128)), reads=["kT_d"], writes=["kT_sb"])
            P.dma("act", "lc1", lambda e: e.dma_start(out=qT_sb[:], in_=qT_d.rearrange("(t p) n -> p t n", p=128)), reads=["qT_d"], writes=["qT_sb"])
            P.dma("act", "lc1", lambda e: e.dma_start(out=Vctx[:], in_=v_d[0:CTX, :].rearrange("(t p) n -> p t n", p=128)), reads=["v_d"], writes=["Vctx"])
            P.op("pool", lambda e: e.memset(ones_b[:], 1.0), writes=["ones_b"])
            with phase() as st:
                rpbB = sbuf(st, "rpbB", [128, 8 * 14, 31]); tmpC = sbuf(st, "tmpC", [128, 8 * 14, 64])
                Dm = sbuf(st, "Dm", [128, 64]); eqm = [sbuf(st, "eqm%d" % i, [128, 64]) for i in range(2)]
                c0t = sbuf(st, "c0t", [128, 64]); kcv = sbuf(st, "kcv", [128, 64]); m2 = sbuf(st, "m2c", [128, 64])
                for half in range(2):
                    sl = slice(half * 64, (half + 1) * 64)
                    P.dma("sp", "lc2", (lambda sl, half: lambda e: e.dma_start(out=rpbB[sl].rearrange("p (h j) m -> p h (j m)", h=8),
                                                                              in_=rpb[:, half:half + 14, :].rearrange("h j m -> h (j m)").unsqueeze(0).to_broadcast([64, 8, 14 * 31])))(sl, half),
                          writes=["rpbB"])
                    P.op("pool", (lambda sl: lambda e: e.iota(Dm[sl], pattern=[[-1, 64]], base=15, channel_multiplier=1, allow_small_or_imprecise_dtypes=True))(sl), writes=["Dm"])
                    P.op("pool", (lambda sl: lambda e: e.iota(kcv[sl], pattern=[[0, 64]], base=0, channel_multiplier=1, allow_small_or_imprecise_dtypes=True))(sl), writes=["kcv"])
                P.op("pool", lambda e: e.iota(c0t[:], pattern=[[1, 64]], base=-8, channel_multiplier=0, allow_small_or_imprecise_dtypes=True), writes=["c0t"])
                P.op("dve", lambda e: e.tensor_scalar(out=c0t[:], in0=c0t[:], scalar1=0.0, scalar2=48.0, op0=ALU.max, op1=ALU.min), reads=["c0t"], writes=["c0t"])
                P.op("dve", lambda e: e.tensor_tensor(out=kcv[:], in0=kcv[:], in1=c0t[:], op=ALU.subtract), reads=["kcv", "c0t"], writes=["kcv"])
                P.op("dve", lambda e: e.tensor_single_scalar(out=m2[:], in_=kcv[:], scalar=0.0, op=ALU.is_ge), reads=["kcv"], writes=["m2c"])
                P.op("dve", lambda e: e.tensor_single_scalar(out=kcv[:], in_=kcv[:], scalar=15.0, op=ALU.is_le), reads=["kcv", "m2c"], writes=["kcv"])
                P.op("dve", lambda e: e.tensor_tensor(out=m2[:], in0=m2[:], in1=kcv[:], op=ALU.mult), reads=["kcv", "m2c"], writes=["m2c"])
                P.op("dve", lambda e: e.tensor_scalar(out=m2[:], in0=m2[:], scalar1=-1.0, scalar2=-NEG, op0=ALU.add, op1=ALU.mult), reads=["m2c"], writes=["m2c"])
                P.op("dve", lambda e: e.tensor_copy(out=BiasTT[:], in_=m2[:].unsqueeze(1).to_broadcast([128, 112, 64])), reads=["m2c"], writes=["BiasTT"])
                for m in range(31):
                    ei = m % 2; ek = "eqm%d" % ei
                    P.op("dve", (lambda ei, m: lambda e: e.tensor_single_scalar(out=eqm[ei][:], in_=Dm[:], scalar=float(m), op=ALU.is_equal))(ei, m), reads=["Dm"], writes=[ek])
                    for hh in range(2):
                        hs = slice(hh * 56, (hh + 1) * 56); tk_ = "tmpC%d" % hh
                        P.op("pool", (lambda ei, m, hh, hs: lambda e: e.tensor_tensor(out=tmpC[:, hs, :], in0=eqm[ei][:].unsqueeze(1).to_broadcast([128, 56, 64]),
                                                                                      in1=rpbB[:, hs, m:m + 1].to_broadcast([128, 56, 64]), op=ALU.mult))(ei, m, hh, hs),
                             reads=[ek, "rpbB"], writes=[tk_])
                        P.op("dve", (lambda hs: lambda e: e.tensor_tensor(out=BiasTT[:, hs, :], in0=BiasTT[:, hs, :], in1=tmpC[:, hs, :], op=ALU.add))(hs), reads=[tk_, "BiasTT"], writes=["BiasTT%d" % hh])
            P.barrier()
            with phase() as st:
                Vb = [sbuf(st, "Vb%d" % i, [128, 4, 512], BF16) for i in range(3)]; vbr = Rot("Vb", 3)
                ssb = [sbuf(st, "ssb%d" % i, [128, 4, 64]) for i in range(3)]; ssr2 = Rot("ssb", 3)
                pT = [sbuf(st, "pT%d" % i, [128, 384], BF16) for i in range(3)]; ptr = Rot("pT", 3)
                rden = [sbuf(st, "rden%d" % i, [128, 64]) for i in range(2)]; rdr = Rot("rden", 2)
                ps_ = [psum(st, "psc%d" % i, [128, 512]) for i in range(3)]; psr = Rot("psc", 3)
                po_ = [psum(st, "poc%d" % i, [128, 512]) for i in range(2)]; por = Rot("poc", 2)
                pd_ = [psum(st, "pdc%d" % i, [128, 512]) for i in range(2)]; pdr = Rot("pdc", 2)
                B4 = BiasTT[:].rearrange("p (h j) q -> p h j q", h=8)
                cf = [sbuf(st, "cvf%d" % i, [128, 2048]) for i in range(3)]; cb = [sbuf(st, "cvb%d" % i, [128, 2048], BF16) for i in range(3)]

                def convert_tile(ti):
                    bi = ti % 3
                    P.dma("sp", "cvf%d" % bi, lambda e: e.dma_start(out=cf[bi][:], in_=peer_uv[ti * 128:(ti + 1) * 128, :]), writes=["cvf%d" % bi])
                    P.op("pool", lambda e: e.tensor_copy(out=cb[bi][:], in_=cf[bi][:]), reads=["cvf%d" % bi], writes=["cvb%d" % bi])
                    P.dma("pool", "cvb%d" % bi, lambda e: e.dma_start(out=uvb_d[ti * 128:(ti + 1) * 128, :], in_=cb[bi][:]), reads=["cvb%d" % bi], writes=["uvb_d"])
                def c_scores(r, h, vi):
                    r0 = min(max(r - 4, 0), 56)
                    t = h // 2; po = (h % 2) * 64; psl = slice(po, po + 64)
                    si, skey = psr.next()
                    qsl = slice(r * 64, (r + 1) * 64)
                    for j in range(6):
                        k0 = (CTX + (r0 + 2 * j) * 64) if j < 4 else (j - 4) * 128
                        P.op("pe", (lambda j, k0: lambda e: e.matmul(ps_[si][:, j * 64:(j + 1) * 64], lhsT=kT_sb[psl, t, k0:k0 + 128], rhs=qT_sb[psl, t, qsl], start=True, stop=True))(j, k0),
                             reads=["kT_sb", "qT_sb"], writes=[skey])
                    return (r, h, vi, si, skey)

                def c_part1(state):
                    r, h, vi, si, skey = state
                    r0 = min(max(r - 4, 0), 56); dr0 = r0 - r + 7
                    bi2, bkey2 = ssr2.next()
                    P.op("dve", lambda e: e.scalar_tensor_tensor(out=ssb[bi2][:], in0=ps_[si][:, 0:256].rearrange("p (j q) -> p j q", j=4), scalar=0.125,
                                                                 in1=B4[:, h, dr0:dr0 + 7:2, :], op0=ALU.mult, op1=ALU.add), reads=[skey, "BiasTT"], writes=[bkey2])
                    ti, tkey = ptr.next()
                    P.op("act", lambda e: e.activation(out=pT[ti][:, 0:256], in_=ssb[bi2][:].rearrange("p j q -> p (j q)"), func=AF.Exp), reads=[bkey2], writes=[tkey])
                    P.op("act", lambda e: e.activation(out=pT[ti][:, 256:384], in_=ps_[si][:, 256:384], func=AF.Exp, scale=0.125), reads=[skey], writes=[tkey])
                    return (r, h, vi, ti, tkey)

                def c_part2(state):
                    r, h, vi, ti, tkey = state
                    vkey = "Vb%d" % vi
                    t = h // 2; po = (h % 2) * 64; psl = slice(po, po + 64)
                    qsl = slice(r * 64, (r + 1) * 64)
                    oi, okey = por.next(); di, dkey = pdr.next()
                    hp = (h // 2) * 128
                    for j in range(6):
                        vsrc = (Vb[vi][:, j, hp:hp + 128] if j < 4 else Vctx[:, j - 4, hp:hp + 128])
                        P.op("pe", (lambda j, vsrc: lambda e: e.matmul(po_[oi][:, 0:64], lhsT=vsrc, rhs=pT[ti][:, j * 64:(j + 1) * 64], start=(j == 0), stop=(j == 5)))(j, vsrc),
                             reads=[vkey, "Vctx", tkey], writes=[okey])
                    for j in range(6):
                        P.op("pe", (lambda j: lambda e: e.matmul(pd_[di][:, 0:64], lhsT=ones_b[:], rhs=pT[ti][:, j * 64:(j + 1) * 64], start=(j == 0), stop=(j == 5)))(j),
                             reads=["ones_b", tkey], writes=[dkey])
                    ri, rkey = rdr.next()
                    P.op("dve", lambda e: e.reciprocal(out=rden[ri][psl, :], in_=pd_[di][psl, 0:64]), reads=[dkey], writes=[rkey])
                    P.op("dve", lambda e: e.tensor_tensor(out=attT_sb[psl, t, qsl], in0=po_[oi][psl, 0:64], in1=rden[ri][psl, :], op=ALU.mult), reads=[okey, rkey], writes=["attT_sb"])

                pend1 = None; pend2 = None
                for r in range(64):
                    convert_tile(2 * r); convert_tile(2 * r + 1)
                    r0 = min(max(r - 4, 0), 56)
                    vi, vkey = vbr.next()
                    P.dma("sp", vkey, (lambda vi, r0: lambda e: e.dma_start(out=Vb[vi][:], in_=v_d[CTX + r0 * 64:CTX + (r0 + 8) * 64, :].rearrange("(j p) n -> p j n", p=128)))(vi, r0),
                          reads=["v_d"], writes=[vkey])
                    for h in range(8):
                        stt = c_scores(r, h, vi)
                        nxt2 = c_part1(pend1) if pend1 is not None else None
                        if pend2 is not None:
                            c_part2(pend2)
                        pend2 = nxt2
                        pend1 = stt
                nxt2 = c_part1(pend1)
                if pend2 is not None:
                    c_part2(pend2)
                c_part2(nxt2)
            if debug:
                P.dma("sp", "dbga", lambda e: e.dma_start(out=attT_d.rearrange("(t p) n -> p t n", p=128), in_=attT_sb[:]), reads=["attT_sb"], writes=["attT_d"])
            P.barrier()
            stC2.close()
            with phase() as st:
                wstgC_t = sbuf(st, "wstgC", [128, 4, 1024]); w_bb_sb = sbuf(st, "w_bb_sb", [128, 4, D], BF16)
                g_sb = [sbuf(st, "g_sb%d" % i, [128, 16, 512], BF16) for i in range(2)]
                ba_sb = [sbuf(st, "ba_sb%d" % i, [128, 8, 512], BF16) for i in range(2)]
                t1 = [sbuf(st, "t1c%d" % i, [128, 512]) for i in range(2)]; t1r = Rot("t1c", 2)
                t2 = [sbuf(st, "t2c%d" % i, [128, 512]) for i in range(2)]; t2r = Rot("t2c", 2)
                st_mg = [sbuf(st, "st_mg%d" % i, [128, 8, 512], BF16) for i in range(2)]
                pbb = [psum(st, "pbb%d" % i, [128, 512]) for i in range(3)]; pbr2 = Rot("pbb", 3)
                P.dma("sp", "wc0", lambda e: e.dma_start(out=wstgC_t[:], in_=w_bb.rearrange("(k p) n -> p k n", p=128)), writes=["wstgC"])
                P.op("dve", lambda e: e.tensor_copy(out=w_bb_sb[:], in_=wstgC_t[:]), reads=["wstgC"], writes=["w_bb_sb"])
                for sgi in range(8):
                    b2 = sgi % 2; ssl = slice(sgi * 512, (sgi + 1) * 512)
                    gk, bk, mk = "g_sb%d" % b2, "ba_sb%d" % b2, "st_mg%d" % b2
                    P.dma("sp", gk, (lambda b2, ssl: lambda e: e.dma_start(out=g_sb[b2][:], in_=gT_d.rearrange("(t p) n -> p t n", p=128)[:, :, ssl]))(b2, ssl), reads=["gT_d"], writes=[gk])
                    P.dma("act", bk, (lambda b2, ssl: lambda e: e.dma_start(out=ba_sb[b2][:], in_=baT_d.rearrange("(t p) n -> p t n", p=128)[:, :, ssl]))(b2, ssl), reads=["baT_d"], writes=[bk])
                    for ct2 in range(8):
                        qi, qk = pbr2.next()
                        for k in range(4):
                            P.op("pe", (lambda qi, k, ct2, ssl: lambda e: e.matmul(pbb[qi][:], lhsT=w_bb_sb[:, k, ct2 * 128:(ct2 + 1) * 128], rhs=attT_sb[:, k, ssl],
                                                                                   start=(k == 0), stop=(k == 3)))(qi, k, ct2, ssl),
                                 reads=["w_bb_sb", "attT_sb"], writes=[qk])
                        i1, k1 = t1r.next(); i2, k2 = t2r.next()
                        P.op("dve", (lambda i1, qi, b2, ct2: lambda e: e.tensor_tensor(out=t1[i1][:], in0=pbb[qi][:], in1=g_sb[b2][:, 8 + ct2, :], op=ALU.mult))(i1, qi, b2, ct2),
                             reads=[qk, gk], writes=[k1])
                        P.op("pool", (lambda i2, b2, ct2: lambda e: e.tensor_tensor(out=t2[i2][:], in0=ba_sb[b2][:, ct2, :], in1=g_sb[b2][:, ct2, :], op=ALU.mult))(i2, b2, ct2),
                             reads=[bk, gk], writes=[k2])
                        P.op("dve", (lambda b2, ct2, i1, i2: lambda e: e.tensor_tensor(out=st_mg[b2][:, ct2, :], in0=t1[i1][:], in1=t2[i2][:], op=ALU.add))(b2, ct2, i1, i2),
                             reads=[k1, k2], writes=[mk])
                    P.dma("sp", mk, (lambda b2, ssl: lambda e: e.dma_start(out=mgT_d.rearrange("(t p) n -> p t n", p=128)[:, :, ssl], in_=st_mg[b2][:]))(b2, ssl),
                          reads=[mk], writes=["mgT_d"])
        P.barrier()
        if stage <= 3:
            return finish(nc, P, out)

        x1_d = dscr("x1_d", [SEQ, D], F32) if debug else None
        pf_d = dscr("pf_d", [SEQ, D], F32) if debug else None
        NT = 32 if stage >= 5 else int(stage * 10) % 10 or 1
        if not debug:
            NT = 32
        if "KNT" in os.environ:
            NT = int(os.environ["KNT"])
        gate1B = modB[:, 2 * D:3 * D]; S2B = modB[:, 3 * D:4 * D]; G2B = modB[:, 4 * D:5 * D]; gate2B = modB[:, 5 * D:6 * D]
        with ExitStack() as stD:
            w_out_sb = sbuf(stD, "w_out_sb", [128, 8, D], BF16); w_q_sb = sbuf(stD, "w_q_sb", [128, 8, 2048], BF16)
            skT = sbuf(stD, "skT", [128, 2, 128], BF16); fgB = sbuf(stD, "fgB", [128, D])
            with phase() as st:
                wstgD_t = [sbuf(st, "wstgD%d" % i, [128, 8, 512]) for i in range(2)]
                skf = sbuf(st, "skf", [128, 2, 128]); skb = sbuf(st, "skb", [128, 2, 128], BF16)
                ptk = psum(st, "ptk", [128, 2, 128], BF16)
                for i in range(6):
                    bi = i % 2
                    srcw = (w_out if i < 2 else w_q).rearrange("(k p) n -> p k n", p=128)
                    c0 = (i * 512) if i < 2 else (i - 2) * 512
                    dstw = w_out_sb if i < 2 else w_q_sb
                    P.dma("sp", "wd%d" % bi, (lambda bi, srcw, c0: lambda e: e.dma_start(out=wstgD_t[bi][:], in_=srcw[:, :, c0:c0 + 512]))(bi, srcw, c0), writes=["wstgD%d" % bi])
                    P.op(("dve", "pool")[bi], (lambda bi, dstw, c0: lambda e: e.tensor_copy(out=dstw[:, :, c0:c0 + 512], in_=wstgD_t[bi][:]))(bi, dstw, c0),
                         reads=["wstgD%d" % bi], writes=["wD"])
                P.dma("act", "wd2", lambda e: e.dma_start(out=skf[:], in_=subkeys.rearrange("n k d -> k n d")), writes=["skf"])
                P.dma("act", "wd2", lambda e: e.dma_start(out=fgB[:], in_=final_g.to_broadcast([128, D])), writes=["fgB"])
                P.op("dve", lambda e: e.tensor_copy(out=skb[:], in_=skf[:]), reads=["skf"], writes=["skb"])
                for n_ in range(2):
                    P.op("pe", (lambda n_: lambda e: e.transpose(ptk[:, n_, :], skb[:, n_, :], identb[:]))(n_), reads=["skb", "identb"], writes=["ptk"])
                P.op("dve", lambda e: e.tensor_copy(out=skT[:], in_=ptk[:]), reads=["ptk"], writes=["skT"])
            P.barrier()
            P.barrier()
            if stage == 3.5:
                return finish(nc, P, out)
            with phase() as st:
                NB = 2
                xtD_t = sbuf(st, "xtD", [128, D])
                x1 = [sbuf(st, "x1_%d" % i, [128, D]) for i in range(NB)]
                h2 = [sbuf(st, "h2_%d" % i, [128, D]) for i in range(NB)]
                eid = [sbuf(st, "eid%d" % i, [128, 128], I32) for i in range(NB)]
                gate = [sbuf(st, "gate%d" % i, [128, 128]) for i in range(NB)]
                tmpD = sbuf(st, "tmpD", [128, D]); tmpF = tmpD
                acc = None
                junkD = sbuf(st, "junkD", [128, D], BF16); NJ = int(os.environ.get("KJ", "2"))
                junkE3 = [sbuf(st, "junkE%d" % i, [128, D], BF16) for i in range(NJ)]; jer = Rot("junkE", NJ)
                mg_sb = sbuf(st, "mg_sb", [128, 8, 128], BF16); h2b2 = [sbuf(st, "h2b%d" % i, [128, D], BF16) for i in range(NB)]; h2T = sbuf(st, "h2T", [128, 8, 128], BF16)
                qT_sb2 = sbuf(st, "qT_sb2", [128, 16, 128], BF16); s_sb = sbuf(st, "s_sb", [128, 16, 128]); work = sbuf(st, "workD", [128, 16, 128])
                topv = sbuf(st, "topv", [128, 16, 16]); idxu = sbuf(st, "idxu", [128, 16, 16], U32); idxf = sbuf(st, "idxf", [128, 16, 16])
                cand = sbuf(st, "cand", [128, 8, 256]); cidx = s_sb[:].rearrange("p a b -> p (a b)").rearrange("p (h c) -> p h c", h=8)
                best = sbuf(st, "best", [128, 8, 16]); eg = sbuf(st, "eg", [128, 8, 16]); sm = sbuf(st, "smD", [128, 32]); sm2 = sbuf(st, "smE", [128, 8]); eidf = sbuf(st, "eidf", [128, 128])
                aD = sbuf(st, "aD", [128, 128]); gl_ = sbuf(st, "gl_", [128, 128]); wD = sbuf(st, "wDw", [128, 128])
                NG = int(os.environ.get("KNG", "12")); GJ = int(os.environ.get("KGJ", "4"))
                FY = int(os.environ.get("KFY", "1")); KAR = int(os.environ.get("KAR", "0"))
                posI = sbuf(st, "posI", [128, 256], I32); mskI = sbuf(st, "mskI", [128, 1], I32)
                c4I = sbuf(st, "c4I", [128, 1], I32); c15I = sbuf(st, "c15I", [128, 1], I32); iota16 = sbuf(st, "iota16", [128, 16])
                pab = sbuf(st, "pab", [128, 2, 8, 16], I32); pabf = sbuf(st, "pabf", [128, 2, 8, 16]); e3b = sbuf(st, "e3b", [128, 8, 16])
                oh = cand[:].rearrange("p h (k a) -> p h k a", a=16)
                P.op("pool", lambda e: e.iota(c4I[:], pattern=[[0, 1]], base=4, channel_multiplier=0), writes=["posI"])
                P.op("pool", lambda e: e.iota(c15I[:], pattern=[[0, 1]], base=15, channel_multiplier=0), writes=["posI"])
                P.op("pool", lambda e: e.iota(iota16[:], pattern=[[1, 16]], base=0, channel_multiplier=0, allow_small_or_imprecise_dtypes=True), writes=["posI"])
                P.op("pool", lambda e: e.iota(posI[:], pattern=[[1, 256]], base=0, channel_multiplier=0), writes=["posI"])
                P.op("pool", lambda e: e.iota(mskI[:], pattern=[[0, 1]], base=-256, channel_multiplier=0), writes=["posI"])
                UVg = [sbuf(st, "UVg%d" % i, [128, 2, D], BF16) for i in range(NG)]
                dg = [sbuf(st, "dg%d" % i, [128, 128], BF16) for i in range(8)]; dgr = Rot("dg", 8)
                pmo = [psum(st, "pmo%d" % i, [128, 512]) for i in range(2)]
                pacc = [psum(st, "pacc%d" % i, [128, 512]) for i in range(2)]
                tpD = psum(st, "tpD", [128, 8, 128], BF16)
                pq = [psum(st, "pq%d" % i, [128, 4, 128]) for i in range(2)]; pqr = Rot("pq", 2)
                uv_i = [0]

                def rstd_chain(src_ap, srckey, smt, smk, jk, jkey):
                    P.op("act", lambda e: e.activation(out=jk[:], in_=src_ap, func=AF.Square, accum_out=smt[:, 0:1]), reads=[srckey], writes=[smk, jkey])
                    P.op("dve", lambda e: e.tensor_scalar(out=smt[:, 1:2], in0=smt[:, 0:1], scalar1=1.0 / D, scalar2=EPS, op0=ALU.mult, op1=ALU.add), reads=[smk], writes=[smk])
                    P.op("act", lambda e: e.sqrt(out=smt[:, 2:3], in_=smt[:, 1:2]), reads=[smk], writes=[smk])
                    P.op("dve", lambda e: e.reciprocal(out=smt[:, 3:4], in_=smt[:, 2:3]), reads=[smk], writes=[smk])

                def front(i):
                    b = i % NB; t0 = i * 128
                    xk, x1k, h2k, ek, gk = "xtD", "x1_%d" % b, "h2_%d" % b, "eid%d" % b, "gate%d" % b
                    P.dma("sp", xk, lambda e: e.dma_start(out=xtD_t[:], in_=x[t0:t0 + 128, :]), writes=[xk])
                    P.dma("sp", "mgl", lambda e: e.dma_start(out=mg_sb[:], in_=mgT_d.rearrange("(k p) n -> p k n", p=128)[:, :, t0:t0 + 128]), reads=["mgT_d"], writes=["mg_sb"])
                    for hf in range(2):
                        for k in range(8):
                            P.op("pe", (lambda hf, k: lambda e: e.matmul(pmo[hf][:], lhsT=mg_sb[:, k, :], rhs=w_out_sb[:, k, hf * 512:(hf + 1) * 512], start=(k == 0), stop=(k == 7)))(hf, k),
                                 reads=["mg_sb", "wD"], writes=["pmo%d" % hf])
                        yield
                        P.op("dve", (lambda hf: lambda e: e.tensor_tensor(out=tmpF[:, hf * 512:(hf + 1) * 512], in0=pmo[hf][:], in1=gate1B[:, hf * 512:(hf + 1) * 512], op=ALU.mult))(hf),
                             reads=["pmo%d" % hf, "modB"], writes=["tmpD"])
                        yield
                    P.op("dve", lambda e: e.tensor_tensor(out=x1[b][:], in0=tmpF[:], in1=xtD_t[:], op=ALU.add), reads=["tmpD", xk], writes=[x1k])
                    yield
                    if debug:
                        P.dma("sp", "dbgx1", lambda e: e.dma_start(out=x1_d[t0:t0 + 128, :], in_=x1[b][:]), reads=[x1k], writes=["x1_d"])
                    rstd_chain(x1[b][:], x1k, sm[:, 0:4], "smD", junkD, "junkD")
                    P.op("dve", lambda e: e.scalar_tensor_tensor(out=tmpF[:], in0=x1[b][:], scalar=sm[:, 3:4], in1=G2B, op0=ALU.mult, op1=ALU.mult), reads=[x1k, "smD", "modB"], writes=["tmpD"])
                    yield
                    P.op("dve", lambda e: e.tensor_tensor(out=h2[b][:], in0=tmpF[:], in1=S2B, op=ALU.add), reads=["tmpD", "modB"], writes=[h2k])
                    yield
                    P.op("act", lambda e: e.copy(out=h2b2[b][:], in_=h2[b][:]), reads=[h2k], writes=["h2b%d" % b])
                    for k in range(8):
                        P.op("pe", (lambda k: lambda e: e.transpose(tpD[:, k, :], h2b2[b][:, k * 128:(k + 1) * 128], identb[:]))(k), reads=["h2b%d" % b, "identb"], writes=["tpD"])
                    P.op("act", lambda e: e.copy(out=h2T[:], in_=tpD[:]), reads=["tpD"], writes=["h2T"])
                    yield
                    for g4 in range(4):
                        qi, qk = pqr.next()
                        for bl in range(4):
                            blk = g4 * 4 + bl
                            for k in range(8):
                                P.op("pe", (lambda qi, bl, blk, k: lambda e: e.matmul(pq[qi][:, bl, :], lhsT=w_q_sb[:, k, blk * 128:(blk + 1) * 128], rhs=h2T[:, k, :], start=(k == 0), stop=(k == 7)))(qi, bl, blk, k),
                                     reads=["wD", "h2T"], writes=[qk])
                        P.op("act", (lambda qi, g4: lambda e: e.copy(out=qT_sb2[:, g4 * 4:(g4 + 1) * 4, :], in_=pq[qi][:]))(qi, g4), reads=[qk], writes=["qT_sb2"])
                        yield
                    for g4 in range(4):
                        si, sk = pqr.next()
                        for bl in range(4):
                            blk = g4 * 4 + bl
                            P.op("pe", (lambda si, bl, blk: lambda e: e.matmul(pq[si][:, bl, :], lhsT=qT_sb2[:, blk, :], rhs=skT[:, blk % 2, :], start=True, stop=True))(si, bl, blk),
                                 reads=["qT_sb2", "skT"], writes=[sk])
                        P.op("act", (lambda si, g4: lambda e: e.copy(out=s_sb[:, g4 * 4:(g4 + 1) * 4, :], in_=pq[si][:]))(si, g4), reads=[sk], writes=["s_sb"])
                        yield
                    TK = ["s_sb", "topk"]
                    BK = ["tk%d" % q for q in range(16)]
                    for blk in range(16):
                        P.op("dve", (lambda blk: lambda e: e.max(out=topv[:, blk, 0:8], in_=s_sb[:, blk, :]))(blk), reads=TK, writes=[BK[blk]])
                    yield
                    for blk in range(16):
                        P.op("dve", (lambda blk: lambda e: e.max_index(out=idxu[:, blk, 0:8], in_max=topv[:, blk, 0:8], in_values=s_sb[:, blk, :]))(blk), reads=["s_sb", BK[blk]], writes=[BK[blk]])
                        if blk % 8 == 7:
                            yield
                    for blk in range(16):
                        P.op("dve", (lambda blk: lambda e: e.match_replace(out=work[:, blk, :], in_to_replace=topv[:, blk, 0:8], in_values=s_sb[:, blk, :], imm_value=-1e30))(blk), reads=["s_sb", BK[blk]], writes=[BK[blk]])
                        if blk % 8 == 7:
                            yield
                    for blk in range(16):
                        P.op("dve", (lambda blk: lambda e: e.max(out=topv[:, blk, 8:16], in_=work[:, blk, :]))(blk), reads=[BK[blk]], writes=[BK[blk]])
                    yield
                    for blk in range(16):
                        P.op("dve", (lambda blk: lambda e: e.max_index(out=idxu[:, blk, 8:16], in_max=topv[:, blk, 8:16], in_values=work[:, blk, :]))(blk), reads=[BK[blk]], writes=[BK[blk]])
                        if blk % 8 == 7:
                            yield
                    TK = TK + BK
                    P.op("dve", lambda e: e.tensor_copy(out=idxf[:], in_=idxu[:]), reads=TK, writes=["topk"])
                    tv4 = topv[:].rearrange("p (h n) a -> p h n a", n=2); ix4 = idxf[:].rearrange("p (h n) a -> p h n a", n=2)
                    c4 = cand[:].rearrange("p h (a b) -> p h a b", a=16); ci4 = cidx.rearrange("p h (a b) -> p h a b", a=16)
                    P.op("dve", lambda e: e.tensor_tensor(out=c4, in0=tv4[:, :, 0, :].unsqueeze(3).to_broadcast([128, 8, 16, 16]),
                                                          in1=tv4[:, :, 1, :].unsqueeze(2).to_broadcast([128, 8, 16, 16]), op=ALU.add), reads=TK, writes=["topk"])
                    yield
                    candI = cand[:].bitcast(I32)
                    P.op("dve", lambda e: e.tensor_scalar(out=candI, in0=candI, scalar1=mskI[:, 0:1], scalar2=None, op0=ALU.bitwise_and), reads=TK + ["posI"], writes=["topk"])
                    P.op("dve", lambda e: e.tensor_tensor(out=candI, in0=candI, in1=posI[:].unsqueeze(1).to_broadcast([128, 8, 256]), op=ALU.bitwise_or), reads=TK + ["posI"], writes=["topk"])
                    P.op("dve", lambda e: e.tensor_single_scalar(out=ix4[:, :, 0, :], in_=ix4[:, :, 0, :], scalar=128.0, op=ALU.mult), reads=TK, writes=["topk"])
                    yield
                    w2 = work[:].rearrange("p (h n) k -> p h (n k)", n=2)
                    HK = ["hk%d" % q for q in range(8)]
                    for h in range(8):
                        P.op("dve", (lambda h: lambda e: e.max(out=best[:, h, 0:8], in_=cand[:, h, :]))(h), reads=TK, writes=[HK[h]])
                    yield
                    for h in range(8):
                        P.op("dve", (lambda h: lambda e: e.match_replace(out=w2[:, h, :], in_to_replace=best[:, h, 0:8], in_values=cand[:, h, :], imm_value=-1e30))(h), reads=TK + [HK[h]], writes=[HK[h]])
                    yield
                    for h in range(8):
                        P.op("dve", (lambda h: lambda e: e.max(out=best[:, h, 8:16], in_=w2[:, h, :]))(h), reads=[HK[h]], writes=[HK[h]])
                    yield
                    TK = TK + HK
                    P.op("dve", lambda e: e.tensor_single_scalar(out=sm[:, 8:16], in_=best[:, :, 0], scalar=-1.0, op=ALU.mult), reads=TK + ["smD"], writes=["smD"])
                    for h in range(8):
                        P.op("act", (lambda h: lambda e: e.activation(out=eg[:, h, :], in_=best[:, h, :], func=AF.Exp, bias=sm[:, 8 + h:9 + h], accum_out=sm[:, 16 + h:17 + h]))(h),
                             reads=TK + ["smD"], writes=["eg", "smD"])
                    P.op("dve", lambda e: e.reciprocal(out=sm[:, 24:32], in_=sm[:, 16:24]), reads=["smD"], writes=["smD"])
                    P.op("dve", lambda e: e.tensor_tensor(out=gate[b][:].rearrange("p (h k) -> p h k", h=8), in0=eg[:], in1=sm[:, 24:32].unsqueeze(2).to_broadcast([128, 8, 16]), op=ALU.mult),
                         reads=["eg", "smD"], writes=[gk])
                    yield
                    bestI = best[:].bitcast(I32)
                    P.op("dve", lambda e: e.tensor_scalar(out=pab[:, 0], in0=bestI, scalar1=c4I[:, 0:1], scalar2=None, op0=ALU.arith_shift_right), reads=TK + ["posI"], writes=["topk"])
                    P.op("dve", lambda e: e.tensor_scalar(out=pab[:, 0], in0=pab[:, 0], scalar1=c15I[:, 0:1], scalar2=None, op0=ALU.bitwise_and), reads=TK + ["posI"], writes=["topk"])
                    P.op("dve", lambda e: e.tensor_scalar(out=pab[:, 1], in0=bestI, scalar1=c15I[:, 0:1], scalar2=None, op0=ALU.bitwise_and), reads=TK + ["posI"], writes=["topk"])
                    P.op("dve", lambda e: e.tensor_copy(out=pabf[:], in_=pab[:]), reads=TK, writes=["topk"])
                    yield
                    e3 = eidf[:].rearrange("p (h k) -> p h k", h=8)
                    for n_ in range(2):
                        P.op("dve", (lambda n_: lambda e: e.tensor_tensor(out=oh[:], in0=pabf[:, n_].unsqueeze(3).to_broadcast([128, 8, 16, 16]),
                                                                          in1=iota16[:].unsqueeze(1).unsqueeze(1).to_broadcast([128, 8, 16, 16]), op=ALU.is_equal))(n_), reads=TK + ["posI"], writes=["topk"])
                        P.op("dve", (lambda n_: lambda e: e.tensor_tensor(out=oh[:], in0=oh[:], in1=ix4[:, :, n_, :].unsqueeze(2).to_broadcast([128, 8, 16, 16]), op=ALU.mult))(n_), reads=TK, writes=["topk"])
                        P.op("dve", (lambda n_: lambda e: e.tensor_reduce(out=(e3 if n_ == 0 else e3b[:]), in_=oh[:], axis=AX.X, op=ALU.add))(n_), reads=TK, writes=["topk"])
                        yield
                    P.op("dve", lambda e: e.tensor_tensor(out=e3, in0=e3, in1=e3b[:], op=ALU.add), reads=TK, writes=["topk"])
                    P.op("dve", lambda e: e.tensor_scalar(out=eidf[:], in0=eidf[:], scalar1=0.0, scalar2=16383.0, op0=ALU.max, op1=ALU.min), reads=TK, writes=["topk"])
                    P.op("dve", lambda e: e.tensor_copy(out=eid[b][:], in_=eidf[:]), reads=TK, writes=[ek])
                    yield

                def consume(i, fgen):
                    b = i % NB; t0 = i * 128
                    x1k, h2k, ek, gk = "x1_%d" % b, "h2_%d" % b, "eid%d" % b, "gate%d" % b
                    ngrp = 128 // GJ
                    pend = None

                    def finish_group(g, bufs):
                        j0 = g * GJ
                        if "nofin" in KD:
                            return
                        P.op("act", lambda e: e.activation(out=gl_[:, j0:j0 + GJ], in_=aD[:, j0:j0 + GJ], func=AF.Gelu_apprx_tanh), reads=["aD%d_%d" % (g, q) for q in range(GJ)], writes=["gl_%d" % g])
                        for jj in range(GJ):
                            P.op("act", (lambda jj: lambda e: e.mul(out=wD[:, j0 + jj:j0 + jj + 1], in_=gl_[:, j0 + jj:j0 + jj + 1], mul=gate[b][:, j0 + jj:j0 + jj + 1]))(jj),
                                 reads=["gl_%d" % g, gk], writes=["wDw%d" % g])
                        for jj in range(GJ):
                            j = j0 + jj; gi, ugk = bufs[jj]
                            di, dk = dgr.next()
                            P.op("act", (lambda di, j: lambda e: e.activation(out=dg[di][:], in_=identb[:], func=AF.Copy, scale=wD[:, j:j + 1]))(di, j), reads=["wDw%d" % g, "identb"], writes=[dk])
                            for hf in range(2):
                                if "nov" in KD and j not in (0, 127):
                                    continue
                                P.op("pe", (lambda di, gi, hf, j: lambda e: e.matmul(pacc[hf][:], lhsT=dg[di][:], rhs=UVg[gi][:, 1, hf * 512:(hf + 1) * 512], start=(j == 0), stop=(j == 127)))(di, gi, hf, j),
                                     reads=[dk, ugk], writes=["pacc%d" % hf])

                    for g in range(ngrp):
                        bufs = []
                        for jj in range(GJ):
                            j = g * GJ + jj
                            gi = uv_i[0] % NG; uv_i[0] += 1; ugk = "UVg%d" % gi
                            bufs.append((gi, ugk))
                            if "nog" in KD:
                                P.op("pool", (lambda gi: lambda e: e.memset(UVg[gi][:], 1.0))(gi), reads=[ek], writes=[ugk])
                            else:
                                P.dma("pool", ugk, (lambda gi, j: lambda e: e.indirect_dma_start(out=UVg[gi][:].rearrange("p a d -> p (a d)"), out_offset=None, in_=uvb_d,
                                                                                               in_offset=bass.IndirectOffsetOnAxis(ap=eid[b][:, j:j + 1], axis=0)))(gi, j), reads=[ek], writes=[ugk])
                        for jj in range(GJ):
                            j = g * GJ + jj; gi, ugk = bufs[jj]
                            if "noa" in KD:
                                continue
                            ji, jkey_ = jer.next()
                            jE = junkE3[ji]
                            if jj < KAR:
                                P.op("dve", (lambda gi, jE: lambda e: e.tensor_tensor(out=jE[:], in0=UVg[gi][:, 0, :], in1=h2b2[b][:], op=ALU.mult))(gi, jE),
                                     reads=[ugk, "h2b%d" % b], writes=[jkey_])
                                P.op("act", (lambda j, jE: lambda e: e.activation(out=jE[:], in_=jE[:], func=AF.Copy, accum_out=aD[:, j:j + 1]))(j, jE),
                                     reads=[jkey_], writes=[jkey_, "aD%d_%d" % (g, jj)])
                            else:
                                P.op("dve", (lambda gi, j, jE: lambda e: e.scalar_tensor_tensor(out=jE[:], in0=UVg[gi][:, 0, :], scalar=1.0, in1=h2[b][:], op0=ALU.mult, op1=ALU.mult, accum_out=aD[:, j:j + 1]))(gi, j, jE),
                                     reads=[ugk, h2k], writes=["aD%d_%d" % (g, jj), jkey_])
                        finish_group(g, bufs)
                        if fgen is not None:
                            for _ in range(FY):
                                next(fgen, None)
                    if fgen is not None:
                        for _ in fgen:
                            pass
                    if "dump23" in KD and i == 23:
                        for nm, src_t in (("d_x1", x1[b]), ("d_wD", wD), ("d_aD", aD), ("d_gate", gate[b]), ("d_h2", h2[b])):
                            dd = nc.dram_tensor(nm, list(src_t[:].shape), F32, kind="ExternalOutput").ap()
                            P.dma("sp", "dump", (lambda dd, src_t: lambda e: e.dma_start(out=dd, in_=src_t[:]))(dd, src_t), reads=[x1k, gk, h2k] + ["wDw%d" % q for q in range(32)] + ["aD%d" % q for q in range(32)], writes=["dumpo"])
                        dd2 = nc.dram_tensor("d_eid", [128, 128], I32, kind="ExternalOutput").ap()
                        P.dma("sp", "dump", lambda e: e.dma_start(out=dd2, in_=eid[b][:]), reads=[ek], writes=["dumpo"])
                    if "notail" in KD:
                        return
                    for hf in range(2):
                        P.op("dve", (lambda hf: lambda e: e.tensor_tensor(out=tmpD[:, hf * 512:(hf + 1) * 512], in0=pacc[hf][:], in1=gate2B[:, hf * 512:(hf + 1) * 512], op=ALU.mult))(hf),
                             reads=["pacc%d" % hf, "modB"], writes=["tmpD"])
                    P.op("dve", lambda e: e.tensor_tensor(out=x1[b][:], in0=tmpD[:], in1=x1[b][:], op=ALU.add), reads=["tmpD", x1k], writes=[x1k])
                    rstd_chain(x1[b][:], x1k, sm2[:, 0:4], "smE", junkD, "junkD")
                    P.op("dve", lambda e: e.scalar_tensor_tensor(out=tmpD[:], in0=x1[b][:], scalar=sm2[:, 3:4], in1=fgB[:], op0=ALU.mult, op1=ALU.mult), reads=[x1k, "smE", "fgB"], writes=["tmpD"])
                    P.dma("sp", "outst", lambda e: e.dma_start(out=out[t0:t0 + 128, :], in_=tmpD[:]), reads=["tmpD"], writes=["out"])

                for _ in front(0):
                    pass
                for i in range(NT):
                    if "noc" in KD:
                        break
                    if "noil" in KD:
                        consume(i, None)
                        if i + 1 < NT:
                            for _ in front(i + 1):
                                pass
                        continue
                    consume(i, front(i + 1) if i + 1 < NT else None)
        return finish(nc, P, out)
    return nc


def finish(nc, P, out):
    P.barrier()
    P.close()
    return nc


def core_inputs(inp, b):
    f = lambda a: np.ascontiguousarray(a, dtype=np.float32)
    return {
        "x": f(inp["x"][b]), "c": f(inp["c"][b:b + 1]), "ctx": f(inp["ctx"][b]), "c_ctx": f(inp["c_ctx"].reshape(1, D)),
        "w_mod": f(inp["w_mod"][0]), "b_mod": f(inp["b_mod"][0].reshape(1, -1)),
        "norm1_g": f(inp["norm1_g"][0].reshape(1, D)), "norm2_g": f(inp["norm2_g"][0].reshape(1, D)),
        "final_g": f(inp["final_g"].reshape(1, D)), "w_in": f(inp["w_in"][0]),
        "ssm_a_re": f(inp["ssm_a_re"][0]), "ssm_a_im": f(inp["ssm_a_im"][0]), "ssm_log_dt": f(inp["ssm_log_dt"][0]),
        "ssm_b_re": f(inp["ssm_b_re"][0]), "ssm_b_im": f(inp["ssm_b_im"][0]),
        "ssm_c_re": f(inp["ssm_c_re"][0]), "ssm_c_im": f(inp["ssm_c_im"][0]),
        "ssm_d": f(inp["ssm_d"][0].reshape(512, 1)), "w_glu": f(inp["w_glu"][0]), "b_glu": f(inp["b_glu"][0].reshape(512, 1)),
        "w_branch_a": f(inp["w_branch_a"][0]), "w_branch_b": f(inp["w_branch_b"][0]), "na_rpb": f(inp["na_rpb"][0]),
        "w_out": f(inp["w_out"][0]), "peer_w_q": f(inp["peer_w_q"][0]), "peer_subkeys": f(inp["peer_subkeys"][0]),
        "peer_uv": f(np.concatenate([inp["peer_u"][0], inp["peer_v"][0]], axis=1)),
    }


def kernel(**inputs):
    nc = build()
    in_maps = [core_inputs(inputs, b) for b in range(8)]
    res = run_bass_kernel_spmd(nc, in_maps, core_ids=list(range(8)))
    return np.stack([np.asarray(r["out"], dtype=np.float32) for r in res.results], axis=0)
```

```python
import math
import numpy as np
import concourse.bass as bass
import concourse.mybir as mybir
from concourse.bass_utils import run_bass_kernel_spmd

F32 = mybir.dt.float32
BF16 = mybir.dt.bfloat16
I32 = mybir.dt.int32
U32 = mybir.dt.uint32
AF = mybir.ActivationFunctionType
ALU = mybir.AluOpType
AX = mybir.AxisListType


class _Op:
    __slots__ = ("eng", "fn", "deps", "seq", "is_dma", "semkey", "signal", "count", "waits")

    def __init__(self, eng, fn, seq, is_dma=False, semkey=None):
        self.eng = eng
        self.fn = fn
        self.deps = []
        self.seq = seq
        self.is_dma = is_dma
        self.semkey = semkey
        self.signal = is_dma
        self.count = 0
        self.waits = []


class Prog:
    ENGS = ("pe", "dve", "act", "pool", "sp")

    def __init__(self, nc):
        self.nc = nc
        self.ops = []
        self.writer = {}
        self.readers = {}
        import os
        self.same_sync = os.environ.get("KSAME", "1") == "1"

    def _add(self, op, reads, writes):
        deps = []
        for r in reads:
            w = self.writer.get(r)
            if w is not None:
                deps.append(w)
        for w_ in writes:
            w = self.writer.get(w_)
            if w is not None:
                deps.append(w)
            deps.extend(self.readers.get(w_, ()))
        op.deps = [d for d in set(deps) if d is not op]
        for r in reads:
            self.readers.setdefault(r, []).append(op)
        for w_ in writes:
            self.writer[w_] = op
            self.readers[w_] = []
        self.ops.append(op)
        return op

    def op(self, eng, fn, reads=(), writes=()):
        return self._add(_Op(eng, fn, len(self.ops)), reads, writes)

    def dma(self, eng, semkey, fn, reads=(), writes=()):
        return self._add(_Op(eng, fn, len(self.ops), True, semkey), reads, writes)

    def emit(self):
        import bisect
        from contextlib import ExitStack
        nc = self.nc
        if not hasattr(self, "_st"):
            self._st = ExitStack(); self._sem = {}; self._cnt = {}; self._hist = {}
            self._seen = {e: {} for e in self.ENGS}; self._done = 0
        ops = self.ops[self._done:]
        self._done = len(self.ops)
        if not ops:
            return

        def same(d, o):
            return d.eng == o.eng and not o.is_dma and (d.eng == "pe" or not self.same_sync)
        for o in ops:
            for d in o.deps:
                if d.is_dma or same(d, o):
                    continue
                assert d.count == 0 or d.signal, "dependency on an already-emitted non-signalling op"
                d.signal = True
        for o in ops:
            if not o.signal:
                continue
            k = ("dma", o.semkey) if o.is_dma else ("eng", o.eng)
            if k not in self._sem:
                self._sem[k] = self._st.enter_context(nc.semaphore("s%d_%s" % (len(self._sem), str(k[1]).replace(" ", ""))))
                self._cnt[k] = 0
            self._cnt[k] += 1
            o.count = self._cnt[k]
            if o.is_dma:
                self._hist.setdefault(o.semkey, []).append(o.seq)
        for o in ops:
            need = {}
            for d in o.deps:
                if d.is_dma:
                    k = ("dma", d.semkey)
                    v = 16 * bisect.bisect_left(self._hist[d.semkey], o.seq)
                else:
                    if same(d, o):
                        continue
                    k = ("eng", d.eng)
                    v = d.count
                if need.get(k, 0) < v:
                    need[k] = v
            sn = self._seen[o.eng]
            o.waits = []
            for k, v in need.items():
                if sn.get(k, 0) < v:
                    sn[k] = v
                    o.waits.append((k, v))
        self.n_sems = len(self._sem)
        sem = self._sem
        with nc.Block() as block:
            per = {e: [o for o in ops if o.eng == e] for e in self.ENGS}

            def run(engobj, lst):
                for o in lst:
                    for k, v in o.waits:
                        engobj.wait_ge(sem[k], v)
                    ins = o.fn(engobj)
                    if o.signal:
                        k = ("dma", o.semkey) if o.is_dma else ("eng", o.eng)
                        ins.then_inc(sem[k], 16 if o.is_dma else 1)
                    o.fn = None

            @block.tensor
            def _(e):
                run(e, per["pe"])

            @block.vector
            def _(e):
                run(e, per["dve"])

            @block.scalar
            def _(e):
                run(e, per["act"])

            @block.gpsimd
            def _(e):
                run(e, per["pool"])

            @block.sync
            def _(e):
                run(e, per["sp"])

    def close(self):
        self.emit()
        if hasattr(self, "_st"):
            self._st.close()

    def barrier(self, flush=True):
        start = getattr(self, "_done", 0)
        last = {}
        dmas = {}
        for o in self.ops[start:]:
            if o.is_dma:
                dmas[o.semkey] = o
            else:
                last[o.eng] = o
        for e, o in getattr(self, "_bar", {}).items():
            last.setdefault(e, o)
        deps = list(last.values()) + list(dmas.values())
        self._bar = {}
        for e in self.ENGS:
            o = _Op(e, lambda eng: eng.nop(), len(self.ops))
            o.deps = [d for d in deps]
            o.signal = True
            self.ops.append(o)
            self._bar[e] = o
        self.writer = {}
        self.readers = {}
        if flush:
            self.emit()


class Rot:
    def __init__(self, name, n):
        self.name, self.n, self.i = name, n, -1

    def next(self):
        self.i = (self.i + 1) % self.n
        return self.i, "%s%d" % (self.name, self.i)


D = 1024
SEQ = 4096
CTX = 256
NTOK = SEQ + CTX
EPS = 1e-6


def build(stage=99, debug=False):
    import os
    KD = os.environ.get("KDBG", "")
    from contextlib import ExitStack
    nc = bass.Bass("TRN2", target_bir_lowering=False)
    P = Prog(nc)

    def din(name, shape, dt=F32):
        return nc.dram_tensor(name, shape, dt, kind="ExternalInput").ap()

    def dscr(name, shape, dt):
        return nc.dram_tensor(name, shape, dt, kind=("ExternalOutput" if debug else "Internal")).ap()

    x = din("x", [SEQ, D]); c = din("c", [1, D]); ctx = din("ctx", [CTX, D]); c_ctx = din("c_ctx", [1, D])
    w_mod = din("w_mod", [D, 6 * D]); b_mod = din("b_mod", [1, 6 * D])
    norm1_g = din("norm1_g", [1, D]); norm2_g = din("norm2_g", [1, D]); final_g = din("final_g", [1, D])
    w_in = din("w_in", [D, 4096])
    a_re = din("ssm_a_re", [2, 32, 64]); a_im = din("ssm_a_im", [2, 32, 64]); log_dt = din("ssm_log_dt", [2, 32])
    b_re = din("ssm_b_re", [2, 32, 64, 16]); b_im = din("ssm_b_im", [2, 32, 64, 16])
    c_re = din("ssm_c_re", [2, 32, 16, 64]); c_im = din("ssm_c_im", [2, 32, 16, 64])
    ssm_d = din("ssm_d", [512, 1]); w_glu = din("w_glu", [512, 512]); b_glu = din("b_glu", [512, 1])
    w_ba = din("w_branch_a", [512, D]); w_bb = din("w_branch_b", [512, D]); rpb = din("na_rpb", [8, 15, 31])
    w_out = din("w_out", [D, D]); w_q = din("peer_w_q", [D, 2048]); subkeys = din("peer_subkeys", [2, 128, 128])
    peer_uv = din("peer_uv", [16384, 2 * D])
    out = nc.dram_tensor("out", [SEQ, D], F32, kind="ExternalOutput").ap()

    uT_d = dscr("uT_d", [512, NTOK], F32)
    kT_d = dscr("kT_d", [512, NTOK], BF16)
    qT_d = dscr("qT_d", [512, SEQ], BF16)
    v_d = dscr("v_d", [NTOK, 512], BF16)
    gT_d = dscr("gT_d", [2048, SEQ], BF16)
    baT_d = dscr("baT_d", [D, SEQ], BF16)
    mgT_d = dscr("mgT_d", [D, SEQ], BF16)
    uvb_d = nc.dram_tensor("uvb_d", [16384, 2 * D], BF16, kind=("ExternalOutput" if (debug and stage == 3.5) else "Internal")).ap()

    from contextlib import contextmanager

    @contextmanager
    def phase():
        stk = ExitStack()
        try:
            yield stk
            P.barrier()
        finally:
            stk.close()

    top = ExitStack()
    with top:
        def sbuf(st, n, s, d=F32):
            return st.enter_context(nc.sbuf_tensor(n, s, d))

        def psum(st, n, s, d=F32):
            return st.enter_context(nc.psum_tensor(n, s, d))

        identf = sbuf(top, "identf", [128, 128])
        identb = sbuf(top, "identb", [128, 128], BF16)
        modB = sbuf(top, "modB", [128, 6 * D])
        P.op("pool", lambda e: e.iota(identf[:], pattern=[[1, 128]], base=0, channel_multiplier=-1,
                                      allow_small_or_imprecise_dtypes=True), writes=["identf"])
        P.op("dve", lambda e: e.tensor_single_scalar(out=identf[:], in_=identf[:], scalar=0.0, op=ALU.is_equal),
             reads=["identf"], writes=["identf"])
        P.op("dve", lambda e: e.tensor_copy(out=identb[:], in_=identf[:]), reads=["identf"], writes=["identb"])

        with phase() as st:
            modcB = sbuf(st, "modcB", [128, 2 * D])
            wm = [sbuf(st, "wm%d" % i, [128, 8, 512]) for i in range(2)]
            w_in_sb = sbuf(st, "w_in_sb", [128, 8, 4096], BF16)
            st0 = ExitStack()
            cc = sbuf(st0, "cc", [128, 2, 8]); sc = sbuf(st0, "sc", [128, 2, 8]); scB = sbuf(st0, "scB", [128, 2, 8, 128])
            bmB = sbuf(st0, "bmB", [128, 6 * D]); gB = sbuf(st0, "gB", [128, 2, D])
            pmod = [psum(st0, "pmod%d" % i, [128, 512]) for i in range(2)]

            P.dma("sp", "c0", lambda e: e.dma_start(out=cc[:, 0, :], in_=c.rearrange("o (k p) -> p (o k)", p=128),
                                                    allow_slow_non_contiguous=True), writes=["cc"])
            P.dma("sp", "c0", lambda e: e.dma_start(out=cc[:, 1, :], in_=c_ctx.rearrange("o (k p) -> p (o k)", p=128),
                                                    allow_slow_non_contiguous=True), writes=["cc"])
            P.dma("act", "c1", lambda e: e.dma_start(out=bmB[:], in_=b_mod.to_broadcast([128, 6 * D])), writes=["bmB"])
            P.dma("act", "c1", lambda e: e.dma_start(out=gB[:, 0, :], in_=norm1_g.to_broadcast([128, D])), writes=["gB"])
            P.dma("act", "c1", lambda e: e.dma_start(out=gB[:, 1, :], in_=norm2_g.to_broadcast([128, D])), writes=["gB"])
            P.op("act", lambda e: e.activation(out=sc[:], in_=cc[:], func=AF.Silu), reads=["cc"], writes=["sc"])
            P.op("dve", lambda e: e.tensor_copy(out=scB[:], in_=sc[:].unsqueeze(3).to_broadcast([128, 2, 8, 128])),
                 reads=["sc"], writes=["scB"])
            w_mod_v = w_mod.rearrange("(k p) n -> p k n", p=128)
            for cch in range(12):
                bi = cch % 2
                P.dma("sp", "wm%d" % bi, (lambda bi, cch: lambda e: e.dma_start(out=wm[bi][:], in_=w_mod_v[:, :, cch * 512:(cch + 1) * 512]))(bi, cch),
                      writes=["wm%d" % bi])
                for which in range(2 if cch < 4 else 1):
                    for k in range(8):
                        P.op("pe", (lambda bi, which, k: lambda e: e.matmul(pmod[which][:], lhsT=scB[:, which, k, :], rhs=wm[bi][:, k, :],
                                                                            start=(k == 0), stop=(k == 7)))(bi, which, k),
                             reads=["scB", "wm%d" % bi], writes=["pmod%d" % which])
                    dst = modB if which == 0 else modcB
                    P.op("dve", (lambda dst, which, cch: lambda e: e.tensor_tensor(out=dst[:, cch * 512:(cch + 1) * 512], in0=pmod[which][:],
                                                                                   in1=bmB[:, cch * 512:(cch + 1) * 512], op=ALU.add))(dst, which, cch),
                         reads=["pmod%d" % which, "bmB"], writes=["modB" if which == 0 else "modcB"])
            for dst, key, off, gi in ((modB, "modB", D, 0), (modcB, "modcB", D, 0), (modB, "modB", 4 * D, 1)):
                P.op("dve", (lambda dst, off, gi: lambda e: e.scalar_tensor_tensor(out=dst[:, off:off + D], in0=dst[:, off:off + D], scalar=1.0,
                                                                                  in1=gB[:, gi, :], op0=ALU.add, op1=ALU.mult))(dst, off, gi),
                     reads=[key, "gB"], writes=[key])

            P.barrier()
            st0.close()
            w_in_v = w_in.rearrange("(k p) n -> p k n", p=128)
            for cch in range(8):
                bi = cch % 2
                P.dma("sp", "wm%d" % bi, (lambda bi, cch: lambda e: e.dma_start(out=wm[bi][:], in_=w_in_v[:, :, cch * 512:(cch + 1) * 512]))(bi, cch),
                      reads=[], writes=["wm%d" % bi])
                eng = ("pool", "dve")[cch % 2]
                P.op(eng, (lambda bi, cch: lambda e: e.tensor_copy(out=w_in_sb[:, :, cch * 512:(cch + 1) * 512], in_=wm[bi][:]))(bi, cch),
                     reads=["wm%d" % bi], writes=["w_in_sb"])

            xt = [sbuf(st, "xt%d" % i, [128, D]) for i in range(3)]; xr = Rot("xt", 3)
            junk = sbuf(st, "junkA", [128, D]); tmpA = sbuf(st, "tmpA", [128, D])
            ss = [sbuf(st, "ss%d" % i, [128, 4]) for i in range(2)]; ssr = Rot("ss", 2)
            hxb = [sbuf(st, "hxb%d" % i, [128, D], BF16) for i in range(2)]; hr = Rot("hxb", 2)
            hxT = [sbuf(st, "hxT%d" % i, [128, 8, 512], BF16) for i in range(2)]; hTr = Rot("hxT", 2)
            st_u = sbuf(st, "st_u", [128, 4, 512]); st_k = sbuf(st, "st_k", [128, 4, 512], BF16)
            st_q = sbuf(st, "st_q", [128, 4, 512], BF16); st_g = sbuf(st, "st_g", [128, 16, 512], BF16)
            st_v = sbuf(st, "st_v", [128, 4, 512], BF16)
            tp = [psum(st, "tpA%d" % i, [128, 8, 128], BF16) for i in range(2)]; tpr = Rot("tpA", 2)
            pj = [psum(st, "pj%d" % i, [128, 512]) for i in range(4)]; pjr = Rot("pj", 4)
            evac_i = [0]

            def evac(dst_ap, src_ap, reads, writes, func=None):
                if func is not None:
                    P.op("act", lambda e: e.activation(out=dst_ap, in_=src_ap, func=func), reads, writes)
                    return
                evac_i[0] += 1
                if evac_i[0] % 2:
                    P.op("act", lambda e: e.copy(out=dst_ap, in_=src_ap), reads, writes)
                else:
                    P.op("dve", lambda e: e.tensor_copy(out=dst_ap, in_=src_ap), reads, writes)

            for i_ in range(2):
                P.op("pool", (lambda i_: lambda e: e.memset(hxT[i_][:], 0.0))(i_), writes=["hxT%d" % i_])
            chunks = [("ctx", 0, 256)] + [("lat", i * 512, 512) for i in range(8)]
            for kind, t0, n in chunks:
                src = ctx if kind == "ctx" else x
                mB, mkey = (modcB, "modcB") if kind == "ctx" else (modB, "modB")
                col0 = t0 if kind == "ctx" else CTX + t0
                hi, hkey = hTr.next()
                for t in range(n // 128):
                    xi, xkey = xr.next()
                    si, skey = ssr.next()
                    bi, bkey = hr.next()
                    pi, pkey = tpr.next()
                    r0 = t0 + t * 128
                    P.dma("sp", xkey, (lambda xi, r0, src: lambda e: e.dma_start(out=xt[xi][:], in_=src[r0:r0 + 128, :]))(xi, r0, src), writes=[xkey])
                    P.op("act", (lambda xi, si: lambda e: e.activation(out=junk[:], in_=xt[xi][:], func=AF.Square, accum_out=ss[si][:, 0:1]))(xi, si),
                         reads=[xkey], writes=["junkA", skey])
                    P.op("dve", (lambda si: lambda e: e.tensor_scalar(out=ss[si][:, 1:2], in0=ss[si][:, 0:1], scalar1=1.0 / D, scalar2=EPS,
                                                                      op0=ALU.mult, op1=ALU.add))(si), reads=[skey], writes=[skey])
                    P.op("act", (lambda si: lambda e: e.sqrt(out=ss[si][:, 2:3], in_=ss[si][:, 1:2]))(si), reads=[skey], writes=[skey])
                    P.op("dve", (lambda si: lambda e: e.reciprocal(out=ss[si][:, 3:4], in_=ss[si][:, 2:3]))(si), reads=[skey], writes=[skey])
                    P.op("dve", (lambda xi, si, mB: lambda e: e.scalar_tensor_tensor(out=tmpA[:], in0=xt[xi][:], scalar=ss[si][:, 3:4], in1=mB[:, D:2 * D],
                                                                                     op0=ALU.mult, op1=ALU.mult))(xi, si, mB),
                         reads=[xkey, skey, mkey], writes=["tmpA"])
                    P.op("dve", (lambda bi, mB: lambda e: e.tensor_tensor(out=hxb[bi][:], in0=tmpA[:], in1=mB[:, 0:D], op=ALU.add))(bi, mB),
                         reads=["tmpA", mkey], writes=[bkey])
                    for k in range(8):
                        P.op("pe", (lambda pi, bi, k: lambda e: e.transpose(tp[pi][:, k, :], hxb[bi][:, k * 128:(k + 1) * 128], identb[:]))(pi, bi, k),
                             reads=[bkey, "identb"], writes=[pkey])
                    P.op("act", (lambda hi, pi, t: lambda e: e.copy(out=hxT[hi][:, :, t * 128:(t + 1) * 128], in_=tp[pi][:]))(hi, pi, t),
                         reads=[pkey], writes=[hkey])
                cts = list(range(0, 8)) + (list(range(12, 32)) if kind == "lat" else [])
                for ct in cts:
                    qi, qkey = pjr.next()
                    for k in range(8):
                        P.op("pe", (lambda qi, hi, k, ct: lambda e: e.matmul(pj[qi][:, 0:n], lhsT=w_in_sb[:, k, ct * 128:(ct + 1) * 128], rhs=hxT[hi][:, k, 0:n],
                                                                             start=(k == 0), stop=(k == 7)))(qi, hi, k, ct),
                             reads=["w_in_sb", hkey], writes=[qkey])
                    if ct < 4:
                        evac(st_u[:, ct, 0:n], pj[qi][:, 0:n], [qkey], ["st_u"])
                    elif ct < 8:
                        evac(st_k[:, ct - 4, 0:n], pj[qi][:, 0:n], [qkey], ["st_k"])
                    elif ct < 16:
                        evac(st_q[:, ct - 12, 0:n], pj[qi][:, 0:n], [qkey], ["st_q"])
                    else:
                        evac(st_g[:, ct - 16, 0:n], pj[qi][:, 0:n], [qkey], ["st_g"], func=AF.Sigmoid)
                P.dma("pool", "stu", (lambda col0, n: lambda e: e.dma_start(out=uT_d.rearrange("(t p) n -> p t n", p=128)[:, :, col0:col0 + n], in_=st_u[:, :, 0:n]))(col0, n),
                      reads=["st_u"], writes=["uT_d"])
                P.dma("pool", "stk", (lambda col0, n: lambda e: e.dma_start(out=kT_d.rearrange("(t p) n -> p t n", p=128)[:, :, col0:col0 + n], in_=st_k[:, :, 0:n]))(col0, n),
                      reads=["st_k"], writes=["kT_d"])
                if kind == "lat":
                    P.dma("pool", "stq", (lambda t0: lambda e: e.dma_start(out=qT_d.rearrange("(t p) n -> p t n", p=128)[:, :, t0:t0 + 512], in_=st_q[:]))(t0),
                          reads=["st_q"], writes=["qT_d"])
                    P.dma("pool", "stg", (lambda t0: lambda e: e.dma_start(out=gT_d.rearrange("(t p) n -> p t n", p=128)[:, :, t0:t0 + 512], in_=st_g[:]))(t0),
                          reads=["st_g"], writes=["gT_d"])
                for t in range(n // 128):
                    qi, qkey = pjr.next()
                    for k in range(8):
                        P.op("pe", (lambda qi, hi, k, t: lambda e: e.matmul(pj[qi][:], lhsT=hxT[hi][:, k, t * 128:(t + 1) * 128], rhs=w_in_sb[:, k, 1024:1536],
                                                                            start=(k == 0), stop=(k == 7)))(qi, hi, k, t),
                             reads=["w_in_sb", hkey], writes=[qkey])
                    evac(st_v[:, t, :], pj[qi][:], [qkey], ["st_v"])
                nt = n // 128
                P.dma("pool", "stv", (lambda col0, nt: lambda e: e.dma_start(out=v_d[col0:col0 + nt * 128, :].rearrange("(t p) n -> p t n", p=128), in_=st_v[:, 0:nt, :]))(col0, nt),
                      reads=["st_v"], writes=["v_d"])
        P.barrier()
        if stage <= 1:
            return finish(nc, P, out)

        yT_d = dscr("yT_d", [512, SEQ], F32) if debug else None
        TWO_PI = 2.0 * math.pi
        with ExitStack() as stB:
            zT = sbuf(stB, "zT", [128, 4, SEQ], BF16)
            with phase() as st:
                def t32(n):
                    return sbuf(st, n, [128, 32])
                are, aim, ldt = t32("are"), t32("aim"), t32("ldt")
                Bn = [sbuf(st, "Bn%d" % i, [128, 32, 16]) for i in range(2)]
                bb = [sbuf(st, "bb%d" % i, [128, 32, 16]) for i in range(2)]
                tmpb = sbuf(st, "tmpb", [128, 32, 16])
                Cn2 = [sbuf(st, "Cn2%d" % i, [128, 8, 2, 64]) for i in range(2)]
                dsk = sbuf(st, "dsk", [128, 4])
                maskf = sbuf(st, "maskf", [128, 4, 2]); mask2 = sbuf(st, "mask2", [128, 4, 2])
                pwr = sbuf(st, "pwr", [128, 13, 32]); pwi = sbuf(st, "pwi", [128, 13, 32]); npwi = sbuf(st, "npwi", [128, 13, 32])
                kint = sbuf(st, "kint", [128, 32], I32)
                names = ["dt", "er", "th", "mag", "kf", "rr", "half", "sn", "ah", "cq", "sinr", "cosr", "nre", "den", "rden",
                         "fre", "fim", "t1", "t2"]
                T = {n: t32("p_" + n) for n in names}
                uT_sb = [sbuf(st, "uT_sb%d" % i, [128, NTOK]) for i in range(1)]
                PL = [sbuf(st, "PL%d" % i, [128, 2, NTOK]) for i in range(2)]
                yT = sbuf(st, "yT", [128, SEQ])
                Z = [sbuf(st, "Z%d" % i, [128, 2, 128]) for i in range(2)]
                Zc = [sbuf(st, "Zc%d" % i, [128, 2, 128]) for i in range(2)]
                LB = [sbuf(st, "LB%d" % i, [128, 2, 128]) for i in range(2)]
                LC = [sbuf(st, "LC%d" % i, [128, 2, 128]) for i in range(2)]
                pz = [psum(st, "pz%d" % i, [128, 2, 128]) for i in range(2)]; pzr = Rot("pz", 2)
                pb = [psum(st, "pb%d" % i, [128, 512]) for i in range(3)]; pbr = Rot("pb", 3)
                py = [psum(st, "py%d" % i, [128, 512]) for i in range(2)]; pyr = Rot("py", 2)

                for gl in range(2):
                    sl = slice(gl * 64, (gl + 1) * 64)
                    for dst, srcp, key in ((are, a_re, "are"), (aim, a_im, "aim")):
                        P.dma("act", "pb0", (lambda dst, srcp, sl, gl: lambda e: e.dma_start(
                            out=dst[sl, :].rearrange("p (d g) -> p d g", d=2),
                            in_=srcp.rearrange("d (gp gl) p -> gl p d gp", gl=2)[gl], allow_slow_non_contiguous=True))(dst, srcp, sl, gl), writes=[key])
                    P.dma("act", "pb0", (lambda sl, gl: lambda e: e.dma_start(
                        out=ldt[sl, :].rearrange("p (d g) -> p d g", d=2),
                        in_=log_dt.rearrange("d (gp gl) -> gl d gp", gl=2)[gl:gl + 1].to_broadcast([64, 2, 16]), allow_slow_non_contiguous=True))(sl, gl), writes=["ldt"])
                    for i, srcp in enumerate((b_re, b_im)):
                        P.dma("act", "pb0", (lambda i, srcp, sl, gl: lambda e: e.dma_start(
                            out=Bn[i][sl].rearrange("p (d g) h -> p d g h", d=2),
                            in_=srcp.rearrange("d (gp gl) p h -> gl p d gp h", gl=2)[gl]))(i, srcp, sl, gl), writes=["Bn%d" % i])
                for i, srcp in enumerate((c_re, c_im)):
                    for j in range(2):
                        P.dma("act", "pb0", (lambda i, srcp, j: lambda e: e.dma_start(
                            out=Cn2[i][:, :, j, :].rearrange("p (d u) q -> p d u q", d=2),
                            in_=srcp.rearrange("d (ut g8) h p -> (g8 h) d ut p", g8=8)))(i, srcp, j), writes=["Cn2%d" % i])
                P.dma("act", "pb0", lambda e: e.dma_start(out=dsk[:], in_=ssm_d.rearrange("(ut p) o -> p (ut o)", p=128), allow_slow_non_contiguous=True), writes=["dsk"])
                P.op("pool", lambda e: e.iota(maskf[:], pattern=[[-32, 4], [-16, 2]], base=0, channel_multiplier=1, allow_small_or_imprecise_dtypes=True), writes=["maskf"])
                P.op("dve", lambda e: e.tensor_single_scalar(out=mask2[:], in_=maskf[:], scalar=0.0, op=ALU.is_ge), reads=["maskf"], writes=["mask2"])
                P.op("dve", lambda e: e.tensor_single_scalar(out=maskf[:], in_=maskf[:], scalar=16.0, op=ALU.is_lt), reads=["maskf", "mask2"], writes=["maskf"])
                P.op("dve", lambda e: e.tensor_tensor(out=maskf[:], in0=maskf[:], in1=mask2[:], op=ALU.mult), reads=["maskf", "mask2"], writes=["maskf"])

                PK = ["are", "aim", "ldt", "Bn0", "Bn1", "prm"]

                def dve(fn):
                    P.op("dve", fn, reads=PK, writes=["prm"])

                def act(fn):
                    P.op("act", fn, reads=PK, writes=["prm"])
                act(lambda e: e.activation(out=T["dt"][:], in_=ldt[:], func=AF.Exp))
                dve(lambda e: e.tensor_tensor(out=T["er"][:], in0=are[:], in1=T["dt"][:], op=ALU.mult))
                dve(lambda e: e.tensor_tensor(out=T["th"][:], in0=aim[:], in1=T["dt"][:], op=ALU.mult))
                act(lambda e: e.activation(out=T["mag"][:], in_=T["er"][:], func=AF.Exp))
                dve(lambda e: e.tensor_single_scalar(out=T["kf"][:], in_=T["th"][:], scalar=1.0 / TWO_PI, op=ALU.mult))
                dve(lambda e: e.tensor_copy(out=kint[:], in_=T["kf"][:]))
                dve(lambda e: e.tensor_copy(out=T["kf"][:], in_=kint[:]))
                dve(lambda e: e.scalar_tensor_tensor(out=T["rr"][:], in0=T["kf"][:], scalar=-TWO_PI, in1=T["th"][:], op0=ALU.mult, op1=ALU.add))
                dve(lambda e: e.tensor_single_scalar(out=T["half"][:], in_=T["rr"][:], scalar=0.5, op=ALU.mult))
                act(lambda e: e.activation(out=T["ah"][:], in_=T["half"][:], func=AF.Abs))
                dve(lambda e: e.tensor_scalar(out=T["t1"][:], in0=T["ah"][:], scalar1=-1.0, scalar2=math.pi / 2, op0=ALU.mult, op1=ALU.add))
                act(lambda e: e.activation(out=T["sn"][:], in_=T["half"][:], func=AF.Sin))
                act(lambda e: e.activation(out=T["cq"][:], in_=T["t1"][:], func=AF.Sin))
                dve(lambda e: e.scalar_tensor_tensor(out=T["sinr"][:], in0=T["sn"][:], scalar=2.0, in1=T["cq"][:], op0=ALU.mult, op1=ALU.mult))
                dve(lambda e: e.scalar_tensor_tensor(out=T["t2"][:], in0=T["sn"][:], scalar=-2.0, in1=T["sn"][:], op0=ALU.mult, op1=ALU.mult))
                dve(lambda e: e.tensor_single_scalar(out=T["cosr"][:], in_=T["t2"][:], scalar=1.0, op=ALU.add))
                dve(lambda e: e.tensor_tensor(out=pwr[:, 0, :], in0=T["mag"][:], in1=T["cosr"][:], op=ALU.mult))
                dve(lambda e: e.tensor_tensor(out=pwi[:, 0, :], in0=T["mag"][:], in1=T["sinr"][:], op=ALU.mult))
                dve(lambda e: e.tensor_single_scalar(out=T["nre"][:], in_=pwr[:, 0, :], scalar=-1.0, op=ALU.add))
                dve(lambda e: e.tensor_tensor(out=T["den"][:], in0=are[:], in1=are[:], op=ALU.mult))
                dve(lambda e: e.tensor_tensor(out=T["t1"][:], in0=aim[:], in1=aim[:], op=ALU.mult))
                dve(lambda e: e.tensor_tensor(out=T["den"][:], in0=T["den"][:], in1=T["t1"][:], op=ALU.add))
                dve(lambda e: e.reciprocal(out=T["rden"][:], in_=T["den"][:]))
                dve(lambda e: e.tensor_tensor(out=T["t1"][:], in0=T["nre"][:], in1=are[:], op=ALU.mult))
                dve(lambda e: e.tensor_tensor(out=T["t2"][:], in0=pwi[:, 0, :], in1=aim[:], op=ALU.mult))
                dve(lambda e: e.tensor_tensor(out=T["t1"][:], in0=T["t1"][:], in1=T["t2"][:], op=ALU.add))
                dve(lambda e: e.tensor_tensor(out=T["fre"][:], in0=T["t1"][:], in1=T["rden"][:], op=ALU.mult))
                dve(lambda e: e.tensor_tensor(out=T["t1"][:], in0=pwi[:, 0, :], in1=are[:], op=ALU.mult))
                dve(lambda e: e.tensor_tensor(out=T["t2"][:], in0=T["nre"][:], in1=aim[:], op=ALU.mult))
                dve(lambda e: e.tensor_tensor(out=T["t1"][:], in0=T["t1"][:], in1=T["t2"][:], op=ALU.subtract))
                dve(lambda e: e.tensor_tensor(out=T["fim"][:], in0=T["t1"][:], in1=T["rden"][:], op=ALU.mult))
                fr = T["fre"][:].unsqueeze(2).to_broadcast([128, 32, 16]); fi = T["fim"][:].unsqueeze(2).to_broadcast([128, 32, 16])
                dve(lambda e: e.tensor_tensor(out=bb[0][:], in0=Bn[0][:], in1=fr, op=ALU.mult))
                dve(lambda e: e.tensor_tensor(out=tmpb[:], in0=Bn[1][:], in1=fi, op=ALU.mult))
                dve(lambda e: e.tensor_tensor(out=bb[0][:], in0=bb[0][:], in1=tmpb[:], op=ALU.subtract))
                dve(lambda e: e.tensor_tensor(out=bb[1][:], in0=Bn[1][:], in1=fr, op=ALU.mult))
                dve(lambda e: e.tensor_tensor(out=tmpb[:], in0=Bn[0][:], in1=fi, op=ALU.mult))
                dve(lambda e: e.tensor_tensor(out=bb[1][:], in0=bb[1][:], in1=tmpb[:], op=ALU.add))
                for k in range(12):
                    dve((lambda k: lambda e: e.tensor_tensor(out=T["t1"][:], in0=pwr[:, k, :], in1=pwr[:, k, :], op=ALU.mult))(k))
                    dve((lambda k: lambda e: e.tensor_tensor(out=T["t2"][:], in0=pwi[:, k, :], in1=pwi[:, k, :], op=ALU.mult))(k))
                    dve((lambda k: lambda e: e.tensor_tensor(out=pwr[:, k + 1, :], in0=T["t1"][:], in1=T["t2"][:], op=ALU.subtract))(k))
                    dve((lambda k: lambda e: e.scalar_tensor_tensor(out=pwi[:, k + 1, :], in0=pwr[:, k, :], scalar=2.0, in1=pwi[:, k, :], op0=ALU.mult, op1=ALU.mult))(k))
                dve(lambda e: e.tensor_single_scalar(out=npwi[:], in_=pwi[:], scalar=-1.0, op=ALU.mult))

                chain = {}

                def cmul_acc(hi_re, hi_im, lo_re, lo_im, k, u, key):
                    sr = pwr[:, k, u:u + 1]; si = pwi[:, k, u:u + 1]; nsi = npwi[:, k, u:u + 1]
                    prev = chain.get(key)
                    if prev is None:
                        prev = [w for w in (P.writer.get(key), P.writer.get("prm")) if w is not None]
                    ops_ = []
                    for n_, (o_, a_, s_) in enumerate(((hi_re, lo_re, sr), (hi_im, lo_re, si), (hi_re, lo_im, nsi), (hi_im, lo_im, sr))):
                        op = _Op("dve", (lambda o_, a_, s_: lambda e: e.scalar_tensor_tensor(out=o_, in0=a_, scalar=s_, in1=o_, op0=ALU.mult, op1=ALU.add))(o_, a_, s_), len(P.ops))
                        op.deps = list(prev) if n_ < 2 else [ops_[n_ - 2]]
                        P.ops.append(op)
                        ops_.append(op)
                    chain[key] = [ops_[3]]

                def scan_done(key):
                    P.writer[key] = chain.pop(key)[0]
                    P.readers[key] = []

                def bk_scan(pl, c0, n, rev, u, key, up_only=False):
                    L = n.bit_length() - 1
                    re = pl[:, 0, c0:c0 + n]; im = pl[:, 1, c0:c0 + n]
                    for k in range(L):
                        s_ = 2 << k; h_ = 1 << k
                        vr = re.rearrange("p (m s) -> p m s", s=s_); vi = im.rearrange("p (m s) -> p m s", s=s_)
                        if not rev:
                            cmul_acc(vr[:, :, s_ - 1], vi[:, :, s_ - 1], vr[:, :, h_ - 1], vi[:, :, h_ - 1], k, u, key)
                        else:
                            cmul_acc(vr[:, :, 0], vi[:, :, 0], vr[:, :, h_], vi[:, :, h_], k, u, key)
                    for k in (range(L - 2, -1, -1) if not up_only else ()):
                        s_ = 2 << k; h_ = 1 << k
                        vr = re.rearrange("p (m s) -> p m s", s=s_); vi = im.rearrange("p (m s) -> p m s", s=s_)
                        if not rev:
                            cmul_acc(vr[:, 1:, h_ - 1], vi[:, 1:, h_ - 1], vr[:, :-1, s_ - 1], vi[:, :-1, s_ - 1], k, u, key)
                        else:
                            cmul_acc(vr[:, :-1, h_], vi[:, :-1, h_], vr[:, 1:, 0], vi[:, 1:, 0], k, u, key)

                segs = [(0, 256)] + [(CTX + i * 512, 512) for i in range(8)]
                units = [(ut, d_, gpl) for ut in range(4) for d_ in range(2) for gpl in range(4)]

                def stA(ix):
                    ut, d_, gpl = units[ix]
                    u = d_ * 16 + ut * 4 + gpl
                    bi = ix % 2; ub = 0; ukey = "uT_sb0"
                    zk, zck, lbk, lck, plk = "Z%d" % bi, "Zc%d" % bi, "LB%d" % bi, "LC%d" % bi, "PL%d" % bi
                    if ix % 8 == 0:
                        P.dma("sp", ukey, lambda e: e.dma_start(out=uT_sb[ub][:], in_=uT_d[ut * 128:(ut + 1) * 128, :]), reads=["uT_d"], writes=[ukey])
                    P.op("pool", lambda e: e.memset(Z[bi][:], 0.0), writes=[zk])
                    for j in range(2):
                        for gl in range(2):
                            cs = (2 * gpl + gl) * 16
                            P.op("pool", (lambda j, gl, cs: lambda e: e.tensor_copy(out=Z[bi][gl * 64:(gl + 1) * 64, j, cs:cs + 16], in_=bb[j][gl * 64:(gl + 1) * 64, u, :]))(j, gl, cs),
                                 reads=["prm"], writes=[zk])
                    zi, zkey = pzr.next()
                    for j in range(2):
                        P.op("pe", (lambda zi, j: lambda e: e.matmul(pz[zi][:, j, :], lhsT=Z[bi][:, j, :], rhs=identf[:], start=True, stop=True))(zi, j), reads=[zk, "identf"], writes=[zkey])
                    P.op("act", (lambda zi: lambda e: e.copy(out=LB[bi][:], in_=pz[zi][:]))(zi), reads=[zkey], writes=[lbk])
                    for j in range(2):
                        P.op("pool", (lambda j: lambda e: e.tensor_tensor(out=Zc[bi][:, j, :].rearrange("p (g q) -> p g q", g=2), in0=Cn2[j][:, d_ * 4 + ut, :, :],
                                                                         in1=maskf[:, gpl, :].unsqueeze(2).to_broadcast([128, 2, 64]), op=ALU.mult))(j),
                             reads=["Cn2%d" % j, "maskf"], writes=[zck])
                    zi2, zkey2 = pzr.next()
                    for j in range(2):
                        P.op("pe", (lambda zi2, j: lambda e: e.matmul(pz[zi2][:, j, :], lhsT=Zc[bi][:, j, :], rhs=identf[:], start=True, stop=True))(zi2, j), reads=[zck, "identf"], writes=[zkey2])
                    P.op("act", lambda e: e.copy(out=LC[bi][:, 0, :], in_=pz[zi2][:, 0, :]), reads=[zkey2], writes=[lck])
                    P.op("act", lambda e: e.mul(out=LC[bi][:, 1, :], in_=pz[zi2][:, 1, :], mul=-1.0), reads=[zkey2], writes=[lck])
                    for (c0, n) in segs:
                        for j in range(2):
                            qi, qkey = pbr.next()
                            P.op("pe", (lambda qi, j, c0, n: lambda e: e.matmul(pb[qi][:, 0:n], lhsT=LB[bi][:, j, :], rhs=uT_sb[ub][:, c0:c0 + n], start=True, stop=True))(qi, j, c0, n),
                                 reads=[lbk, ukey], writes=[qkey])
                            P.op("act", (lambda qi, j, c0, n: lambda e: e.copy(out=PL[bi][:, j, c0:c0 + n], in_=pb[qi][:, 0:n]))(qi, j, c0, n), reads=[qkey], writes=[plk])

                def stB(ix):
                    ut, d_, gpl = units[ix]
                    u = d_ * 16 + ut * 4 + gpl
                    bi = ix % 2; plk = "PL%d" % bi
                    rev = (d_ == 1)
                    bk_scan(PL[bi], 0, CTX, rev, u, plk, up_only=True)
                    if not rev:
                        cmul_acc(PL[bi][:, 0, CTX:CTX + 1], PL[bi][:, 1, CTX:CTX + 1], PL[bi][:, 0, CTX - 1:CTX], PL[bi][:, 1, CTX - 1:CTX], 0, u, plk)
                    else:
                        cmul_acc(PL[bi][:, 0, NTOK - 1:NTOK], PL[bi][:, 1, NTOK - 1:NTOK], PL[bi][:, 0, 0:1], PL[bi][:, 1, 0:1], 0, u, plk)
                    bk_scan(PL[bi], CTX, SEQ, rev, u, plk)
                    scan_done(plk)

                def stC(ix):
                    ut, d_, gpl = units[ix]
                    bi = ix % 2; ub = 0; ukey = "uT_sb0"; lck, plk = "LC%d" % bi, "PL%d" % bi
                    first = (ix % 8 == 0)
                    for sgi in range(8):
                        c0 = CTX + sgi * 512
                        yi, ykey = pyr.next()
                        for j in range(2):
                            P.op("pe", (lambda yi, j, c0: lambda e: e.matmul(py[yi][:], lhsT=LC[bi][:, j, :], rhs=PL[bi][:, j, c0:c0 + 512], start=(j == 0), stop=(j == 1)))(yi, j, c0),
                                 reads=[lck, plk], writes=[ykey])
                        ysl = slice(sgi * 512, (sgi + 1) * 512)
                        if first:
                            P.op("dve", (lambda yi, c0, ysl: lambda e: e.scalar_tensor_tensor(out=yT[:, ysl], in0=uT_sb[ub][:, c0:c0 + 512], scalar=dsk[:, ut:ut + 1],
                                                                                              in1=py[yi][:], op0=ALU.mult, op1=ALU.add))(yi, c0, ysl),
                                 reads=[ykey, ukey, "dsk"], writes=["yT"])
                        else:
                            P.op("dve", (lambda yi, ysl: lambda e: e.tensor_tensor(out=yT[:, ysl], in0=yT[:, ysl], in1=py[yi][:], op=ALU.add))(yi, ysl), reads=[ykey], writes=["yT"])
                    if ix % 8 == 7:
                        if debug:
                            P.dma("sp", "dbgy", lambda e: e.dma_start(out=yT_d[ut * 128:(ut + 1) * 128, :], in_=yT[:]), reads=["yT"], writes=["yT_d"])
                        P.op("act", lambda e: e.activation(out=zT[:, ut, :], in_=yT[:], func=AF.Gelu_apprx_tanh), reads=["yT"], writes=["zT"])

                stA(0)
                for ix in range(32):
                    if ix + 1 < 32:
                        stA(ix + 1)
                    stB(ix)
                    stC(ix)
            P.barrier()
            with phase() as st:
                wstgB_t = sbuf(st, "wstgB", [128, 4, 1024])
                w_glu_sb = sbuf(st, "w_glu_sb", [128, 4, 512], BF16); w_ba_sb = sbuf(st, "w_ba_sb", [128, 4, D], BF16)
                bglu = sbuf(st, "bglu", [128, 4])
                sg = [sbuf(st, "sg%d" % i, [128, 512], BF16) for i in range(2)]; sgr = Rot("sg", 2)
                glu = [sbuf(st, "glu%d" % i, [128, 4, 512], BF16) for i in range(2)]
                st_ba = [sbuf(st, "st_ba%d" % i, [128, 8, 512], BF16) for i in range(2)]
                pg = [psum(st, "pg%d" % i, [128, 512]) for i in range(3)]; pgr = Rot("pg", 3)
                pa = [psum(st, "pa%d" % i, [128, 512]) for i in range(3)]; par = Rot("pa", 3)
                P.dma("sp", "wl0", lambda e: e.dma_start(out=wstgB_t[:, :, 0:512], in_=w_glu.rearrange("(k p) n -> p k n", p=128)), writes=["wstgB"])
                P.op("dve", lambda e: e.tensor_copy(out=w_glu_sb[:], in_=wstgB_t[:, :, 0:512]), reads=["wstgB"], writes=["w_glu_sb"])
                P.dma("sp", "wl0", lambda e: e.dma_start(out=wstgB_t[:], in_=w_ba.rearrange("(k p) n -> p k n", p=128)), reads=["wstgB"], writes=["wstgB"])
                P.op("dve", lambda e: e.tensor_copy(out=w_ba_sb[:], in_=wstgB_t[:]), reads=["wstgB"], writes=["w_ba_sb"])
                P.dma("act", "wl1", lambda e: e.dma_start(out=bglu[:], in_=b_glu.rearrange("(k p) o -> p (k o)", p=128), allow_slow_non_contiguous=True), writes=["bglu"])
                for sgi in range(8):
                    gb_ = sgi % 2; gkey = "glu%d" % gb_; bakey = "st_ba%d" % gb_
                    ssl = slice(sgi * 512, (sgi + 1) * 512)
                    for ct in range(4):
                        gi, gk = pgr.next()
                        for k in range(4):
                            P.op("pe", (lambda gi, k, ct, ssl: lambda e: e.matmul(pg[gi][:], lhsT=w_glu_sb[:, k, ct * 128:(ct + 1) * 128], rhs=zT[:, k, ssl],
                                                                                  start=(k == 0), stop=(k == 3)))(gi, k, ct, ssl),
                                 reads=["w_glu_sb", "zT"], writes=[gk])
                        si_, sk_ = sgr.next()
                        P.op("act", (lambda si_, gi, ct: lambda e: e.activation(out=sg[si_][:], in_=pg[gi][:], func=AF.Sigmoid, bias=bglu[:, ct:ct + 1]))(si_, gi, ct),
                             reads=[gk, "bglu"], writes=[sk_])
                        P.op("dve", (lambda gb_, ct, si_, ssl: lambda e: e.tensor_tensor(out=glu[gb_][:, ct, :], in0=sg[si_][:], in1=zT[:, ct, ssl], op=ALU.mult))(gb_, ct, si_, ssl),
                             reads=[sk_, "zT"], writes=[gkey])
                    for ct2 in range(8):
                        ai, ak = par.next()
                        for k in range(4):
                            P.op("pe", (lambda ai, k, ct2, gb_: lambda e: e.matmul(pa[ai][:], lhsT=w_ba_sb[:, k, ct2 * 128:(ct2 + 1) * 128], rhs=glu[gb_][:, k, :],
                                                                                   start=(k == 0), stop=(k == 3)))(ai, k, ct2, gb_),
                                 reads=["w_ba_sb", gkey], writes=[ak])
                        if ct2 % 2:
                            P.op("act", (lambda gb_, ct2, ai: lambda e: e.copy(out=st_ba[gb_][:, ct2, :], in_=pa[ai][:]))(gb_, ct2, ai), reads=[ak], writes=[bakey])
                        else:
                            P.op("dve", (lambda gb_, ct2, ai: lambda e: e.tensor_copy(out=st_ba[gb_][:, ct2, :], in_=pa[ai][:]))(gb_, ct2, ai), reads=[ak], writes=[bakey])
                    P.dma("sp", bakey, (lambda gb_, ssl: lambda e: e.dma_start(out=baT_d.rearrange("(t p) n -> p t n", p=128)[:, :, ssl], in_=st_ba[gb_][:]))(gb_, ssl),
                          reads=[bakey], writes=["baT_d"])
        P.barrier()
        if stage <= 2:
            return finish(nc, P, out)

        attT_d = dscr("attT_d", [512, SEQ], BF16) if debug else None
        NEG = -30000.0
        with ExitStack() as stC:
            attT_sb = sbuf(stC, "attT_sb", [128, 4, SEQ], BF16)
            stC2 = ExitStack()
            kT_sb = sbuf(stC2, "kT_sb", [128, 4, NTOK], BF16); qT_sb = sbuf(stC2, "qT_sb", [128, 4, SEQ], BF16)
            BiasTT = sbuf(stC2, "BiasTT", [128, 8 * 14, 64])
            Vctx = sbuf(stC2, "Vctx", [128, 2, 512], BF16)
            ones_b = sbuf(stC2, "ones_b", [128, 128], BF16)
            P.dma("sp", "lc0", lambda e: e.dma_start(out=kT_sb[:], in_=kT_d.rearrange("(t p) n -> p t n", p=128)), reads=["kT_d"], writes=["kT_sb"])
            P.dma("act", "lc1", lambda e: e.dma_start(out=qT_sb[:], in_=qT_d.rearrange("(t p) n -> p t n", p=128)), reads=["qT_d"], writes=["qT_sb"])
            P.dma("act", "lc1", lambda e: e.dma_start(out=Vctx[:], in_=v_d[0:CTX, :].rearrange("(t p) n -> p t n", p=128)), reads=["v_d"], writes=["Vctx"])
            P.op("pool", lambda e: e.memset(ones_b[:], 1.0), writes=["ones_b"])
            with phase() as st:
                rpbB = sbuf(st, "rpbB", [128, 8 * 14, 31]); tmpC = sbuf(st, "tmpC", [128, 8 * 14, 64])
                Dm = sbuf(st, "Dm", [128, 64]); eqm = [sbuf(st, "eqm%d" % i, [128, 64]) for i in range(2)]
                c0t = sbuf(st, "c0t", [128, 64]); kcv = sbuf(st, "kcv", [128, 64]); m2 = sbuf(st, "m2c", [128, 64])
                for half in range(2):
                    sl = slice(half * 64, (half + 1) * 64)
                    P.dma("sp", "lc2", (lambda sl, half: lambda e: e.dma_start(out=rpbB[sl].rearrange("p (h j) m -> p h (j m)", h=8),
                                                                              in_=rpb[:, half:half + 14, :].rearrange("h j m -> h (j m)").unsqueeze(0).to_broadcast([64, 8, 14 * 31])))(sl, half),
                          writes=["rpbB"])
                    P.op("pool", (lambda sl: lambda e: e.iota(Dm[sl], pattern=[[-1, 64]], base=15, channel_multiplier=1, allow_small_or_imprecise_dtypes=True))(sl), writes=["Dm"])
                    P.op("pool", (lambda sl: lambda e: e.iota(kcv[sl], pattern=[[0, 64]], base=0, channel_multiplier=1, allow_small_or_imprecise_dtypes=True))(sl), writes=["kcv"])
                P.op("pool", lambda e: e.iota(c0t[:], pattern=[[1, 64]], base=-8, channel_multiplier=0, allow_small_or_imprecise_dtypes=True), writes=["c0t"])
                P.op("dve", lambda e: e.tensor_scalar(out=c0t[:], in0=c0t[:], scalar1=0.0, scalar2=48.0, op0=ALU.max, op1=ALU.min), reads=["c0t"], writes=["c0t"])
                P.op("dve", lambda e: e.tensor_tensor(out=kcv[:], in0=kcv[:], in1=c0t[:], op=ALU.subtract), reads=["kcv", "c0t"], writes=["kcv"])
                P.op("dve", lambda e: e.tensor_single_scalar(out=m2[:], in_=kcv[:], scalar=0.0, op=ALU.is_ge), reads=["kcv"], writes=["m2c"])
                P.op("dve", lambda e: e.tensor_single_scalar(out=kcv[:], in_=kcv[:], scalar=15.0, op=ALU.is_le), reads=["kcv", "m2c"], writes=["kcv"])
                P.op("dve", lambda e: e.tensor_tensor(out=m2[:], in0=m2[:], in1=kcv[:], op=ALU.mult), reads=["kcv", "m2c"], writes=["m2c"])
                P.op("dve", lambda e: e.tensor_scalar(out=m2[:], in0=m2[:], scalar1=-1.0, scalar2=-NEG, op0=ALU.add, op1=ALU.mult), reads=["m2c"], writes=["m2c"])
                P.op("dve", lambda e: e.tensor_copy(out=BiasTT[:], in_=m2[:].unsqueeze(1).to_broadcast([128, 112, 64])), reads=["m2c"], writes=["BiasTT"])
                for m in range(31):
                    ei = m % 2; ek = "eqm%d" % ei
                    P.op("dve", (lambda ei, m: lambda e: e.tensor_single_scalar(out=eqm[ei][:], in_=Dm[:], scalar=float(m), op=ALU.is_equal))(ei, m), reads=["Dm"], writes=[ek])
                    for hh in range(2):
                        hs = slice(hh * 56, (hh + 1) * 56); tk_ = "tmpC%d" % hh
                        P.op("pool", (lambda ei, m, hh, hs: lambda e: e.tensor_tensor(out=tmpC[:, hs, :], in0=eqm[ei][:].unsqueeze(1).to_broadcast([128, 56, 64]),
                                                                                      in1=rpbB[:, hs, m:m + 1].to_broadcast([128, 56, 64]), op=ALU.mult))(ei, m, hh, hs),
                             reads=[ek, "rpbB"], writes=[tk_])
                        P.op("dve", (lambda hs: lambda e: e.tensor_tensor(out=BiasTT[:, hs, :], in0=BiasTT[:, hs, :], in1=tmpC[:, hs, :], op=ALU.add))(hs), reads=[tk_, "BiasTT"], writes=["BiasTT%d" % hh])
            P.barrier()
            with phase() as st:
                Vb = [sbuf(st, "Vb%d" % i, [128, 4, 512], BF16) for i in range(3)]; vbr = Rot("Vb", 3)
                ssb = [sbuf(st, "ssb%d" % i, [128, 4, 64]) for i in range(3)]; ssr2 = Rot("ssb", 3)
                pT = [sbuf(st, "pT%d" % i, [128, 384], BF16) for i in range(3)]; ptr = Rot("pT", 3)
                rden = [sbuf(st, "rden%d" % i, [128, 64]) for i in range(2)]; rdr = Rot("rden", 2)
                ps_ = [psum(st, "psc%d" % i, [128, 512]) for i in range(3)]; psr = Rot("psc", 3)
                po_ = [psum(st, "poc%d" % i, [128, 512]) for i in range(2)]; por = Rot("poc", 2)
                pd_ = [psum(st, "pdc%d" % i, [128, 512]) for i in range(2)]; pdr = Rot("pdc", 2)
                B4 = BiasTT[:].rearrange("p (h j) q -> p h j q", h=8)
                cf = [sbuf(st, "cvf%d" % i, [128, 2048]) for i in range(3)]; cb = [sbuf(st, "cvb%d" % i, [128, 2048], BF16) for i in range(3)]

                def convert_tile(ti):
                    bi = ti % 3
                    P.dma("sp", "cvf%d" % bi, lambda e: e.dma_start(out=cf[bi][:], in_=peer_uv[ti * 128:(ti + 1) * 128, :]), writes=["cvf%d" % bi])
                    P.op("pool", lambda e: e.tensor_copy(out=cb[bi][:], in_=cf[bi][:]), reads=["cvf%d" % bi], writes=["cvb%d" % bi])
                    P.dma("pool", "cvb%d" % bi, lambda e: e.dma_start(out=uvb_d[ti * 128:(ti + 1) * 128, :], in_=cb[bi][:]), reads=["cvb%d" % bi], writes=["uvb_d"])
                def c_scores(r, h, vi):
                    r0 = min(max(r - 4, 0), 56)
                    t = h // 2; po = (h % 2) * 64; psl = slice(po, po + 64)
                    si, skey = psr.next()
                    qsl = slice(r * 64, (r + 1) * 64)
                    for j in range(6):
                        k0 = (CTX + (r0 + 2 * j) * 64) if j < 4 else (j - 4) * 128
                        P.op("pe", (lambda j, k0: lambda e: e.matmul(ps_[si][:, j * 64:(j + 1) * 64], lhsT=kT_sb[psl, t, k0:k0 + 128], rhs=qT_sb[psl, t, qsl], start=True, stop=True))(j, k0),
                             reads=["kT_sb", "qT_sb"], writes=[skey])
                    return (r, h, vi, si, skey)

                def c_part1(state):
                    r, h, vi, si, skey = state
                    r0 = min(max(r - 4, 0), 56); dr0 = r0 - r + 7
                    bi2, bkey2 = ssr2.next()
                    P.op("dve", lambda e: e.scalar_tensor_tensor(out=ssb[bi2][:], in0=ps_[si][:, 0:256].rearrange("p (j q) -> p j q", j=4), scalar=0.125,
                                                                 in1=B4[:, h, dr0:dr0 + 7:2, :], op0=ALU.mult, op1=ALU.add), reads=[skey, "BiasTT"], writes=[bkey2])
                    ti, tkey = ptr.next()
                    P.op("act", lambda e: e.activation(out=pT[ti][:, 0:256], in_=ssb[bi2][:].rearrange("p j q -> p (j q)"), func=AF.Exp), reads=[bkey2], writes=[tkey])
                    P.op("act", lambda e: e.activation(out=pT[ti][:, 256:384], in_=ps_[si][:, 256:384], func=AF.Exp, scale=0.125), reads=[skey], writes=[tkey])
                    return (r, h, vi, ti, tkey)

                def c_part2(state):
                    r, h, vi, ti, tkey = state
                    vkey = "Vb%d" % vi
                    t = h // 2; po = (h % 2) * 64; psl = slice(po, po + 64)
                    qsl = slice(r * 64, (r + 1) * 64)
                    oi, okey = por.next(); di, dkey = pdr.next()
                    hp = (h // 2) * 128
                    for j in range(6):
                        vsrc = (Vb[vi][:, j, hp:hp + 128] if j < 4 else Vctx[:, j - 4, hp:hp + 128])
                        P.op("pe", (lambda j, vsrc: lambda e: e.matmul(po_[oi][:, 0:64], lhsT=vsrc, rhs=pT[ti][:, j * 64:(j + 1) * 64], start=(j == 0), stop=(j == 5)))(j, vsrc),
                             reads=[vkey, "Vctx", tkey], writes=[okey])
                    for j in range(6):
                        P.op("pe", (lambda j: lambda e: e.matmul(pd_[di][:, 0:64], lhsT=ones_b[:], rhs=pT[ti][:, j * 64:(j + 1) * 64], start=(j == 0), stop=(j == 5)))(j),
                             reads=["ones_b", tkey], writes=[dkey])
                    ri, rkey = rdr.next()
                    P.op("dve", lambda e: e.reciprocal(out=rden[ri][psl, :], in_=pd_[di][psl, 0:64]), reads=[dkey], writes=[rkey])
                    P.op("dve", lambda e: e.tensor_tensor(out=attT_sb[psl, t, qsl], in0=po_[oi][psl, 0:64], in1=rden[ri][psl, :], op=ALU.mult), reads=[okey, rkey], writes=["attT_sb"])

                pend1 = None; pend2 = None
                for r in range(64):
                    convert_tile(2 * r); convert_tile(2 * r + 1)
                    r0 = min(max(r - 4, 0), 56)
                    vi, vkey = vbr.next()
                    P.dma("sp", vkey, (lambda vi, r0: lambda e: e.dma_start(out=Vb[vi][:], in_=v_d[CTX + r0 * 64:CTX + (r0 + 8) * 64, :].rearrange("(j p) n -> p j n", p=128)))(vi, r0),
                          reads=["v_d"], writes=[vkey])
                    for h in range(8):
                        stt = c_scores(r, h, vi)
                        nxt2 = c_part1(pend1) if pend1 is not None else None
                        if pend2 is not None:
                            c_part2(pend2)
                        pend2 = nxt2
                        pend1 = stt
                nxt2 = c_part1(pend1)
                if pend2 is not None:
                    c_part2(pend2)
                c_part2(nxt2)
            if debug:
                P.dma("sp", "dbga", lambda e: e.dma_start(out=attT_d.rearrange("(t p) n -> p t n", p=128), in_=attT_sb[:]), reads=["attT_sb"], writes=["attT_d"])
            P.barrier()
            stC2.close()
            with phase() as st:
                wstgC_t = sbuf(st, "wstgC", [128, 4, 1024]); w_bb_sb = sbuf(st, "w_bb_sb", [128, 4, D], BF16)
                g_sb = [sbuf(st, "g_sb%d" % i, [128, 16, 512], BF16) for i in range(2)]
                ba_sb = [sbuf(st, "ba_sb%d" % i, [128, 8, 512], BF16) for i in range(2)]
                t1 = [sbuf(st, "t1c%d" % i, [128, 512]) for i in range(2)]; t1r = Rot("t1c", 2)
                t2 = [sbuf(st, "t2c%d" % i, [128, 512]) for i in range(2)]; t2r = Rot("t2c", 2)
                st_mg = [sbuf(st, "st_mg%d" % i, [128, 8, 512], BF16) for i in range(2)]
                pbb = [psum(st, "pbb%d" % i, [128, 512]) for i in range(3)]; pbr2 = Rot("pbb", 3)
                P.dma("sp", "wc0", lambda e: e.dma_start(out=wstgC_t[:], in_=w_bb.rearrange("(k p) n -> p k n", p=128)), writes=["wstgC"])
                P.op("dve", lambda e: e.tensor_copy(out=w_bb_sb[:], in_=wstgC_t[:]), reads=["wstgC"], writes=["w_bb_sb"])
                for sgi in range(8):
                    b2 = sgi % 2; ssl = slice(sgi * 512, (sgi + 1) * 512)
                    gk, bk, mk = "g_sb%d" % b2, "ba_sb%d" % b2, "st_mg%d" % b2
                    P.dma("act", gk, (lambda b2, ssl: lambda e: e.dma_start(out=g_sb[b2][:], in_=gT_d.rearrange("(t p) n -> p t n", p=128)[:, :, ssl]))(b2, ssl), reads=["gT_d"], writes=[gk])
                    P.dma("act", bk, (lambda b2, ssl: lambda e: e.dma_start(out=ba_sb[b2][:], in_=baT_d.rearrange("(t p) n -> p t n", p=128)[:, :, ssl]))(b2, ssl), reads=["baT_d"], writes=[bk])
                    for ct2 in range(8):
                        qi, qk = pbr2.next()
                        for k in range(4):
                            P.op("pe", (lambda qi, k, ct2, ssl: lambda e: e.matmul(pbb[qi][:], lhsT=w_bb_sb[:, k, ct2 * 128:(ct2 + 1) * 128], rhs=attT_sb[:, k, ssl],
                                                                                   start=(k == 0), stop=(k == 3)))(qi, k, ct2, ssl),
                                 reads=["w_bb_sb", "attT_sb"], writes=[qk])
                        i1, k1 = t1r.next(); i2, k2 = t2r.next()
                        P.op("dve", (lambda i1, qi, b2, ct2: lambda e: e.tensor_tensor(out=t1[i1][:], in0=pbb[qi][:], in1=g_sb[b2][:, 8 + ct2, :], op=ALU.mult))(i1, qi, b2, ct2),
                             reads=[qk, gk], writes=[k1])
                        P.op("pool", (lambda i2, b2, ct2: lambda e: e.tensor_tensor(out=t2[i2][:], in0=ba_sb[b2][:, ct2, :], in1=g_sb[b2][:, ct2, :], op=ALU.mult))(i2, b2, ct2),
                             reads=[bk, gk], writes=[k2])
                        P.op("dve", (lambda b2, ct2, i1, i2: lambda e: e.tensor_tensor(out=st_mg[b2][:, ct2, :], in0=t1[i1][:], in1=t2[i2][:], op=ALU.add))(b2, ct2, i1, i2),
                             reads=[k1, k2], writes=[mk])
                    P.dma("sp", mk, (lambda b2, ssl: lambda e: e.dma_start(out=mgT_d.rearrange("(t p) n -> p t n", p=128)[:, :, ssl], in_=st_mg[b2][:]))(b2, ssl),
                          reads=[mk], writes=["mgT_d"])
        P.barrier()
        if stage <= 3:
            return finish(nc, P, out)

        x1_d = dscr("x1_d", [SEQ, D], F32) if debug else None
        pf_d = dscr("pf_d", [SEQ, D], F32) if debug else None
        NT = 32 if stage >= 5 else int(stage * 10) % 10 or 1
        if not debug:
            NT = 32
        if "KNT" in os.environ:
            NT = int(os.environ["KNT"])
        gate1B = modB[:, 2 * D:3 * D]; S2B = modB[:, 3 * D:4 * D]; G2B = modB[:, 4 * D:5 * D]; gate2B = modB[:, 5 * D:6 * D]
        with ExitStack() as stD:
            w_out_sb = sbuf(stD, "w_out_sb", [128, 8, D], BF16); w_q_sb = sbuf(stD, "w_q_sb", [128, 8, 2048], BF16)
            skT = sbuf(stD, "skT", [128, 2, 128], BF16); fgB = sbuf(stD, "fgB", [128, D])
            with phase() as st:
                wstgD_t = [sbuf(st, "wstgD%d" % i, [128, 8, 512]) for i in range(2)]
                skf = sbuf(st, "skf", [128, 2, 128]); skb = sbuf(st, "skb", [128, 2, 128], BF16)
                ptk = psum(st, "ptk", [128, 2, 128], BF16)
                for i in range(6):
                    bi = i % 2
                    srcw = (w_out if i < 2 else w_q).rearrange("(k p) n -> p k n", p=128)
                    c0 = (i * 512) if i < 2 else (i - 2) * 512
                    dstw = w_out_sb if i < 2 else w_q_sb
                    P.dma("sp", "wd%d" % bi, (lambda bi, srcw, c0: lambda e: e.dma_start(out=wstgD_t[bi][:], in_=srcw[:, :, c0:c0 + 512]))(bi, srcw, c0), writes=["wstgD%d" % bi])
                    P.op(("dve", "pool")[bi], (lambda bi, dstw, c0: lambda e: e.tensor_copy(out=dstw[:, :, c0:c0 + 512], in_=wstgD_t[bi][:]))(bi, dstw, c0),
                         reads=["wstgD%d" % bi], writes=["wD"])
                P.dma("act", "wd2", lambda e: e.dma_start(out=skf[:], in_=subkeys.rearrange("n k d -> k n d")), writes=["skf"])
                P.dma("act", "wd2", lambda e: e.dma_start(out=fgB[:], in_=final_g.to_broadcast([128, D])), writes=["fgB"])
                P.op("dve", lambda e: e.tensor_copy(out=skb[:], in_=skf[:]), reads=["skf"], writes=["skb"])
                for n_ in range(2):
                    P.op("pe", (lambda n_: lambda e: e.transpose(ptk[:, n_, :], skb[:, n_, :], identb[:]))(n_), reads=["skb", "identb"], writes=["ptk"])
                P.op("dve", lambda e: e.tensor_copy(out=skT[:], in_=ptk[:]), reads=["ptk"], writes=["skT"])
            P.barrier()
            P.barrier()
            if stage == 3.5:
                return finish(nc, P, out)
            with phase() as st:
                NB = 2
                xtD_t = sbuf(st, "xtD", [128, D])
                x1 = [sbuf(st, "x1_%d" % i, [128, D]) for i in range(NB)]
                h2 = [sbuf(st, "h2_%d" % i, [128, D]) for i in range(NB)]
                eid = [sbuf(st, "eid%d" % i, [128, 128], I32) for i in range(NB)]
                gate = [sbuf(st, "gate%d" % i, [128, 128]) for i in range(NB)]
                tmpD = sbuf(st, "tmpD", [128, D]); tmpF = tmpD
                acc = None
                junkD = sbuf(st, "junkD", [128, D], BF16); NJ = int(os.environ.get("KJ", "2"))
                junkE3 = [sbuf(st, "junkE%d" % i, [128, D], BF16) for i in range(NJ)]; jer = Rot("junkE", NJ)
                mg_sb = sbuf(st, "mg_sb", [128, 8, 128], BF16); h2b2 = [sbuf(st, "h2b%d" % i, [128, D], BF16) for i in range(NB)]; h2T = sbuf(st, "h2T", [128, 8, 128], BF16)
                qT_sb2 = sbuf(st, "qT_sb2", [128, 16, 128], BF16); s_sb = sbuf(st, "s_sb", [128, 16, 128]); work = sbuf(st, "workD", [128, 16, 128])
                topv = sbuf(st, "topv", [128, 16, 16]); idxu = sbuf(st, "idxu", [128, 16, 16], U32); idxf = sbuf(st, "idxf", [128, 16, 16])
                cand = sbuf(st, "cand", [128, 8, 256]); cidx = s_sb[:].rearrange("p a b -> p (a b)").rearrange("p (h c) -> p h c", h=8)
                best = sbuf(st, "best", [128, 8, 16]); eg = sbuf(st, "eg", [128, 8, 16]); sm = sbuf(st, "smD", [128, 32]); sm2 = sbuf(st, "smE", [128, 8]); eidf = sbuf(st, "eidf", [128, 128])
                aD = sbuf(st, "aD", [128, 128]); gl_ = sbuf(st, "gl_", [128, 128]); wD = sbuf(st, "wDw", [128, 128])
                NG = int(os.environ.get("KNG", "12")); GJ = int(os.environ.get("KGJ", "4"))
                FY = int(os.environ.get("KFY", "1")); KAR = int(os.environ.get("KAR", "0"))
                posI = sbuf(st, "posI", [128, 256], I32); mskI = sbuf(st, "mskI", [128, 1], I32)
                c4I = sbuf(st, "c4I", [128, 1], I32); c15I = sbuf(st, "c15I", [128, 1], I32); iota16 = sbuf(st, "iota16", [128, 16])
                pab = sbuf(st, "pab", [128, 2, 8, 16], I32); pabf = sbuf(st, "pabf", [128, 2, 8, 16]); e3b = sbuf(st, "e3b", [128, 8, 16])
                oh = cand[:].rearrange("p h (k a) -> p h k a", a=16)
                P.op("pool", lambda e: e.iota(c4I[:], pattern=[[0, 1]], base=4, channel_multiplier=0), writes=["posI"])
                P.op("pool", lambda e: e.iota(c15I[:], pattern=[[0, 1]], base=15, channel_multiplier=0), writes=["posI"])
                P.op("pool", lambda e: e.iota(iota16[:], pattern=[[1, 16]], base=0, channel_multiplier=0, allow_small_or_imprecise_dtypes=True), writes=["posI"])
                P.op("pool", lambda e: e.iota(posI[:], pattern=[[1, 256]], base=0, channel_multiplier=0), writes=["posI"])
                P.op("pool", lambda e: e.iota(mskI[:], pattern=[[0, 1]], base=-256, channel_multiplier=0), writes=["posI"])
                UVg = [sbuf(st, "UVg%d" % i, [128, 2, D], BF16) for i in range(NG)]
                dg = [sbuf(st, "dg%d" % i, [128, 128], BF16) for i in range(8)]; dgr = Rot("dg", 8)
                pmo = [psum(st, "pmo%d" % i, [128, 512]) for i in range(2)]
                pacc = [psum(st, "pacc%d" % i, [128, 512]) for i in range(2)]
                tpD = psum(st, "tpD", [128, 8, 128], BF16)
                pq = [psum(st, "pq%d" % i, [128, 4, 128]) for i in range(2)]; pqr = Rot("pq", 2)
                uv_i = [0]

                def rstd_chain(src_ap, srckey, smt, smk, jk, jkey):
                    P.op("act", lambda e: e.activation(out=jk[:], in_=src_ap, func=AF.Square, accum_out=smt[:, 0:1]), reads=[srckey], writes=[smk, jkey])
                    P.op("dve", lambda e: e.tensor_scalar(out=smt[:, 1:2], in0=smt[:, 0:1], scalar1=1.0 / D, scalar2=EPS, op0=ALU.mult, op1=ALU.add), reads=[smk], writes=[smk])
                    P.op("act", lambda e: e.sqrt(out=smt[:, 2:3], in_=smt[:, 1:2]), reads=[smk], writes=[smk])
                    P.op("dve", lambda e: e.reciprocal(out=smt[:, 3:4], in_=smt[:, 2:3]), reads=[smk], writes=[smk])

                def front(i):
                    b = i % NB; t0 = i * 128
                    xk, x1k, h2k, ek, gk = "xtD", "x1_%d" % b, "h2_%d" % b, "eid%d" % b, "gate%d" % b
                    P.dma("sp", xk, lambda e: e.dma_start(out=xtD_t[:], in_=x[t0:t0 + 128, :]), writes=[xk])
                    P.dma("sp", "mgl", lambda e: e.dma_start(out=mg_sb[:], in_=mgT_d.rearrange("(k p) n -> p k n", p=128)[:, :, t0:t0 + 128]), reads=["mgT_d"], writes=["mg_sb"])
                    for hf in range(2):
                        for k in range(8):
                            P.op("pe", (lambda hf, k: lambda e: e.matmul(pmo[hf][:], lhsT=mg_sb[:, k, :], rhs=w_out_sb[:, k, hf * 512:(hf + 1) * 512], start=(k == 0), stop=(k == 7)))(hf, k),
                                 reads=["mg_sb", "wD"], writes=["pmo%d" % hf])
                        yield
                        P.op("dve", (lambda hf: lambda e: e.tensor_tensor(out=tmpF[:, hf * 512:(hf + 1) * 512], in0=pmo[hf][:], in1=gate1B[:, hf * 512:(hf + 1) * 512], op=ALU.mult))(hf),
                             reads=["pmo%d" % hf, "modB"], writes=["tmpD"])
                        yield
                    P.op("dve", lambda e: e.tensor_tensor(out=x1[b][:], in0=tmpF[:], in1=xtD_t[:], op=ALU.add), reads=["tmpD", xk], writes=[x1k])
                    yield
                    if debug:
                        P.dma("sp", "dbgx1", lambda e: e.dma_start(out=x1_d[t0:t0 + 128, :], in_=x1[b][:]), reads=[x1k], writes=["x1_d"])
                    rstd_chain(x1[b][:], x1k, sm[:, 0:4], "smD", junkD, "junkD")
                    P.op("dve", lambda e: e.scalar_tensor_tensor(out=tmpF[:], in0=x1[b][:], scalar=sm[:, 3:4], in1=G2B, op0=ALU.mult, op1=ALU.mult), reads=[x1k, "smD", "modB"], writes=["tmpD"])
                    yield
                    P.op("dve", lambda e: e.tensor_tensor(out=h2[b][:], in0=tmpF[:], in1=S2B, op=ALU.add), reads=["tmpD", "modB"], writes=[h2k])
                    yield
                    P.op("act", lambda e: e.copy(out=h2b2[b][:], in_=h2[b][:]), reads=[h2k], writes=["h2b%d" % b])
                    for k in range(8):
                        P.op("pe", (lambda k: lambda e: e.transpose(tpD[:, k, :], h2b2[b][:, k * 128:(k + 1) * 128], identb[:]))(k), reads=["h2b%d" % b, "identb"], writes=["tpD"])
                    P.op("act", lambda e: e.copy(out=h2T[:], in_=tpD[:]), reads=["tpD"], writes=["h2T"])
                    yield
                    for g4 in range(4):
                        qi, qk = pqr.next()
                        for bl in range(4):
                            blk = g4 * 4 + bl
                            for k in range(8):
                                P.op("pe", (lambda qi, bl, blk, k: lambda e: e.matmul(pq[qi][:, bl, :], lhsT=w_q_sb[:, k, blk * 128:(blk + 1) * 128], rhs=h2T[:, k, :], start=(k == 0), stop=(k == 7)))(qi, bl, blk, k),
                                     reads=["wD", "h2T"], writes=[qk])
                        P.op("act", (lambda qi, g4: lambda e: e.copy(out=qT_sb2[:, g4 * 4:(g4 + 1) * 4, :], in_=pq[qi][:]))(qi, g4), reads=[qk], writes=["qT_sb2"])
                        yield
                    for g4 in range(4):
                        si, sk = pqr.next()
                        for bl in range(4):
                            blk = g4 * 4 + bl
                            P.op("pe", (lambda si, bl, blk: lambda e: e.matmul(pq[si][:, bl, :], lhsT=qT_sb2[:, blk, :], rhs=skT[:, blk % 2, :], start=True, stop=True))(si, bl, blk),
                                 reads=["qT_sb2", "skT"], writes=[sk])
                        P.op("act", (lambda si, g4: lambda e: e.copy(out=s_sb[:, g4 * 4:(g4 + 1) * 4, :], in_=pq[si][:]))(si, g4), reads=[sk], writes=["s_sb"])
                        yield
                    TK = ["s_sb", "topk"]
                    BK = ["tk%d" % q for q in range(16)]
                    for blk in range(16):
                        P.op("dve", (lambda blk: lambda e: e.max(out=topv[:, blk, 0:8], in_=s_sb[:, blk, :]))(blk), reads=TK, writes=[BK[blk]])
                    yield
                    for blk in range(16):
                        P.op("dve", (lambda blk: lambda e: e.max_index(out=idxu[:, blk, 0:8], in_max=topv[:, blk, 0:8], in_values=s_sb[:, blk, :]))(blk), reads=["s_sb", BK[blk]], writes=[BK[blk]])
                        if blk % 8 == 7:
                            yield
                    for blk in range(16):
                        P.op("dve", (lambda blk: lambda e: e.match_replace(out=work[:, blk, :], in_to_replace=topv[:, blk, 0:8], in_values=s_sb[:, blk, :], imm_value=-1e30))(blk), reads=["s_sb", BK[blk]], writes=[BK[blk]])
                        if blk % 8 == 7:
                            yield
                    for blk in range(16):
                        P.op("dve", (lambda blk: lambda e: e.max(out=topv[:, blk, 8:16], in_=work[:, blk, :]))(blk), reads=[BK[blk]], writes=[BK[blk]])
                    yield
                    for blk in range(16):
                        P.op("dve", (lambda blk: lambda e: e.max_index(out=idxu[:, blk, 8:16], in_max=topv[:, blk, 8:16], in_values=work[:, blk, :]))(blk), reads=[BK[blk]], writes=[BK[blk]])
                        if blk % 8 == 7:
                            yield
                    TK = TK + BK
                    P.op("dve", lambda e: e.tensor_copy(out=idxf[:], in_=idxu[:]), reads=TK, writes=["topk"])
                    tv4 = topv[:].rearrange("p (h n) a -> p h n a", n=2); ix4 = idxf[:].rearrange("p (h n) a -> p h n a", n=2)
                    c4 = cand[:].rearrange("p h (a b) -> p h a b", a=16); ci4 = cidx.rearrange("p h (a b) -> p h a b", a=16)
                    P.op("dve", lambda e: e.tensor_tensor(out=c4, in0=tv4[:, :, 0, :].unsqueeze(3).to_broadcast([128, 8, 16, 16]),
                                                          in1=tv4[:, :, 1, :].unsqueeze(2).to_broadcast([128, 8, 16, 16]), op=ALU.add), reads=TK, writes=["topk"])
                    yield
                    candI = cand[:].bitcast(I32)
                    P.op("dve", lambda e: e.tensor_scalar(out=candI, in0=candI, scalar1=mskI[:, 0:1], scalar2=None, op0=ALU.bitwise_and), reads=TK + ["posI"], writes=["topk"])
                    P.op("dve", lambda e: e.tensor_tensor(out=candI, in0=candI, in1=posI[:].unsqueeze(1).to_broadcast([128, 8, 256]), op=ALU.bitwise_or), reads=TK + ["posI"], writes=["topk"])
                    P.op("dve", lambda e: e.tensor_single_scalar(out=ix4[:, :, 0, :], in_=ix4[:, :, 0, :], scalar=128.0, op=ALU.mult), reads=TK, writes=["topk"])
                    yield
                    w2 = work[:].rearrange("p (h n) k -> p h (n k)", n=2)
                    HK = ["hk%d" % q for q in range(8)]
                    for h in range(8):
                        P.op("dve", (lambda h: lambda e: e.max(out=best[:, h, 0:8], in_=cand[:, h, :]))(h), reads=TK, writes=[HK[h]])
                    yield
                    for h in range(8):
                        P.op("dve", (lambda h: lambda e: e.match_replace(out=w2[:, h, :], in_to_replace=best[:, h, 0:8], in_values=cand[:, h, :], imm_value=-1e30))(h), reads=TK + [HK[h]], writes=[HK[h]])
                    yield
                    for h in range(8):
                        P.op("dve", (lambda h: lambda e: e.max(out=best[:, h, 8:16], in_=w2[:, h, :]))(h), reads=[HK[h]], writes=[HK[h]])
                    yield
                    TK = TK + HK
                    P.op("dve", lambda e: e.tensor_single_scalar(out=sm[:, 8:16], in_=best[:, :, 0], scalar=-1.0, op=ALU.mult), reads=TK + ["smD"], writes=["smD"])
                    for h in range(8):
                        P.op("act", (lambda h: lambda e: e.activation(out=eg[:, h, :], in_=best[:, h, :], func=AF.Exp, bias=sm[:, 8 + h:9 + h], accum_out=sm[:, 16 + h:17 + h]))(h),
                             reads=TK + ["smD"], writes=["eg", "smD"])
                    P.op("dve", lambda e: e.reciprocal(out=sm[:, 24:32], in_=sm[:, 16:24]), reads=["smD"], writes=["smD"])
                    P.op("dve", lambda e: e.tensor_tensor(out=gate[b][:].rearrange("p (h k) -> p h k", h=8), in0=eg[:], in1=sm[:, 24:32].unsqueeze(2).to_broadcast([128, 8, 16]), op=ALU.mult),
                         reads=["eg", "smD"], writes=[gk])
                    yield
                    bestI = best[:].bitcast(I32)
                    P.op("dve", lambda e: e.tensor_scalar(out=pab[:, 0], in0=bestI, scalar1=c4I[:, 0:1], scalar2=None, op0=ALU.arith_shift_right), reads=TK + ["posI"], writes=["topk"])
                    P.op("dve", lambda e: e.tensor_scalar(out=pab[:, 0], in0=pab[:, 0], scalar1=c15I[:, 0:1], scalar2=None, op0=ALU.bitwise_and), reads=TK + ["posI"], writes=["topk"])
                    P.op("dve", lambda e: e.tensor_scalar(out=pab[:, 1], in0=bestI, scalar1=c15I[:, 0:1], scalar2=None, op0=ALU.bitwise_and), reads=TK + ["posI"], writes=["topk"])
                    P.op("dve", lambda e: e.tensor_copy(out=pabf[:], in_=pab[:]), reads=TK, writes=["topk"])
                    yield
                    e3 = eidf[:].rearrange("p (h k) -> p h k", h=8)
                    for n_ in range(2):
                        P.op("dve", (lambda n_: lambda e: e.tensor_tensor(out=oh[:], in0=pabf[:, n_].unsqueeze(3).to_broadcast([128, 8, 16, 16]),
                                                                          in1=iota16[:].unsqueeze(1).unsqueeze(1).to_broadcast([128, 8, 16, 16]), op=ALU.is_equal))(n_), reads=TK + ["posI"], writes=["topk"])
                        P.op("dve", (lambda n_: lambda e: e.tensor_tensor(out=oh[:], in0=oh[:], in1=ix4[:, :, n_, :].unsqueeze(2).to_broadcast([128, 8, 16, 16]), op=ALU.mult))(n_), reads=TK, writes=["topk"])
                        P.op("dve", (lambda n_: lambda e: e.tensor_reduce(out=(e3 if n_ == 0 else e3b[:]), in_=oh[:], axis=AX.X, op=ALU.add))(n_), reads=TK, writes=["topk"])
                        yield
                    P.op("dve", lambda e: e.tensor_tensor(out=e3, in0=e3, in1=e3b[:], op=ALU.add), reads=TK, writes=["topk"])
                    P.op("dve", lambda e: e.tensor_scalar(out=eidf[:], in0=eidf[:], scalar1=0.0, scalar2=16383.0, op0=ALU.max, op1=ALU.min), reads=TK, writes=["topk"])
                    P.op("dve", lambda e: e.tensor_copy(out=eid[b][:], in_=eidf[:]), reads=TK, writes=[ek])
                    yield

                def consume(i, fgen):
                    b = i % NB; t0 = i * 128
                    x1k, h2k, ek, gk = "x1_%d" % b, "h2_%d" % b, "eid%d" % b, "gate%d" % b
                    ngrp = 128 // GJ
                    pend = None

                    def finish_group(g, bufs):
                        j0 = g * GJ
                        if "nofin" in KD:
                            return
                        P.op("act", lambda e: e.activation(out=gl_[:, j0:j0 + GJ], in_=aD[:, j0:j0 + GJ], func=AF.Gelu_apprx_tanh), reads=["aD%d_%d" % (g, q) for q in range(GJ)], writes=["gl_%d" % g])
                        for jj in range(GJ):
                            P.op("act", (lambda jj: lambda e: e.mul(out=wD[:, j0 + jj:j0 + jj + 1], in_=gl_[:, j0 + jj:j0 + jj + 1], mul=gate[b][:, j0 + jj:j0 + jj + 1]))(jj),
                                 reads=["gl_%d" % g, gk], writes=["wDw%d" % g])
                        for jj in range(GJ):
                            j = j0 + jj; gi, ugk = bufs[jj]
                            di, dk = dgr.next()
                            P.op("act", (lambda di, j: lambda e: e.activation(out=dg[di][:], in_=identb[:], func=AF.Copy, scale=wD[:, j:j + 1]))(di, j), reads=["wDw%d" % g, "identb"], writes=[dk])
                            for hf in range(2):
                                if "nov" in KD and j not in (0, 127):
                                    continue
                                P.op("pe", (lambda di, gi, hf, j: lambda e: e.matmul(pacc[hf][:], lhsT=dg[di][:], rhs=UVg[gi][:, 1, hf * 512:(hf + 1) * 512], start=(j == 0), stop=(j == 127)))(di, gi, hf, j),
                                     reads=[dk, ugk], writes=["pacc%d" % hf])

                    for g in range(ngrp):
                        bufs = []
                        for jj in range(GJ):
                            j = g * GJ + jj
                            gi = uv_i[0] % NG; uv_i[0] += 1; ugk = "UVg%d" % gi
                            bufs.append((gi, ugk))
                            if "nog" in KD:
                                P.op("pool", (lambda gi: lambda e: e.memset(UVg[gi][:], 1.0))(gi), reads=[ek], writes=[ugk])
                            else:
                                P.dma("pool", ugk, (lambda gi, j: lambda e: e.indirect_dma_start(out=UVg[gi][:].rearrange("p a d -> p (a d)"), out_offset=None, in_=uvb_d,
                                                                                               in_offset=bass.IndirectOffsetOnAxis(ap=eid[b][:, j:j + 1], axis=0)))(gi, j), reads=[ek], writes=[ugk])
                        for jj in range(GJ):
                            j = g * GJ + jj; gi, ugk = bufs[jj]
                            if "noa" in KD:
                                continue
                            ji, jkey_ = jer.next()
                            jE = junkE3[ji]
                            if jj < KAR:
                                P.op("dve", (lambda gi, jE: lambda e: e.tensor_tensor(out=jE[:], in0=UVg[gi][:, 0, :], in1=h2b2[b][:], op=ALU.mult))(gi, jE),
                                     reads=[ugk, "h2b%d" % b], writes=[jkey_])
                                P.op("act", (lambda j, jE: lambda e: e.activation(out=jE[:], in_=jE[:], func=AF.Copy, accum_out=aD[:, j:j + 1]))(j, jE),
                                     reads=[jkey_], writes=[jkey_, "aD%d_%d" % (g, jj)])
                            else:
                                P.op("dve", (lambda gi, j, jE: lambda e: e.scalar_tensor_tensor(out=jE[:], in0=UVg[gi][:, 0, :], scalar=1.0, in1=h2[b][:], op0=ALU.mult, op1=ALU.mult, accum_out=aD[:, j:j + 1]))(gi, j, jE),
                                     reads=[ugk, h2k], writes=["aD%d_%d" % (g, jj), jkey_])
                        finish_group(g, bufs)
                        if fgen is not None:
                            for _ in range(FY):
                                next(fgen, None)
                    if fgen is not None:
                        for _ in fgen:
                            pass
                    if "dump23" in KD and i == 23:
                        for nm, src_t in (("d_x1", x1[b]), ("d_wD", wD), ("d_aD", aD), ("d_gate", gate[b]), ("d_h2", h2[b])):
                            dd = nc.dram_tensor(nm, list(src_t[:].shape), F32, kind="ExternalOutput").ap()
                            P.dma("sp", "dump", (lambda dd, src_t: lambda e: e.dma_start(out=dd, in_=src_t[:]))(dd, src_t), reads=[x1k, gk, h2k] + ["wDw%d" % q for q in range(32)] + ["aD%d" % q for q in range(32)], writes=["dumpo"])
                        dd2 = nc.dram_tensor("d_eid", [128, 128], I32, kind="ExternalOutput").ap()
                        P.dma("sp", "dump", lambda e: e.dma_start(out=dd2, in_=eid[b][:]), reads=[ek], writes=["dumpo"])
                    if "notail" in KD:
                        return
                    for hf in range(2):
                        P.op("dve", (lambda hf: lambda e: e.tensor_tensor(out=tmpD[:, hf * 512:(hf + 1) * 512], in0=pacc[hf][:], in1=gate2B[:, hf * 512:(hf + 1) * 512], op=ALU.mult))(hf),
                             reads=["pacc%d" % hf, "modB"], writes=["tmpD"])
                    P.op("dve", lambda e: e.tensor_tensor(out=x1[b][:], in0=tmpD[:], in1=x1[b][:], op=ALU.add), reads=["tmpD", x1k], writes=[x1k])
                    rstd_chain(x1[b][:], x1k, sm2[:, 0:4], "smE", junkD, "junkD")
                    P.op("dve", lambda e: e.scalar_tensor_tensor(out=tmpD[:], in0=x1[b][:], scalar=sm2[:, 3:4], in1=fgB[:], op0=ALU.mult, op1=ALU.mult), reads=[x1k, "smE", "fgB"], writes=["tmpD"])
                    P.dma("sp", "outst", lambda e: e.dma_start(out=out[t0:t0 + 128, :], in_=tmpD[:]), reads=["tmpD"], writes=["out"])

                for _ in front(0):
                    pass
                for i in range(NT):
                    if "noc" in KD:
                        break
                    if "noil" in KD:
                        consume(i, None)
                        if i + 1 < NT:
                            for _ in front(i + 1):
                                pass
                        continue
                    consume(i, front(i + 1) if i + 1 < NT else None)
        return finish(nc, P, out)
    return nc


def finish(nc, P, out):
    P.barrier()
    P.close()
    return nc


def core_inputs(inp, b):
    f = lambda a: np.ascontiguousarray(a, dtype=np.float32)
    return {
        "x": f(inp["x"][b]), "c": f(inp["c"][b:b + 1]), "ctx": f(inp["ctx"][b]), "c_ctx": f(inp["c_ctx"].reshape(1, D)),
        "w_mod": f(inp["w_mod"][0]), "b_mod": f(inp["b_mod"][0].reshape(1, -1)),
        "norm1_g": f(inp["norm1_g"][0].reshape(1, D)), "norm2_g": f(inp["norm2_g"][0].reshape(1, D)),
        "final_g": f(inp["final_g"].reshape(1, D)), "w_in": f(inp["w_in"][0]),
        "ssm_a_re": f(inp["ssm_a_re"][0]), "ssm_a_im": f(inp["ssm_a_im"][0]), "ssm_log_dt": f(inp["ssm_log_dt"][0]),
        "ssm_b_re": f(inp["ssm_b_re"][0]), "ssm_b_im": f(inp["ssm_b_im"][0]),
        "ssm_c_re": f(inp["ssm_c_re"][0]), "ssm_c_im": f(inp["ssm_c_im"][0]),
        "ssm_d": f(inp["ssm_d"][0].reshape(512, 1)), "w_glu": f(inp["w_glu"][0]), "b_glu": f(inp["b_glu"][0].reshape(512, 1)),
        "w_branch_a": f(inp["w_branch_a"][0]), "w_branch_b": f(inp["w_branch_b"][0]), "na_rpb": f(inp["na_rpb"][0]),
        "w_out": f(inp["w_out"][0]), "peer_w_q": f(inp["peer_w_q"][0]), "peer_subkeys": f(inp["peer_subkeys"][0]),
        "peer_uv": f(np.concatenate([inp["peer_u"][0], inp["peer_v"][0]], axis=1)),
    }


def kernel(**inputs):
    nc = build()
    in_maps = [core_inputs(inputs, b) for b in range(8)]
    res = run_bass_kernel_spmd(nc, in_maps, core_ids=list(range(8)))
    return np.stack([np.asarray(r["out"], dtype=np.float32) for r in res.results], axis=0)
```

```python
import math
import numpy as np
import concourse.bass as bass
import concourse.mybir as mybir
from concourse.bass_utils import run_bass_kernel_spmd

F32 = mybir.dt.float32
BF16 = mybir.dt.bfloat16
I32 = mybir.dt.int32
U32 = mybir.dt.uint32
AF = mybir.ActivationFunctionType
ALU = mybir.AluOpType
AX = mybir.AxisListType


class _Op:
    __slots__ = ("eng", "fn", "deps", "seq", "is_dma", "semkey", "signal", "count", "waits")

    def __init__(self, eng, fn, seq, is_dma=False, semkey=None):
        self.eng = eng
        self.fn = fn
        self.deps = []
        self.seq = seq
        self.is_dma = is_dma
        self.semkey = semkey
        self.signal = is_dma
        self.count = 0
        self.waits = []


class Prog:
    ENGS = ("pe", "dve", "act", "pool", "sp")

    def __init__(self, nc):
        self.nc = nc
        self.ops = []
        self.writer = {}
        self.readers = {}
        import os
        self.same_sync = os.environ.get("KSAME", "1") == "1"

    def _add(self, op, reads, writes):
        deps = []
        for r in reads:
            w = self.writer.get(r)
            if w is not None:
                deps.append(w)
        for w_ in writes:
            w = self.writer.get(w_)
            if w is not None:
                deps.append(w)
            deps.extend(self.readers.get(w_, ()))
        op.deps = [d for d in set(deps) if d is not op]
        for r in reads:
            self.readers.setdefault(r, []).append(op)
        for w_ in writes:
            self.writer[w_] = op
            self.readers[w_] = []
        self.ops.append(op)
        return op

    def op(self, eng, fn, reads=(), writes=()):
        return self._add(_Op(eng, fn, len(self.ops)), reads, writes)

    def dma(self, eng, semkey, fn, reads=(), writes=()):
        return self._add(_Op(eng, fn, len(self.ops), True, semkey), reads, writes)

    def emit(self):
        import bisect
        from contextlib import ExitStack
        nc = self.nc
        if not hasattr(self, "_st"):
            self._st = ExitStack(); self._sem = {}; self._cnt = {}; self._hist = {}
            self._seen = {e: {} for e in self.ENGS}; self._done = 0
        ops = self.ops[self._done:]
        self._done = len(self.ops)
        if not ops:
            return

        def same(d, o):
            return d.eng == o.eng and not o.is_dma and (d.eng == "pe" or not self.same_sync)
        for o in ops:
            for d in o.deps:
                if d.is_dma or same(d, o):
                    continue
                assert d.count == 0 or d.signal, "dependency on an already-emitted non-signalling op"
                d.signal = True
        for o in ops:
            if not o.signal:
                continue
            k = ("dma", o.semkey) if o.is_dma else ("eng", o.eng)
            if k not in self._sem:
                self._sem[k] = self._st.enter_context(nc.semaphore("s%d_%s" % (len(self._sem), str(k[1]).replace(" ", ""))))
                self._cnt[k] = 0
            self._cnt[k] += 1
            o.count = self._cnt[k]
            if o.is_dma:
                self._hist.setdefault(o.semkey, []).append(o.seq)
        for o in ops:
            need = {}
            for d in o.deps:
                if d.is_dma:
                    k = ("dma", d.semkey)
                    v = 16 * bisect.bisect_left(self._hist[d.semkey], o.seq)
                else:
                    if same(d, o):
                        continue
                    k = ("eng", d.eng)
                    v = d.count
                if need.get(k, 0) < v:
                    need[k] = v
            sn = self._seen[o.eng]
            o.waits = []
            for k, v in need.items():
                if sn.get(k, 0) < v:
                    sn[k] = v
                    o.waits.append((k, v))
        self.n_sems = len(self._sem)
        sem = self._sem
        with nc.Block() as block:
            per = {e: [o for o in ops if o.eng == e] for e in self.ENGS}

            def run(engobj, lst):
                for o in lst:
                    for k, v in o.waits:
                        engobj.wait_ge(sem[k], v)
                    ins = o.fn(engobj)
                    if o.signal:
                        k = ("dma", o.semkey) if o.is_dma else ("eng", o.eng)
                        ins.then_inc(sem[k], 16 if o.is_dma else 1)
                    o.fn = None

            @block.tensor
            def _(e):
                run(e, per["pe"])

            @block.vector
            def _(e):
                run(e, per["dve"])

            @block.scalar
            def _(e):
                run(e, per["act"])

            @block.gpsimd
            def _(e):
                run(e, per["pool"])

            @block.sync
            def _(e):
                run(e, per["sp"])

    def close(self):
        self.emit()
        if hasattr(self, "_st"):
            self._st.close()

    def barrier(self, flush=True):
        start = getattr(self, "_done", 0)
        last = {}
        dmas = {}
        for o in self.ops[start:]:
            if o.is_dma:
                dmas[o.semkey] = o
            else:
                last[o.eng] = o
        for e, o in getattr(self, "_bar", {}).items():
            last.setdefault(e, o)
        deps = list(last.values()) + list(dmas.values())
        self._bar = {}
        for e in self.ENGS:
            o = _Op(e, lambda eng: eng.nop(), len(self.ops))
            o.deps = [d for d in deps]
            o.signal = True
            self.ops.append(o)
            self._bar[e] = o
        self.writer = {}
        self.readers = {}
        if flush:
            self.emit()


class Rot:
    def __init__(self, name, n):
        self.name, self.n, self.i = name, n, -1

    def next(self):
        self.i = (self.i + 1) % self.n
        return self.i, "%s%d" % (self.name, self.i)


D = 1024
SEQ = 4096
CTX = 256
NTOK = SEQ + CTX
EPS = 1e-6


def build(stage=99, debug=False):
    import os
    KD = os.environ.get("KDBG", "")
    from contextlib import ExitStack
    nc = bass.Bass("TRN2", target_bir_lowering=False)
    P = Prog(nc)

    def din(name, shape, dt=F32):
        return nc.dram_tensor(name, shape, dt, kind="ExternalInput").ap()

    def dscr(name, shape, dt):
        return nc.dram_tensor(name, shape, dt, kind=("ExternalOutput" if debug else "Internal")).ap()

    x = din("x", [SEQ, D]); c = din("c", [1, D]); ctx = din("ctx", [CTX, D]); c_ctx = din("c_ctx", [1, D])
    w_mod = din("w_mod", [D, 6 * D]); b_mod = din("b_mod", [1, 6 * D])
    norm1_g = din("norm1_g", [1, D]); norm2_g = din("norm2_g", [1, D]); final_g = din("final_g", [1, D])
    w_in = din("w_in", [D, 4096])
    a_re = din("ssm_a_re", [2, 32, 64]); a_im = din("ssm_a_im", [2, 32, 64]); log_dt = din("ssm_log_dt", [2, 32])
    b_re = din("ssm_b_re", [2, 32, 64, 16]); b_im = din("ssm_b_im", [2, 32, 64, 16])
    c_re = din("ssm_c_re", [2, 32, 16, 64]); c_im = din("ssm_c_im", [2, 32, 16, 64])
    ssm_d = din("ssm_d", [512, 1]); w_glu = din("w_glu", [512, 512]); b_glu = din("b_glu", [512, 1])
    w_ba = din("w_branch_a", [512, D]); w_bb = din("w_branch_b", [512, D]); rpb = din("na_rpb", [8, 15, 31])
    w_out = din("w_out", [D, D]); w_q = din("peer_w_q", [D, 2048]); subkeys = din("peer_subkeys", [2, 128, 128])
    peer_uv = din("peer_uv", [16384, 2 * D])
    out = nc.dram_tensor("out", [SEQ, D], F32, kind="ExternalOutput").ap()

    uT_d = dscr("uT_d", [512, NTOK], F32)
    kT_d = dscr("kT_d", [512, NTOK], BF16)
    qT_d = dscr("qT_d", [512, SEQ], BF16)
    v_d = dscr("v_d", [NTOK, 512], BF16)
    gT_d = dscr("gT_d", [2048, SEQ], BF16)
    baT_d = dscr("baT_d", [D, SEQ], BF16)
    mgT_d = dscr("mgT_d", [D, SEQ], BF16)
    uvb_d = nc.dram_tensor("uvb_d", [16384, 2 * D], BF16, kind=("ExternalOutput" if (debug and stage == 3.5) else "Internal")).ap()

    from contextlib import contextmanager

    @contextmanager
    def phase():
        stk = ExitStack()
        try:
            yield stk
            P.barrier()
        finally:
            stk.close()

    top = ExitStack()
    with top:
        def sbuf(st, n, s, d=F32):
            return st.enter_context(nc.sbuf_tensor(n, s, d))

        def psum(st, n, s, d=F32):
            return st.enter_context(nc.psum_tensor(n, s, d))

        identf = sbuf(top, "identf", [128, 128])
        identb = sbuf(top, "identb", [128, 128], BF16)
        modB = sbuf(top, "modB", [128, 6 * D])
        P.op("pool", lambda e: e.iota(identf[:], pattern=[[1, 128]], base=0, channel_multiplier=-1,
                                      allow_small_or_imprecise_dtypes=True), writes=["identf"])
        P.op("dve", lambda e: e.tensor_single_scalar(out=identf[:], in_=identf[:], scalar=0.0, op=ALU.is_equal),
             reads=["identf"], writes=["identf"])
        P.op("dve", lambda e: e.tensor_copy(out=identb[:], in_=identf[:]), reads=["identf"], writes=["identb"])

        with phase() as st:
            modcB = sbuf(st, "modcB", [128, 2 * D])
            wm = [sbuf(st, "wm%d" % i, [128, 8, 512]) for i in range(2)]
            w_in_sb = sbuf(st, "w_in_sb", [128, 8, 4096], BF16)
            st0 = ExitStack()
            cc = sbuf(st0, "cc", [128, 2, 8]); sc = sbuf(st0, "sc", [128, 2, 8]); scB = sbuf(st0, "scB", [128, 2, 8, 128])
            bmB = sbuf(st0, "bmB", [128, 6 * D]); gB = sbuf(st0, "gB", [128, 2, D])
            pmod = [psum(st0, "pmod%d" % i, [128, 512]) for i in range(2)]

            P.dma("sp", "c0", lambda e: e.dma_start(out=cc[:, 0, :], in_=c.rearrange("o (k p) -> p (o k)", p=128),
                                                    allow_slow_non_contiguous=True), writes=["cc"])
            P.dma("sp", "c0", lambda e: e.dma_start(out=cc[:, 1, :], in_=c_ctx.rearrange("o (k p) -> p (o k)", p=128),
                                                    allow_slow_non_contiguous=True), writes=["cc"])
            P.dma("act", "c1", lambda e: e.dma_start(out=bmB[:], in_=b_mod.to_broadcast([128, 6 * D])), writes=["bmB"])
            P.dma("act", "c1", lambda e: e.dma_start(out=gB[:, 0, :], in_=norm1_g.to_broadcast([128, D])), writes=["gB"])
            P.dma("act", "c1", lambda e: e.dma_start(out=gB[:, 1, :], in_=norm2_g.to_broadcast([128, D])), writes=["gB"])
            P.op("act", lambda e: e.activation(out=sc[:], in_=cc[:], func=AF.Silu), reads=["cc"], writes=["sc"])
            P.op("dve", lambda e: e.tensor_copy(out=scB[:], in_=sc[:].unsqueeze(3).to_broadcast([128, 2, 8, 128])),
                 reads=["sc"], writes=["scB"])
            w_mod_v = w_mod.rearrange("(k p) n -> p k n", p=128)
            for cch in range(12):
                bi = cch % 2
                P.dma("sp", "wm%d" % bi, (lambda bi, cch: lambda e: e.dma_start(out=wm[bi][:], in_=w_mod_v[:, :, cch * 512:(cch + 1) * 512]))(bi, cch),
                      writes=["wm%d" % bi])
                for which in range(2 if cch < 4 else 1):
                    for k in range(8):
                        P.op("pe", (lambda bi, which, k: lambda e: e.matmul(pmod[which][:], lhsT=scB[:, which, k, :], rhs=wm[bi][:, k, :],
                                                                            start=(k == 0), stop=(k == 7)))(bi, which, k),
                             reads=["scB", "wm%d" % bi], writes=["pmod%d" % which])
                    dst = modB if which == 0 else modcB
                    P.op("dve", (lambda dst, which, cch: lambda e: e.tensor_tensor(out=dst[:, cch * 512:(cch + 1) * 512], in0=pmod[which][:],
                                                                                   in1=bmB[:, cch * 512:(cch + 1) * 512], op=ALU.add))(dst, which, cch),
                         reads=["pmod%d" % which, "bmB"], writes=["modB" if which == 0 else "modcB"])
            for dst, key, off, gi in ((modB, "modB", D, 0), (modcB, "modcB", D, 0), (modB, "modB", 4 * D, 1)):
                P.op("dve", (lambda dst, off, gi: lambda e: e.scalar_tensor_tensor(out=dst[:, off:off + D], in0=dst[:, off:off + D], scalar=1.0,
                                                                                  in1=gB[:, gi, :], op0=ALU.add, op1=ALU.mult))(dst, off, gi),
                     reads=[key, "gB"], writes=[key])

            P.barrier()
            st0.close()
            w_in_v = w_in.rearrange("(k p) n -> p k n", p=128)
            for cch in range(8):
                bi = cch % 2
                P.dma("sp", "wm%d" % bi, (lambda bi, cch: lambda e: e.dma_start(out=wm[bi][:], in_=w_in_v[:, :, cch * 512:(cch + 1) * 512]))(bi, cch),
                      reads=[], writes=["wm%d" % bi])
                eng = ("pool", "dve")[cch % 2]
                P.op(eng, (lambda bi, cch: lambda e: e.tensor_copy(out=w_in_sb[:, :, cch * 512:(cch + 1) * 512], in_=wm[bi][:]))(bi, cch),
                     reads=["wm%d" % bi], writes=["w_in_sb"])

            xt = [sbuf(st, "xt%d" % i, [128, D]) for i in range(3)]; xr = Rot("xt", 3)
            junk = sbuf(st, "junkA", [128, D]); tmpA = sbuf(st, "tmpA", [128, D])
            ss = [sbuf(st, "ss%d" % i, [128, 4]) for i in range(2)]; ssr = Rot("ss", 2)
            hxb = [sbuf(st, "hxb%d" % i, [128, D], BF16) for i in range(2)]; hr = Rot("hxb", 2)
            hxT = [sbuf(st, "hxT%d" % i, [128, 8, 512], BF16) for i in range(2)]; hTr = Rot("hxT", 2)
            st_u = sbuf(st, "st_u", [128, 4, 512]); st_k = sbuf(st, "st_k", [128, 4, 512], BF16)
            st_q = sbuf(st, "st_q", [128, 4, 512], BF16); st_g = sbuf(st, "st_g", [128, 16, 512], BF16)
            st_v = sbuf(st, "st_v", [128, 4, 512], BF16)
            tp = [psum(st, "tpA%d" % i, [128, 8, 128], BF16) for i in range(2)]; tpr = Rot("tpA", 2)
            pj = [psum(st, "pj%d" % i, [128, 512]) for i in range(4)]; pjr = Rot("pj", 4)
            evac_i = [0]

            def evac(dst_ap, src_ap, reads, writes, func=None):
                if func is not None:
                    P.op("act", lambda e: e.activation(out=dst_ap, in_=src_ap, func=func), reads, writes)
                    return
                evac_i[0] += 1
                if evac_i[0] % 2:
                    P.op("act", lambda e: e.copy(out=dst_ap, in_=src_ap), reads, writes)
                else:
                    P.op("dve", lambda e: e.tensor_copy(out=dst_ap, in_=src_ap), reads, writes)

            for i_ in range(2):
                P.op("pool", (lambda i_: lambda e: e.memset(hxT[i_][:], 0.0))(i_), writes=["hxT%d" % i_])
            chunks = [("ctx", 0, 256)] + [("lat", i * 512, 512) for i in range(8)]
            for kind, t0, n in chunks:
                src = ctx if kind == "ctx" else x
                mB, mkey = (modcB, "modcB") if kind == "ctx" else (modB, "modB")
                col0 = t0 if kind == "ctx" else CTX + t0
                hi, hkey = hTr.next()
                for t in range(n // 128):
                    xi, xkey = xr.next()
                    si, skey = ssr.next()
                    bi, bkey = hr.next()
                    pi, pkey = tpr.next()
                    r0 = t0 + t * 128
                    P.dma("sp", xkey, (lambda xi, r0, src: lambda e: e.dma_start(out=xt[xi][:], in_=src[r0:r0 + 128, :]))(xi, r0, src), writes=[xkey])
                    P.op("act", (lambda xi, si: lambda e: e.activation(out=junk[:], in_=xt[xi][:], func=AF.Square, accum_out=ss[si][:, 0:1]))(xi, si),
                         reads=[xkey], writes=["junkA", skey])
                    P.op("dve", (lambda si: lambda e: e.tensor_scalar(out=ss[si][:, 1:2], in0=ss[si][:, 0:1], scalar1=1.0 / D, scalar2=EPS,
                                                                      op0=ALU.mult, op1=ALU.add))(si), reads=[skey], writes=[skey])
                    P.op("act", (lambda si: lambda e: e.sqrt(out=ss[si][:, 2:3], in_=ss[si][:, 1:2]))(si), reads=[skey], writes=[skey])
                    P.op("dve", (lambda si: lambda e: e.reciprocal(out=ss[si][:, 3:4], in_=ss[si][:, 2:3]))(si), reads=[skey], writes=[skey])
                    P.op("dve", (lambda xi, si, mB: lambda e: e.scalar_tensor_tensor(out=tmpA[:], in0=xt[xi][:], scalar=ss[si][:, 3:4], in1=mB[:, D:2 * D],
                                                                                     op0=ALU.mult, op1=ALU.mult))(xi, si, mB),
                         reads=[xkey, skey, mkey], writes=["tmpA"])
                    P.op("dve", (lambda bi, mB: lambda e: e.tensor_tensor(out=hxb[bi][:], in0=tmpA[:], in1=mB[:, 0:D], op=ALU.add))(bi, mB),
                         reads=["tmpA", mkey], writes=[bkey])
                    for k in range(8):
                        P.op("pe", (lambda pi, bi, k: lambda e: e.transpose(tp[pi][:, k, :], hxb[bi][:, k * 128:(k + 1) * 128], identb[:]))(pi, bi, k),
                             reads=[bkey, "identb"], writes=[pkey])
                    P.op("act", (lambda hi, pi, t: lambda e: e.copy(out=hxT[hi][:, :, t * 128:(t + 1) * 128], in_=tp[pi][:]))(hi, pi, t),
                         reads=[pkey], writes=[hkey])
                cts = list(range(0, 8)) + (list(range(12, 32)) if kind == "lat" else [])
                for ct in cts:
                    qi, qkey = pjr.next()
                    for k in range(8):
                        P.op("pe", (lambda qi, hi, k, ct: lambda e: e.matmul(pj[qi][:, 0:n], lhsT=w_in_sb[:, k, ct * 128:(ct + 1) * 128], rhs=hxT[hi][:, k, 0:n],
                                                                             start=(k == 0), stop=(k == 7)))(qi, hi, k, ct),
                             reads=["w_in_sb", hkey], writes=[qkey])
                    if ct < 4:
                        evac(st_u[:, ct, 0:n], pj[qi][:, 0:n], [qkey], ["st_u"])
                    elif ct < 8:
                        evac(st_k[:, ct - 4, 0:n], pj[qi][:, 0:n], [qkey], ["st_k"])
                    elif ct < 16:
                        evac(st_q[:, ct - 12, 0:n], pj[qi][:, 0:n], [qkey], ["st_q"])
                    else:
                        evac(st_g[:, ct - 16, 0:n], pj[qi][:, 0:n], [qkey], ["st_g"], func=AF.Sigmoid)
                P.dma("pool", "stu", (lambda col0, n: lambda e: e.dma_start(out=uT_d.rearrange("(t p) n -> p t n", p=128)[:, :, col0:col0 + n], in_=st_u[:, :, 0:n]))(col0, n),
                      reads=["st_u"], writes=["uT_d"])
                P.dma("pool", "stk", (lambda col0, n: lambda e: e.dma_start(out=kT_d.rearrange("(t p) n -> p t n", p=128)[:, :, col0:col0 + n], in_=st_k[:, :, 0:n]))(col0, n),
                      reads=["st_k"], writes=["kT_d"])
                if kind == "lat":
                    P.dma("pool", "stq", (lambda t0: lambda e: e.dma_start(out=qT_d.rearrange("(t p) n -> p t n", p=128)[:, :, t0:t0 + 512], in_=st_q[:]))(t0),
                          reads=["st_q"], writes=["qT_d"])
                    P.dma("pool", "stg", (lambda t0: lambda e: e.dma_start(out=gT_d.rearrange("(t p) n -> p t n", p=128)[:, :, t0:t0 + 512], in_=st_g[:]))(t0),
                          reads=["st_g"], writes=["gT_d"])
                for t in range(n // 128):
                    qi, qkey = pjr.next()
                    for k in range(8):
                        P.op("pe", (lambda qi, hi, k, t: lambda e: e.matmul(pj[qi][:], lhsT=hxT[hi][:, k, t * 128:(t + 1) * 128], rhs=w_in_sb[:, k, 1024:1536],
                                                                            start=(k == 0), stop=(k == 7)))(qi, hi, k, t),
                             reads=["w_in_sb", hkey], writes=[qkey])
                    evac(st_v[:, t, :], pj[qi][:], [qkey], ["st_v"])
                nt = n // 128
                P.dma("pool", "stv", (lambda col0, nt: lambda e: e.dma_start(out=v_d[col0:col0 + nt * 128, :].rearrange("(t p) n -> p t n", p=128), in_=st_v[:, 0:nt, :]))(col0, nt),
                      reads=["st_v"], writes=["v_d"])
        P.barrier()
        if stage <= 1:
            return finish(nc, P, out)

        yT_d = dscr("yT_d", [512, SEQ], F32) if debug else None
        TWO_PI = 2.0 * math.pi
        with ExitStack() as stB:
            zT = sbuf(stB, "zT", [128, 4, SEQ], BF16)
            with phase() as st:
                def t32(n):
                    return sbuf(st, n, [128, 32])
                are, aim, ldt = t32("are"), t32("aim"), t32("ldt")
                Bn = [sbuf(st, "Bn%d" % i, [128, 32, 16]) for i in range(2)]
                bb = [sbuf(st, "bb%d" % i, [128, 32, 16]) for i in range(2)]
                tmpb = sbuf(st, "tmpb", [128, 32, 16])
                Cn2 = [sbuf(st, "Cn2%d" % i, [128, 8, 2, 64]) for i in range(2)]
                dsk = sbuf(st, "dsk", [128, 4])
                maskf = sbuf(st, "maskf", [128, 4, 2]); mask2 = sbuf(st, "mask2", [128, 4, 2])
                pwr = sbuf(st, "pwr", [128, 13, 32]); pwi = sbuf(st, "pwi", [128, 13, 32]); npwi = sbuf(st, "npwi", [128, 13, 32])
                kint = sbuf(st, "kint", [128, 32], I32)
                names = ["dt", "er", "th", "mag", "kf", "rr", "half", "sn", "ah", "cq", "sinr", "cosr", "nre", "den", "rden",
                         "fre", "fim", "t1", "t2"]
                T = {n: t32("p_" + n) for n in names}
                uT_sb = [sbuf(st, "uT_sb%d" % i, [128, NTOK]) for i in range(1)]
                PL = [sbuf(st, "PL%d" % i, [128, 2, NTOK]) for i in range(2)]
                yT = sbuf(st, "yT", [128, SEQ])
                Z = [sbuf(st, "Z%d" % i, [128, 2, 128]) for i in range(2)]
                Zc = [sbuf(st, "Zc%d" % i, [128, 2, 128]) for i in range(2)]
                LB = [sbuf(st, "LB%d" % i, [128, 2, 128]) for i in range(2)]
                LC = [sbuf(st, "LC%d" % i, [128, 2, 128]) for i in range(2)]
                pz = [psum(st, "pz%d" % i, [128, 2, 128]) for i in range(2)]; pzr = Rot("pz", 2)
                pb = [psum(st, "pb%d" % i, [128, 512]) for i in range(3)]; pbr = Rot("pb", 3)
                py = [psum(st, "py%d" % i, [128, 512]) for i in range(2)]; pyr = Rot("py", 2)

                for gl in range(2):
                    sl = slice(gl * 64, (gl + 1) * 64)
                    for dst, srcp, key in ((are, a_re, "are"), (aim, a_im, "aim")):
                        P.dma("act", "pb0", (lambda dst, srcp, sl, gl: lambda e: e.dma_start(
                            out=dst[sl, :].rearrange("p (d g) -> p d g", d=2),
                            in_=srcp.rearrange("d (gp gl) p -> gl p d gp", gl=2)[gl], allow_slow_non_contiguous=True))(dst, srcp, sl, gl), writes=[key])
                    P.dma("act", "pb0", (lambda sl, gl: lambda e: e.dma_start(
                        out=ldt[sl, :].rearrange("p (d g) -> p d g", d=2),
                        in_=log_dt.rearrange("d (gp gl) -> gl d gp", gl=2)[gl:gl + 1].to_broadcast([64, 2, 16]), allow_slow_non_contiguous=True))(sl, gl), writes=["ldt"])
                    for i, srcp in enumerate((b_re, b_im)):
                        P.dma("act", "pb0", (lambda i, srcp, sl, gl: lambda e: e.dma_start(
                            out=Bn[i][sl].rearrange("p (d g) h -> p d g h", d=2),
                            in_=srcp.rearrange("d (gp gl) p h -> gl p d gp h", gl=2)[gl]))(i, srcp, sl, gl), writes=["Bn%d" % i])
                for i, srcp in enumerate((c_re, c_im)):
                    for j in range(2):
                        P.dma("act", "pb0", (lambda i, srcp, j: lambda e: e.dma_start(
                            out=Cn2[i][:, :, j, :].rearrange("p (d u) q -> p d u q", d=2),
                            in_=srcp.rearrange("d (ut g8) h p -> (g8 h) d ut p", g8=8)))(i, srcp, j), writes=["Cn2%d" % i])
                P.dma("act", "pb0", lambda e: e.dma_start(out=dsk[:], in_=ssm_d.rearrange("(ut p) o -> p (ut o)", p=128), allow_slow_non_contiguous=True), writes=["dsk"])
                P.op("pool", lambda e: e.iota(maskf[:], pattern=[[-32, 4], [-16, 2]], base=0, channel_multiplier=1, allow_small_or_imprecise_dtypes=True), writes=["maskf"])
                P.op("dve", lambda e: e.tensor_single_scalar(out=mask2[:], in_=maskf[:], scalar=0.0, op=ALU.is_ge), reads=["maskf"], writes=["mask2"])
                P.op("dve", lambda e: e.tensor_single_scalar(out=maskf[:], in_=maskf[:], scalar=16.0, op=ALU.is_lt), reads=["maskf", "mask2"], writes=["maskf"])
                P.op("dve", lambda e: e.tensor_tensor(out=maskf[:], in0=maskf[:], in1=mask2[:], op=ALU.mult), reads=["maskf", "mask2"], writes=["maskf"])

                PK = ["are", "aim", "ldt", "Bn0", "Bn1", "prm"]

                def dve(fn):
                    P.op("dve", fn, reads=PK, writes=["prm"])

                def act(fn):
                    P.op("act", fn, reads=PK, writes=["prm"])
                act(lambda e: e.activation(out=T["dt"][:], in_=ldt[:], func=AF.Exp))
                dve(lambda e: e.tensor_tensor(out=T["er"][:], in0=are[:], in1=T["dt"][:], op=ALU.mult))
                dve(lambda e: e.tensor_tensor(out=T["th"][:], in0=aim[:], in1=T["dt"][:], op=ALU.mult))
                act(lambda e: e.activation(out=T["mag"][:], in_=T["er"][:], func=AF.Exp))
                dve(lambda e: e.tensor_single_scalar(out=T["kf"][:], in_=T["th"][:], scalar=1.0 / TWO_PI, op=ALU.mult))
                dve(lambda e: e.tensor_copy(out=kint[:], in_=T["kf"][:]))
                dve(lambda e: e.tensor_copy(out=T["kf"][:], in_=kint[:]))
                dve(lambda e: e.scalar_tensor_tensor(out=T["rr"][:], in0=T["kf"][:], scalar=-TWO_PI, in1=T["th"][:], op0=ALU.mult, op1=ALU.add))
                dve(lambda e: e.tensor_single_scalar(out=T["half"][:], in_=T["rr"][:], scalar=0.5, op=ALU.mult))
                act(lambda e: e.activation(out=T["ah"][:], in_=T["half"][:], func=AF.Abs))
                dve(lambda e: e.tensor_scalar(out=T["t1"][:], in0=T["ah"][:], scalar1=-1.0, scalar2=math.pi / 2, op0=ALU.mult, op1=ALU.add))
                act(lambda e: e.activation(out=T["sn"][:], in_=T["half"][:], func=AF.Sin))
                act(lambda e: e.activation(out=T["cq"][:], in_=T["t1"][:], func=AF.Sin))
                dve(lambda e: e.scalar_tensor_tensor(out=T["sinr"][:], in0=T["sn"][:], scalar=2.0, in1=T["cq"][:], op0=ALU.mult, op1=ALU.mult))
                dve(lambda e: e.scalar_tensor_tensor(out=T["t2"][:], in0=T["sn"][:], scalar=-2.0, in1=T["sn"][:], op0=ALU.mult, op1=ALU.mult))
                dve(lambda e: e.tensor_single_scalar(out=T["cosr"][:], in_=T["t2"][:], scalar=1.0, op=ALU.add))
                dve(lambda e: e.tensor_tensor(out=pwr[:, 0, :], in0=T["mag"][:], in1=T["cosr"][:], op=ALU.mult))
                dve(lambda e: e.tensor_tensor(out=pwi[:, 0, :], in0=T["mag"][:], in1=T["sinr"][:], op=ALU.mult))
                dve(lambda e: e.tensor_single_scalar(out=T["nre"][:], in_=pwr[:, 0, :], scalar=-1.0, op=ALU.add))
                dve(lambda e: e.tensor_tensor(out=T["den"][:], in0=are[:], in1=are[:], op=ALU.mult))
                dve(lambda e: e.tensor_tensor(out=T["t1"][:], in0=aim[:], in1=aim[:], op=ALU.mult))
                dve(lambda e: e.tensor_tensor(out=T["den"][:], in0=T["den"][:], in1=T["t1"][:], op=ALU.add))
                dve(lambda e: e.reciprocal(out=T["rden"][:], in_=T["den"][:]))
                dve(lambda e: e.tensor_tensor(out=T["t1"][:], in0=T["nre"][:], in1=are[:], op=ALU.mult))
                dve(lambda e: e.tensor_tensor(out=T["t2"][:], in0=pwi[:, 0, :], in1=aim[:], op=ALU.mult))
                dve(lambda e: e.tensor_tensor(out=T["t1"][:], in0=T["t1"][:], in1=T["t2"][:], op=ALU.add))
                dve(lambda e: e.tensor_tensor(out=T["fre"][:], in0=T["t1"][:], in1=T["rden"][:], op=ALU.mult))
                dve(lambda e: e.tensor_tensor(out=T["t1"][:], in0=pwi[:, 0, :], in1=are[:], op=ALU.mult))
                dve(lambda e: e.tensor_tensor(out=T["t2"][:], in0=T["nre"][:], in1=aim[:], op=ALU.mult))
                dve(lambda e: e.tensor_tensor(out=T["t1"][:], in0=T["t1"][:], in1=T["t2"][:], op=ALU.subtract))
                dve(lambda e: e.tensor_tensor(out=T["fim"][:], in0=T["t1"][:], in1=T["rden"][:], op=ALU.mult))
                fr = T["fre"][:].unsqueeze(2).to_broadcast([128, 32, 16]); fi = T["fim"][:].unsqueeze(2).to_broadcast([128, 32, 16])
                dve(lambda e: e.tensor_tensor(out=bb[0][:], in0=Bn[0][:], in1=fr, op=ALU.mult))
                dve(lambda e: e.tensor_tensor(out=tmpb[:], in0=Bn[1][:], in1=fi, op=ALU.mult))
                dve(lambda e: e.tensor_tensor(out=bb[0][:], in0=bb[0][:], in1=tmpb[:], op=ALU.subtract))
                dve(lambda e: e.tensor_tensor(out=bb[1][:], in0=Bn[1][:], in1=fr, op=ALU.mult))
                dve(lambda e: e.tensor_tensor(out=tmpb[:], in0=Bn[0][:], in1=fi, op=ALU.mult))
                dve(lambda e: e.tensor_tensor(out=bb[1][:], in0=bb[1][:], in1=tmpb[:], op=ALU.add))
                for k in range(12):
                    dve((lambda k: lambda e: e.tensor_tensor(out=T["t1"][:], in0=pwr[:, k, :], in1=pwr[:, k, :], op=ALU.mult))(k))
                    dve((lambda k: lambda e: e.tensor_tensor(out=T["t2"][:], in0=pwi[:, k, :], in1=pwi[:, k, :], op=ALU.mult))(k))
                    dve((lambda k: lambda e: e.tensor_tensor(out=pwr[:, k + 1, :], in0=T["t1"][:], in1=T["t2"][:], op=ALU.subtract))(k))
                    dve((lambda k: lambda e: e.scalar_tensor_tensor(out=pwi[:, k + 1, :], in0=pwr[:, k, :], scalar=2.0, in1=pwi[:, k, :], op0=ALU.mult, op1=ALU.mult))(k))
                dve(lambda e: e.tensor_single_scalar(out=npwi[:], in_=pwi[:], scalar=-1.0, op=ALU.mult))

                chain = {}

                def cmul_acc(hi_re, hi_im, lo_re, lo_im, k, u, key):
                    sr = pwr[:, k, u:u + 1]; si = pwi[:, k, u:u + 1]; nsi = npwi[:, k, u:u + 1]
                    prev = chain.get(key)
                    if prev is None:
                        prev = [w for w in (P.writer.get(key), P.writer.get("prm")) if w is not None]
                    ops_ = []
                    for n_, (o_, a_, s_) in enumerate(((hi_re, lo_re, sr), (hi_im, lo_re, si), (hi_re, lo_im, nsi), (hi_im, lo_im, sr))):
                        op = _Op("dve", (lambda o_, a_, s_: lambda e: e.scalar_tensor_tensor(out=o_, in0=a_, scalar=s_, in1=o_, op0=ALU.mult, op1=ALU.add))(o_, a_, s_), len(P.ops))
                        op.deps = list(prev) if n_ < 2 else [ops_[n_ - 2]]
                        P.ops.append(op)
                        ops_.append(op)
                    chain[key] = [ops_[3]]

                def scan_done(key):
                    P.writer[key] = chain.pop(key)[0]
                    P.readers[key] = []

                def bk_scan(pl, c0, n, rev, u, key, up_only=False):
                    L = n.bit_length() - 1
                    re = pl[:, 0, c0:c0 + n]; im = pl[:, 1, c0:c0 + n]
                    for k in range(L):
                        s_ = 2 << k; h_ = 1 << k
                        vr = re.rearrange("p (m s) -> p m s", s=s_); vi = im.rearrange("p (m s) -> p m s", s=s_)
                        if not rev:
                            cmul_acc(vr[:, :, s_ - 1], vi[:, :, s_ - 1], vr[:, :, h_ - 1], vi[:, :, h_ - 1], k, u, key)
                        else:
                            cmul_acc(vr[:, :, 0], vi[:, :, 0], vr[:, :, h_], vi[:, :, h_], k, u, key)
                    for k in (range(L - 2, -1, -1) if not up_only else ()):
                        s_ = 2 << k; h_ = 1 << k
                        vr = re.rearrange("p (m s) -> p m s", s=s_); vi = im.rearrange("p (m s) -> p m s", s=s_)
                        if not rev:
                            cmul_acc(vr[:, 1:, h_ - 1], vi[:, 1:, h_ - 1], vr[:, :-1, s_ - 1], vi[:, :-1, s_ - 1], k, u, key)
                        else:
                            cmul_acc(vr[:, :-1, h_], vi[:, :-1, h_], vr[:, 1:, 0], vi[:, 1:, 0], k, u, key)

                segs = [(0, 256)] + [(CTX + i * 512, 512) for i in range(8)]
                units = [(ut, d_, gpl) for ut in range(4) for d_ in range(2) for gpl in range(4)]

                def stA(ix):
                    ut, d_, gpl = units[ix]
                    u = d_ * 16 + ut * 4 + gpl
                    bi = ix % 2; ub = 0; ukey = "uT_sb0"
                    zk, zck, lbk, lck, plk = "Z%d" % bi, "Zc%d" % bi, "LB%d" % bi, "LC%d" % bi, "PL%d" % bi
                    if ix % 8 == 0:
                        P.dma("sp", ukey, lambda e: e.dma_start(out=uT_sb[ub][:], in_=uT_d[ut * 128:(ut + 1) * 128, :]), reads=["uT_d"], writes=[ukey])
                    P.op("pool", lambda e: e.memset(Z[bi][:], 0.0), writes=[zk])
                    for j in range(2):
                        for gl in range(2):
                            cs = (2 * gpl + gl) * 16
                            P.op("pool", (lambda j, gl, cs: lambda e: e.tensor_copy(out=Z[bi][gl * 64:(gl + 1) * 64, j, cs:cs + 16], in_=bb[j][gl * 64:(gl + 1) * 64, u, :]))(j, gl, cs),
                                 reads=["prm"], writes=[zk])
                    zi, zkey = pzr.next()
                    for j in range(2):
                        P.op("pe", (lambda zi, j: lambda e: e.matmul(pz[zi][:, j, :], lhsT=Z[bi][:, j, :], rhs=identf[:], start=True, stop=True))(zi, j), reads=[zk, "identf"], writes=[zkey])
                    P.op("act", (lambda zi: lambda e: e.copy(out=LB[bi][:], in_=pz[zi][:]))(zi), reads=[zkey], writes=[lbk])
                    for j in range(2):
                        P.op("pool", (lambda j: lambda e: e.tensor_tensor(out=Zc[bi][:, j, :].rearrange("p (g q) -> p g q", g=2), in0=Cn2[j][:, d_ * 4 + ut, :, :],
                                                                         in1=maskf[:, gpl, :].unsqueeze(2).to_broadcast([128, 2, 64]), op=ALU.mult))(j),
                             reads=["Cn2%d" % j, "maskf"], writes=[zck])
                    zi2, zkey2 = pzr.next()
                    for j in range(2):
                        P.op("pe", (lambda zi2, j: lambda e: e.matmul(pz[zi2][:, j, :], lhsT=Zc[bi][:, j, :], rhs=identf[:], start=True, stop=True))(zi2, j), reads=[zck, "identf"], writes=[zkey2])
                    P.op("act", lambda e: e.copy(out=LC[bi][:, 0, :], in_=pz[zi2][:, 0, :]), reads=[zkey2], writes=[lck])
                    P.op("act", lambda e: e.mul(out=LC[bi][:, 1, :], in_=pz[zi2][:, 1, :], mul=-1.0), reads=[zkey2], writes=[lck])
                    for (c0, n) in segs:
                        for j in range(2):
                            qi, qkey = pbr.next()
                            P.op("pe", (lambda qi, j, c0, n: lambda e: e.matmul(pb[qi][:, 0:n], lhsT=LB[bi][:, j, :], rhs=uT_sb[ub][:, c0:c0 + n], start=True, stop=True))(qi, j, c0, n),
                                 reads=[lbk, ukey], writes=[qkey])
                            P.op("act", (lambda qi, j, c0, n: lambda e: e.copy(out=PL[bi][:, j, c0:c0 + n], in_=pb[qi][:, 0:n]))(qi, j, c0, n), reads=[qkey], writes=[plk])

                def stB(ix):
                    ut, d_, gpl = units[ix]
                    u = d_ * 16 + ut * 4 + gpl
                    bi = ix % 2; plk = "PL%d" % bi
                    rev = (d_ == 1)
                    bk_scan(PL[bi], 0, CTX, rev, u, plk, up_only=True)
                    if not rev:
                        cmul_acc(PL[bi][:, 0, CTX:CTX + 1], PL[bi][:, 1, CTX:CTX + 1], PL[bi][:, 0, CTX - 1:CTX], PL[bi][:, 1, CTX - 1:CTX], 0, u, plk)
                    else:
                        cmul_acc(PL[bi][:, 0, NTOK - 1:NTOK], PL[bi][:, 1, NTOK - 1:NTOK], PL[bi][:, 0, 0:1], PL[bi][:, 1, 0:1], 0, u, plk)
                    bk_scan(PL[bi], CTX, SEQ, rev, u, plk)
                    scan_done(plk)

                def stC(ix):
                    ut, d_, gpl = units[ix]
                    bi = ix % 2; ub = 0; ukey = "uT_sb0"; lck, plk = "LC%d" % bi, "PL%d" % bi
                    first = (ix % 8 == 0)
                    for sgi in range(8):
                        c0 = CTX + sgi * 512
                        yi, ykey = pyr.next()
                        for j in range(2):
                            P.op("pe", (lambda yi, j, c0: lambda e: e.matmul(py[yi][:], lhsT=LC[bi][:, j, :], rhs=PL[bi][:, j, c0:c0 + 512], start=(j == 0), stop=(j == 1)))(yi, j, c0),
                                 reads=[lck, plk], writes=[ykey])
                        ysl = slice(sgi * 512, (sgi + 1) * 512)
                        if first:
                            P.op("dve", (lambda yi, c0, ysl: lambda e: e.scalar_tensor_tensor(out=yT[:, ysl], in0=uT_sb[ub][:, c0:c0 + 512], scalar=dsk[:, ut:ut + 1],
                                                                                              in1=py[yi][:], op0=ALU.mult, op1=ALU.add))(yi, c0, ysl),
                                 reads=[ykey, ukey, "dsk"], writes=["yT"])
                        else:
                            P.op("dve", (lambda yi, ysl: lambda e: e.tensor_tensor(out=yT[:, ysl], in0=yT[:, ysl], in1=py[yi][:], op=ALU.add))(yi, ysl), reads=[ykey], writes=["yT"])
                    if ix % 8 == 7:
                        if debug:
                            P.dma("sp", "dbgy", lambda e: e.dma_start(out=yT_d[ut * 128:(ut + 1) * 128, :], in_=yT[:]), reads=["yT"], writes=["yT_d"])
                        P.op("act", lambda e: e.activation(out=zT[:, ut, :], in_=yT[:], func=AF.Gelu_apprx_tanh), reads=["yT"], writes=["zT"])

                stA(0)
                for ix in range(32):
                    if ix + 1 < 32:
                        stA(ix + 1)
                    stB(ix)
                    stC(ix)
            P.barrier()
            with phase() as st:
                wstgB_t = sbuf(st, "wstgB", [128, 4, 1024])
                w_glu_sb = sbuf(st, "w_glu_sb", [128, 4, 512], BF16); w_ba_sb = sbuf(st, "w_ba_sb", [128, 4, D], BF16)
                bglu = sbuf(st, "bglu", [128, 4])
                sg = [sbuf(st, "sg%d" % i, [128, 512], BF16) for i in range(2)]; sgr = Rot("sg", 2)
                glu = [sbuf(st, "glu%d" % i, [128, 4, 512], BF16) for i in range(2)]
                st_ba = [sbuf(st, "st_ba%d" % i, [128, 8, 512], BF16) for i in range(2)]
                pg = [psum(st, "pg%d" % i, [128, 512]) for i in range(3)]; pgr = Rot("pg", 3)
                pa = [psum(st, "pa%d" % i, [128, 512]) for i in range(3)]; par = Rot("pa", 3)
                P.dma("sp", "wl0", lambda e: e.dma_start(out=wstgB_t[:, :, 0:512], in_=w_glu.rearrange("(k p) n -> p k n", p=128)), writes=["wstgB"])
                P.op("dve", lambda e: e.tensor_copy(out=w_glu_sb[:], in_=wstgB_t[:, :, 0:512]), reads=["wstgB"], writes=["w_glu_sb"])
                P.dma("sp", "wl0", lambda e: e.dma_start(out=wstgB_t[:], in_=w_ba.rearrange("(k p) n -> p k n", p=128)), reads=["wstgB"], writes=["wstgB"])
                P.op("dve", lambda e: e.tensor_copy(out=w_ba_sb[:], in_=wstgB_t[:]), reads=["wstgB"], writes=["w_ba_sb"])
                P.dma("act", "wl1", lambda e: e.dma_start(out=bglu[:], in_=b_glu.rearrange("(k p) o -> p (k o)", p=128), allow_slow_non_contiguous=True), writes=["bglu"])
                for sgi in range(8):
                    gb_ = sgi % 2; gkey = "glu%d" % gb_; bakey = "st_ba%d" % gb_
                    ssl = slice(sgi * 512, (sgi + 1) * 512)
                    for ct in range(4):
                        gi, gk = pgr.next()
                        for k in range(4):
                            P.op("pe", (lambda gi, k, ct, ssl: lambda e: e.matmul(pg[gi][:], lhsT=w_glu_sb[:, k, ct * 128:(ct + 1) * 128], rhs=zT[:, k, ssl],
                                                                                  start=(k == 0), stop=(k == 3)))(gi, k, ct, ssl),
                                 reads=["w_glu_sb", "zT"], writes=[gk])
                        si_, sk_ = sgr.next()
                        P.op("act", (lambda si_, gi, ct: lambda e: e.activation(out=sg[si_][:], in_=pg[gi][:], func=AF.Sigmoid, bias=bglu[:, ct:ct + 1]))(si_, gi, ct),
                             reads=[gk, "bglu"], writes=[sk_])
                        P.op("dve", (lambda gb_, ct, si_, ssl: lambda e: e.tensor_tensor(out=glu[gb_][:, ct, :], in0=sg[si_][:], in1=zT[:, ct, ssl], op=ALU.mult))(gb_, ct, si_, ssl),
                             reads=[sk_, "zT"], writes=[gkey])
                    for ct2 in range(8):
                        ai, ak = par.next()
                        for k in range(4):
                            P.op("pe", (lambda ai, k, ct2, gb_: lambda e: e.matmul(pa[ai][:], lhsT=w_ba_sb[:, k, ct2 * 128:(ct2 + 1) * 128], rhs=glu[gb_][:, k, :],
                                                                                   start=(k == 0), stop=(k == 3)))(ai, k, ct2, gb_),
                                 reads=["w_ba_sb", gkey], writes=[ak])
                        if ct2 % 2:
                            P.op("act", (lambda gb_, ct2, ai: lambda e: e.copy(out=st_ba[gb_][:, ct2, :], in_=pa[ai][:]))(gb_, ct2, ai), reads=[ak], writes=[bakey])
                        else:
                            P.op("dve", (lambda gb_, ct2, ai: lambda e: e.tensor_copy(out=st_ba[gb_][:, ct2, :], in_=pa[ai][:]))(gb_, ct2, ai), reads=[ak], writes=[bakey])
                    P.dma("sp", bakey, (lambda gb_, ssl: lambda e: e.dma_start(out=baT_d.rearrange("(t p) n -> p t n", p=128)[:, :, ssl], in_=st_ba[gb_][:]))(gb_, ssl),
                          reads=[bakey], writes=["baT_d"])
        P.barrier()
        if stage <= 2:
            return finish(nc, P, out)

        attT_d = dscr("attT_d", [512, SEQ], BF16) if debug else None
        NEG = -30000.0
        with ExitStack() as stC:
            attT_sb = sbuf(stC, "attT_sb", [128, 4, SEQ], BF16)
            stC2 = ExitStack()
            kT_sb = sbuf(stC2, "kT_sb", [128, 4, NTOK], BF16); qT_sb = sbuf(stC2, "qT_sb", [128, 4, SEQ], BF16)
            BiasTT = sbuf(stC2, "BiasTT", [128, 8 * 14, 64])
            Vctx = sbuf(stC2, "Vctx", [128, 2, 512], BF16)
            ones_b = sbuf(stC2, "ones_b", [128, 128], BF16)
            P.dma("sp", "lc0", lambda e: e.dma_start(out=kT_sb[:], in_=kT_d.rearrange("(t p) n -> p t n", p=128)), reads=["kT_d"], writes=["kT_sb"])
            P.dma("act", "lc1", lambda e: e.dma_start(out=qT_sb[:], in_=qT_d.rearrange("(t p) n -> p t n", p=128)), reads=["qT_d"], writes=["qT_sb"])
            P.dma("act", "lc1", lambda e: e.dma_start(out=Vctx[:], in_=v_d[0:CTX, :].rearrange("(t p) n -> p t n", p=128)), reads=["v_d"], writes=["Vctx"])
            P.op("pool", lambda e: e.memset(ones_b[:], 1.0), writes=["ones_b"])
            with phase() as st:
                rpbB = sbuf(st, "rpbB", [128, 8 * 14, 31]); tmpC = sbuf(st, "tmpC", [128, 8 * 14, 64])
                Dm = sbuf(st, "Dm", [128, 64]); eqm = [sbuf(st, "eqm%d" % i, [128, 64]) for i in range(2)]
                c0t = sbuf(st, "c0t", [128, 64]); kcv = sbuf(st, "kcv", [128, 64]); m2 = sbuf(st, "m2c", [128, 64])
                for half in range(2):
                    sl = slice(half * 64, (half + 1) * 64)
                    P.dma("sp", "lc2", (lambda sl, half: lambda e: e.dma_start(out=rpbB[sl].rearrange("p (h j) m -> p h (j m)", h=8),
                                                                              in_=rpb[:, half:half + 14, :].rearrange("h j m -> h (j m)").unsqueeze(0).to_broadcast([64, 8, 14 * 31])))(sl, half),
                          writes=["rpbB"])
                    P.op("pool", (lambda sl: lambda e: e.iota(Dm[sl], pattern=[[-1, 64]], base=15, channel_multiplier=1, allow_small_or_imprecise_dtypes=True))(sl), writes=["Dm"])
                    P.op("pool", (lambda sl: lambda e: e.iota(kcv[sl], pattern=[[0, 64]], base=0, channel_multiplier=1, allow_small_or_imprecise_dtypes=True))(sl), writes=["kcv"])
                P.op("pool", lambda e: e.iota(c0t[:], pattern=[[1, 64]], base=-8, channel_multiplier=0, allow_small_or_imprecise_dtypes=True), writes=["c0t"])
                P.op("dve", lambda e: e.tensor_scalar(out=c0t[:], in0=c0t[:], scalar1=0.0, scalar2=48.0, op0=ALU.max, op1=ALU.min), reads=["c0t"], writes=["c0t"])
                P.op("dve", lambda e: e.tensor_tensor(out=kcv[:], in0=kcv[:], in1=c0t[:], op=ALU.subtract), reads=["kcv", "c0t"], writes=["kcv"])
                P.op("dve", lambda e: e.tensor_single_scalar(out=m2[:], in_=kcv[:], scalar=0.0, op=ALU.is_ge), reads=["kcv"], writes=["m2c"])
                P.op("dve", lambda e: e.tensor_single_scalar(out=kcv[:], in_=kcv[:], scalar=15.0, op=ALU.is_le), reads=["kcv", "m2c"], writes=["kcv"])
                P.op("dve", lambda e: e.tensor_tensor(out=m2[:], in0=m2[:], in1=kcv[:], op=ALU.mult), reads=["kcv", "m2c"], writes=["m2c"])
                P.op("dve", lambda e: e.tensor_scalar(out=m2[:], in0=m2[:], scalar1=-1.0, scalar2=-NEG, op0=ALU.add, op1=ALU.mult), reads=["m2c"], writes=["m2c"])
                P.op("dve", lambda e: e.tensor_copy(out=BiasTT[:], in_=m2[:].unsqueeze(1).to_broadcast([128, 112, 64])), reads=["m2c"], writes=["BiasTT"])
                for m in range(31):
                    ei = m % 2; ek = "eqm%d" % ei
                    P.op("dve", (lambda ei, m: lambda e: e.tensor_single_scalar(out=eqm[ei][:], in_=Dm[:], scalar=float(m), op=ALU.is_equal))(ei, m), reads=["Dm"], writes=[ek])
                    for hh in range(2):
                        hs = slice(hh * 56, (hh + 1) * 56); tk_ = "tmpC%d" % hh
                        P.op("pool", (lambda ei, m, hh, hs: lambda e: e.tensor_tensor(out=tmpC[:, hs, :], in0=eqm[ei][:].unsqueeze(1).to_broadcast([128, 56, 64]),
                                                                                      in1=rpbB[:, hs, m:m + 1].to_broadcast([128, 56, 64]), op=ALU.mult))(ei, m, hh, hs),
                             reads=[ek, "rpbB"], writes=[tk_])
                        P.op("dve", (lambda hs: lambda e: e.tensor_tensor(out=BiasTT[:, hs, :], in0=BiasTT[:, hs, :], in1=tmpC[:, hs, :], op=ALU.add))(hs), reads=[tk_, "BiasTT"], writes=["BiasTT%d" % hh])
            P.barrier()
            with phase() as st:
                Vb = [sbuf(st, "Vb%d" % i, [128, 4, 512], BF16) for i in range(3)]; vbr = Rot("Vb", 3)
                ssb = [sbuf(st, "ssb%d" % i, [128, 4, 64]) for i in range(3)]; ssr2 = Rot("ssb", 3)
                pT = [sbuf(st, "pT%d" % i, [128, 384], BF16) for i in range(3)]; ptr = Rot("pT", 3)
                rden = [sbuf(st, "rden%d" % i, [128, 64]) for i in range(2)]; rdr = Rot("rden", 2)
                ps_ = [psum(st, "psc%d" % i, [128, 512]) for i in range(3)]; psr = Rot("psc", 3)
                po_ = [psum(st, "poc%d" % i, [128, 512]) for i in range(2)]; por = Rot("poc", 2)
                pd_ = [psum(st, "pdc%d" % i, [128, 512]) for i in range(2)]; pdr = Rot("pdc", 2)
                B4 = BiasTT[:].rearrange("p (h j) q -> p h j q", h=8)
                cf = [sbuf(st, "cvf%d" % i, [128, 2048]) for i in range(3)]; cb = [sbuf(st, "cvb%d" % i, [128, 2048], BF16) for i in range(3)]

                def convert_tile(ti):
                    bi = ti % 3
                    P.dma("sp", "cvf%d" % bi, lambda e: e.dma_start(out=cf[bi][:], in_=peer_uv[ti * 128:(ti + 1) * 128, :]), writes=["cvf%d" % bi])
                    P.op("pool", lambda e: e.tensor_copy(out=cb[bi][:], in_=cf[bi][:]), reads=["cvf%d" % bi], writes=["cvb%d" % bi])
                    P.dma("pool", "cvb%d" % bi, lambda e: e.dma_start(out=uvb_d[ti * 128:(ti + 1) * 128, :], in_=cb[bi][:]), reads=["cvb%d" % bi], writes=["uvb_d"])
                def c_scores(r, h, vi):
                    r0 = min(max(r - 4, 0), 56)
                    t = h // 2; po = (h % 2) * 64; psl = slice(po, po + 64)
                    si, skey = psr.next()
                    qsl = slice(r * 64, (r + 1) * 64)
                    for j in range(6):
                        k0 = (CTX + (r0 + 2 * j) * 64) if j < 4 else (j - 4) * 128
                        P.op("pe", (lambda j, k0: lambda e: e.matmul(ps_[si][:, j * 64:(j + 1) * 64], lhsT=kT_sb[psl, t, k0:k0 + 128], rhs=qT_sb[psl, t, qsl], start=True, stop=True))(j, k0),
                             reads=["kT_sb", "qT_sb"], writes=[skey])
                    return (r, h, vi, si, skey)

                def c_part1(state):
                    r, h, vi, si, skey = state
                    r0 = min(max(r - 4, 0), 56); dr0 = r0 - r + 7
                    bi2, bkey2 = ssr2.next()
                    P.op("dve", lambda e: e.scalar_tensor_tensor(out=ssb[bi2][:], in0=ps_[si][:, 0:256].rearrange("p (j q) -> p j q", j=4), scalar=0.125,
                                                                 in1=B4[:, h, dr0:dr0 + 7:2, :], op0=ALU.mult, op1=ALU.add), reads=[skey, "BiasTT"], writes=[bkey2])
                    ti, tkey = ptr.next()
                    P.op("act", lambda e: e.activation(out=pT[ti][:, 0:256], in_=ssb[bi2][:].rearrange("p j q -> p (j q)"), func=AF.Exp), reads=[bkey2], writes=[tkey])
                    P.op("act", lambda e: e.activation(out=pT[ti][:, 256:384], in_=ps_[si][:, 256:384], func=AF.Exp, scale=0.125), reads=[skey], writes=[tkey])
                    return (r, h, vi, ti, tkey)

                def c_part2(state):
                    r, h, vi, ti, tkey = state
                    vkey = "Vb%d" % vi
                    t = h // 2; po = (h % 2) * 64; psl = slice(po, po + 64)
                    qsl = slice(r * 64, (r + 1) * 64)
                    oi, okey = por.next(); di, dkey = pdr.next()
                    hp = (h // 2) * 128
                    for j in range(6):
                        vsrc = (Vb[vi][:, j, hp:hp + 128] if j < 4 else Vctx[:, j - 4, hp:hp + 128])
                        P.op("pe", (lambda j, vsrc: lambda e: e.matmul(po_[oi][:, 0:64], lhsT=vsrc, rhs=pT[ti][:, j * 64:(j + 1) * 64], start=(j == 0), stop=(j == 5)))(j, vsrc),
                             reads=[vkey, "Vctx", tkey], writes=[okey])
                    for j in range(6):
                        P.op("pe", (lambda j: lambda e: e.matmul(pd_[di][:, 0:64], lhsT=ones_b[:], rhs=pT[ti][:, j * 64:(j + 1) * 64], start=(j == 0), stop=(j == 5)))(j),
                             reads=["ones_b", tkey], writes=[dkey])
                    ri, rkey = rdr.next()
                    P.op("dve", lambda e: e.reciprocal(out=rden[ri][psl, :], in_=pd_[di][psl, 0:64]), reads=[dkey], writes=[rkey])
                    P.op("dve", lambda e: e.tensor_tensor(out=attT_sb[psl, t, qsl], in0=po_[oi][psl, 0:64], in1=rden[ri][psl, :], op=ALU.mult), reads=[okey, rkey], writes=["attT_sb"])

                pend1 = None; pend2 = None
                for r in range(64):
                    convert_tile(2 * r); convert_tile(2 * r + 1)
                    r0 = min(max(r - 4, 0), 56)
                    vi, vkey = vbr.next()
                    P.dma("sp", vkey, (lambda vi, r0: lambda e: e.dma_start(out=Vb[vi][:], in_=v_d[CTX + r0 * 64:CTX + (r0 + 8) * 64, :].rearrange("(j p) n -> p j n", p=128)))(vi, r0),
                          reads=["v_d"], writes=[vkey])
                    for h in range(8):
                        stt = c_scores(r, h, vi)
                        nxt2 = c_part1(pend1) if pend1 is not None else None
                        if pend2 is not None:
                            c_part2(pend2)
                        pend2 = nxt2
                        pend1 = stt
                nxt2 = c_part1(pend1)
                if pend2 is not None:
                    c_part2(pend2)
                c_part2(nxt2)
            if debug:
                P.dma("sp", "dbga", lambda e: e.dma_start(out=attT_d.rearrange("(t p) n -> p t n", p=128), in_=attT_sb[:]), reads=["attT_sb"], writes=["attT_d"])
            P.barrier()
            stC2.close()
            with phase() as st:
                wstgC_t = sbuf(st, "wstgC", [128, 4, 1024]); w_bb_sb = sbuf(st, "w_bb_sb", [128, 4, D], BF16)
                g_sb = [sbuf(st, "g_sb%d" % i, [128, 16, 512], BF16) for i in range(2)]
                ba_sb = [sbuf(st, "ba_sb%d" % i, [128, 8, 512], BF16) for i in range(2)]
                t1 = [sbuf(st, "t1c%d" % i, [128, 512]) for i in range(2)]; t1r = Rot("t1c", 2)
                t2 = [sbuf(st, "t2c%d" % i, [128, 512]) for i in range(2)]; t2r = Rot("t2c", 2)
                st_mg = [sbuf(st, "st_mg%d" % i, [128, 8, 512], BF16) for i in range(2)]
                pbb = [psum(st, "pbb%d" % i, [128, 512]) for i in range(3)]; pbr2 = Rot("pbb", 3)
                P.dma("sp", "wc0", lambda e: e.dma_start(out=wstgC_t[:], in_=w_bb.rearrange("(k p) n -> p k n", p=128)), writes=["wstgC"])
                P.op("dve", lambda e: e.tensor_copy(out=w_bb_sb[:], in_=wstgC_t[:]), reads=["wstgC"], writes=["w_bb_sb"])
                for sgi in range(8):
                    b2 = sgi % 2; ssl = slice(sgi * 512, (sgi + 1) * 512)
                    gk, bk, mk = "g_sb%d" % b2, "ba_sb%d" % b2, "st_mg%d" % b2
                    P.dma("act", gk, (lambda b2, ssl: lambda e: e.dma_start(out=g_sb[b2][:], in_=gT_d.rearrange("(t p) n -> p t n", p=128)[:, :, ssl]))(b2, ssl), reads=["gT_d"], writes=[gk])
                    P.dma("act", bk, (lambda b2, ssl: lambda e: e.dma_start(out=ba_sb[b2][:], in_=baT_d.rearrange("(t p) n -> p t n", p=128)[:, :, ssl]))(b2, ssl), reads=["baT_d"], writes=[bk])
                    for ct2 in range(8):
                        qi, qk = pbr2.next()
                        for k in range(4):
                            P.op("pe", (lambda qi, k, ct2, ssl: lambda e: e.matmul(pbb[qi][:], lhsT=w_bb_sb[:, k, ct2 * 128:(ct2 + 1) * 128], rhs=attT_sb[:, k, ssl],
                                                                                   start=(k == 0), stop=(k == 3)))(qi, k, ct2, ssl),
                                 reads=["w_bb_sb", "attT_sb"], writes=[qk])
                        i1, k1 = t1r.next(); i2, k2 = t2r.next()
                        P.op("dve", (lambda i1, qi, b2, ct2: lambda e: e.tensor_tensor(out=t1[i1][:], in0=pbb[qi][:], in1=g_sb[b2][:, 8 + ct2, :], op=ALU.mult))(i1, qi, b2, ct2),
                             reads=[qk, gk], writes=[k1])
                        P.op("pool", (lambda i2, b2, ct2: lambda e: e.tensor_tensor(out=t2[i2][:], in0=ba_sb[b2][:, ct2, :], in1=g_sb[b2][:, ct2, :], op=ALU.mult))(i2, b2, ct2),
                             reads=[bk, gk], writes=[k2])
                        P.op("dve", (lambda b2, ct2, i1, i2: lambda e: e.tensor_tensor(out=st_mg[b2][:, ct2, :], in0=t1[i1][:], in1=t2[i2][:], op=ALU.add))(b2, ct2, i1, i2),
                             reads=[k1, k2], writes=[mk])
                    P.dma("sp", mk, (lambda b2, ssl: lambda e: e.dma_start(out=mgT_d.rearrange("(t p) n -> p t n", p=128)[:, :, ssl], in_=st_mg[b2][:]))(b2, ssl),
                          reads=[mk], writes=["mgT_d"])
        P.barrier()
        if stage <= 3:
            return finish(nc, P, out)

        x1_d = dscr("x1_d", [SEQ, D], F32) if debug else None
        pf_d = dscr("pf_d", [SEQ, D], F32) if debug else None
        NT = 32 if stage >= 5 else int(stage * 10) % 10 or 1
        if not debug:
            NT = 32
        if "KNT" in os.environ:
            NT = int(os.environ["KNT"])
        gate1B = modB[:, 2 * D:3 * D]; S2B = modB[:, 3 * D:4 * D]; G2B = modB[:, 4 * D:5 * D]; gate2B = modB[:, 5 * D:6 * D]
        with ExitStack() as stD:
            w_out_sb = sbuf(stD, "w_out_sb", [128, 8, D], BF16); w_q_sb = sbuf(stD, "w_q_sb", [128, 8, 2048], BF16)
            skT = sbuf(stD, "skT", [128, 2, 128], BF16); fgB = sbuf(stD, "fgB", [128, D])
            with phase() as st:
                wstgD_t = [sbuf(st, "wstgD%d" % i, [128, 8, 512]) for i in range(2)]
                skf = sbuf(st, "skf", [128, 2, 128]); skb = sbuf(st, "skb", [128, 2, 128], BF16)
                ptk = psum(st, "ptk", [128, 2, 128], BF16)
                for i in range(6):
                    bi = i % 2
                    srcw = (w_out if i < 2 else w_q).rearrange("(k p) n -> p k n", p=128)
                    c0 = (i * 512) if i < 2 else (i - 2) * 512
                    dstw = w_out_sb if i < 2 else w_q_sb
                    P.dma("sp", "wd%d" % bi, (lambda bi, srcw, c0: lambda e: e.dma_start(out=wstgD_t[bi][:], in_=srcw[:, :, c0:c0 + 512]))(bi, srcw, c0), writes=["wstgD%d" % bi])
                    P.op(("dve", "pool")[bi], (lambda bi, dstw, c0: lambda e: e.tensor_copy(out=dstw[:, :, c0:c0 + 512], in_=wstgD_t[bi][:]))(bi, dstw, c0),
                         reads=["wstgD%d" % bi], writes=["wD"])
                P.dma("act", "wd2", lambda e: e.dma_start(out=skf[:], in_=subkeys.rearrange("n k d -> k n d")), writes=["skf"])
                P.dma("act", "wd2", lambda e: e.dma_start(out=fgB[:], in_=final_g.to_broadcast([128, D])), writes=["fgB"])
                P.op("dve", lambda e: e.tensor_copy(out=skb[:], in_=skf[:]), reads=["skf"], writes=["skb"])
                for n_ in range(2):
                    P.op("pe", (lambda n_: lambda e: e.transpose(ptk[:, n_, :], skb[:, n_, :], identb[:]))(n_), reads=["skb", "identb"], writes=["ptk"])
                P.op("dve", lambda e: e.tensor_copy(out=skT[:], in_=ptk[:]), reads=["ptk"], writes=["skT"])
            P.barrier()
            P.barrier()
            if stage == 3.5:
                return finish(nc, P, out)
            with phase() as st:
                NB = 2
                xtD_t = sbuf(st, "xtD", [128, D])
                x1 = [sbuf(st, "x1_%d" % i, [128, D]) for i in range(NB)]
                h2 = [sbuf(st, "h2_%d" % i, [128, D]) for i in range(NB)]
                eid = [sbuf(st, "eid%d" % i, [128, 128], I32) for i in range(NB)]
                gate = [sbuf(st, "gate%d" % i, [128, 128]) for i in range(NB)]
                tmpD = sbuf(st, "tmpD", [128, D]); tmpF = tmpD
                acc = None
                junkD = sbuf(st, "junkD", [128, D], BF16); NJ = int(os.environ.get("KJ", "2"))
                junkE3 = [sbuf(st, "junkE%d" % i, [128, D], BF16) for i in range(NJ)]; jer = Rot("junkE", NJ)
                mg_sb = sbuf(st, "mg_sb", [128, 8, 128], BF16); h2b2 = [sbuf(st, "h2b%d" % i, [128, D], BF16) for i in range(NB)]; h2T = sbuf(st, "h2T", [128, 8, 128], BF16)
                qT_sb2 = sbuf(st, "qT_sb2", [128, 16, 128], BF16); s_sb = sbuf(st, "s_sb", [128, 16, 128]); work = sbuf(st, "workD", [128, 16, 128])
                topv = sbuf(st, "topv", [128, 16, 16]); idxu = sbuf(st, "idxu", [128, 16, 16], U32); idxf = sbuf(st, "idxf", [128, 16, 16])
                cand = sbuf(st, "cand", [128, 8, 256]); cidx = s_sb[:].rearrange("p a b -> p (a b)").rearrange("p (h c) -> p h c", h=8)
                best = sbuf(st, "best", [128, 8, 16]); eg = sbuf(st, "eg", [128, 8, 16]); sm = sbuf(st, "smD", [128, 32]); sm2 = sbuf(st, "smE", [128, 8]); eidf = sbuf(st, "eidf", [128, 128])
                aD = sbuf(st, "aD", [128, 128]); gl_ = sbuf(st, "gl_", [128, 128]); wD = sbuf(st, "wDw", [128, 128])
                NG = int(os.environ.get("KNG", "12")); GJ = int(os.environ.get("KGJ", "4"))
                FY = int(os.environ.get("KFY", "1")); KAR = int(os.environ.get("KAR", "0")); KPEND = int(os.environ.get("KPEND", "1"))
                posI = sbuf(st, "posI", [128, 256], I32); mskI = sbuf(st, "mskI", [128, 1], I32)
                c4I = sbuf(st, "c4I", [128, 1], I32); c15I = sbuf(st, "c15I", [128, 1], I32); iota16 = sbuf(st, "iota16", [128, 16])
                pab = sbuf(st, "pab", [128, 2, 8, 16], I32); pabf = sbuf(st, "pabf", [128, 2, 8, 16]); e3b = sbuf(st, "e3b", [128, 8, 16])
                oh = cand[:].rearrange("p h (k a) -> p h k a", a=16)
                P.op("pool", lambda e: e.iota(c4I[:], pattern=[[0, 1]], base=4, channel_multiplier=0), writes=["posI"])
                P.op("pool", lambda e: e.iota(c15I[:], pattern=[[0, 1]], base=15, channel_multiplier=0), writes=["posI"])
                P.op("pool", lambda e: e.iota(iota16[:], pattern=[[1, 16]], base=0, channel_multiplier=0, allow_small_or_imprecise_dtypes=True), writes=["posI"])
                P.op("pool", lambda e: e.iota(posI[:], pattern=[[1, 256]], base=0, channel_multiplier=0), writes=["posI"])
                P.op("pool", lambda e: e.iota(mskI[:], pattern=[[0, 1]], base=-256, channel_multiplier=0), writes=["posI"])
                UVg = [sbuf(st, "UVg%d" % i, [128, 2, D], BF16) for i in range(NG)]
                dg = [sbuf(st, "dg%d" % i, [128, 128], BF16) for i in range(8)]; dgr = Rot("dg", 8)
                pmo = [psum(st, "pmo%d" % i, [128, 512]) for i in range(2)]
                pacc = [psum(st, "pacc%d" % i, [128, 512]) for i in range(2)]
                tpD = psum(st, "tpD", [128, 8, 128], BF16)
                pq = [psum(st, "pq%d" % i, [128, 4, 128]) for i in range(2)]; pqr = Rot("pq", 2)
                uv_i = [0]

                def rstd_chain(src_ap, srckey, smt, smk, jk, jkey):
                    P.op("act", lambda e: e.activation(out=jk[:], in_=src_ap, func=AF.Square, accum_out=smt[:, 0:1]), reads=[srckey], writes=[smk, jkey])
                    P.op("dve", lambda e: e.tensor_scalar(out=smt[:, 1:2], in0=smt[:, 0:1], scalar1=1.0 / D, scalar2=EPS, op0=ALU.mult, op1=ALU.add), reads=[smk], writes=[smk])
                    P.op("act", lambda e: e.sqrt(out=smt[:, 2:3], in_=smt[:, 1:2]), reads=[smk], writes=[smk])
                    P.op("dve", lambda e: e.reciprocal(out=smt[:, 3:4], in_=smt[:, 2:3]), reads=[smk], writes=[smk])

                def front(i):
                    b = i % NB; t0 = i * 128
                    xk, x1k, h2k, ek, gk = "xtD", "x1_%d" % b, "h2_%d" % b, "eid%d" % b, "gate%d" % b
                    P.dma("sp", xk, lambda e: e.dma_start(out=xtD_t[:], in_=x[t0:t0 + 128, :]), writes=[xk])
                    P.dma("sp", "mgl", lambda e: e.dma_start(out=mg_sb[:], in_=mgT_d.rearrange("(k p) n -> p k n", p=128)[:, :, t0:t0 + 128]), reads=["mgT_d"], writes=["mg_sb"])
                    for hf in range(2):
                        for k in range(8):
                            P.op("pe", (lambda hf, k: lambda e: e.matmul(pmo[hf][:], lhsT=mg_sb[:, k, :], rhs=w_out_sb[:, k, hf * 512:(hf + 1) * 512], start=(k == 0), stop=(k == 7)))(hf, k),
                                 reads=["mg_sb", "wD"], writes=["pmo%d" % hf])
                        yield
                        P.op("dve", (lambda hf: lambda e: e.tensor_tensor(out=tmpF[:, hf * 512:(hf + 1) * 512], in0=pmo[hf][:], in1=gate1B[:, hf * 512:(hf + 1) * 512], op=ALU.mult))(hf),
                             reads=["pmo%d" % hf, "modB"], writes=["tmpD"])
                        yield
                    P.op("dve", lambda e: e.tensor_tensor(out=x1[b][:], in0=tmpF[:], in1=xtD_t[:], op=ALU.add), reads=["tmpD", xk], writes=[x1k])
                    yield
                    if debug:
                        P.dma("sp", "dbgx1", lambda e: e.dma_start(out=x1_d[t0:t0 + 128, :], in_=x1[b][:]), reads=[x1k], writes=["x1_d"])
                    rstd_chain(x1[b][:], x1k, sm[:, 0:4], "smD", junkD, "junkD")
                    P.op("dve", lambda e: e.scalar_tensor_tensor(out=tmpF[:], in0=x1[b][:], scalar=sm[:, 3:4], in1=G2B, op0=ALU.mult, op1=ALU.mult), reads=[x1k, "smD", "modB"], writes=["tmpD"])
                    yield
                    P.op("dve", lambda e: e.tensor_tensor(out=h2[b][:], in0=tmpF[:], in1=S2B, op=ALU.add), reads=["tmpD", "modB"], writes=[h2k])
                    yield
                    P.op("act", lambda e: e.copy(out=h2b2[b][:], in_=h2[b][:]), reads=[h2k], writes=["h2b%d" % b])
                    for k in range(8):
                        P.op("pe", (lambda k: lambda e: e.transpose(tpD[:, k, :], h2b2[b][:, k * 128:(k + 1) * 128], identb[:]))(k), reads=["h2b%d" % b, "identb"], writes=["tpD"])
                    P.op("act", lambda e: e.copy(out=h2T[:], in_=tpD[:]), reads=["tpD"], writes=["h2T"])
                    yield
                    for g4 in range(4):
                        qi, qk = pqr.next()
                        for bl in range(4):
                            blk = g4 * 4 + bl
                            for k in range(8):
                                P.op("pe", (lambda qi, bl, blk, k: lambda e: e.matmul(pq[qi][:, bl, :], lhsT=w_q_sb[:, k, blk * 128:(blk + 1) * 128], rhs=h2T[:, k, :], start=(k == 0), stop=(k == 7)))(qi, bl, blk, k),
                                     reads=["wD", "h2T"], writes=[qk])
                        P.op("act", (lambda qi, g4: lambda e: e.copy(out=qT_sb2[:, g4 * 4:(g4 + 1) * 4, :], in_=pq[qi][:]))(qi, g4), reads=[qk], writes=["qT_sb2"])
                        yield
                    for g4 in range(4):
                        si, sk = pqr.next()
                        for bl in range(4):
                            blk = g4 * 4 + bl
                            P.op("pe", (lambda si, bl, blk: lambda e: e.matmul(pq[si][:, bl, :], lhsT=qT_sb2[:, blk, :], rhs=skT[:, blk % 2, :], start=True, stop=True))(si, bl, blk),
                                 reads=["qT_sb2", "skT"], writes=[sk])
                        P.op("act", (lambda si, g4: lambda e: e.copy(out=s_sb[:, g4 * 4:(g4 + 1) * 4, :], in_=pq[si][:]))(si, g4), reads=[sk], writes=["s_sb"])
                        yield
                    TK = ["s_sb", "topk"]
                    BK = ["tk%d" % q for q in range(16)]
                    for blk in range(16):
                        P.op("dve", (lambda blk: lambda e: e.max(out=topv[:, blk, 0:8], in_=s_sb[:, blk, :]))(blk), reads=TK, writes=[BK[blk]])
                    yield
                    for blk in range(16):
                        P.op("dve", (lambda blk: lambda e: e.max_index(out=idxu[:, blk, 0:8], in_max=topv[:, blk, 0:8], in_values=s_sb[:, blk, :]))(blk), reads=["s_sb", BK[blk]], writes=[BK[blk]])
                        if blk % 8 == 7:
                            yield
                    for blk in range(16):
                        P.op("dve", (lambda blk: lambda e: e.match_replace(out=work[:, blk, :], in_to_replace=topv[:, blk, 0:8], in_values=s_sb[:, blk, :], imm_value=-1e30))(blk), reads=["s_sb", BK[blk]], writes=[BK[blk]])
                        if blk % 8 == 7:
                            yield
                    for blk in range(16):
                        P.op("dve", (lambda blk: lambda e: e.max(out=topv[:, blk, 8:16], in_=work[:, blk, :]))(blk), reads=[BK[blk]], writes=[BK[blk]])
                    yield
                    for blk in range(16):
                        P.op("dve", (lambda blk: lambda e: e.max_index(out=idxu[:, blk, 8:16], in_max=topv[:, blk, 8:16], in_values=work[:, blk, :]))(blk), reads=[BK[blk]], writes=[BK[blk]])
                        if blk % 8 == 7:
                            yield
                    TK = TK + BK
                    P.op("dve", lambda e: e.tensor_copy(out=idxf[:], in_=idxu[:]), reads=TK, writes=["topk"])
                    tv4 = topv[:].rearrange("p (h n) a -> p h n a", n=2); ix4 = idxf[:].rearrange("p (h n) a -> p h n a", n=2)
                    c4 = cand[:].rearrange("p h (a b) -> p h a b", a=16); ci4 = cidx.rearrange("p h (a b) -> p h a b", a=16)
                    P.op("dve", lambda e: e.tensor_tensor(out=c4, in0=tv4[:, :, 0, :].unsqueeze(3).to_broadcast([128, 8, 16, 16]),
                                                          in1=tv4[:, :, 1, :].unsqueeze(2).to_broadcast([128, 8, 16, 16]), op=ALU.add), reads=TK, writes=["topk"])
                    yield
                    candI = cand[:].bitcast(I32)
                    P.op("dve", lambda e: e.tensor_scalar(out=candI, in0=candI, scalar1=mskI[:, 0:1], scalar2=None, op0=ALU.bitwise_and), reads=TK + ["posI"], writes=["topk"])
                    P.op("dve", lambda e: e.tensor_tensor(out=candI, in0=candI, in1=posI[:].unsqueeze(1).to_broadcast([128, 8, 256]), op=ALU.bitwise_or), reads=TK + ["posI"], writes=["topk"])
                    P.op("dve", lambda e: e.tensor_single_scalar(out=ix4[:, :, 0, :], in_=ix4[:, :, 0, :], scalar=128.0, op=ALU.mult), reads=TK, writes=["topk"])
                    yield
                    w2 = work[:].rearrange("p (h n) k -> p h (n k)", n=2)
                    HK = ["hk%d" % q for q in range(8)]
                    for h in range(8):
                        P.op("dve", (lambda h: lambda e: e.max(out=best[:, h, 0:8], in_=cand[:, h, :]))(h), reads=TK, writes=[HK[h]])
                    yield
                    for h in range(8):
                        P.op("dve", (lambda h: lambda e: e.match_replace(out=w2[:, h, :], in_to_replace=best[:, h, 0:8], in_values=cand[:, h, :], imm_value=-1e30))(h), reads=TK + [HK[h]], writes=[HK[h]])
                    yield
                    for h in range(8):
                        P.op("dve", (lambda h: lambda e: e.max(out=best[:, h, 8:16], in_=w2[:, h, :]))(h), reads=[HK[h]], writes=[HK[h]])
                    yield
                    TK = TK + HK
                    P.op("dve", lambda e: e.tensor_single_scalar(out=sm[:, 8:16], in_=best[:, :, 0], scalar=-1.0, op=ALU.mult), reads=TK + ["smD"], writes=["smD"])
                    for h in range(8):
                        P.op("act", (lambda h: lambda e: e.activation(out=eg[:, h, :], in_=best[:, h, :], func=AF.Exp, bias=sm[:, 8 + h:9 + h], accum_out=sm[:, 16 + h:17 + h]))(h),
                             reads=TK + ["smD"], writes=["eg", "smD"])
                    P.op("dve", lambda e: e.reciprocal(out=sm[:, 24:32], in_=sm[:, 16:24]), reads=["smD"], writes=["smD"])
                    P.op("dve", lambda e: e.tensor_tensor(out=gate[b][:].rearrange("p (h k) -> p h k", h=8), in0=eg[:], in1=sm[:, 24:32].unsqueeze(2).to_broadcast([128, 8, 16]), op=ALU.mult),
                         reads=["eg", "smD"], writes=[gk])
                    yield
                    bestI = best[:].bitcast(I32)
                    P.op("dve", lambda e: e.tensor_scalar(out=pab[:, 0], in0=bestI, scalar1=c4I[:, 0:1], scalar2=None, op0=ALU.arith_shift_right), reads=TK + ["posI"], writes=["topk"])
                    P.op("dve", lambda e: e.tensor_scalar(out=pab[:, 0], in0=pab[:, 0], scalar1=c15I[:, 0:1], scalar2=None, op0=ALU.bitwise_and), reads=TK + ["posI"], writes=["topk"])
                    P.op("dve", lambda e: e.tensor_scalar(out=pab[:, 1], in0=bestI, scalar1=c15I[:, 0:1], scalar2=None, op0=ALU.bitwise_and), reads=TK + ["posI"], writes=["topk"])
                    P.op("dve", lambda e: e.tensor_copy(out=pabf[:], in_=pab[:]), reads=TK, writes=["topk"])
                    yield
                    e3 = eidf[:].rearrange("p (h k) -> p h k", h=8)
                    for n_ in range(2):
                        P.op("dve", (lambda n_: lambda e: e.tensor_tensor(out=oh[:], in0=pabf[:, n_].unsqueeze(3).to_broadcast([128, 8, 16, 16]),
                                                                          in1=iota16[:].unsqueeze(1).unsqueeze(1).to_broadcast([128, 8, 16, 16]), op=ALU.is_equal))(n_), reads=TK + ["posI"], writes=["topk"])
                        P.op("dve", (lambda n_: lambda e: e.tensor_tensor(out=oh[:], in0=oh[:], in1=ix4[:, :, n_, :].unsqueeze(2).to_broadcast([128, 8, 16, 16]), op=ALU.mult))(n_), reads=TK, writes=["topk"])
                        P.op("dve", (lambda n_: lambda e: e.tensor_reduce(out=(e3 if n_ == 0 else e3b[:]), in_=oh[:], axis=AX.X, op=ALU.add))(n_), reads=TK, writes=["topk"])
                        yield
                    P.op("dve", lambda e: e.tensor_tensor(out=e3, in0=e3, in1=e3b[:], op=ALU.add), reads=TK, writes=["topk"])
                    P.op("dve", lambda e: e.tensor_scalar(out=eidf[:], in0=eidf[:], scalar1=0.0, scalar2=16383.0, op0=ALU.max, op1=ALU.min), reads=TK, writes=["topk"])
                    P.op("dve", lambda e: e.tensor_copy(out=eid[b][:], in_=eidf[:]), reads=TK, writes=[ek])
                    yield

                def consume(i, fgen):
                    b = i % NB; t0 = i * 128
                    x1k, h2k, ek, gk = "x1_%d" % b, "h2_%d" % b, "eid%d" % b, "gate%d" % b
                    ngrp = 128 // GJ
                    pend = None

                    def finish_group(g, bufs):
                        j0 = g * GJ
                        if "nofin" in KD:
                            return
                        P.op("act", lambda e: e.activation(out=gl_[:, j0:j0 + GJ], in_=aD[:, j0:j0 + GJ], func=AF.Gelu_apprx_tanh), reads=["aD%d_%d" % (g, q) for q in range(GJ)], writes=["gl_%d" % g])
                        for jj in range(GJ):
                            P.op("act", (lambda jj: lambda e: e.mul(out=wD[:, j0 + jj:j0 + jj + 1], in_=gl_[:, j0 + jj:j0 + jj + 1], mul=gate[b][:, j0 + jj:j0 + jj + 1]))(jj),
                                 reads=["gl_%d" % g, gk], writes=["wDw%d" % g])
                        for jj in range(GJ):
                            j = j0 + jj; gi, ugk = bufs[jj]
                            di, dk = dgr.next()
                            P.op("act", (lambda di, j: lambda e: e.activation(out=dg[di][:], in_=identb[:], func=AF.Copy, scale=wD[:, j:j + 1]))(di, j), reads=["wDw%d" % g, "identb"], writes=[dk])
                            for hf in range(2):
                                if "nov" in KD and j not in (0, 127):
                                    continue
                                P.op("pe", (lambda di, gi, hf, j: lambda e: e.matmul(pacc[hf][:], lhsT=dg[di][:], rhs=UVg[gi][:, 1, hf * 512:(hf + 1) * 512], start=(j == 0), stop=(j == 127)))(di, gi, hf, j),
                                     reads=[dk, ugk], writes=["pacc%d" % hf])

                    for g in range(ngrp):
                        bufs = []
                        for jj in range(GJ):
                            j = g * GJ + jj
                            gi = uv_i[0] % NG; uv_i[0] += 1; ugk = "UVg%d" % gi
                            bufs.append((gi, ugk))
                            if "nog" in KD:
                                P.op("pool", (lambda gi: lambda e: e.memset(UVg[gi][:], 1.0))(gi), reads=[ek], writes=[ugk])
                            else:
                                P.dma("pool", ugk, (lambda gi, j: lambda e: e.indirect_dma_start(out=UVg[gi][:].rearrange("p a d -> p (a d)"), out_offset=None, in_=uvb_d,
                                                                                               in_offset=bass.IndirectOffsetOnAxis(ap=eid[b][:, j:j + 1], axis=0)))(gi, j), reads=[ek], writes=[ugk])
                        for jj in range(GJ):
                            j = g * GJ + jj; gi, ugk = bufs[jj]
                            if "noa" in KD:
                                continue
                            ji, jkey_ = jer.next()
                            jE = junkE3[ji]
                            if jj < KAR:
                                P.op("dve", (lambda gi, jE: lambda e: e.tensor_tensor(out=jE[:], in0=UVg[gi][:, 0, :], in1=h2b2[b][:], op=ALU.mult))(gi, jE),
                                     reads=[ugk, "h2b%d" % b], writes=[jkey_])
                                P.op("act", (lambda j, jE: lambda e: e.activation(out=jE[:], in_=jE[:], func=AF.Copy, accum_out=aD[:, j:j + 1]))(j, jE),
                                     reads=[jkey_], writes=[jkey_, "aD%d_%d" % (g, jj)])
                            else:
                                P.op("dve", (lambda gi, j, jE: lambda e: e.scalar_tensor_tensor(out=jE[:], in0=UVg[gi][:, 0, :], scalar=1.0, in1=h2[b][:], op0=ALU.mult, op1=ALU.mult, accum_out=aD[:, j:j + 1]))(gi, j, jE),
                                     reads=[ugk, h2k], writes=["aD%d_%d" % (g, jj), jkey_])
                        if KPEND:
                            if pend is not None:
                                finish_group(*pend)
                            pend = (g, bufs)
                        else:
                            finish_group(g, bufs)
                        if fgen is not None:
                            for _ in range(FY):
                                next(fgen, None)
                    if KPEND and pend is not None:
                        finish_group(*pend)
                    if fgen is not None:
                        for _ in fgen:
                            pass
                    if "dump23" in KD and i == 23:
                        for nm, src_t in (("d_x1", x1[b]), ("d_wD", wD), ("d_aD", aD), ("d_gate", gate[b]), ("d_h2", h2[b])):
                            dd = nc.dram_tensor(nm, list(src_t[:].shape), F32, kind="ExternalOutput").ap()
                            P.dma("sp", "dump", (lambda dd, src_t: lambda e: e.dma_start(out=dd, in_=src_t[:]))(dd, src_t), reads=[x1k, gk, h2k] + ["wDw%d" % q for q in range(32)] + ["aD%d" % q for q in range(32)], writes=["dumpo"])
                        dd2 = nc.dram_tensor("d_eid", [128, 128], I32, kind="ExternalOutput").ap()
                        P.dma("sp", "dump", lambda e: e.dma_start(out=dd2, in_=eid[b][:]), reads=[ek], writes=["dumpo"])
                    if "notail" in KD:
                        return
                    for hf in range(2):
                        P.op("dve", (lambda hf: lambda e: e.tensor_tensor(out=tmpD[:, hf * 512:(hf + 1) * 512], in0=pacc[hf][:], in1=gate2B[:, hf * 512:(hf + 1) * 512], op=ALU.mult))(hf),
                             reads=["pacc%d" % hf, "modB"], writes=["tmpD"])
                    P.op("dve", lambda e: e.tensor_tensor(out=x1[b][:], in0=tmpD[:], in1=x1[b][:], op=ALU.add), reads=["tmpD", x1k], writes=[x1k])
                    rstd_chain(x1[b][:], x1k, sm2[:, 0:4], "smE", junkD, "junkD")
                    P.op("dve", lambda e: e.scalar_tensor_tensor(out=tmpD[:], in0=x1[b][:], scalar=sm2[:, 3:4], in1=fgB[:], op0=ALU.mult, op1=ALU.mult), reads=[x1k, "smE", "fgB"], writes=["tmpD"])
                    P.dma("sp", "outst", lambda e: e.dma_start(out=out[t0:t0 + 128, :], in_=tmpD[:]), reads=["tmpD"], writes=["out"])

                for _ in front(0):
                    pass
                for i in range(NT):
                    if "noc" in KD:
                        break
                    if "noil" in KD:
                        consume(i, None)
                        if i + 1 < NT:
                            for _ in front(i + 1):
                                pass
                        continue
                    consume(i, front(i + 1) if i + 1 < NT else None)
        return finish(nc, P, out)
    return nc


def finish(nc, P, out):
    P.barrier()
    P.close()
    return nc


def core_inputs(inp, b):
    f = lambda a: np.ascontiguousarray(a, dtype=np.float32)
    return {
        "x": f(inp["x"][b]), "c": f(inp["c"][b:b + 1]), "ctx": f(inp["ctx"][b]), "c_ctx": f(inp["c_ctx"].reshape(1, D)),
        "w_mod": f(inp["w_mod"][0]), "b_mod": f(inp["b_mod"][0].reshape(1, -1)),
        "norm1_g": f(inp["norm1_g"][0].reshape(1, D)), "norm2_g": f(inp["norm2_g"][0].reshape(1, D)),
        "final_g": f(inp["final_g"].reshape(1, D)), "w_in": f(inp["w_in"][0]),
        "ssm_a_re": f(inp["ssm_a_re"][0]), "ssm_a_im": f(inp["ssm_a_im"][0]), "ssm_log_dt": f(inp["ssm_log_dt"][0]),
        "ssm_b_re": f(inp["ssm_b_re"][0]), "ssm_b_im": f(inp["ssm_b_im"][0]),
        "ssm_c_re": f(inp["ssm_c_re"][0]), "ssm_c_im": f(inp["ssm_c_im"][0]),
        "ssm_d": f(inp["ssm_d"][0].reshape(512, 1)), "w_glu": f(inp["w_glu"][0]), "b_glu": f(inp["b_glu"][0].reshape(512, 1)),
        "w_branch_a": f(inp["w_branch_a"][0]), "w_branch_b": f(inp["w_branch_b"][0]), "na_rpb": f(inp["na_rpb"][0]),
        "w_out": f(inp["w_out"][0]), "peer_w_q": f(inp["peer_w_q"][0]), "peer_subkeys": f(inp["peer_subkeys"][0]),
        "peer_uv": f(np.concatenate([inp["peer_u"][0], inp["peer_v"][0]], axis=1)),
    }


def kernel(**inputs):
    nc = build()
    in_maps = [core_inputs(inputs, b) for b in range(8)]
    res = run_bass_kernel_spmd(nc, in_maps, core_ids=list(range(8)))
    return np.stack([np.asarray(r["out"], dtype=np.float32) for r in res.results], axis=0)
```

```python
import math
import numpy as np
import concourse.bass as bass
import concourse.mybir as mybir
from concourse.bass_utils import run_bass_kernel_spmd

F32 = mybir.dt.float32
BF16 = mybir.dt.bfloat16
I32 = mybir.dt.int32
U32 = mybir.dt.uint32
AF = mybir.ActivationFunctionType
ALU = mybir.AluOpType
AX = mybir.AxisListType


class _Op:
    __slots__ = ("eng", "fn", "deps", "seq", "is_dma", "semkey", "signal", "count", "waits")

    def __init__(self, eng, fn, seq, is_dma=False, semkey=None):
        self.eng = eng
        self.fn = fn
        self.deps = []
        self.seq = seq
        self.is_dma = is_dma
        self.semkey = semkey
        self.signal = is_dma
        self.count = 0
        self.waits = []


class Prog:
    ENGS = ("pe", "dve", "act", "pool", "sp")

    def __init__(self, nc):
        self.nc = nc
        self.ops = []
        self.writer = {}
        self.readers = {}
        import os
        self.same_sync = os.environ.get("KSAME", "1") == "1"

    def _add(self, op, reads, writes):
        deps = []
        for r in reads:
            w = self.writer.get(r)
            if w is not None:
                deps.append(w)
        for w_ in writes:
            w = self.writer.get(w_)
            if w is not None:
                deps.append(w)
            deps.extend(self.readers.get(w_, ()))
        op.deps = [d for d in set(deps) if d is not op]
        for r in reads:
            self.readers.setdefault(r, []).append(op)
        for w_ in writes:
            self.writer[w_] = op
            self.readers[w_] = []
        self.ops.append(op)
        return op

    def op(self, eng, fn, reads=(), writes=()):
        return self._add(_Op(eng, fn, len(self.ops)), reads, writes)

    def dma(self, eng, semkey, fn, reads=(), writes=()):
        return self._add(_Op(eng, fn, len(self.ops), True, semkey), reads, writes)

    def emit(self):
        import bisect
        from contextlib import ExitStack
        nc = self.nc
        if not hasattr(self, "_st"):
            self._st = ExitStack(); self._sem = {}; self._cnt = {}; self._hist = {}
            self._seen = {e: {} for e in self.ENGS}; self._done = 0
        ops = self.ops[self._done:]
        self._done = len(self.ops)
        if not ops:
            return

        def same(d, o):
            return d.eng == o.eng and not o.is_dma and (d.eng == "pe" or not self.same_sync)
        for o in ops:
            for d in o.deps:
                if d.is_dma or same(d, o):
                    continue
                assert d.count == 0 or d.signal, "dependency on an already-emitted non-signalling op"
                d.signal = True
        for o in ops:
            if not o.signal:
                continue
            k = ("dma", o.semkey) if o.is_dma else ("eng", o.eng)
            if k not in self._sem:
                self._sem[k] = self._st.enter_context(nc.semaphore("s%d_%s" % (len(self._sem), str(k[1]).replace(" ", ""))))
                self._cnt[k] = 0
            self._cnt[k] += 1
            o.count = self._cnt[k]
            if o.is_dma:
                self._hist.setdefault(o.semkey, []).append(o.seq)
        for o in ops:
            need = {}
            for d in o.deps:
                if d.is_dma:
                    k = ("dma", d.semkey)
                    v = 16 * bisect.bisect_left(self._hist[d.semkey], o.seq)
                else:
                    if same(d, o):
                        continue
                    k = ("eng", d.eng)
                    v = d.count
                if need.get(k, 0) < v:
                    need[k] = v
            sn = self._seen[o.eng]
            o.waits = []
            for k, v in need.items():
                if sn.get(k, 0) < v:
                    sn[k] = v
                    o.waits.append((k, v))
        self.n_sems = len(self._sem)
        sem = self._sem
        with nc.Block() as block:
            per = {e: [o for o in ops if o.eng == e] for e in self.ENGS}

            def run(engobj, lst):
                for o in lst:
                    for k, v in o.waits:
                        engobj.wait_ge(sem[k], v)
                    ins = o.fn(engobj)
                    if o.signal:
                        k = ("dma", o.semkey) if o.is_dma else ("eng", o.eng)
                        ins.then_inc(sem[k], 16 if o.is_dma else 1)
                    o.fn = None

            @block.tensor
            def _(e):
                run(e, per["pe"])

            @block.vector
            def _(e):
                run(e, per["dve"])

            @block.scalar
            def _(e):
                run(e, per["act"])

            @block.gpsimd
            def _(e):
                run(e, per["pool"])

            @block.sync
            def _(e):
                run(e, per["sp"])

    def close(self):
        self.emit()
        if hasattr(self, "_st"):
            self._st.close()

    def barrier(self, flush=True):
        start = getattr(self, "_done", 0)
        last = {}
        dmas = {}
        for o in self.ops[start:]:
            if o.is_dma:
                dmas[o.semkey] = o
            else:
                last[o.eng] = o
        for e, o in getattr(self, "_bar", {}).items():
            last.setdefault(e, o)
        deps = list(last.values()) + list(dmas.values())
        self._bar = {}
        for e in self.ENGS:
            o = _Op(e, lambda eng: eng.nop(), len(self.ops))
            o.deps = [d for d in deps]
            o.signal = True
            self.ops.append(o)
            self._bar[e] = o
        self.writer = {}
        self.readers = {}
        if flush:
            self.emit()


class Rot:
    def __init__(self, name, n):
        self.name, self.n, self.i = name, n, -1

    def next(self):
        self.i = (self.i + 1) % self.n
        return self.i, "%s%d" % (self.name, self.i)


D = 1024
SEQ = 4096
CTX = 256
NTOK = SEQ + CTX
EPS = 1e-6


def build(stage=99, debug=False):
    import os
    KD = os.environ.get("KDBG", "")
    from contextlib import ExitStack
    nc = bass.Bass("TRN2", target_bir_lowering=False)
    P = Prog(nc)

    def din(name, shape, dt=F32):
        return nc.dram_tensor(name, shape, dt, kind="ExternalInput").ap()

    def dscr(name, shape, dt):
        return nc.dram_tensor(name, shape, dt, kind=("ExternalOutput" if debug else "Internal")).ap()

    x = din("x", [SEQ, D]); c = din("c", [1, D]); ctx = din("ctx", [CTX, D]); c_ctx = din("c_ctx", [1, D])
    w_mod = din("w_mod", [D, 6 * D]); b_mod = din("b_mod", [1, 6 * D])
    norm1_g = din("norm1_g", [1, D]); norm2_g = din("norm2_g", [1, D]); final_g = din("final_g", [1, D])
    w_in = din("w_in", [D, 4096])
    a_re = din("ssm_a_re", [2, 32, 64]); a_im = din("ssm_a_im", [2, 32, 64]); log_dt = din("ssm_log_dt", [2, 32])
    b_re = din("ssm_b_re", [2, 32, 64, 16]); b_im = din("ssm_b_im", [2, 32, 64, 16])
    c_re = din("ssm_c_re", [2, 32, 16, 64]); c_im = din("ssm_c_im", [2, 32, 16, 64])
    ssm_d = din("ssm_d", [512, 1]); w_glu = din("w_glu", [512, 512]); b_glu = din("b_glu", [512, 1])
    w_ba = din("w_branch_a", [512, D]); w_bb = din("w_branch_b", [512, D]); rpb = din("na_rpb", [8, 15, 31])
    w_out = din("w_out", [D, D]); w_q = din("peer_w_q", [D, 2048]); subkeys = din("peer_subkeys", [2, 128, 128])
    peer_uv = din("peer_uv", [16384, 2 * D])
    out = nc.dram_tensor("out", [SEQ, D], F32, kind="ExternalOutput").ap()

    uT_d = dscr("uT_d", [512, NTOK], F32)
    kT_d = dscr("kT_d", [512, NTOK], BF16)
    qT_d = dscr("qT_d", [512, SEQ], BF16)
    v_d = dscr("v_d", [NTOK, 512], BF16)
    gT_d = dscr("gT_d", [2048, SEQ], BF16)
    baT_d = dscr("baT_d", [D, SEQ], BF16)
    mgT_d = dscr("mgT_d", [D, SEQ], BF16)
    uvb_d = nc.dram_tensor("uvb_d", [16384, 2 * D], BF16, kind=("ExternalOutput" if (debug and stage == 3.5) else "Internal")).ap()

    from contextlib import contextmanager

    @contextmanager
    def phase():
        stk = ExitStack()
        try:
            yield stk
            P.barrier()
        finally:
            stk.close()

    top = ExitStack()
    with top:
        def sbuf(st, n, s, d=F32):
            return st.enter_context(nc.sbuf_tensor(n, s, d))

        def psum(st, n, s, d=F32):
            return st.enter_context(nc.psum_tensor(n, s, d))

        identf = sbuf(top, "identf", [128, 128])
        identb = sbuf(top, "identb", [128, 128], BF16)
        modB = sbuf(top, "modB", [128, 6 * D])
        P.op("pool", lambda e: e.iota(identf[:], pattern=[[1, 128]], base=0, channel_multiplier=-1,
                                      allow_small_or_imprecise_dtypes=True), writes=["identf"])
        P.op("dve", lambda e: e.tensor_single_scalar(out=identf[:], in_=identf[:], scalar=0.0, op=ALU.is_equal),
             reads=["identf"], writes=["identf"])
        P.op("dve", lambda e: e.tensor_copy(out=identb[:], in_=identf[:]), reads=["identf"], writes=["identb"])

        with phase() as st:
            modcB = sbuf(st, "modcB", [128, 2 * D])
            wm = [sbuf(st, "wm%d" % i, [128, 8, 512]) for i in range(2)]
            w_in_sb = sbuf(st, "w_in_sb", [128, 8, 4096], BF16)
            st0 = ExitStack()
            cc = sbuf(st0, "cc", [128, 2, 8]); sc = sbuf(st0, "sc", [128, 2, 8]); scB = sbuf(st0, "scB", [128, 2, 8, 128])
            bmB = sbuf(st0, "bmB", [128, 6 * D]); gB = sbuf(st0, "gB", [128, 2, D])
            pmod = [psum(st0, "pmod%d" % i, [128, 512]) for i in range(2)]

            P.dma("sp", "c0", lambda e: e.dma_start(out=cc[:, 0, :], in_=c.rearrange("o (k p) -> p (o k)", p=128),
                                                    allow_slow_non_contiguous=True), writes=["cc"])
            P.dma("sp", "c0", lambda e: e.dma_start(out=cc[:, 1, :], in_=c_ctx.rearrange("o (k p) -> p (o k)", p=128),
                                                    allow_slow_non_contiguous=True), writes=["cc"])
            P.dma("act", "c1", lambda e: e.dma_start(out=bmB[:], in_=b_mod.to_broadcast([128, 6 * D])), writes=["bmB"])
            P.dma("act", "c1", lambda e: e.dma_start(out=gB[:, 0, :], in_=norm1_g.to_broadcast([128, D])), writes=["gB"])
            P.dma("act", "c1", lambda e: e.dma_start(out=gB[:, 1, :], in_=norm2_g.to_broadcast([128, D])), writes=["gB"])
            P.op("act", lambda e: e.activation(out=sc[:], in_=cc[:], func=AF.Silu), reads=["cc"], writes=["sc"])
            P.op("dve", lambda e: e.tensor_copy(out=scB[:], in_=sc[:].unsqueeze(3).to_broadcast([128, 2, 8, 128])),
                 reads=["sc"], writes=["scB"])
            w_mod_v = w_mod.rearrange("(k p) n -> p k n", p=128)
            for cch in range(12):
                bi = cch % 2
                P.dma("sp", "wm%d" % bi, (lambda bi, cch: lambda e: e.dma_start(out=wm[bi][:], in_=w_mod_v[:, :, cch * 512:(cch + 1) * 512]))(bi, cch),
                      writes=["wm%d" % bi])
                for which in range(2 if cch < 4 else 1):
                    for k in range(8):
                        P.op("pe", (lambda bi, which, k: lambda e: e.matmul(pmod[which][:], lhsT=scB[:, which, k, :], rhs=wm[bi][:, k, :],
                                                                            start=(k == 0), stop=(k == 7)))(bi, which, k),
                             reads=["scB", "wm%d" % bi], writes=["pmod%d" % which])
                    dst = modB if which == 0 else modcB
                    P.op("dve", (lambda dst, which, cch: lambda e: e.tensor_tensor(out=dst[:, cch * 512:(cch + 1) * 512], in0=pmod[which][:],
                                                                                   in1=bmB[:, cch * 512:(cch + 1) * 512], op=ALU.add))(dst, which, cch),
                         reads=["pmod%d" % which, "bmB"], writes=["modB" if which == 0 else "modcB"])
            for dst, key, off, gi in ((modB, "modB", D, 0), (modcB, "modcB", D, 0), (modB, "modB", 4 * D, 1)):
                P.op("dve", (lambda dst, off, gi: lambda e: e.scalar_tensor_tensor(out=dst[:, off:off + D], in0=dst[:, off:off + D], scalar=1.0,
                                                                                  in1=gB[:, gi, :], op0=ALU.add, op1=ALU.mult))(dst, off, gi),
                     reads=[key, "gB"], writes=[key])

            P.barrier()
            st0.close()
            w_in_v = w_in.rearrange("(k p) n -> p k n", p=128)
            for cch in range(8):
                bi = cch % 2
                P.dma("sp", "wm%d" % bi, (lambda bi, cch: lambda e: e.dma_start(out=wm[bi][:], in_=w_in_v[:, :, cch * 512:(cch + 1) * 512]))(bi, cch),
                      reads=[], writes=["wm%d" % bi])
                eng = ("pool", "dve")[cch % 2]
                P.op(eng, (lambda bi, cch: lambda e: e.tensor_copy(out=w_in_sb[:, :, cch * 512:(cch + 1) * 512], in_=wm[bi][:]))(bi, cch),
                     reads=["wm%d" % bi], writes=["w_in_sb"])

            xt = [sbuf(st, "xt%d" % i, [128, D]) for i in range(3)]; xr = Rot("xt", 3)
            junk = sbuf(st, "junkA", [128, D]); tmpA = sbuf(st, "tmpA", [128, D])
            ss = [sbuf(st, "ss%d" % i, [128, 4]) for i in range(2)]; ssr = Rot("ss", 2)
            hxb = [sbuf(st, "hxb%d" % i, [128, D], BF16) for i in range(2)]; hr = Rot("hxb", 2)
            hxT = [sbuf(st, "hxT%d" % i, [128, 8, 512], BF16) for i in range(2)]; hTr = Rot("hxT", 2)
            st_u = sbuf(st, "st_u", [128, 4, 512]); st_k = sbuf(st, "st_k", [128, 4, 512], BF16)
            st_q = sbuf(st, "st_q", [128, 4, 512], BF16); st_g = sbuf(st, "st_g", [128, 16, 512], BF16)
            st_v = sbuf(st, "st_v", [128, 4, 512], BF16)
            tp = [psum(st, "tpA%d" % i, [128, 8, 128], BF16) for i in range(2)]; tpr = Rot("tpA", 2)
            pj = [psum(st, "pj%d" % i, [128, 512]) for i in range(4)]; pjr = Rot("pj", 4)
            evac_i = [0]

            def evac(dst_ap, src_ap, reads, writes, func=None):
                if func is not None:
                    P.op("act", lambda e: e.activation(out=dst_ap, in_=src_ap, func=func), reads, writes)
                    return
                evac_i[0] += 1
                if evac_i[0] % 2:
                    P.op("act", lambda e: e.copy(out=dst_ap, in_=src_ap), reads, writes)
                else:
                    P.op("dve", lambda e: e.tensor_copy(out=dst_ap, in_=src_ap), reads, writes)

            for i_ in range(2):
                P.op("pool", (lambda i_: lambda e: e.memset(hxT[i_][:], 0.0))(i_), writes=["hxT%d" % i_])
            chunks = [("ctx", 0, 256)] + [("lat", i * 512, 512) for i in range(8)]
            for kind, t0, n in chunks:
                src = ctx if kind == "ctx" else x
                mB, mkey = (modcB, "modcB") if kind == "ctx" else (modB, "modB")
                col0 = t0 if kind == "ctx" else CTX + t0
                hi, hkey = hTr.next()
                for t in range(n // 128):
                    xi, xkey = xr.next()
                    si, skey = ssr.next()
                    bi, bkey = hr.next()
                    pi, pkey = tpr.next()
                    r0 = t0 + t * 128
                    P.dma("sp", xkey, (lambda xi, r0, src: lambda e: e.dma_start(out=xt[xi][:], in_=src[r0:r0 + 128, :]))(xi, r0, src), writes=[xkey])
                    P.op("act", (lambda xi, si: lambda e: e.activation(out=junk[:], in_=xt[xi][:], func=AF.Square, accum_out=ss[si][:, 0:1]))(xi, si),
                         reads=[xkey], writes=["junkA", skey])
                    P.op("dve", (lambda si: lambda e: e.tensor_scalar(out=ss[si][:, 1:2], in0=ss[si][:, 0:1], scalar1=1.0 / D, scalar2=EPS,
                                                                      op0=ALU.mult, op1=ALU.add))(si), reads=[skey], writes=[skey])
                    P.op("act", (lambda si: lambda e: e.sqrt(out=ss[si][:, 2:3], in_=ss[si][:, 1:2]))(si), reads=[skey], writes=[skey])
                    P.op("dve", (lambda si: lambda e: e.reciprocal(out=ss[si][:, 3:4], in_=ss[si][:, 2:3]))(si), reads=[skey], writes=[skey])
                    P.op("dve", (lambda xi, si, mB: lambda e: e.scalar_tensor_tensor(out=tmpA[:], in0=xt[xi][:], scalar=ss[si][:, 3:4], in1=mB[:, D:2 * D],
                                                                                     op0=ALU.mult, op1=ALU.mult))(xi, si, mB),
                         reads=[xkey, skey, mkey], writes=["tmpA"])
                    P.op("dve", (lambda bi, mB: lambda e: e.tensor_tensor(out=hxb[bi][:], in0=tmpA[:], in1=mB[:, 0:D], op=ALU.add))(bi, mB),
                         reads=["tmpA", mkey], writes=[bkey])
                    for k in range(8):
                        P.op("pe", (lambda pi, bi, k: lambda e: e.transpose(tp[pi][:, k, :], hxb[bi][:, k * 128:(k + 1) * 128], identb[:]))(pi, bi, k),
                             reads=[bkey, "identb"], writes=[pkey])
                    P.op("act", (lambda hi, pi, t: lambda e: e.copy(out=hxT[hi][:, :, t * 128:(t + 1) * 128], in_=tp[pi][:]))(hi, pi, t),
                         reads=[pkey], writes=[hkey])
                cts = list(range(0, 8)) + (list(range(12, 32)) if kind == "lat" else [])
                for ct in cts:
                    qi, qkey = pjr.next()
                    for k in range(8):
                        P.op("pe", (lambda qi, hi, k, ct: lambda e: e.matmul(pj[qi][:, 0:n], lhsT=w_in_sb[:, k, ct * 128:(ct + 1) * 128], rhs=hxT[hi][:, k, 0:n],
                                                                             start=(k == 0), stop=(k == 7)))(qi, hi, k, ct),
                             reads=["w_in_sb", hkey], writes=[qkey])
                    if ct < 4:
                        evac(st_u[:, ct, 0:n], pj[qi][:, 0:n], [qkey], ["st_u"])
                    elif ct < 8:
                        evac(st_k[:, ct - 4, 0:n], pj[qi][:, 0:n], [qkey], ["st_k"])
                    elif ct < 16:
                        evac(st_q[:, ct - 12, 0:n], pj[qi][:, 0:n], [qkey], ["st_q"])
                    else:
                        evac(st_g[:, ct - 16, 0:n], pj[qi][:, 0:n], [qkey], ["st_g"], func=AF.Sigmoid)
                P.dma("pool", "stu", (lambda col0, n: lambda e: e.dma_start(out=uT_d.rearrange("(t p) n -> p t n", p=128)[:, :, col0:col0 + n], in_=st_u[:, :, 0:n]))(col0, n),
                      reads=["st_u"], writes=["uT_d"])
                P.dma("pool", "stk", (lambda col0, n: lambda e: e.dma_start(out=kT_d.rearrange("(t p) n -> p t n", p=128)[:, :, col0:col0 + n], in_=st_k[:, :, 0:n]))(col0, n),
                      reads=["st_k"], writes=["kT_d"])
                if kind == "lat":
                    P.dma("pool", "stq", (lambda t0: lambda e: e.dma_start(out=qT_d.rearrange("(t p) n -> p t n", p=128)[:, :, t0:t0 + 512], in_=st_q[:]))(t0),
                          reads=["st_q"], writes=["qT_d"])
                    P.dma("pool", "stg", (lambda t0: lambda e: e.dma_start(out=gT_d.rearrange("(t p) n -> p t n", p=128)[:, :, t0:t0 + 512], in_=st_g[:]))(t0),
                          reads=["st_g"], writes=["gT_d"])
                for t in range(n // 128):
                    qi, qkey = pjr.next()
                    for k in range(8):
                        P.op("pe", (lambda qi, hi, k, t: lambda e: e.matmul(pj[qi][:], lhsT=hxT[hi][:, k, t * 128:(t + 1) * 128], rhs=w_in_sb[:, k, 1024:1536],
                                                                            start=(k == 0), stop=(k == 7)))(qi, hi, k, t),
                             reads=["w_in_sb", hkey], writes=[qkey])
                    evac(st_v[:, t, :], pj[qi][:], [qkey], ["st_v"])
                nt = n // 128
                P.dma("pool", "stv", (lambda col0, nt: lambda e: e.dma_start(out=v_d[col0:col0 + nt * 128, :].rearrange("(t p) n -> p t n", p=128), in_=st_v[:, 0:nt, :]))(col0, nt),
                      reads=["st_v"], writes=["v_d"])
        P.barrier()
        if stage <= 1:
            return finish(nc, P, out)

        yT_d = dscr("yT_d", [512, SEQ], F32) if debug else None
        TWO_PI = 2.0 * math.pi
        with ExitStack() as stB:
            zT = sbuf(stB, "zT", [128, 4, SEQ], BF16)
            with phase() as st:
                def t32(n):
                    return sbuf(st, n, [128, 32])
                are, aim, ldt = t32("are"), t32("aim"), t32("ldt")
                Bn = [sbuf(st, "Bn%d" % i, [128, 32, 16]) for i in range(2)]
                bb = [sbuf(st, "bb%d" % i, [128, 32, 16]) for i in range(2)]
                tmpb = sbuf(st, "tmpb", [128, 32, 16])
                Cn2 = [sbuf(st, "Cn2%d" % i, [128, 8, 2, 64]) for i in range(2)]
                dsk = sbuf(st, "dsk", [128, 4])
                maskf = sbuf(st, "maskf", [128, 4, 2]); mask2 = sbuf(st, "mask2", [128, 4, 2])
                pwr = sbuf(st, "pwr", [128, 13, 32]); pwi = sbuf(st, "pwi", [128, 13, 32]); npwi = sbuf(st, "npwi", [128, 13, 32])
                kint = sbuf(st, "kint", [128, 32], I32)
                names = ["dt", "er", "th", "mag", "kf", "rr", "half", "sn", "ah", "cq", "sinr", "cosr", "nre", "den", "rden",
                         "fre", "fim", "t1", "t2"]
                T = {n: t32("p_" + n) for n in names}
                uT_sb = [sbuf(st, "uT_sb%d" % i, [128, NTOK]) for i in range(1)]
                PL = [sbuf(st, "PL%d" % i, [128, 2, NTOK]) for i in range(2)]
                yT = sbuf(st, "yT", [128, SEQ])
                Z = [sbuf(st, "Z%d" % i, [128, 2, 128]) for i in range(2)]
                Zc = [sbuf(st, "Zc%d" % i, [128, 2, 128]) for i in range(2)]
                LB = [sbuf(st, "LB%d" % i, [128, 2, 128]) for i in range(2)]
                LC = [sbuf(st, "LC%d" % i, [128, 2, 128]) for i in range(2)]
                pz = [psum(st, "pz%d" % i, [128, 2, 128]) for i in range(2)]; pzr = Rot("pz", 2)
                pb = [psum(st, "pb%d" % i, [128, 512]) for i in range(4)]; pbr = Rot("pb", 4)
                py = [psum(st, "py%d" % i, [128, 512]) for i in range(2)]; pyr = Rot("py", 2)

                for gl in range(2):
                    sl = slice(gl * 64, (gl + 1) * 64)
                    for dst, srcp, key in ((are, a_re, "are"), (aim, a_im, "aim")):
                        P.dma("act", "pb0", (lambda dst, srcp, sl, gl: lambda e: e.dma_start(
                            out=dst[sl, :].rearrange("p (d g) -> p d g", d=2),
                            in_=srcp.rearrange("d (gp gl) p -> gl p d gp", gl=2)[gl], allow_slow_non_contiguous=True))(dst, srcp, sl, gl), writes=[key])
                    P.dma("act", "pb0", (lambda sl, gl: lambda e: e.dma_start(
                        out=ldt[sl, :].rearrange("p (d g) -> p d g", d=2),
                        in_=log_dt.rearrange("d (gp gl) -> gl d gp", gl=2)[gl:gl + 1].to_broadcast([64, 2, 16]), allow_slow_non_contiguous=True))(sl, gl), writes=["ldt"])
                    for i, srcp in enumerate((b_re, b_im)):
                        P.dma("act", "pb0", (lambda i, srcp, sl, gl: lambda e: e.dma_start(
                            out=Bn[i][sl].rearrange("p (d g) h -> p d g h", d=2),
                            in_=srcp.rearrange("d (gp gl) p h -> gl p d gp h", gl=2)[gl]))(i, srcp, sl, gl), writes=["Bn%d" % i])
                for i, srcp in enumerate((c_re, c_im)):
                    for j in range(2):
                        P.dma("act", "pb0", (lambda i, srcp, j: lambda e: e.dma_start(
                            out=Cn2[i][:, :, j, :].rearrange("p (d u) q -> p d u q", d=2),
                            in_=srcp.rearrange("d (ut g8) h p -> (g8 h) d ut p", g8=8)))(i, srcp, j), writes=["Cn2%d" % i])
                P.dma("act", "pb0", lambda e: e.dma_start(out=dsk[:], in_=ssm_d.rearrange("(ut p) o -> p (ut o)", p=128), allow_slow_non_contiguous=True), writes=["dsk"])
                P.op("pool", lambda e: e.iota(maskf[:], pattern=[[-32, 4], [-16, 2]], base=0, channel_multiplier=1, allow_small_or_imprecise_dtypes=True), writes=["maskf"])
                P.op("dve", lambda e: e.tensor_single_scalar(out=mask2[:], in_=maskf[:], scalar=0.0, op=ALU.is_ge), reads=["maskf"], writes=["mask2"])
                P.op("dve", lambda e: e.tensor_single_scalar(out=maskf[:], in_=maskf[:], scalar=16.0, op=ALU.is_lt), reads=["maskf", "mask2"], writes=["maskf"])
                P.op("dve", lambda e: e.tensor_tensor(out=maskf[:], in0=maskf[:], in1=mask2[:], op=ALU.mult), reads=["maskf", "mask2"], writes=["maskf"])

                PK = ["are", "aim", "ldt", "Bn0", "Bn1", "prm"]

                def dve(fn):
                    P.op("dve", fn, reads=PK, writes=["prm"])

                def act(fn):
                    P.op("act", fn, reads=PK, writes=["prm"])
                act(lambda e: e.activation(out=T["dt"][:], in_=ldt[:], func=AF.Exp))
                dve(lambda e: e.tensor_tensor(out=T["er"][:], in0=are[:], in1=T["dt"][:], op=ALU.mult))
                dve(lambda e: e.tensor_tensor(out=T["th"][:], in0=aim[:], in1=T["dt"][:], op=ALU.mult))
                act(lambda e: e.activation(out=T["mag"][:], in_=T["er"][:], func=AF.Exp))
                dve(lambda e: e.tensor_single_scalar(out=T["kf"][:], in_=T["th"][:], scalar=1.0 / TWO_PI, op=ALU.mult))
                dve(lambda e: e.tensor_copy(out=kint[:], in_=T["kf"][:]))
                dve(lambda e: e.tensor_copy(out=T["kf"][:], in_=kint[:]))
                dve(lambda e: e.scalar_tensor_tensor(out=T["rr"][:], in0=T["kf"][:], scalar=-TWO_PI, in1=T["th"][:], op0=ALU.mult, op1=ALU.add))
                dve(lambda e: e.tensor_single_scalar(out=T["half"][:], in_=T["rr"][:], scalar=0.5, op=ALU.mult))
                act(lambda e: e.activation(out=T["ah"][:], in_=T["half"][:], func=AF.Abs))
                dve(lambda e: e.tensor_scalar(out=T["t1"][:], in0=T["ah"][:], scalar1=-1.0, scalar2=math.pi / 2, op0=ALU.mult, op1=ALU.add))
                act(lambda e: e.activation(out=T["sn"][:], in_=T["half"][:], func=AF.Sin))
                act(lambda e: e.activation(out=T["cq"][:], in_=T["t1"][:], func=AF.Sin))
                dve(lambda e: e.scalar_tensor_tensor(out=T["sinr"][:], in0=T["sn"][:], scalar=2.0, in1=T["cq"][:], op0=ALU.mult, op1=ALU.mult))
                dve(lambda e: e.scalar_tensor_tensor(out=T["t2"][:], in0=T["sn"][:], scalar=-2.0, in1=T["sn"][:], op0=ALU.mult, op1=ALU.mult))
                dve(lambda e: e.tensor_single_scalar(out=T["cosr"][:], in_=T["t2"][:], scalar=1.0, op=ALU.add))
                dve(lambda e: e.tensor_tensor(out=pwr[:, 0, :], in0=T["mag"][:], in1=T["cosr"][:], op=ALU.mult))
                dve(lambda e: e.tensor_tensor(out=pwi[:, 0, :], in0=T["mag"][:], in1=T["sinr"][:], op=ALU.mult))
                dve(lambda e: e.tensor_single_scalar(out=T["nre"][:], in_=pwr[:, 0, :], scalar=-1.0, op=ALU.add))
                dve(lambda e: e.tensor_tensor(out=T["den"][:], in0=are[:], in1=are[:], op=ALU.mult))
                dve(lambda e: e.tensor_tensor(out=T["t1"][:], in0=aim[:], in1=aim[:], op=ALU.mult))
                dve(lambda e: e.tensor_tensor(out=T["den"][:], in0=T["den"][:], in1=T["t1"][:], op=ALU.add))
                dve(lambda e: e.reciprocal(out=T["rden"][:], in_=T["den"][:]))
                dve(lambda e: e.tensor_tensor(out=T["t1"][:], in0=T["nre"][:], in1=are[:], op=ALU.mult))
                dve(lambda e: e.tensor_tensor(out=T["t2"][:], in0=pwi[:, 0, :], in1=aim[:], op=ALU.mult))
                dve(lambda e: e.tensor_tensor(out=T["t1"][:], in0=T["t1"][:], in1=T["t2"][:], op=ALU.add))
                dve(lambda e: e.tensor_tensor(out=T["fre"][:], in0=T["t1"][:], in1=T["rden"][:], op=ALU.mult))
                dve(lambda e: e.tensor_tensor(out=T["t1"][:], in0=pwi[:, 0, :], in1=are[:], op=ALU.mult))
                dve(lambda e: e.tensor_tensor(out=T["t2"][:], in0=T["nre"][:], in1=aim[:], op=ALU.mult))
                dve(lambda e: e.tensor_tensor(out=T["t1"][:], in0=T["t1"][:], in1=T["t2"][:], op=ALU.subtract))
                dve(lambda e: e.tensor_tensor(out=T["fim"][:], in0=T["t1"][:], in1=T["rden"][:], op=ALU.mult))
                fr = T["fre"][:].unsqueeze(2).to_broadcast([128, 32, 16]); fi = T["fim"][:].unsqueeze(2).to_broadcast([128, 32, 16])
                dve(lambda e: e.tensor_tensor(out=bb[0][:], in0=Bn[0][:], in1=fr, op=ALU.mult))
                dve(lambda e: e.tensor_tensor(out=tmpb[:], in0=Bn[1][:], in1=fi, op=ALU.mult))
                dve(lambda e: e.tensor_tensor(out=bb[0][:], in0=bb[0][:], in1=tmpb[:], op=ALU.subtract))
                dve(lambda e: e.tensor_tensor(out=bb[1][:], in0=Bn[1][:], in1=fr, op=ALU.mult))
                dve(lambda e: e.tensor_tensor(out=tmpb[:], in0=Bn[0][:], in1=fi, op=ALU.mult))
                dve(lambda e: e.tensor_tensor(out=bb[1][:], in0=bb[1][:], in1=tmpb[:], op=ALU.add))
                for k in range(12):
                    dve((lambda k: lambda e: e.tensor_tensor(out=T["t1"][:], in0=pwr[:, k, :], in1=pwr[:, k, :], op=ALU.mult))(k))
                    dve((lambda k: lambda e: e.tensor_tensor(out=T["t2"][:], in0=pwi[:, k, :], in1=pwi[:, k, :], op=ALU.mult))(k))
                    dve((lambda k: lambda e: e.tensor_tensor(out=pwr[:, k + 1, :], in0=T["t1"][:], in1=T["t2"][:], op=ALU.subtract))(k))
                    dve((lambda k: lambda e: e.scalar_tensor_tensor(out=pwi[:, k + 1, :], in0=pwr[:, k, :], scalar=2.0, in1=pwi[:, k, :], op0=ALU.mult, op1=ALU.mult))(k))
                dve(lambda e: e.tensor_single_scalar(out=npwi[:], in_=pwi[:], scalar=-1.0, op=ALU.mult))

                chain = {}

                def cmul_acc(hi_re, hi_im, lo_re, lo_im, k, u, key):
                    sr = pwr[:, k, u:u + 1]; si = pwi[:, k, u:u + 1]; nsi = npwi[:, k, u:u + 1]
                    prev = chain.get(key)
                    if prev is None:
                        prev = [w for w in (P.writer.get(key), P.writer.get("prm")) if w is not None]
                    ops_ = []
                    for n_, (o_, a_, s_) in enumerate(((hi_re, lo_re, sr), (hi_im, lo_re, si), (hi_re, lo_im, nsi), (hi_im, lo_im, sr))):
                        op = _Op("dve", (lambda o_, a_, s_: lambda e: e.scalar_tensor_tensor(out=o_, in0=a_, scalar=s_, in1=o_, op0=ALU.mult, op1=ALU.add))(o_, a_, s_), len(P.ops))
                        op.deps = list(prev) if n_ < 2 else [ops_[n_ - 2]]
                        P.ops.append(op)
                        ops_.append(op)
                    chain[key] = [ops_[3]]

                def scan_done(key):
                    P.writer[key] = chain.pop(key)[0]
                    P.readers[key] = []

                def bk_scan(pl, c0, n, rev, u, key, up_only=False):
                    L = n.bit_length() - 1
                    re = pl[:, 0, c0:c0 + n]; im = pl[:, 1, c0:c0 + n]
                    for k in range(L):
                        s_ = 2 << k; h_ = 1 << k
                        vr = re.rearrange("p (m s) -> p m s", s=s_); vi = im.rearrange("p (m s) -> p m s", s=s_)
                        if not rev:
                            cmul_acc(vr[:, :, s_ - 1], vi[:, :, s_ - 1], vr[:, :, h_ - 1], vi[:, :, h_ - 1], k, u, key)
                        else:
                            cmul_acc(vr[:, :, 0], vi[:, :, 0], vr[:, :, h_], vi[:, :, h_], k, u, key)
                    for k in (range(L - 2, -1, -1) if not up_only else ()):
                        s_ = 2 << k; h_ = 1 << k
                        vr = re.rearrange("p (m s) -> p m s", s=s_); vi = im.rearrange("p (m s) -> p m s", s=s_)
                        if not rev:
                            cmul_acc(vr[:, 1:, h_ - 1], vi[:, 1:, h_ - 1], vr[:, :-1, s_ - 1], vi[:, :-1, s_ - 1], k, u, key)
                        else:
                            cmul_acc(vr[:, :-1, h_], vi[:, :-1, h_], vr[:, 1:, 0], vi[:, 1:, 0], k, u, key)

                segs = [(0, 256)] + [(CTX + i * 512, 512) for i in range(8)]
                units = [(ut, d_, gpl) for ut in range(4) for d_ in range(2) for gpl in range(4)]

                def stA(ix):
                    ut, d_, gpl = units[ix]
                    u = d_ * 16 + ut * 4 + gpl
                    bi = ix % 2; ub = 0; ukey = "uT_sb0"
                    zk, zck, lbk, lck, plk = "Z%d" % bi, "Zc%d" % bi, "LB%d" % bi, "LC%d" % bi, "PL%d" % bi
                    if ix % 8 == 0:
                        P.dma("sp", ukey, lambda e: e.dma_start(out=uT_sb[ub][:], in_=uT_d[ut * 128:(ut + 1) * 128, :]), reads=["uT_d"], writes=[ukey])
                    P.op("pool", lambda e: e.memset(Z[bi][:], 0.0), writes=[zk])
                    for j in range(2):
                        for gl in range(2):
                            cs = (2 * gpl + gl) * 16
                            P.op("pool", (lambda j, gl, cs: lambda e: e.tensor_copy(out=Z[bi][gl * 64:(gl + 1) * 64, j, cs:cs + 16], in_=bb[j][gl * 64:(gl + 1) * 64, u, :]))(j, gl, cs),
                                 reads=["prm"], writes=[zk])
                    zi, zkey = pzr.next()
                    for j in range(2):
                        P.op("pe", (lambda zi, j: lambda e: e.matmul(pz[zi][:, j, :], lhsT=Z[bi][:, j, :], rhs=identf[:], start=True, stop=True))(zi, j), reads=[zk, "identf"], writes=[zkey])
                    P.op("act", (lambda zi: lambda e: e.copy(out=LB[bi][:], in_=pz[zi][:]))(zi), reads=[zkey], writes=[lbk])
                    for j in range(2):
                        P.op("pool", (lambda j: lambda e: e.tensor_tensor(out=Zc[bi][:, j, :].rearrange("p (g q) -> p g q", g=2), in0=Cn2[j][:, d_ * 4 + ut, :, :],
                                                                         in1=maskf[:, gpl, :].unsqueeze(2).to_broadcast([128, 2, 64]), op=ALU.mult))(j),
                             reads=["Cn2%d" % j, "maskf"], writes=[zck])
                    zi2, zkey2 = pzr.next()
                    for j in range(2):
                        P.op("pe", (lambda zi2, j: lambda e: e.matmul(pz[zi2][:, j, :], lhsT=Zc[bi][:, j, :], rhs=identf[:], start=True, stop=True))(zi2, j), reads=[zck, "identf"], writes=[zkey2])
                    P.op("act", lambda e: e.copy(out=LC[bi][:, 0, :], in_=pz[zi2][:, 0, :]), reads=[zkey2], writes=[lck])
                    P.op("act", lambda e: e.mul(out=LC[bi][:, 1, :], in_=pz[zi2][:, 1, :], mul=-1.0), reads=[zkey2], writes=[lck])
                    for (c0, n) in segs:
                        for j in range(2):
                            qi, qkey = pbr.next()
                            P.op("pe", (lambda qi, j, c0, n: lambda e: e.matmul(pb[qi][:, 0:n], lhsT=LB[bi][:, j, :], rhs=uT_sb[ub][:, c0:c0 + n], start=True, stop=True))(qi, j, c0, n),
                                 reads=[lbk, ukey], writes=[qkey])
                            P.op("act", (lambda qi, j, c0, n: lambda e: e.copy(out=PL[bi][:, j, c0:c0 + n], in_=pb[qi][:, 0:n]))(qi, j, c0, n), reads=[qkey], writes=[plk])

                def stB(ix):
                    ut, d_, gpl = units[ix]
                    u = d_ * 16 + ut * 4 + gpl
                    bi = ix % 2; plk = "PL%d" % bi
                    rev = (d_ == 1)
                    bk_scan(PL[bi], 0, CTX, rev, u, plk, up_only=True)
                    if not rev:
                        cmul_acc(PL[bi][:, 0, CTX:CTX + 1], PL[bi][:, 1, CTX:CTX + 1], PL[bi][:, 0, CTX - 1:CTX], PL[bi][:, 1, CTX - 1:CTX], 0, u, plk)
                    else:
                        cmul_acc(PL[bi][:, 0, NTOK - 1:NTOK], PL[bi][:, 1, NTOK - 1:NTOK], PL[bi][:, 0, 0:1], PL[bi][:, 1, 0:1], 0, u, plk)
                    bk_scan(PL[bi], CTX, SEQ, rev, u, plk)
                    scan_done(plk)

                def stC(ix):
                    ut, d_, gpl = units[ix]
                    bi = ix % 2; ub = 0; ukey = "uT_sb0"; lck, plk = "LC%d" % bi, "PL%d" % bi
                    first = (ix % 8 == 0)
                    for sgi in range(8):
                        c0 = CTX + sgi * 512
                        yi, ykey = pyr.next()
                        for j in range(2):
                            P.op("pe", (lambda yi, j, c0: lambda e: e.matmul(py[yi][:], lhsT=LC[bi][:, j, :], rhs=PL[bi][:, j, c0:c0 + 512], start=(j == 0), stop=(j == 1)))(yi, j, c0),
                                 reads=[lck, plk], writes=[ykey])
                        ysl = slice(sgi * 512, (sgi + 1) * 512)
                        if first:
                            P.op("dve", (lambda yi, c0, ysl: lambda e: e.scalar_tensor_tensor(out=yT[:, ysl], in0=uT_sb[ub][:, c0:c0 + 512], scalar=dsk[:, ut:ut + 1],
                                                                                              in1=py[yi][:], op0=ALU.mult, op1=ALU.add))(yi, c0, ysl),
                                 reads=[ykey, ukey, "dsk"], writes=["yT"])
                        else:
                            P.op("dve", (lambda yi, ysl: lambda e: e.tensor_tensor(out=yT[:, ysl], in0=yT[:, ysl], in1=py[yi][:], op=ALU.add))(yi, ysl), reads=[ykey], writes=["yT"])
                    if ix % 8 == 7:
                        if debug:
                            P.dma("sp", "dbgy", lambda e: e.dma_start(out=yT_d[ut * 128:(ut + 1) * 128, :], in_=yT[:]), reads=["yT"], writes=["yT_d"])
                        P.op("act", lambda e: e.activation(out=zT[:, ut, :], in_=yT[:], func=AF.Gelu_apprx_tanh), reads=["yT"], writes=["zT"])

                stA(0)
                for ix in range(32):
                    if ix + 1 < 32:
                        stA(ix + 1)
                    stB(ix)
                    stC(ix)
            P.barrier()
            with phase() as st:
                wstgB_t = sbuf(st, "wstgB", [128, 4, 1024])
                w_glu_sb = sbuf(st, "w_glu_sb", [128, 4, 512], BF16); w_ba_sb = sbuf(st, "w_ba_sb", [128, 4, D], BF16)
                bglu = sbuf(st, "bglu", [128, 4])
                sg = [sbuf(st, "sg%d" % i, [128, 512], BF16) for i in range(2)]; sgr = Rot("sg", 2)
                glu = [sbuf(st, "glu%d" % i, [128, 4, 512], BF16) for i in range(2)]
                st_ba = [sbuf(st, "st_ba%d" % i, [128, 8, 512], BF16) for i in range(2)]
                pg = [psum(st, "pg%d" % i, [128, 512]) for i in range(3)]; pgr = Rot("pg", 3)
                pa = [psum(st, "pa%d" % i, [128, 512]) for i in range(3)]; par = Rot("pa", 3)
                P.dma("sp", "wl0", lambda e: e.dma_start(out=wstgB_t[:, :, 0:512], in_=w_glu.rearrange("(k p) n -> p k n", p=128)), writes=["wstgB"])
                P.op("dve", lambda e: e.tensor_copy(out=w_glu_sb[:], in_=wstgB_t[:, :, 0:512]), reads=["wstgB"], writes=["w_glu_sb"])
                P.dma("sp", "wl0", lambda e: e.dma_start(out=wstgB_t[:], in_=w_ba.rearrange("(k p) n -> p k n", p=128)), reads=["wstgB"], writes=["wstgB"])
                P.op("dve", lambda e: e.tensor_copy(out=w_ba_sb[:], in_=wstgB_t[:]), reads=["wstgB"], writes=["w_ba_sb"])
                P.dma("act", "wl1", lambda e: e.dma_start(out=bglu[:], in_=b_glu.rearrange("(k p) o -> p (k o)", p=128), allow_slow_non_contiguous=True), writes=["bglu"])
                for sgi in range(8):
                    gb_ = sgi % 2; gkey = "glu%d" % gb_; bakey = "st_ba%d" % gb_
                    ssl = slice(sgi * 512, (sgi + 1) * 512)
                    for ct in range(4):
                        gi, gk = pgr.next()
                        for k in range(4):
                            P.op("pe", (lambda gi, k, ct, ssl: lambda e: e.matmul(pg[gi][:], lhsT=w_glu_sb[:, k, ct * 128:(ct + 1) * 128], rhs=zT[:, k, ssl],
                                                                                  start=(k == 0), stop=(k == 3)))(gi, k, ct, ssl),
                                 reads=["w_glu_sb", "zT"], writes=[gk])
                        si_, sk_ = sgr.next()
                        P.op("act", (lambda si_, gi, ct: lambda e: e.activation(out=sg[si_][:], in_=pg[gi][:], func=AF.Sigmoid, bias=bglu[:, ct:ct + 1]))(si_, gi, ct),
                             reads=[gk, "bglu"], writes=[sk_])
                        P.op("dve", (lambda gb_, ct, si_, ssl: lambda e: e.tensor_tensor(out=glu[gb_][:, ct, :], in0=sg[si_][:], in1=zT[:, ct, ssl], op=ALU.mult))(gb_, ct, si_, ssl),
                             reads=[sk_, "zT"], writes=[gkey])
                    for ct2 in range(8):
                        ai, ak = par.next()
                        for k in range(4):
                            P.op("pe", (lambda ai, k, ct2, gb_: lambda e: e.matmul(pa[ai][:], lhsT=w_ba_sb[:, k, ct2 * 128:(ct2 + 1) * 128], rhs=glu[gb_][:, k, :],
                                                                                   start=(k == 0), stop=(k == 3)))(ai, k, ct2, gb_),
                                 reads=["w_ba_sb", gkey], writes=[ak])
                        if ct2 % 2:
                            P.op("act", (lambda gb_, ct2, ai: lambda e: e.copy(out=st_ba[gb_][:, ct2, :], in_=pa[ai][:]))(gb_, ct2, ai), reads=[ak], writes=[bakey])
                        else:
                            P.op("dve", (lambda gb_, ct2, ai: lambda e: e.tensor_copy(out=st_ba[gb_][:, ct2, :], in_=pa[ai][:]))(gb_, ct2, ai), reads=[ak], writes=[bakey])
                    P.dma("sp", bakey, (lambda gb_, ssl: lambda e: e.dma_start(out=baT_d.rearrange("(t p) n -> p t n", p=128)[:, :, ssl], in_=st_ba[gb_][:]))(gb_, ssl),
                          reads=[bakey], writes=["baT_d"])
        P.barrier()
        if stage <= 2:
            return finish(nc, P, out)

        attT_d = dscr("attT_d", [512, SEQ], BF16) if debug else None
        NEG = -30000.0
        with ExitStack() as stC:
            attT_sb = sbuf(stC, "attT_sb", [128, 4, SEQ], BF16)
            stC2 = ExitStack()
            kT_sb = sbuf(stC2, "kT_sb", [128, 4, NTOK], BF16); qT_sb = sbuf(stC2, "qT_sb", [128, 4, SEQ], BF16)
            BiasTT = sbuf(stC2, "BiasTT", [128, 8 * 14, 64])
            Vctx = sbuf(stC2, "Vctx", [128, 2, 512], BF16)
            ones_b = sbuf(stC2, "ones_b", [128, 128], BF16)
            P.dma("sp", "lc0", lambda e: e.dma_start(out=kT_sb[:], in_=kT_d.rearrange("(t p) n -> p t n", p=128)), reads=["kT_d"], writes=["kT_sb"])
            P.dma("act", "lc1", lambda e: e.dma_start(out=qT_sb[:], in_=qT_d.rearrange("(t p) n -> p t n", p=128)), reads=["qT_d"], writes=["qT_sb"])
            P.dma("act", "lc1", lambda e: e.dma_start(out=Vctx[:], in_=v_d[0:CTX, :].rearrange("(t p) n -> p t n", p=128)), reads=["v_d"], writes=["Vctx"])
            P.op("pool", lambda e: e.memset(ones_b[:], 1.0), writes=["ones_b"])
            with phase() as st:
                rpbB = sbuf(st, "rpbB", [128, 8 * 14, 31]); tmpC = sbuf(st, "tmpC", [128, 8 * 14, 64])
                Dm = sbuf(st, "Dm", [128, 64]); eqm = [sbuf(st, "eqm%d" % i, [128, 64]) for i in range(2)]
                c0t = sbuf(st, "c0t", [128, 64]); kcv = sbuf(st, "kcv", [128, 64]); m2 = sbuf(st, "m2c", [128, 64])
                for half in range(2):
                    sl = slice(half * 64, (half + 1) * 64)
                    P.dma("sp", "lc2", (lambda sl, half: lambda e: e.dma_start(out=rpbB[sl].rearrange("p (h j) m -> p h (j m)", h=8),
                                                                              in_=rpb[:, half:half + 14, :].rearrange("h j m -> h (j m)").unsqueeze(0).to_broadcast([64, 8, 14 * 31])))(sl, half),
                          writes=["rpbB"])
                    P.op("pool", (lambda sl: lambda e: e.iota(Dm[sl], pattern=[[-1, 64]], base=15, channel_multiplier=1, allow_small_or_imprecise_dtypes=True))(sl), writes=["Dm"])
                    P.op("pool", (lambda sl: lambda e: e.iota(kcv[sl], pattern=[[0, 64]], base=0, channel_multiplier=1, allow_small_or_imprecise_dtypes=True))(sl), writes=["kcv"])
                P.op("pool", lambda e: e.iota(c0t[:], pattern=[[1, 64]], base=-8, channel_multiplier=0, allow_small_or_imprecise_dtypes=True), writes=["c0t"])
                P.op("dve", lambda e: e.tensor_scalar(out=c0t[:], in0=c0t[:], scalar1=0.0, scalar2=48.0, op0=ALU.max, op1=ALU.min), reads=["c0t"], writes=["c0t"])
                P.op("dve", lambda e: e.tensor_tensor(out=kcv[:], in0=kcv[:], in1=c0t[:], op=ALU.subtract), reads=["kcv", "c0t"], writes=["kcv"])
                P.op("dve", lambda e: e.tensor_single_scalar(out=m2[:], in_=kcv[:], scalar=0.0, op=ALU.is_ge), reads=["kcv"], writes=["m2c"])
                P.op("dve", lambda e: e.tensor_single_scalar(out=kcv[:], in_=kcv[:], scalar=15.0, op=ALU.is_le), reads=["kcv", "m2c"], writes=["kcv"])
                P.op("dve", lambda e: e.tensor_tensor(out=m2[:], in0=m2[:], in1=kcv[:], op=ALU.mult), reads=["kcv", "m2c"], writes=["m2c"])
                P.op("dve", lambda e: e.tensor_scalar(out=m2[:], in0=m2[:], scalar1=-1.0, scalar2=-NEG, op0=ALU.add, op1=ALU.mult), reads=["m2c"], writes=["m2c"])
                P.op("dve", lambda e: e.tensor_copy(out=BiasTT[:], in_=m2[:].unsqueeze(1).to_broadcast([128, 112, 64])), reads=["m2c"], writes=["BiasTT"])
                for m in range(31):
                    ei = m % 2; ek = "eqm%d" % ei
                    P.op("dve", (lambda ei, m: lambda e: e.tensor_single_scalar(out=eqm[ei][:], in_=Dm[:], scalar=float(m), op=ALU.is_equal))(ei, m), reads=["Dm"], writes=[ek])
                    for hh in range(2):
                        hs = slice(hh * 56, (hh + 1) * 56); tk_ = "tmpC%d" % hh
                        P.op("pool", (lambda ei, m, hh, hs: lambda e: e.tensor_tensor(out=tmpC[:, hs, :], in0=eqm[ei][:].unsqueeze(1).to_broadcast([128, 56, 64]),
                                                                                      in1=rpbB[:, hs, m:m + 1].to_broadcast([128, 56, 64]), op=ALU.mult))(ei, m, hh, hs),
                             reads=[ek, "rpbB"], writes=[tk_])
                        P.op("dve", (lambda hs: lambda e: e.tensor_tensor(out=BiasTT[:, hs, :], in0=BiasTT[:, hs, :], in1=tmpC[:, hs, :], op=ALU.add))(hs), reads=[tk_, "BiasTT"], writes=["BiasTT%d" % hh])
            P.barrier()
            with phase() as st:
                Vb = [sbuf(st, "Vb%d" % i, [128, 4, 512], BF16) for i in range(3)]; vbr = Rot("Vb", 3)
                ssb = [sbuf(st, "ssb%d" % i, [128, 4, 64]) for i in range(3)]; ssr2 = Rot("ssb", 3)
                pT = [sbuf(st, "pT%d" % i, [128, 384], BF16) for i in range(3)]; ptr = Rot("pT", 3)
                rden = [sbuf(st, "rden%d" % i, [128, 64]) for i in range(2)]; rdr = Rot("rden", 2)
                ps_ = [psum(st, "psc%d" % i, [128, 512]) for i in range(3)]; psr = Rot("psc", 3)
                po_ = [psum(st, "poc%d" % i, [128, 512]) for i in range(2)]; por = Rot("poc", 2)
                pd_ = [psum(st, "pdc%d" % i, [128, 512]) for i in range(2)]; pdr = Rot("pdc", 2)
                B4 = BiasTT[:].rearrange("p (h j) q -> p h j q", h=8)
                cf = [sbuf(st, "cvf%d" % i, [128, 2048]) for i in range(3)]; cb = [sbuf(st, "cvb%d" % i, [128, 2048], BF16) for i in range(3)]

                def convert_tile(ti):
                    bi = ti % 3
                    P.dma("sp", "cvf%d" % bi, lambda e: e.dma_start(out=cf[bi][:], in_=peer_uv[ti * 128:(ti + 1) * 128, :]), writes=["cvf%d" % bi])
                    P.op("pool", lambda e: e.tensor_copy(out=cb[bi][:], in_=cf[bi][:]), reads=["cvf%d" % bi], writes=["cvb%d" % bi])
                    P.dma("pool", "cvb%d" % bi, lambda e: e.dma_start(out=uvb_d[ti * 128:(ti + 1) * 128, :], in_=cb[bi][:]), reads=["cvb%d" % bi], writes=["uvb_d"])
                def c_scores(r, h, vi):
                    r0 = min(max(r - 4, 0), 56)
                    t = h // 2; po = (h % 2) * 64; psl = slice(po, po + 64)
                    si, skey = psr.next()
                    qsl = slice(r * 64, (r + 1) * 64)
                    for j in range(6):
                        k0 = (CTX + (r0 + 2 * j) * 64) if j < 4 else (j - 4) * 128
                        P.op("pe", (lambda j, k0: lambda e: e.matmul(ps_[si][:, j * 64:(j + 1) * 64], lhsT=kT_sb[psl, t, k0:k0 + 128], rhs=qT_sb[psl, t, qsl], start=True, stop=True))(j, k0),
                             reads=["kT_sb", "qT_sb"], writes=[skey])
                    return (r, h, vi, si, skey)

                def c_part1(state):
                    r, h, vi, si, skey = state
                    r0 = min(max(r - 4, 0), 56); dr0 = r0 - r + 7
                    bi2, bkey2 = ssr2.next()
                    P.op("dve", lambda e: e.scalar_tensor_tensor(out=ssb[bi2][:], in0=ps_[si][:, 0:256].rearrange("p (j q) -> p j q", j=4), scalar=0.125,
                                                                 in1=B4[:, h, dr0:dr0 + 7:2, :], op0=ALU.mult, op1=ALU.add), reads=[skey, "BiasTT"], writes=[bkey2])
                    ti, tkey = ptr.next()
                    P.op("act", lambda e: e.activation(out=pT[ti][:, 0:256], in_=ssb[bi2][:].rearrange("p j q -> p (j q)"), func=AF.Exp), reads=[bkey2], writes=[tkey])
                    P.op("act", lambda e: e.activation(out=pT[ti][:, 256:384], in_=ps_[si][:, 256:384], func=AF.Exp, scale=0.125), reads=[skey], writes=[tkey])
                    return (r, h, vi, ti, tkey)

                def c_part2(state):
                    r, h, vi, ti, tkey = state
                    vkey = "Vb%d" % vi
                    t = h // 2; po = (h % 2) * 64; psl = slice(po, po + 64)
                    qsl = slice(r * 64, (r + 1) * 64)
                    oi, okey = por.next(); di, dkey = pdr.next()
                    hp = (h // 2) * 128
                    for j in range(6):
                        vsrc = (Vb[vi][:, j, hp:hp + 128] if j < 4 else Vctx[:, j - 4, hp:hp + 128])
                        P.op("pe", (lambda j, vsrc: lambda e: e.matmul(po_[oi][:, 0:64], lhsT=vsrc, rhs=pT[ti][:, j * 64:(j + 1) * 64], start=(j == 0), stop=(j == 5)))(j, vsrc),
                             reads=[vkey, "Vctx", tkey], writes=[okey])
                    for j in range(6):
                        P.op("pe", (lambda j: lambda e: e.matmul(pd_[di][:, 0:64], lhsT=ones_b[:], rhs=pT[ti][:, j * 64:(j + 1) * 64], start=(j == 0), stop=(j == 5)))(j),
                             reads=["ones_b", tkey], writes=[dkey])
                    ri, rkey = rdr.next()
                    P.op("dve", lambda e: e.reciprocal(out=rden[ri][psl, :], in_=pd_[di][psl, 0:64]), reads=[dkey], writes=[rkey])
                    P.op("dve", lambda e: e.tensor_tensor(out=attT_sb[psl, t, qsl], in0=po_[oi][psl, 0:64], in1=rden[ri][psl, :], op=ALU.mult), reads=[okey, rkey], writes=["attT_sb"])

                pend1 = None; pend2 = None
                for r in range(64):
                    convert_tile(2 * r); convert_tile(2 * r + 1)
                    r0 = min(max(r - 4, 0), 56)
                    vi, vkey = vbr.next()
                    P.dma("sp", vkey, (lambda vi, r0: lambda e: e.dma_start(out=Vb[vi][:], in_=v_d[CTX + r0 * 64:CTX + (r0 + 8) * 64, :].rearrange("(j p) n -> p j n", p=128)))(vi, r0),
                          reads=["v_d"], writes=[vkey])
                    for h in range(8):
                        stt = c_scores(r, h, vi)
                        nxt2 = c_part1(pend1) if pend1 is not None else None
                        if pend2 is not None:
                            c_part2(pend2)
                        pend2 = nxt2
                        pend1 = stt
                nxt2 = c_part1(pend1)
                if pend2 is not None:
                    c_part2(pend2)
                c_part2(nxt2)
            if debug:
                P.dma("sp", "dbga", lambda e: e.dma_start(out=attT_d.rearrange("(t p) n -> p t n", p=128), in_=attT_sb[:]), reads=["attT_sb"], writes=["attT_d"])
            P.barrier()
            stC2.close()
            with phase() as st:
                wstgC_t = sbuf(st, "wstgC", [128, 4, 1024]); w_bb_sb = sbuf(st, "w_bb_sb", [128, 4, D], BF16)
                g_sb = [sbuf(st, "g_sb%d" % i, [128, 16, 512], BF16) for i in range(2)]
                ba_sb = [sbuf(st, "ba_sb%d" % i, [128, 8, 512], BF16) for i in range(2)]
                t1 = [sbuf(st, "t1c%d" % i, [128, 512]) for i in range(2)]; t1r = Rot("t1c", 2)
                t2 = [sbuf(st, "t2c%d" % i, [128, 512]) for i in range(2)]; t2r = Rot("t2c", 2)
                st_mg = [sbuf(st, "st_mg%d" % i, [128, 8, 512], BF16) for i in range(2)]
                pbb = [psum(st, "pbb%d" % i, [128, 512]) for i in range(3)]; pbr2 = Rot("pbb", 3)
                P.dma("sp", "wc0", lambda e: e.dma_start(out=wstgC_t[:], in_=w_bb.rearrange("(k p) n -> p k n", p=128)), writes=["wstgC"])
                P.op("dve", lambda e: e.tensor_copy(out=w_bb_sb[:], in_=wstgC_t[:]), reads=["wstgC"], writes=["w_bb_sb"])
                for sgi in range(8):
                    b2 = sgi % 2; ssl = slice(sgi * 512, (sgi + 1) * 512)
                    gk, bk, mk = "g_sb%d" % b2, "ba_sb%d" % b2, "st_mg%d" % b2
                    P.dma("act", gk, (lambda b2, ssl: lambda e: e.dma_start(out=g_sb[b2][:], in_=gT_d.rearrange("(t p) n -> p t n", p=128)[:, :, ssl]))(b2, ssl), reads=["gT_d"], writes=[gk])
                    P.dma("act", bk, (lambda b2, ssl: lambda e: e.dma_start(out=ba_sb[b2][:], in_=baT_d.rearrange("(t p) n -> p t n", p=128)[:, :, ssl]))(b2, ssl), reads=["baT_d"], writes=[bk])
                    for ct2 in range(8):
                        qi, qk = pbr2.next()
                        for k in range(4):
                            P.op("pe", (lambda qi, k, ct2, ssl: lambda e: e.matmul(pbb[qi][:], lhsT=w_bb_sb[:, k, ct2 * 128:(ct2 + 1) * 128], rhs=attT_sb[:, k, ssl],
                                                                                   start=(k == 0), stop=(k == 3)))(qi, k, ct2, ssl),
                                 reads=["w_bb_sb", "attT_sb"], writes=[qk])
                        i1, k1 = t1r.next(); i2, k2 = t2r.next()
                        P.op("dve", (lambda i1, qi, b2, ct2: lambda e: e.tensor_tensor(out=t1[i1][:], in0=pbb[qi][:], in1=g_sb[b2][:, 8 + ct2, :], op=ALU.mult))(i1, qi, b2, ct2),
                             reads=[qk, gk], writes=[k1])
                        P.op("pool", (lambda i2, b2, ct2: lambda e: e.tensor_tensor(out=t2[i2][:], in0=ba_sb[b2][:, ct2, :], in1=g_sb[b2][:, ct2, :], op=ALU.mult))(i2, b2, ct2),
                             reads=[bk, gk], writes=[k2])
                        P.op("dve", (lambda b2, ct2, i1, i2: lambda e: e.tensor_tensor(out=st_mg[b2][:, ct2, :], in0=t1[i1][:], in1=t2[i2][:], op=ALU.add))(b2, ct2, i1, i2),
                             reads=[k1, k2], writes=[mk])
                    P.dma("sp", mk, (lambda b2, ssl: lambda e: e.dma_start(out=mgT_d.rearrange("(t p) n -> p t n", p=128)[:, :, ssl], in_=st_mg[b2][:]))(b2, ssl),
                          reads=[mk], writes=["mgT_d"])
        P.barrier()
        if stage <= 3:
            return finish(nc, P, out)

        x1_d = dscr("x1_d", [SEQ, D], F32) if debug else None
        pf_d = dscr("pf_d", [SEQ, D], F32) if debug else None
        NT = 32 if stage >= 5 else int(stage * 10) % 10 or 1
        if not debug:
            NT = 32
        if "KNT" in os.environ:
            NT = int(os.environ["KNT"])
        gate1B = modB[:, 2 * D:3 * D]; S2B = modB[:, 3 * D:4 * D]; G2B = modB[:, 4 * D:5 * D]; gate2B = modB[:, 5 * D:6 * D]
        with ExitStack() as stD:
            w_out_sb = sbuf(stD, "w_out_sb", [128, 8, D], BF16); w_q_sb = sbuf(stD, "w_q_sb", [128, 8, 2048], BF16)
            skT = sbuf(stD, "skT", [128, 2, 128], BF16); fgB = sbuf(stD, "fgB", [128, D])
            with phase() as st:
                wstgD_t = [sbuf(st, "wstgD%d" % i, [128, 8, 512]) for i in range(2)]
                skf = sbuf(st, "skf", [128, 2, 128]); skb = sbuf(st, "skb", [128, 2, 128], BF16)
                ptk = psum(st, "ptk", [128, 2, 128], BF16)
                for i in range(6):
                    bi = i % 2
                    srcw = (w_out if i < 2 else w_q).rearrange("(k p) n -> p k n", p=128)
                    c0 = (i * 512) if i < 2 else (i - 2) * 512
                    dstw = w_out_sb if i < 2 else w_q_sb
                    P.dma("sp", "wd%d" % bi, (lambda bi, srcw, c0: lambda e: e.dma_start(out=wstgD_t[bi][:], in_=srcw[:, :, c0:c0 + 512]))(bi, srcw, c0), writes=["wstgD%d" % bi])
                    P.op(("dve", "pool")[bi], (lambda bi, dstw, c0: lambda e: e.tensor_copy(out=dstw[:, :, c0:c0 + 512], in_=wstgD_t[bi][:]))(bi, dstw, c0),
                         reads=["wstgD%d" % bi], writes=["wD"])
                P.dma("act", "wd2", lambda e: e.dma_start(out=skf[:], in_=subkeys.rearrange("n k d -> k n d")), writes=["skf"])
                P.dma("act", "wd2", lambda e: e.dma_start(out=fgB[:], in_=final_g.to_broadcast([128, D])), writes=["fgB"])
                P.op("dve", lambda e: e.tensor_copy(out=skb[:], in_=skf[:]), reads=["skf"], writes=["skb"])
                for n_ in range(2):
                    P.op("pe", (lambda n_: lambda e: e.transpose(ptk[:, n_, :], skb[:, n_, :], identb[:]))(n_), reads=["skb", "identb"], writes=["ptk"])
                P.op("dve", lambda e: e.tensor_copy(out=skT[:], in_=ptk[:]), reads=["ptk"], writes=["skT"])
            P.barrier()
            P.barrier()
            if stage == 3.5:
                return finish(nc, P, out)
            with phase() as st:
                NB = 2
                xtD_t = sbuf(st, "xtD", [128, D])
                x1 = [sbuf(st, "x1_%d" % i, [128, D]) for i in range(NB)]
                h2 = [sbuf(st, "h2_%d" % i, [128, D]) for i in range(NB)]
                eid = [sbuf(st, "eid%d" % i, [128, 128], I32) for i in range(NB)]
                gate = [sbuf(st, "gate%d" % i, [128, 128]) for i in range(NB)]
                tmpD = sbuf(st, "tmpD", [128, D]); tmpF = tmpD
                acc = None
                junkD = sbuf(st, "junkD", [128, D], BF16); NJ = int(os.environ.get("KJ", "2"))
                junkE3 = [sbuf(st, "junkE%d" % i, [128, D], BF16) for i in range(NJ)]; jer = Rot("junkE", NJ)
                mg_sb = sbuf(st, "mg_sb", [128, 8, 128], BF16); h2b2 = [sbuf(st, "h2b%d" % i, [128, D], BF16) for i in range(NB)]; h2T = sbuf(st, "h2T", [128, 8, 128], BF16)
                qT_sb2 = sbuf(st, "qT_sb2", [128, 16, 128], BF16); s_sb = sbuf(st, "s_sb", [128, 16, 128]); work = sbuf(st, "workD", [128, 16, 128])
                topv = sbuf(st, "topv", [128, 16, 16]); idxu = sbuf(st, "idxu", [128, 16, 16], U32); idxf = sbuf(st, "idxf", [128, 16, 16])
                cand = sbuf(st, "cand", [128, 8, 256]); cidx = s_sb[:].rearrange("p a b -> p (a b)").rearrange("p (h c) -> p h c", h=8)
                best = sbuf(st, "best", [128, 8, 16]); eg = sbuf(st, "eg", [128, 8, 16]); sm = sbuf(st, "smD", [128, 32]); sm2 = sbuf(st, "smE", [128, 8]); eidf = sbuf(st, "eidf", [128, 128])
                aD = sbuf(st, "aD", [128, 128]); gl_ = sbuf(st, "gl_", [128, 128]); wD = sbuf(st, "wDw", [128, 128])
                NG = int(os.environ.get("KNG", "12")); GJ = int(os.environ.get("KGJ", "4"))
                FY = int(os.environ.get("KFY", "1")); KAR = int(os.environ.get("KAR", "0")); KPEND = int(os.environ.get("KPEND", "1"))
                posI = sbuf(st, "posI", [128, 256], I32); mskI = sbuf(st, "mskI", [128, 1], I32)
                c4I = sbuf(st, "c4I", [128, 1], I32); c15I = sbuf(st, "c15I", [128, 1], I32); iota16 = sbuf(st, "iota16", [128, 16])
                pab = sbuf(st, "pab", [128, 2, 8, 16], I32); pabf = sbuf(st, "pabf", [128, 2, 8, 16]); e3b = sbuf(st, "e3b", [128, 8, 16])
                oh = cand[:].rearrange("p h (k a) -> p h k a", a=16)
                P.op("pool", lambda e: e.iota(c4I[:], pattern=[[0, 1]], base=4, channel_multiplier=0), writes=["posI"])
                P.op("pool", lambda e: e.iota(c15I[:], pattern=[[0, 1]], base=15, channel_multiplier=0), writes=["posI"])
                P.op("pool", lambda e: e.iota(iota16[:], pattern=[[1, 16]], base=0, channel_multiplier=0, allow_small_or_imprecise_dtypes=True), writes=["posI"])
                P.op("pool", lambda e: e.iota(posI[:], pattern=[[1, 256]], base=0, channel_multiplier=0), writes=["posI"])
                P.op("pool", lambda e: e.iota(mskI[:], pattern=[[0, 1]], base=-256, channel_multiplier=0), writes=["posI"])
                UVg = [sbuf(st, "UVg%d" % i, [128, 2, D], BF16) for i in range(NG)]
                dg = [sbuf(st, "dg%d" % i, [128, 128], BF16) for i in range(8)]; dgr = Rot("dg", 8)
                pmo = [psum(st, "pmo%d" % i, [128, 512]) for i in range(2)]
                pacc = [psum(st, "pacc%d" % i, [128, 512]) for i in range(2)]
                tpD = psum(st, "tpD", [128, 8, 128], BF16)
                pq = [psum(st, "pq%d" % i, [128, 4, 128]) for i in range(2)]; pqr = Rot("pq", 2)
                uv_i = [0]

                def rstd_chain(src_ap, srckey, smt, smk, jk, jkey):
                    P.op("act", lambda e: e.activation(out=jk[:], in_=src_ap, func=AF.Square, accum_out=smt[:, 0:1]), reads=[srckey], writes=[smk, jkey])
                    P.op("dve", lambda e: e.tensor_scalar(out=smt[:, 1:2], in0=smt[:, 0:1], scalar1=1.0 / D, scalar2=EPS, op0=ALU.mult, op1=ALU.add), reads=[smk], writes=[smk])
                    P.op("act", lambda e: e.sqrt(out=smt[:, 2:3], in_=smt[:, 1:2]), reads=[smk], writes=[smk])
                    P.op("dve", lambda e: e.reciprocal(out=smt[:, 3:4], in_=smt[:, 2:3]), reads=[smk], writes=[smk])

                def front(i):
                    b = i % NB; t0 = i * 128
                    xk, x1k, h2k, ek, gk = "xtD", "x1_%d" % b, "h2_%d" % b, "eid%d" % b, "gate%d" % b
                    P.dma("sp", xk, lambda e: e.dma_start(out=xtD_t[:], in_=x[t0:t0 + 128, :]), writes=[xk])
                    P.dma("sp", "mgl", lambda e: e.dma_start(out=mg_sb[:], in_=mgT_d.rearrange("(k p) n -> p k n", p=128)[:, :, t0:t0 + 128]), reads=["mgT_d"], writes=["mg_sb"])
                    for hf in range(2):
                        for k in range(8):
                            P.op("pe", (lambda hf, k: lambda e: e.matmul(pmo[hf][:], lhsT=mg_sb[:, k, :], rhs=w_out_sb[:, k, hf * 512:(hf + 1) * 512], start=(k == 0), stop=(k == 7)))(hf, k),
                                 reads=["mg_sb", "wD"], writes=["pmo%d" % hf])
                        yield
                        P.op("dve", (lambda hf: lambda e: e.tensor_tensor(out=tmpF[:, hf * 512:(hf + 1) * 512], in0=pmo[hf][:], in1=gate1B[:, hf * 512:(hf + 1) * 512], op=ALU.mult))(hf),
                             reads=["pmo%d" % hf, "modB"], writes=["tmpD"])
                        yield
                    P.op("dve", lambda e: e.tensor_tensor(out=x1[b][:], in0=tmpF[:], in1=xtD_t[:], op=ALU.add), reads=["tmpD", xk], writes=[x1k])
                    yield
                    if debug:
                        P.dma("sp", "dbgx1", lambda e: e.dma_start(out=x1_d[t0:t0 + 128, :], in_=x1[b][:]), reads=[x1k], writes=["x1_d"])
                    rstd_chain(x1[b][:], x1k, sm[:, 0:4], "smD", junkD, "junkD")
                    P.op("dve", lambda e: e.scalar_tensor_tensor(out=tmpF[:], in0=x1[b][:], scalar=sm[:, 3:4], in1=G2B, op0=ALU.mult, op1=ALU.mult), reads=[x1k, "smD", "modB"], writes=["tmpD"])
                    yield
                    P.op("dve", lambda e: e.tensor_tensor(out=h2[b][:], in0=tmpF[:], in1=S2B, op=ALU.add), reads=["tmpD", "modB"], writes=[h2k])
                    yield
                    P.op("act", lambda e: e.copy(out=h2b2[b][:], in_=h2[b][:]), reads=[h2k], writes=["h2b%d" % b])
                    for k in range(8):
                        P.op("pe", (lambda k: lambda e: e.transpose(tpD[:, k, :], h2b2[b][:, k * 128:(k + 1) * 128], identb[:]))(k), reads=["h2b%d" % b, "identb"], writes=["tpD"])
                    P.op("act", lambda e: e.copy(out=h2T[:], in_=tpD[:]), reads=["tpD"], writes=["h2T"])
                    yield
                    for g4 in range(4):
                        qi, qk = pqr.next()
                        for bl in range(4):
                            blk = g4 * 4 + bl
                            for k in range(8):
                                P.op("pe", (lambda qi, bl, blk, k: lambda e: e.matmul(pq[qi][:, bl, :], lhsT=w_q_sb[:, k, blk * 128:(blk + 1) * 128], rhs=h2T[:, k, :], start=(k == 0), stop=(k == 7)))(qi, bl, blk, k),
                                     reads=["wD", "h2T"], writes=[qk])
                        P.op("act", (lambda qi, g4: lambda e: e.copy(out=qT_sb2[:, g4 * 4:(g4 + 1) * 4, :], in_=pq[qi][:]))(qi, g4), reads=[qk], writes=["qT_sb2"])
                        yield
                    for g4 in range(4):
                        si, sk = pqr.next()
                        for bl in range(4):
                            blk = g4 * 4 + bl
                            P.op("pe", (lambda si, bl, blk: lambda e: e.matmul(pq[si][:, bl, :], lhsT=qT_sb2[:, blk, :], rhs=skT[:, blk % 2, :], start=True, stop=True))(si, bl, blk),
                                 reads=["qT_sb2", "skT"], writes=[sk])
                        P.op("act", (lambda si, g4: lambda e: e.copy(out=s_sb[:, g4 * 4:(g4 + 1) * 4, :], in_=pq[si][:]))(si, g4), reads=[sk], writes=["s_sb"])
                        yield
                    TK = ["s_sb", "topk"]
                    BK = ["tk%d" % q for q in range(16)]
                    for blk in range(16):
                        P.op("dve", (lambda blk: lambda e: e.max(out=topv[:, blk, 0:8], in_=s_sb[:, blk, :]))(blk), reads=TK, writes=[BK[blk]])
                    yield
                    for blk in range(16):
                        P.op("dve", (lambda blk: lambda e: e.max_index(out=idxu[:, blk, 0:8], in_max=topv[:, blk, 0:8], in_values=s_sb[:, blk, :]))(blk), reads=["s_sb", BK[blk]], writes=[BK[blk]])
                        if blk % 8 == 7:
                            yield
                    for blk in range(16):
                        P.op("dve", (lambda blk: lambda e: e.match_replace(out=work[:, blk, :], in_to_replace=topv[:, blk, 0:8], in_values=s_sb[:, blk, :], imm_value=-1e30))(blk), reads=["s_sb", BK[blk]], writes=[BK[blk]])
                        if blk % 8 == 7:
                            yield
                    for blk in range(16):
                        P.op("dve", (lambda blk: lambda e: e.max(out=topv[:, blk, 8:16], in_=work[:, blk, :]))(blk), reads=[BK[blk]], writes=[BK[blk]])
                    yield
                    for blk in range(16):
                        P.op("dve", (lambda blk: lambda e: e.max_index(out=idxu[:, blk, 8:16], in_max=topv[:, blk, 8:16], in_values=work[:, blk, :]))(blk), reads=[BK[blk]], writes=[BK[blk]])
                        if blk % 8 == 7:
                            yield
                    TK = TK + BK
                    P.op("dve", lambda e: e.tensor_copy(out=idxf[:], in_=idxu[:]), reads=TK, writes=["topk"])
                    tv4 = topv[:].rearrange("p (h n) a -> p h n a", n=2); ix4 = idxf[:].rearrange("p (h n) a -> p h n a", n=2)
                    c4 = cand[:].rearrange("p h (a b) -> p h a b", a=16); ci4 = cidx.rearrange("p h (a b) -> p h a b", a=16)
                    P.op("dve", lambda e: e.tensor_tensor(out=c4, in0=tv4[:, :, 0, :].unsqueeze(3).to_broadcast([128, 8, 16, 16]),
                                                          in1=tv4[:, :, 1, :].unsqueeze(2).to_broadcast([128, 8, 16, 16]), op=ALU.add), reads=TK, writes=["topk"])
                    yield
                    candI = cand[:].bitcast(I32)
                    P.op("dve", lambda e: e.tensor_scalar(out=candI, in0=candI, scalar1=mskI[:, 0:1], scalar2=None, op0=ALU.bitwise_and), reads=TK + ["posI"], writes=["topk"])
                    P.op("dve", lambda e: e.tensor_tensor(out=candI, in0=candI, in1=posI[:].unsqueeze(1).to_broadcast([128, 8, 256]), op=ALU.bitwise_or), reads=TK + ["posI"], writes=["topk"])
                    P.op("dve", lambda e: e.tensor_single_scalar(out=ix4[:, :, 0, :], in_=ix4[:, :, 0, :], scalar=128.0, op=ALU.mult), reads=TK, writes=["topk"])
                    yield
                    w2 = work[:].rearrange("p (h n) k -> p h (n k)", n=2)
                    HK = ["hk%d" % q for q in range(8)]
                    for h in range(8):
                        P.op("dve", (lambda h: lambda e: e.max(out=best[:, h, 0:8], in_=cand[:, h, :]))(h), reads=TK, writes=[HK[h]])
                    yield
                    for h in range(8):
                        P.op("dve", (lambda h: lambda e: e.match_replace(out=w2[:, h, :], in_to_replace=best[:, h, 0:8], in_values=cand[:, h, :], imm_value=-1e30))(h), reads=TK + [HK[h]], writes=[HK[h]])
                    yield
                    for h in range(8):
                        P.op("dve", (lambda h: lambda e: e.max(out=best[:, h, 8:16], in_=w2[:, h, :]))(h), reads=[HK[h]], writes=[HK[h]])
                    yield
                    TK = TK + HK
                    P.op("dve", lambda e: e.tensor_single_scalar(out=sm[:, 8:16], in_=best[:, :, 0], scalar=-1.0, op=ALU.mult), reads=TK + ["smD"], writes=["smD"])
                    for h in range(8):
                        P.op("act", (lambda h: lambda e: e.activation(out=eg[:, h, :], in_=best[:, h, :], func=AF.Exp, bias=sm[:, 8 + h:9 + h], accum_out=sm[:, 16 + h:17 + h]))(h),
                             reads=TK + ["smD"], writes=["eg", "smD"])
                    P.op("dve", lambda e: e.reciprocal(out=sm[:, 24:32], in_=sm[:, 16:24]), reads=["smD"], writes=["smD"])
                    P.op("dve", lambda e: e.tensor_tensor(out=gate[b][:].rearrange("p (h k) -> p h k", h=8), in0=eg[:], in1=sm[:, 24:32].unsqueeze(2).to_broadcast([128, 8, 16]), op=ALU.mult),
                         reads=["eg", "smD"], writes=[gk])
                    yield
                    bestI = best[:].bitcast(I32)
                    P.op("dve", lambda e: e.tensor_scalar(out=pab[:, 0], in0=bestI, scalar1=c4I[:, 0:1], scalar2=None, op0=ALU.arith_shift_right), reads=TK + ["posI"], writes=["topk"])
                    P.op("dve", lambda e: e.tensor_scalar(out=pab[:, 0], in0=pab[:, 0], scalar1=c15I[:, 0:1], scalar2=None, op0=ALU.bitwise_and), reads=TK + ["posI"], writes=["topk"])
                    P.op("dve", lambda e: e.tensor_scalar(out=pab[:, 1], in0=bestI, scalar1=c15I[:, 0:1], scalar2=None, op0=ALU.bitwise_and), reads=TK + ["posI"], writes=["topk"])
                    P.op("dve", lambda e: e.tensor_copy(out=pabf[:], in_=pab[:]), reads=TK, writes=["topk"])
                    yield
                    e3 = eidf[:].rearrange("p (h k) -> p h k", h=8)
                    for n_ in range(2):
                        P.op("dve", (lambda n_: lambda e: e.tensor_tensor(out=oh[:], in0=pabf[:, n_].unsqueeze(3).to_broadcast([128, 8, 16, 16]),
                                                                          in1=iota16[:].unsqueeze(1).unsqueeze(1).to_broadcast([128, 8, 16, 16]), op=ALU.is_equal))(n_), reads=TK + ["posI"], writes=["topk"])
                        P.op("dve", (lambda n_: lambda e: e.tensor_tensor(out=oh[:], in0=oh[:], in1=ix4[:, :, n_, :].unsqueeze(2).to_broadcast([128, 8, 16, 16]), op=ALU.mult))(n_), reads=TK, writes=["topk"])
                        P.op("dve", (lambda n_: lambda e: e.tensor_reduce(out=(e3 if n_ == 0 else e3b[:]), in_=oh[:], axis=AX.X, op=ALU.add))(n_), reads=TK, writes=["topk"])
                        yield
                    P.op("dve", lambda e: e.tensor_tensor(out=e3, in0=e3, in1=e3b[:], op=ALU.add), reads=TK, writes=["topk"])
                    P.op("dve", lambda e: e.tensor_scalar(out=eidf[:], in0=eidf[:], scalar1=0.0, scalar2=16383.0, op0=ALU.max, op1=ALU.min), reads=TK, writes=["topk"])
                    P.op("dve", lambda e: e.tensor_copy(out=eid[b][:], in_=eidf[:]), reads=TK, writes=[ek])
                    yield

                def consume(i, fgen):
                    b = i % NB; t0 = i * 128
                    x1k, h2k, ek, gk = "x1_%d" % b, "h2_%d" % b, "eid%d" % b, "gate%d" % b
                    ngrp = 128 // GJ
                    pend = None

                    def finish_group(g, bufs):
                        j0 = g * GJ
                        if "nofin" in KD:
                            return
                        P.op("act", lambda e: e.activation(out=gl_[:, j0:j0 + GJ], in_=aD[:, j0:j0 + GJ], func=AF.Gelu_apprx_tanh), reads=["aD%d_%d" % (g, q) for q in range(GJ)], writes=["gl_%d" % g])
                        for jj in range(GJ):
                            P.op("act", (lambda jj: lambda e: e.mul(out=wD[:, j0 + jj:j0 + jj + 1], in_=gl_[:, j0 + jj:j0 + jj + 1], mul=gate[b][:, j0 + jj:j0 + jj + 1]))(jj),
                                 reads=["gl_%d" % g, gk], writes=["wDw%d" % g])
                        for jj in range(GJ):
                            j = j0 + jj; gi, ugk = bufs[jj]
                            di, dk = dgr.next()
                            P.op("act", (lambda di, j: lambda e: e.activation(out=dg[di][:], in_=identb[:], func=AF.Copy, scale=wD[:, j:j + 1]))(di, j), reads=["wDw%d" % g, "identb"], writes=[dk])
                            for hf in range(2):
                                if "nov" in KD and j not in (0, 127):
                                    continue
                                P.op("pe", (lambda di, gi, hf, j: lambda e: e.matmul(pacc[hf][:], lhsT=dg[di][:], rhs=UVg[gi][:, 1, hf * 512:(hf + 1) * 512], start=(j == 0), stop=(j == 127)))(di, gi, hf, j),
                                     reads=[dk, ugk], writes=["pacc%d" % hf])

                    for g in range(ngrp):
                        bufs = []
                        for jj in range(GJ):
                            j = g * GJ + jj
                            gi = uv_i[0] % NG; uv_i[0] += 1; ugk = "UVg%d" % gi
                            bufs.append((gi, ugk))
                            if "nog" in KD:
                                P.op("pool", (lambda gi: lambda e: e.memset(UVg[gi][:], 1.0))(gi), reads=[ek], writes=[ugk])
                            else:
                                P.dma("pool", ugk, (lambda gi, j: lambda e: e.indirect_dma_start(out=UVg[gi][:].rearrange("p a d -> p (a d)"), out_offset=None, in_=uvb_d,
                                                                                               in_offset=bass.IndirectOffsetOnAxis(ap=eid[b][:, j:j + 1], axis=0)))(gi, j), reads=[ek], writes=[ugk])
                        for jj in range(GJ):
                            j = g * GJ + jj; gi, ugk = bufs[jj]
                            if "noa" in KD:
                                continue
                            ji, jkey_ = jer.next()
                            jE = junkE3[ji]
                            if jj < KAR:
                                P.op("dve", (lambda gi, jE: lambda e: e.tensor_tensor(out=jE[:], in0=UVg[gi][:, 0, :], in1=h2b2[b][:], op=ALU.mult))(gi, jE),
                                     reads=[ugk, "h2b%d" % b], writes=[jkey_])
                                P.op("act", (lambda j, jE: lambda e: e.activation(out=jE[:], in_=jE[:], func=AF.Copy, accum_out=aD[:, j:j + 1]))(j, jE),
                                     reads=[jkey_], writes=[jkey_, "aD%d_%d" % (g, jj)])
                            else:
                                P.op("dve", (lambda gi, j, jE: lambda e: e.scalar_tensor_tensor(out=jE[:], in0=UVg[gi][:, 0, :], scalar=1.0, in1=h2[b][:], op0=ALU.mult, op1=ALU.mult, accum_out=aD[:, j:j + 1]))(gi, j, jE),
                                     reads=[ugk, h2k], writes=["aD%d_%d" % (g, jj), jkey_])
                        if KPEND:
                            if pend is not None:
                                finish_group(*pend)
                            pend = (g, bufs)
                        else:
                            finish_group(g, bufs)
                        if fgen is not None:
                            for _ in range(FY):
                                next(fgen, None)
                    if KPEND and pend is not None:
                        finish_group(*pend)
                    if fgen is not None:
                        for _ in fgen:
                            pass
                    if "dump23" in KD and i == 23:
                        for nm, src_t in (("d_x1", x1[b]), ("d_wD", wD), ("d_aD", aD), ("d_gate", gate[b]), ("d_h2", h2[b])):
                            dd = nc.dram_tensor(nm, list(src_t[:].shape), F32, kind="ExternalOutput").ap()
                            P.dma("sp", "dump", (lambda dd, src_t: lambda e: e.dma_start(out=dd, in_=src_t[:]))(dd, src_t), reads=[x1k, gk, h2k] + ["wDw%d" % q for q in range(32)] + ["aD%d" % q for q in range(32)], writes=["dumpo"])
                        dd2 = nc.dram_tensor("d_eid", [128, 128], I32, kind="ExternalOutput").ap()
                        P.dma("sp", "dump", lambda e: e.dma_start(out=dd2, in_=eid[b][:]), reads=[ek], writes=["dumpo"])
                    if "notail" in KD:
                        return
                    for hf in range(2):
                        P.op("dve", (lambda hf: lambda e: e.tensor_tensor(out=tmpD[:, hf * 512:(hf + 1) * 512], in0=pacc[hf][:], in1=gate2B[:, hf * 512:(hf + 1) * 512], op=ALU.mult))(hf),
                             reads=["pacc%d" % hf, "modB"], writes=["tmpD"])
                    P.op("dve", lambda e: e.tensor_tensor(out=x1[b][:], in0=tmpD[:], in1=x1[b][:], op=ALU.add), reads=["tmpD", x1k], writes=[x1k])
                    rstd_chain(x1[b][:], x1k, sm2[:, 0:4], "smE", junkD, "junkD")
                    P.op("dve", lambda e: e.scalar_tensor_tensor(out=tmpD[:], in0=x1[b][:], scalar=sm2[:, 3:4], in1=fgB[:], op0=ALU.mult, op1=ALU.mult), reads=[x1k, "smE", "fgB"], writes=["tmpD"])
                    P.dma("sp", "outst", lambda e: e.dma_start(out=out[t0:t0 + 128, :], in_=tmpD[:]), reads=["tmpD"], writes=["out"])

                for _ in front(0):
                    pass
                for i in range(NT):
                    if "noc" in KD:
                        break
                    if "noil" in KD:
                        consume(i, None)
                        if i + 1 < NT:
                            for _ in front(i + 1):
                                pass
                        continue
                    consume(i, front(i + 1) if i + 1 < NT else None)
        return finish(nc, P, out)
    return nc


def finish(nc, P, out):
    P.barrier()
    P.close()
    return nc


def core_inputs(inp, b):
    f = lambda a: np.ascontiguousarray(a, dtype=np.float32)
    return {
        "x": f(inp["x"][b]), "c": f(inp["c"][b:b + 1]), "ctx": f(inp["ctx"][b]), "c_ctx": f(inp["c_ctx"].reshape(1, D)),
        "w_mod": f(inp["w_mod"][0]), "b_mod": f(inp["b_mod"][0].reshape(1, -1)),
        "norm1_g": f(inp["norm1_g"][0].reshape(1, D)), "norm2_g": f(inp["norm2_g"][0].reshape(1, D)),
        "final_g": f(inp["final_g"].reshape(1, D)), "w_in": f(inp["w_in"][0]),
        "ssm_a_re": f(inp["ssm_a_re"][0]), "ssm_a_im": f(inp["ssm_a_im"][0]), "ssm_log_dt": f(inp["ssm_log_dt"][0]),
        "ssm_b_re": f(inp["ssm_b_re"][0]), "ssm_b_im": f(inp["ssm_b_im"][0]),
        "ssm_c_re": f(inp["ssm_c_re"][0]), "ssm_c_im": f(inp["ssm_c_im"][0]),
        "ssm_d": f(inp["ssm_d"][0].reshape(512, 1)), "w_glu": f(inp["w_glu"][0]), "b_glu": f(inp["b_glu"][0].reshape(512, 1)),
        "w_branch_a": f(inp["w_branch_a"][0]), "w_branch_b": f(inp["w_branch_b"][0]), "na_rpb": f(inp["na_rpb"][0]),
        "w_out": f(inp["w_out"][0]), "peer_w_q": f(inp["peer_w_q"][0]), "peer_subkeys": f(inp["peer_subkeys"][0]),
        "peer_uv": f(np.concatenate([inp["peer_u"][0], inp["peer_v"][0]], axis=1)),
    }


def kernel(**inputs):
    nc = build()
    in_maps = [core_inputs(inputs, b) for b in range(8)]
    res = run_bass_kernel_spmd(nc, in_maps, core_ids=list(range(8)))
    return np.stack([np.asarray(r["out"], dtype=np.float32) for r in res.results], axis=0)
```
